# Optimizing a Trainium2 kernel written in Bass

```python
import math
import jax
import jax.numpy as jnp
from jax import lax
import numpy as np

D_MODEL = 1024
BATCH = 4
SEQ = 4096
DEPTH = 4

GRID_W = 64
CTX_LEN = 256
HEAD_DIM = 64
N_MIXERS = 4
GROUP_W = D_MODEL // N_MIXERS
ROPE_THETA = 10000.0
EPS = 1e-6
NEG_INF = -1e30

SWA_HEADS = GROUP_W // HEAD_DIM
SWA_KV_HEADS = SWA_HEADS // 2
SWA_WINDOW = 128
SWA_BLOCK = 128
GDN_HEADS = GROUP_W // HEAD_DIM
GDN_DK = HEAD_DIM
GDN_DV = HEAD_DIM
GDN_CONV = 5
GDN_CHUNK = 64
MLA_HEADS = GROUP_W // HEAD_DIM
MLA_Q_RANK = D_MODEL // 4
MLA_KV_RANK = D_MODEL // 8
MLA_NOPE = HEAD_DIM
MLA_ROPE = HEAD_DIM // 2
MLA_V = HEAD_DIM
MLA_BLOCK = 128
RET_HEADS = GROUP_W // HEAD_DIM
RET_DK = HEAD_DIM
RET_DV = HEAD_DIM
RET_CHUNK = 64
D_FF = 7 * D_MODEL // 2
N_EXPERTS = 8
TOP_K = 2

PROJ_SIZES = (
    SWA_HEADS * HEAD_DIM,
    SWA_KV_HEADS * HEAD_DIM,
    SWA_KV_HEADS * HEAD_DIM,
    GDN_HEADS * (2 * GDN_DK + GDN_DV),
    GDN_HEADS * GDN_DV,
    4 * GDN_HEADS,
    MLA_Q_RANK,
    MLA_KV_RANK,
    MLA_ROPE,
    RET_HEADS * RET_DK,
    RET_HEADS * RET_DK,
    RET_HEADS * RET_DV,
    RET_HEADS * RET_DV,
)
D_IN = sum(PROJ_SIZES)

kernel_name = 'hybrid_parallel_head_diffusion_trunk'


def rms_norm(x, g):
    xf = x.astype(jnp.float32)
    y = xf * lax.rsqrt(jnp.mean(xf * xf, axis=-1, keepdims=True) + EPS)
    return (y * g.astype(jnp.float32)).astype(x.dtype)


def modulate(x, g, shift, scale):
    return rms_norm(x, g) * (1.0 + scale) + shift


def l2_normalize(t):
    return t * lax.rsqrt(jnp.sum(t * t, axis=-1, keepdims=True) + EPS)


def head_group_norm(o, g):
    mu = jnp.mean(o, axis=-1, keepdims=True)
    var = jnp.mean(jnp.square(o - mu), axis=-1, keepdims=True)
    y = (o - mu) * lax.rsqrt(var + EPS)
    b, l, h, d = o.shape
    return y.reshape(b, l, h * d) * g.astype(jnp.float32)


def heads(t, n):
    return t.reshape(t.shape[0], t.shape[1], n, -1)


def split_proj(p):
    offsets = [int(o) for o in np.cumsum(PROJ_SIZES)[:-1]]
    return jnp.split(p, offsets, axis=-1)


def axial_rope_tables(rows, rot_dim):
    n_freq = rot_dim // 4
    inv_freq = ROPE_THETA ** (-jnp.arange(n_freq, dtype=jnp.float32) / n_freq)
    row = jnp.repeat(jnp.arange(rows, dtype=jnp.float32), GRID_W)
    col = jnp.tile(jnp.arange(GRID_W, dtype=jnp.float32), rows)
    ang_r = row[:, None] * inv_freq
    ang_c = col[:, None] * inv_freq
    ang = jnp.concatenate([ang_r, ang_r, ang_c, ang_c], axis=-1)
    return jnp.cos(ang), jnp.sin(ang)


def apply_rope(x, cos, sin):
    x1, x2, x3, x4 = jnp.split(x, 4, axis=-1)
    rot = jnp.concatenate([-x2, x1, -x4, x3], axis=-1)
    return (x * cos[:, None, :] + rot * sin[:, None, :]).astype(x.dtype)


def joint_softmax(scores, sink=None):
    m = scores[0].max(axis=-1, keepdims=True)
    for s in scores[1:]:
        m = jnp.maximum(m, s.max(axis=-1, keepdims=True))
    if sink is not None:
        m = jnp.maximum(m, sink)
    ps = [jnp.exp(s - m) for s in scores]
    denom = sum(p.sum(axis=-1, keepdims=True) for p in ps)
    if sink is not None:
        denom = denom + jnp.exp(sink - m)
    return [p / denom for p in ps]


def swa_latent(q, k, v, k_c, v_c, sink):
    b, n, hq, d = q.shape
    hkv = k.shape[2]
    rep = hq // hkv
    blk = SWA_BLOCK
    nb = n // blk
    scale = d ** -0.5
    qb = q.reshape(b, nb, blk, hkv, rep, d)

    def band(t):
        tb = t.reshape(b, nb, blk, hkv, d)
        pad = jnp.zeros_like(tb[:, :1])
        prev = jnp.concatenate([pad, tb[:, :-1]], axis=1)
        nxt = jnp.concatenate([tb[:, 1:], pad], axis=1)
        return jnp.concatenate([prev, tb, nxt], axis=2)

    kb, vb = band(k), band(v)
    q_off = jnp.arange(blk)[:, None] + blk
    k_off = jnp.arange(3 * blk)[None, :]
    key_pos = (jnp.arange(nb)[:, None] - 1) * blk + jnp.arange(3 * blk)[None, :]
    valid = (jnp.abs(q_off - k_off) <= SWA_WINDOW)[None] & ((key_pos >= 0) & (key_pos < n))[:, None, :]
    s_loc = jnp.einsum('bnqgrd,bnkgd->bngrqk', qb, kb).astype(jnp.float32) * scale
    s_loc = jnp.where(valid[None, :, None, None], s_loc, NEG_INF)
    s_ctx = jnp.einsum('bnqgrd,bmgd->bngrqm', qb, k_c).astype(jnp.float32) * scale
    sink_b = sink.astype(jnp.float32).reshape(1, 1, hkv, rep, 1, 1)
    p_loc, p_ctx = joint_softmax([s_loc, s_ctx], sink_b)
    o = (jnp.einsum('bngrqk,bnkgd->bnqgrd', p_loc.astype(v.dtype), vb)
         + jnp.einsum('bngrqm,bmgd->bnqgrd', p_ctx.astype(v.dtype), v_c))
    return o.reshape(b, n, hq * d)


def swa_context(q, k, v, sink):
    b, m, hq, d = q.shape
    hkv = k.shape[2]
    rep = hq // hkv
    s = jnp.einsum('bqgrd,bkgd->bgrqk', q.reshape(b, m, hkv, rep, d), k).astype(jnp.float32) * d ** -0.5
    (p,) = joint_softmax([s], sink.astype(jnp.float32).reshape(1, hkv, rep, 1, 1))
    return jnp.einsum('bgrqk,bkgd->bqgrd', p.astype(v.dtype), v).reshape(b, m, hq * d)


def directional_scan(chunked_fn, seqs, consts, s0, reverse):
    if reverse:
        seqs = tuple(jnp.flip(t, axis=1) for t in seqs)
    o, s = chunked_fn(*seqs, *consts, s0)
    if reverse:
        o = jnp.flip(o, axis=1)
    return o, s


def prefix_bidirectional_scan(chunked_fn, seqs_c, seqs_x, consts, state_shape):
    out_c, out_x = 0.0, 0.0
    for d in range(2):
        rev = d == 1
        s0 = jnp.zeros(state_shape, jnp.float32)
        oc, s_ctx = directional_scan(chunked_fn, seqs_c[d], consts[d], s0, rev)
        ox, _ = directional_scan(chunked_fn, seqs_x[d], consts[d], s_ctx, rev)
        out_c = out_c + oc
        out_x = out_x + ox
    return out_c, out_x


def short_conv(x, w):
    k = w.shape[0]
    y = lax.conv_general_dilated(
        x, w[:, None, :].astype(x.dtype), window_strides=(1,), padding=[(k // 2, k // 2)],
        dimension_numbers=('NWC', 'WIO', 'NWC'), feature_group_count=x.shape[-1])
    return jax.nn.silu(y)


def gated_delta_chunked(q, k, v, g, beta, s0):
    b, l, h, _ = q.shape
    dv = v.shape[-1]
    cs = GDN_CHUNK
    nc = l // cs

    def chunks(t):
        return t.reshape(b, nc, cs, h, -1).transpose(1, 0, 3, 2, 4)

    qc, kc, vc = chunks(q), chunks(k), chunks(v)
    gc = jnp.cumsum(chunks(g[..., None])[..., 0], axis=-1)
    bc = chunks(beta[..., None])
    idx = jnp.arange(cs)
    lower = idx[:, None] >= idx[None, :]
    strict = idx[:, None] > idx[None, :]
    decay = jnp.exp(jnp.where(lower, gc[..., :, None] - gc[..., None, :], NEG_INF))
    kb = kc * bc
    lmat = jnp.where(strict, jnp.einsum('nbhid,nbhjd->nbhij', kb, kc) * decay, 0.0)
    a_mat = lmat + jnp.eye(cs, dtype=jnp.float32)
    u = lax.linalg.triangular_solve(a_mat, vc * bc, left_side=True, lower=True)
    w = lax.linalg.triangular_solve(a_mat, kb * jnp.exp(gc)[..., None], left_side=True, lower=True)
    attn = jnp.einsum('nbhid,nbhjd->nbhij', qc, kc) * decay

    def step(s, xs):
        q_i, k_i, u_i, w_i, a_i, g_i = xs
        v_new = u_i - jnp.einsum('bhck,bhkv->bhcv', w_i, s)
        o_i = (jnp.einsum('bhck,bhkv->bhcv', q_i * jnp.exp(g_i)[..., None], s)
               + jnp.einsum('bhij,bhjv->bhiv', a_i, v_new))
        g_last = g_i[..., -1:]
        s = (s * jnp.exp(g_last)[..., None]
             + jnp.einsum('bhck,bhcv->bhkv', k_i * jnp.exp(g_last - g_i)[..., None], v_new))
        return s, o_i

    s_fin, o = lax.scan(step, s0, (qc, kc, u, w, attn, gc))
    return o.transpose(1, 0, 3, 2, 4).reshape(b, l, h, dv), s_fin


def gdn_mixer(parts_c, parts_x, need_ctx, conv_w, a_log, dt_bias, norm_g):
    def prep(qkv, ab):
        b, l, _ = qkv.shape
        qkv = short_conv(qkv, conv_w).astype(jnp.float32)
        q, k, v = jnp.split(qkv, [GDN_HEADS * GDN_DK, 2 * GDN_HEADS * GDN_DK], axis=-1)
        q = l2_normalize(q.reshape(b, l, GDN_HEADS, GDN_DK)) * GDN_DK ** -0.5
        k = l2_normalize(k.reshape(b, l, GDN_HEADS, GDN_DK))
        v = v.reshape(b, l, GDN_HEADS, GDN_DV)
        ab = ab.astype(jnp.float32).reshape(b, l, 2, 2, GDN_HEADS)
        g = -jnp.exp(a_log.astype(jnp.float32)) * jax.nn.softplus(ab[:, :, :, 0] + dt_bias.astype(jnp.float32))
        beta = jax.nn.sigmoid(ab[:, :, :, 1])
        return [(q, k, v, g[:, :, d], beta[:, :, d]) for d in range(2)]

    qkv_c, gate_c, ab_c = parts_c
    qkv_x, gate_x, ab_x = parts_x
    s_shape = (qkv_x.shape[0], GDN_HEADS, GDN_DK, GDN_DV)
    o_c, o_x = prefix_bidirectional_scan(gated_delta_chunked, prep(qkv_c, ab_c), prep(qkv_x, ab_x),
                                         [(), ()], s_shape)

    def gated_out(o, gate):
        b, l = gate.shape[:2]
        y = rms_norm(o, norm_g) * jax.nn.silu(gate.astype(jnp.float32)).reshape(o.shape)
        return y.reshape(b, l, GDN_HEADS * GDN_DV).astype(gate.dtype)

    return (gated_out(o_c, gate_c) if need_ctx else None), gated_out(o_x, gate_x)


def mla_queries(cq, q_norm, w_q_up):
    q = heads(rms_norm(cq, q_norm) @ w_q_up, MLA_HEADS)
    return q[..., :MLA_NOPE], q[..., MLA_NOPE:]


def mla_keys_values(ckv, kv_norm, w_kv_up):
    kv = heads(rms_norm(ckv, kv_norm) @ w_kv_up, MLA_HEADS)
    return kv[..., :MLA_NOPE], kv[..., MLA_NOPE:]


def mla_latent(qn, qr, kn, kr, v, kn_c, kr_c, v_c):
    b, n, h, _ = qn.shape
    nb = n // MLA_BLOCK
    scale = (MLA_NOPE + MLA_ROPE) ** -0.5

    def blocks(t):
        return jnp.moveaxis(t.reshape((b, nb, MLA_BLOCK) + t.shape[2:]), 1, 0)

    def one_block(args):
        qn_i, qr_i = args
        s_lat = (jnp.einsum('bqhd,bkhd->bhqk', qn_i, kn)
                 + jnp.einsum('bqhr,bkr->bhqk', qr_i, kr)).astype(jnp.float32) * scale
        s_ctx = (jnp.einsum('bqhd,bkhd->bhqk', qn_i, kn_c)
                 + jnp.einsum('bqhr,bkr->bhqk', qr_i, kr_c)).astype(jnp.float32) * scale
        p_lat, p_ctx = joint_softmax([s_lat, s_ctx])
        return (jnp.einsum('bhqk,bkhd->bqhd', p_lat.astype(v.dtype), v)
                + jnp.einsum('bhqk,bkhd->bqhd', p_ctx.astype(v.dtype), v_c))

    o = lax.map(one_block, (blocks(qn), blocks(qr)))
    return jnp.moveaxis(o, 0, 1).reshape(b, n, h * MLA_V)


def mla_context(qn, qr, kn, kr, v):
    b, m, h, _ = qn.shape
    scale = (MLA_NOPE + MLA_ROPE) ** -0.5
    s = (jnp.einsum('bqhd,bkhd->bhqk', qn, kn)
         + jnp.einsum('bqhr,bkr->bhqk', qr, kr)).astype(jnp.float32) * scale
    (p,) = joint_softmax([s])
    return jnp.einsum('bhqk,bkhd->bqhd', p.astype(v.dtype), v).reshape(b, m, h * MLA_V)


def retention_chunked(q, k, v, log_gamma, s0):
    b, l, h, _ = q.shape
    dv = v.shape[-1]
    cs = RET_CHUNK
    nc = l // cs

    def chunks(t):
        return t.reshape(b, nc, cs, h, -1).transpose(1, 0, 3, 2, 4)

    qc, kc, vc = chunks(q), chunks(k), chunks(v)
    pos = jnp.arange(cs, dtype=jnp.float32)
    lg = log_gamma[:, None]
    rel = pos[:, None] - pos[None, :]
    decay = jnp.where(rel >= 0, jnp.exp(lg[..., None] * jnp.maximum(rel, 0.0)), 0.0)
    q_decay = jnp.exp(lg * (pos + 1.0))[None, :, :, None]
    k_decay = jnp.exp(lg * (cs - 1.0 - pos))[None, :, :, None]
    chunk_decay = jnp.exp(log_gamma * cs)[None, :, None, None]
    intra = jnp.einsum('nbhij,nbhjv->nbhiv', jnp.einsum('nbhid,nbhjd->nbhij', qc, kc) * decay, vc)

    def step(s, xs):
        q_i, k_i, v_i = xs
        o_i = jnp.einsum('bhck,bhkv->bhcv', q_i * q_decay, s)
        s = s * chunk_decay + jnp.einsum('bhck,bhcv->bhkv', k_i * k_decay, v_i)
        return s, o_i

    s_fin, inter = lax.scan(step, s0, (qc, kc, vc))
    o = intra + inter
    return o.transpose(1, 0, 3, 2, 4).reshape(b, l, h, dv), s_fin


def retention_mixer(parts_c, parts_x, need_ctx, log_decay, norm_g, cos, sin):
    def prep(parts, rotate):
        q, k, v, _ = parts
        q = heads(q, RET_HEADS)
        k = heads(k, RET_HEADS)
        if rotate:
            q = apply_rope(q, cos, sin)
            k = apply_rope(k, cos, sin)
        seq = (q.astype(jnp.float32), k.astype(jnp.float32) * RET_DK ** -0.5,
               heads(v, RET_HEADS).astype(jnp.float32))
        return [seq, seq]

    log_gamma = -jnp.exp(log_decay.astype(jnp.float32))
    consts = [(log_gamma[0],), (log_gamma[1],)]
    s_shape = (parts_x[0].shape[0], RET_HEADS, RET_DK, RET_DV)
    o_c, o_x = prefix_bidirectional_scan(retention_chunked, prep(parts_c, False), prep(parts_x, True),
                                         consts, s_shape)

    def gated_out(o, gate):
        return (head_group_norm(o, norm_g) * jax.nn.silu(gate.astype(jnp.float32))).astype(gate.dtype)

    return (gated_out(o_c, parts_c[3]) if need_ctx else None), gated_out(o_x, parts_x[3])


def token_mixers(u_c, u_x, need_ctx, w_in, w_out, swa_sink, gdn_conv, gdn_a_log, gdn_dt_bias, gdn_norm,
                 mla_q_norm, mla_kv_norm, mla_w_q_up, mla_w_kv_up, ret_log_decay, ret_norm,
                 rope_hd, rope_mla):
    cos_h, sin_h = rope_hd
    cos_r, sin_r = rope_mla
    pc = split_proj(u_c @ w_in)
    px = split_proj(u_x @ w_in)
    ak_c, av_c = heads(pc[1], SWA_KV_HEADS), heads(pc[2], SWA_KV_HEADS)
    aq_x = apply_rope(heads(px[0], SWA_HEADS), cos_h, sin_h)
    ak_x = apply_rope(heads(px[1], SWA_KV_HEADS), cos_h, sin_h)
    out_a_x = swa_latent(aq_x, ak_x, heads(px[2], SWA_KV_HEADS), ak_c, av_c, swa_sink)
    out_b_c, out_b_x = gdn_mixer(pc[3:6], px[3:6], need_ctx, gdn_conv, gdn_a_log, gdn_dt_bias, gdn_norm)
    kn_c, v_c = mla_keys_values(pc[7], mla_kv_norm, mla_w_kv_up)
    kn_x, v_x = mla_keys_values(px[7], mla_kv_norm, mla_w_kv_up)
    qn_x, qr_x = mla_queries(px[6], mla_q_norm, mla_w_q_up)
    qr_x = apply_rope(qr_x, cos_r, sin_r)
    kr_x = apply_rope(px[8][:, :, None, :], cos_r, sin_r)[:, :, 0, :]
    out_c_x = mla_latent(qn_x, qr_x, kn_x, kr_x, v_x, kn_c, pc[8], v_c)
    out_d_c, out_d_x = retention_mixer(pc[9:13], px[9:13], need_ctx, ret_log_decay, ret_norm, cos_h, sin_h)
    mix_x = jnp.concatenate([out_a_x, out_b_x, out_c_x, out_d_x], axis=-1) @ w_out
    if not need_ctx:
        return None, mix_x
    out_a_c = swa_context(heads(pc[0], SWA_HEADS), ak_c, av_c, swa_sink)
    qn_c, qr_c = mla_queries(pc[6], mla_q_norm, mla_w_q_up)
    out_c_c = mla_context(qn_c, qr_c, kn_c, pc[8], v_c)
    mix_c = jnp.concatenate([out_a_c, out_b_c, out_c_c, out_d_c], axis=-1) @ w_out
    return mix_c, mix_x


def swiglu(x, w_gate, w_up, w_down):
    return (jax.nn.silu(x @ w_gate) * (x @ w_up)) @ w_down


def moe_swiglu(x, w_router, w_gate, w_up, w_down):
    logits = (x @ w_router).astype(jnp.float32)
    top_val, top_idx = lax.top_k(logits, TOP_K)
    gates = jax.nn.softmax(top_val, axis=-1)
    combine = jnp.sum(jax.nn.one_hot(top_idx, N_EXPERTS, dtype=jnp.float32) * gates[..., None], axis=1)
    out = jnp.zeros_like(x)
    for e in range(N_EXPERTS):
        out = out + combine[:, e:e + 1].astype(x.dtype) * swiglu(x, w_gate[e], w_up[e], w_down[e])
    return out


def setup_inputs(seed: int = 0) -> dict:
    key = jax.random.key(seed)
    keys = iter(jax.random.split(key, 40))
    f32 = jnp.float32
    d = D_MODEL
    n_dense = (DEPTH + 1) // 2
    n_moe = DEPTH // 2

    def normal(shape, scale):
        return jax.random.normal(next(keys), shape, f32) * scale

    def gain(shape):
        return 1.0 + normal(shape, 0.02)

    ret_base = jnp.log(-jnp.log1p(-(2.0 ** (-5.0 - jnp.arange(RET_HEADS, dtype=f32)))))
    dt = jnp.exp(jax.random.uniform(next(keys), (DEPTH, 2, GDN_HEADS), f32, math.log(1e-3), math.log(1e-1)))
    mix_w = N_MIXERS * GROUP_W
    return {
        'x': normal((BATCH, SEQ, d), 1.0),
        'c': normal((BATCH, d), 1.0),
        'ctx': normal((BATCH, CTX_LEN, d), 1.0),
        'c_ctx': normal((d,), 1.0),
        'w_mod': normal((DEPTH, d, 6 * d), 0.5 * d ** -0.5),
        'b_mod': normal((DEPTH, 6 * d), 0.02),
        'norm1': gain((DEPTH, d)),
        'norm2': gain((DEPTH, d)),
        'w_in': normal((DEPTH, d, D_IN), d ** -0.5),
        'w_out': normal((DEPTH, mix_w, d), mix_w ** -0.5),
        'swa_sink': normal((DEPTH, SWA_HEADS), 0.5),
        'gdn_conv': normal((DEPTH, GDN_CONV, GDN_HEADS * (2 * GDN_DK + GDN_DV)), GDN_CONV ** -0.5),
        'gdn_a_log': jnp.log(jax.random.uniform(next(keys), (DEPTH, 2, GDN_HEADS), f32, 1.0, 16.0)),
        'gdn_dt_bias': dt + jnp.log(-jnp.expm1(-dt)),
        'gdn_norm': gain((DEPTH, GDN_DV)),
        'mla_q_norm': gain((DEPTH, MLA_Q_RANK)),
        'mla_kv_norm': gain((DEPTH, MLA_KV_RANK)),
        'mla_w_q_up': normal((DEPTH, MLA_Q_RANK, MLA_HEADS * (MLA_NOPE + MLA_ROPE)), MLA_Q_RANK ** -0.5),
        'mla_w_kv_up': normal((DEPTH, MLA_KV_RANK, MLA_HEADS * (MLA_NOPE + MLA_V)), MLA_KV_RANK ** -0.5),
        'ret_log_decay': ret_base + normal((DEPTH, 2, RET_HEADS), 0.05),
        'ret_norm': gain((DEPTH, RET_HEADS * RET_DV)),
        'ffn_w_gate': normal((n_dense, d, D_FF), d ** -0.5),
        'ffn_w_up': normal((n_dense, d, D_FF), d ** -0.5),
        'ffn_w_down': normal((n_dense, D_FF, d), D_FF ** -0.5),
        'moe_router': normal((n_moe, d, N_EXPERTS), d ** -0.5),
        'moe_w_gate': normal((n_moe, N_EXPERTS, d, D_FF), d ** -0.5),
        'moe_w_up': normal((n_moe, N_EXPERTS, d, D_FF), d ** -0.5),
        'moe_w_down': normal((n_moe, N_EXPERTS, D_FF, d), D_FF ** -0.5),
        'final_norm': gain((d,)),
    }


def reference(x, c, ctx, c_ctx, w_mod, b_mod, norm1, norm2, w_in, w_out, swa_sink, gdn_conv, gdn_a_log,
              gdn_dt_bias, gdn_norm, mla_q_norm, mla_kv_norm, mla_w_q_up, mla_w_kv_up, ret_log_decay,
              ret_norm, ffn_w_gate, ffn_w_up, ffn_w_down, moe_router, moe_w_gate, moe_w_up, moe_w_down,
              final_norm):
    b, n, _ = x.shape
    m = ctx.shape[1]
    rows = n // GRID_W
    rope_hd = axial_rope_tables(rows, HEAD_DIM)
    rope_mla = axial_rope_tables(rows, MLA_ROPE)
    silu_c = jax.nn.silu(c)
    silu_cc = jax.nn.silu(c_ctx)
    h_x, h_c = x, ctx
    for layer in range(DEPTH):
        need_ctx = layer < DEPTH - 1
        mod_x = (silu_c @ w_mod[layer] + b_mod[layer])[:, None, :]
        mod_c = silu_cc @ w_mod[layer] + b_mod[layer]
        sh1_x, sc1_x, g1_x, sh2_x, sc2_x, g2_x = jnp.split(mod_x, 6, axis=-1)
        sh1_c, sc1_c, g1_c, sh2_c, sc2_c, g2_c = jnp.split(mod_c, 6, axis=-1)
        u_x = modulate(h_x, norm1[layer], sh1_x, sc1_x)
        u_c = modulate(h_c, norm1[layer], sh1_c, sc1_c)
        mix_c, mix_x = token_mixers(
            u_c, u_x, need_ctx, w_in[layer], w_out[layer], swa_sink[layer], gdn_conv[layer],
            gdn_a_log[layer], gdn_dt_bias[layer], gdn_norm[layer], mla_q_norm[layer], mla_kv_norm[layer],
            mla_w_q_up[layer], mla_w_kv_up[layer], ret_log_decay[layer], ret_norm[layer], rope_hd, rope_mla)
        h_x = h_x + g1_x * mix_x
        v_x = modulate(h_x, norm2[layer], sh2_x, sc2_x)
        if need_ctx:
            h_c = h_c + g1_c * mix_c
            tokens = jnp.concatenate([modulate(h_c, norm2[layer], sh2_c, sc2_c), v_x], axis=1)
        else:
            tokens = v_x
        lt = tokens.shape[1]
        flat = tokens.reshape(b * lt, D_MODEL)
        i = layer // 2
        if layer % 2 == 0:
            f = swiglu(flat, ffn_w_gate[i], ffn_w_up[i], ffn_w_down[i])
        else:
            f = moe_swiglu(flat, moe_router[i], moe_w_gate[i], moe_w_up[i], moe_w_down[i])
        f = f.reshape(b, lt, D_MODEL)
        if need_ctx:
            h_c = h_c + g2_c * f[:, :m]
            h_x = h_x + g2_x * f[:, m:]
        else:
            h_x = h_x + g2_x * f
    return rms_norm(h_x, final_norm)
```

```python
import numpy as np
import concourse.bass as bass
import concourse.mybir as mybir
from concourse.bass_utils import run_bass_kernel_spmd
from contextlib import ExitStack

F32 = mybir.dt.float32
BF16 = mybir.dt.bfloat16
AF = mybir.ActivationFunctionType
ALU = mybir.AluOpType
AX = mybir.AxisListType

ENGS = ("pe", "act", "dve", "pool", "sp")


class Op:
    __slots__ = ("eng", "fn", "deps", "is_dma", "sem", "val", "marked", "idx", "prewait")

    def __init__(self, eng, fn, is_dma):
        self.eng = eng
        self.fn = fn
        self.deps = []
        self.is_dma = is_dma
        self.sem = None
        self.val = None
        self.marked = False
        self.prewait = None


class Prog:
    def __init__(self, name="k", n_dma_sems=12):
        self.nc = bass.Bass("TRN2", target_bir_lowering=False)
        self.es = ExitStack()
        self.ops = {e: [] for e in ENGS}
        self.last_w = {}
        self.readers = {}
        self.n_dma_sems = n_dma_sems
        self.dma_rr = 0
        self.dma_last = [None] * n_dma_sems
        self.dma_cnt = [0] * n_dma_sems
        self.uid = 0

    def sb(self, shape, dt=F32, name=None):
        self.uid += 1
        return self.es.enter_context(self.nc.sbuf_tensor("s_" + (name or f"sb{self.uid}"), list(shape), dt))

    def ps(self, shape, dt=F32, name=None):
        self.uid += 1
        return self.es.enter_context(self.nc.psum_tensor("p_" + (name or f"ps{self.uid}"), list(shape), dt))

    def dram_in(self, name, shape, dt=F32):
        return self.nc.dram_tensor(name, list(shape), dt, kind="ExternalInput").ap()

    def dram_out(self, name, shape, dt=F32):
        return self.nc.dram_tensor(name, list(shape), dt, kind="ExternalOutput").ap()

    def dram_tmp(self, name, shape, dt=F32):
        return self.nc.dram_tensor(name, list(shape), dt, kind="Internal").ap()

    def _track(self, op, r, w):
        deps = []
        for k in r:
            lw = self.last_w.get(k)
            if lw is not None:
                deps.append(lw)
        for k in w:
            lw = self.last_w.get(k)
            if lw is not None:
                deps.append(lw)
            for rd in self.readers.get(k, ()):
                deps.append(rd)
        for k in r:
            self.readers.setdefault(k, []).append(op)
        for k in w:
            self.last_w[k] = op
            self.readers[k] = []
        op.deps = [d for d in deps if d is not op and not (d.eng == "pe" and op.eng == "pe" and not d.is_dma and not op.is_dma)]
        for d in op.deps:
            d.marked = True

    def add(self, eng, fn, r=(), w=()):
        op = Op(eng, fn, False)
        self._track(op, r, w)
        self.ops[eng].append(op)
        return op

    def dma(self, eng, out, in_, r=(), w=()):
        op = Op(eng, lambda e: e.dma_start(out=out, in_=in_), True)
        s = self.dma_rr
        self.dma_rr = (s + 1) % self.n_dma_sems
        op.prewait = self.dma_last[s]
        self.dma_cnt[s] += 16
        op.sem = s
        op.val = self.dma_cnt[s]
        op.marked = True
        self.dma_last[s] = op
        self._track(op, r, w)
        self.ops[eng].append(op)
        return op

    def finish(self):
        nc = self.nc
        es = self.es
        esem = {e: es.enter_context(nc.semaphore(f"sem_{e}")) for e in ENGS}
        dsem = [es.enter_context(nc.semaphore(f"sem_dma{i}")) for i in range(self.n_dma_sems)]
        for e in ENGS:
            c = 0
            for op in self.ops[e]:
                if op.is_dma:
                    continue
                if op.marked:
                    c += 1
                    op.val = c
                    op.sem = e
        block = es.enter_context(nc.Block())
        ops = self.ops

        def emit(e, eng):
            waited = {}
            for op in ops[e]:
                need = {}
                dl = list(op.deps)
                if op.prewait is not None:
                    dl.append(op.prewait)
                for d in dl:
                    key = ("d", d.sem) if d.is_dma else ("e", d.sem)
                    if need.get(key, 0) < d.val:
                        need[key] = d.val
                for key, v in need.items():
                    if waited.get(key, 0) >= v:
                        continue
                    waited[key] = v
                    sem = dsem[key[1]] if key[0] == "d" else esem[key[1]]
                    eng.wait_ge(sem, v)
                ins = op.fn(eng)
                if op.is_dma:
                    ins.then_inc(dsem[op.sem], 16)
                elif op.marked:
                    ins.then_inc(esem[e], 1)
            if e == "sp":
                for i in range(self.n_dma_sems):
                    if self.dma_cnt[i] > 0:
                        eng.wait_ge(dsem[i], self.dma_cnt[i])

        @block.tensor
        def _(eng):
            emit("pe", eng)

        @block.scalar
        def _(eng):
            emit("act", eng)

        @block.vector
        def _(eng):
            emit("dve", eng)

        @block.gpsimd
        def _(eng):
            emit("pool", eng)

        @block.sync
        def _(eng):
            emit("sp", eng)

        es.close()
        return nc

    def n_ops(self):
        return {e: len(v) for e, v in self.ops.items()}


def run(nc, in_maps, trace=False):
    res = run_bass_kernel_spmd(nc, in_maps, core_ids=list(range(len(in_maps))), trace=trace)
    return res


TILES = [(0, 256), (256, 512), (768, 512), (1280, 512), (1792, 384)]
TC = 2176
EPS = 1e-6
F_BLOCKS = [("Aq", 256), ("AqP", 256), ("Ak", 128), ("AkP", 128), ("Bqkv", 768), ("Bgate", 256), ("Ccq", 256),
            ("Cckv", 128), ("Ckr", 32), ("CkrP", 32), ("Dq", 256), ("DqP", 256), ("Dk", 256), ("DkP", 256), ("Dgate", 256)]
T_BLOCKS = [("Av", 128), ("Bab", 16), ("Dv", 256), ("Dk", 256), ("DkP", 256)]
NF = sum(n for _, n in F_BLOCKS)
NT = sum(n for _, n in T_BLOCKS)


def foff(name, blocks):
    o = 0
    for nm, n in blocks:
        if nm == name:
            return o, n
        o += n
    raise KeyError(name)


class Rot:
    def __init__(self, bufs, name):
        self.bufs = bufs
        self.i = 0
        self.name = name

    def next(self):
        b = self.bufs[self.i % len(self.bufs)]
        k = (self.name, self.i % len(self.bufs))
        self.i += 1
        return b, k


def emit_norm_mod(P, C, ht, hkey, n, Asc, shv, t, u, ukey):
    sq, ssps, rs, tmp = C["sq"], C["ssps"], C["rs"], C["tmp"]
    P.add("act", lambda e: e.activation(out=sq[:, :, :n], in_=ht[:, :, :n], func=AF.Square), r=[hkey], w=["sq"])
    for k in range(8):
        P.add("pe", lambda e, k=k: e.matmul(ssps[:, :n], C["ones"][:, :], sq[:, k, :n], start=(k == 0), stop=(k == 7)),
              r=["sq", "ones"], w=["ssps"])
    P.add("act", lambda e: e.activation(out=rs[:, :n], in_=ssps[:, :n], func=AF.Sqrt, scale=1.0 / 1024, bias=C["epsb"][:, 0:1]),
          r=["ssps", "epsb"], w=["rs"])
    P.add("dve", lambda e: e.reciprocal(out=rs[:, :n], in_=rs[:, :n]), r=["rs"], w=["rs"])
    for k in range(8):
        P.add("dve", lambda e, k=k: e.tensor_tensor(out=tmp[:, k, :n], in0=ht[:, k, :n], in1=rs[:, :n], op=ALU.mult),
              r=[hkey, "rs"], w=[("tmp", k)])
        P.add("act", lambda e, k=k: e.activation(out=u[:, k, :n], in_=tmp[:, k, :n], func=AF.Identity,
                                                scale=Asc[:, t, k:k + 1], bias=shv[:, t, k:k + 1]),
              r=[("tmp", k), "modc"], w=[ukey])


def common_consts(P):
    C = {}
    C["ones"] = P.sb([128, 128], F32, "ones")
    P.add("pool", lambda e: e.memset(C["ones"][:], 1.0), w=["ones"])
    C["epsb"] = P.sb([128, 1], F32, "epsb")
    P.add("pool", lambda e: e.memset(C["epsb"][:], EPS), w=["epsb"])
    C["sq"] = P.sb([128, 8, 512], F32, "sq")
    C["tmp"] = P.sb([128, 8, 512], F32, "tmp")
    C["rs"] = P.sb([128, 512], F32, "rs")
    C["ssps"] = P.ps([128, 512], F32, "ssps")
    return C


def load_mod(P, modt_d, gn_d, kinds):
    modt = P.sb([128, 6, 5, 8], F32, "modt")
    P.dma("sp", modt[:].rearrange("p a b c -> p (a b c)"), modt_d[:, :], w=["modt"])
    return modt


def build_A():
    P = Prog()
    hT = P.dram_in("hT", [1024, TC])
    modt_d = P.dram_in("modt", [128, 240])
    gn_d = P.dram_in("gn", [128, 8])
    win = P.dram_in("win", [1024, NF + NT])
    projT = P.dram_out("projT", [NF, TC])
    projTok = P.dram_out("projTok", [TC, NT])
    C = common_consts(P)
    emit_A_body(P, C, hT, None, modt_d, gn_d, win, projT, projTok)
    return P.finish()


def emit_A_setup(P, C, modt_d, gn_d, win, pre=""):
    modt = P.sb([128, 6, 5, 8], F32, pre + "modt")
    P.dma("sp", modt[:].rearrange("p a b c -> p (a b c)"), modt_d[:, :], w=[pre + "modt"])
    gn = P.sb([128, 8], F32, pre + "gn")
    P.dma("sp", gn[:], gn_d[:, :], w=[pre + "gn"])
    A1 = P.sb([128, 5, 8], F32, pre + "A1")
    for t in range(5):
        P.add("dve", lambda e, t=t: e.scalar_tensor_tensor(out=A1[:, t, :], in0=modt[:, 1, t, :], scalar=1.0, in1=gn[:, :],
                                                          op0=ALU.add, op1=ALU.mult), r=[pre + "modt", pre + "gn"], w=["modc"])
    wb = P.sb([128, 8, NF + NT], BF16, pre + "wb")
    for k in range(8):
        P.dma("pool", wb[:, k, :], win[k * 128:(k + 1) * 128, :], w=[("wb", k)])
    return modt, A1, wb


def emit_A_tile(P, C, S, ti, ht, hkey, projT, projTok):
    modt, A1, wb = S["modt"], S["A1"], S["wb"]
    t0, n = TILES[ti]
    u, ukey = S["u"].next()
    emit_norm_mod(P, C, ht, hkey, n, A1, modt[:, 0], ti, u, ukey)
    wkeys = [("wb", k) for k in range(8)]
    ncc = (NF + 127) // 128
    for cc in range(ncc):
        c0 = cc * 128
        m = min(128, NF - c0)
        ps, pk = S["mmps"].next()
        for k in range(8):
            P.add("pe", lambda e, k=k, ps=ps, c0=c0, m=m: e.matmul(ps[:m, :n], wb[:, k, c0:c0 + m], u[:, k, :n], start=(k == 0), stop=(k == 7)),
                  r=[ukey] + wkeys, w=[pk])
        st, sk = S["stage"].next()
        if cc % 2 == 0:
            P.add("act", lambda e, ps=ps, st=st, m=m: e.copy(out=st[:m, :n], in_=ps[:m, :n]), r=[pk], w=[sk])
        else:
            P.add("dve", lambda e, ps=ps, st=st, m=m: e.tensor_copy(out=st[:m, :n], in_=ps[:m, :n]), r=[pk], w=[sk])
        P.dma("sp", projT[c0:c0 + m, t0:t0 + n], st[:m, :n], r=[sk])
    for s in range(n // 128):
        for (c0, c1) in ((0, 512), (512, NT)):
            ps, pk = S["mmps"].next()
            w_ = c1 - c0
            for k in range(8):
                P.add("pe", lambda e, k=k, ps=ps, c0=c0, w_=w_, s=s: e.matmul(ps[:, :w_], u[:, k, s * 128:(s + 1) * 128], wb[:, k, NF + c0:NF + c0 + w_],
                                                                         start=(k == 0), stop=(k == 7)), r=[ukey] + wkeys, w=[pk])
            st, sk = S["stage"].next()
            P.add("dve" if s % 2 else "act", (lambda e, ps=ps, st=st, w_=w_: e.tensor_copy(out=st[:, :w_], in_=ps[:, :w_])) if s % 2 else
                  (lambda e, ps=ps, st=st, w_=w_: e.copy(out=st[:, :w_], in_=ps[:, :w_])), r=[pk], w=[sk])
            P.dma("sp", projTok[t0 + s * 128:t0 + (s + 1) * 128, c0:c1], st[:, :w_], r=[sk])


def emit_A_body(P, C, hT, _, modt_d, gn_d, win, projT, projTok):
    modt, A1, wb = emit_A_setup(P, C, modt_d, gn_d, win)
    S = {"modt": modt, "A1": A1, "wb": wb}
    S["u"] = Rot([P.sb([128, 8, 512], BF16, f"u{i}") for i in range(2)], "u")
    S["mmps"] = Rot([P.ps([128, 512], F32, f"mmps{i}") for i in range(4)], "mmps")
    S["stage"] = Rot([P.sb([128, 512], F32, f"stg{i}") for i in range(4)], "stg")
    hts = Rot([P.sb([128, 8, 512], F32, f"ht{i}") for i in range(2)], "ht")
    hv = hT.rearrange("(k p) t -> p k t", p=128)
    for ti, (t0, n) in enumerate(TILES):
        ht, hk = hts.next()
        P.dma("sp", ht[:, :, :n], hv[:, :, t0:t0 + n], w=[hk])
        emit_A_tile(P, C, S, ti, ht, hk, projT, projTok)


DFF = 3584
NFG = 7


def build_C(moe, final, ntiles=9):
    TC = ntiles * 256
    CT = [(i * 256, 256) for i in range(ntiles)]
    NE = 8 if moe else 1
    P = Prog()
    hT = P.dram_in("hT", [1024, TC])
    mixT = P.dram_in("mixT", [1024, TC])
    wout = P.dram_in("wout", [1024, 1024])
    modt_d = P.dram_in("modt", [128, 6 * ntiles * 8])
    gn_d = P.dram_in("gn", [128, 8])
    wg = P.dram_in("wg", [NE, 1024, DFF])
    wu = P.dram_in("wu", [NE, 1024, DFF])
    wd = P.dram_in("wd", [NE, DFF, 1024])
    if moe:
        wr_d = P.dram_in("wr", [1024, 8])
        ident_d = P.dram_in("ident", [128, 128])
    if final:
        fn_d = P.dram_in("fn", [128, 8])
    h2T = P.dram_out("h2T", [1024, TC])
    C = common_consts(P)
    for nm in ("sq", "tmp"):
        pass
    modt = P.sb([128, 6, ntiles, 8], F32, "modt")
    P.dma("sp", modt[:].rearrange("p a b c -> p (a b c)"), modt_d[:, :], w=["modt"])
    gn = P.sb([128, 8], F32, "gn")
    P.dma("sp", gn[:], gn_d[:, :], w=["gn"])
    A2 = P.sb([128, ntiles, 8], F32, "A2")
    for t in range(ntiles):
        P.add("dve", lambda e, t=t: e.scalar_tensor_tensor(out=A2[:, t, :], in0=modt[:, 4, t, :], scalar=1.0, in1=gn[:, :],
                                                          op0=ALU.add, op1=ALU.mult), r=["modt", "gn"], w=["modc"])
    wo = P.sb([128, 8, 1024], BF16, "wo")
    for k in range(8):
        P.dma("pool", wo[:, k, :], wout[k * 128:(k + 1) * 128, :], w=["wo"])
    if moe:
        wr = P.sb([128, 8, 8], F32, "wr")
        P.dma("sp", wr[:], wr_d.rearrange("(k p) e -> p k e", p=128), w=["wr"])
        ident = P.sb([128, 128], F32, "ident")
        P.dma("sp", ident[:], ident_d[:, :], w=["ident"])
        lg = P.sb([128, 2, 8], F32, "lg")
        l2 = P.sb([128, 2, 8], F32, "l2")
        mk1 = P.sb([128, 2, 8], F32, "mk1")
        mk2 = P.sb([128, 2, 8], F32, "mk2")
        comb = P.sb([128, 2, 8], F32, "comb")
        m12 = P.sb([128, 2, 4], F32, "m12")
        rep = Rot([P.sb([128, 128], F32, f"rep{i}") for i in range(2)], "rep")
        cB = P.sb([128, 8, 256], F32, "cB")
        uf = P.sb([128, 8, 256], F32, "uf")
    if final:
        fng = P.sb([128, 8], F32, "fng")
        P.dma("sp", fng[:], fn_d[:, :], w=["fng"])
        outT = P.dram_out("outT", [1024, TC])
    ht = P.sb([128, 8, 256], F32, "ht")
    mixb = P.sb([128, 8, 256], BF16, "mixb")
    h1 = P.sb([128, 8, 256], F32, "h1")
    h2 = P.sb([128, 8, 256], F32, "h2")
    u = P.sb([128, 8, 256], BF16, "u")
    hh = Rot([P.sb([128, 4, 256], BF16, f"hh{i}") for i in range(2)], "hh")
    sg = Rot([P.sb([128, 256], F32, f"sg{i}") for i in range(2)], "sg")
    t1 = Rot([P.sb([128, 256], F32, f"t1{i}") for i in range(2)], "t1")
    wgs = Rot([P.sb([128, 8, 512], BF16, f"wgs{i}") for i in range(2)], "wgs")
    wus = Rot([P.sb([128, 8, 512], BF16, f"wus{i}") for i in range(2)], "wus")
    wds = Rot([P.sb([128, 4, 1024], BF16, f"wds{i}") for i in range(2)], "wds")
    gups = Rot([P.ps([128, 2, 256], F32, f"gups{i}") for i in range(2)], "gups")
    yps = [P.ps([128, 2, 256], F32, f"yps{i}") for i in range(4)]
    misc = P.ps([128, 512], F32, "misc")
    mps = Rot([misc[:, 0:256]], "misc")
    cbps = misc[:, 256:512]
    sq, tmp, rs, ssps = C["sq"], C["tmp"], C["rs"], C["ssps"]
    lgps = ssps[:, 384:400].rearrange("p (s e) -> p s e", e=8)
    hv = hT.rearrange("(k p) t -> p k t", p=128)
    mv = mixT.rearrange("(k p) t -> p k t", p=128)
    ov = h2T.rearrange("(k p) t -> p k t", p=128)

    def norm_parts(src, skey, n):
        P.add("act", lambda e: e.activation(out=sq[:, :, :n], in_=src[:, :, :n], func=AF.Square), r=[skey], w=["sq"])
        for k in range(8):
            P.add("pe", lambda e, k=k: e.matmul(ssps[:, :n], C["ones"][:, :], sq[:, k, :n], start=(k == 0), stop=(k == 7)),
                  r=["sq", "ones"], w=["ssps"])
        P.add("act", lambda e: e.activation(out=rs[:, :n], in_=ssps[:, :n], func=AF.Sqrt, scale=1.0 / 1024, bias=C["epsb"][:, 0:1]),
              r=["ssps", "epsb"], w=["rs"])
        P.add("dve", lambda e: e.reciprocal(out=rs[:, :n], in_=rs[:, :n]), r=["rs"], w=["rs"])

    def tile_body(ti, t0, n):
        P.dma("sp", ht[:, :, :n], hv[:, :, t0:t0 + n], w=["ht"])
        P.dma("pool", mixb[:, :, :n], mv[:, :, t0:t0 + n], w=["mixb"])
        for dm in range(8):
            ps, pk = mps.next()
            for k in range(8):
                P.add("pe", lambda e, k=k, dm=dm, ps=ps: e.matmul(ps[:, :n], wo[:, k, dm * 128:(dm + 1) * 128], mixb[:, k, :n],
                                                                 start=(k == 0), stop=(k == 7)), r=["wo", "mixb"], w=[pk])
            P.add("dve", lambda e, dm=dm, ps=ps: e.scalar_tensor_tensor(out=h1[:, dm, :n], in0=ps[:, :n], scalar=modt[:, 2, ti, dm:dm + 1],
                                                                      in1=ht[:, dm, :n], op0=ALU.mult, op1=ALU.add),
                  r=[pk, "ht", "modt"], w=["h1"])
        norm_parts(h1, "h1", n)
        for k in range(8):
            P.add("dve", lambda e, k=k: e.tensor_tensor(out=tmp[:, k, :n], in0=h1[:, k, :n], in1=rs[:, :n], op=ALU.mult),
                  r=["h1", "rs"], w=[("tmp", k)])
            if moe:
                P.add("act", lambda e, k=k: e.activation(out=uf[:, k, :n], in_=tmp[:, k, :n], func=AF.Identity,
                                                        scale=A2[:, ti, k:k + 1], bias=modt[:, 3, ti, k:k + 1]),
                      r=[("tmp", k), "modc", "modt"], w=[("uf", k)])
                P.add("pool", lambda e, k=k: e.tensor_copy(out=u[:, k, :n], in_=uf[:, k, :n]), r=[("uf", k)], w=["u"])
            else:
                P.add("act", lambda e, k=k: e.activation(out=u[:, k, :n], in_=tmp[:, k, :n], func=AF.Identity,
                                                        scale=A2[:, ti, k:k + 1], bias=modt[:, 3, ti, k:k + 1]),
                      r=[("tmp", k), "modc", "modt"], w=["u"])
        ns = n // 128
        if moe:
            for s in range(ns):
                for k in range(8):
                    P.add("pe", lambda e, k=k, s=s: e.matmul(lgps[:, s, :], uf[:, k, s * 128:(s + 1) * 128], wr[:, k, :], start=(k == 0), stop=(k == 7)),
                          r=[("uf", kk) for kk in range(8)] + ["wr"], w=["ssps"])
            P.add("dve", lambda e: e.tensor_copy(out=lg[:, :ns, :], in_=lgps[:, :ns, :]), r=["ssps"], w=["lg"])
            for s in range(ns):
                P.add("dve", lambda e, s=s: e.reduce_max(out=m12[:, s, 0:1], in_=lg[:, s, :], axis=AX.X), r=["lg"], w=["m12"])
                P.add("dve", lambda e, s=s: e.tensor_scalar(out=mk1[:, s, :], in0=lg[:, s, :], scalar1=m12[:, s, 0:1], scalar2=None, op0=ALU.is_equal),
                      r=["lg", "m12"], w=["mk1"])
                P.add("dve", lambda e, s=s: e.scalar_tensor_tensor(out=l2[:, s, :], in0=mk1[:, s, :], scalar=-1e30, in1=lg[:, s, :], op0=ALU.mult, op1=ALU.add),
                      r=["mk1", "lg"], w=["l2"])
                P.add("dve", lambda e, s=s: e.reduce_max(out=m12[:, s, 1:2], in_=l2[:, s, :], axis=AX.X), r=["l2", "m12"], w=["m12"])
                P.add("dve", lambda e, s=s: e.tensor_scalar(out=mk2[:, s, :], in0=l2[:, s, :], scalar1=m12[:, s, 1:2], scalar2=None, op0=ALU.is_equal),
                      r=["l2", "m12"], w=["mk2"])
                P.add("dve", lambda e, s=s: e.tensor_tensor(out=m12[:, s, 2:3], in0=m12[:, s, 1:2], in1=m12[:, s, 0:1], op=ALU.subtract), r=["m12"], w=["m12"])
                P.add("act", lambda e, s=s: e.activation(out=m12[:, s, 2:3], in_=m12[:, s, 2:3], func=AF.Exp), r=["m12"], w=["m12"])
                P.add("dve", lambda e, s=s: e.tensor_scalar(out=m12[:, s, 3:4], in0=m12[:, s, 2:3], scalar1=1.0, scalar2=None, op0=ALU.add), r=["m12"], w=["m12"])
                P.add("dve", lambda e, s=s: e.reciprocal(out=m12[:, s, 3:4], in_=m12[:, s, 3:4]), r=["m12"], w=["m12"])
                P.add("dve", lambda e, s=s: e.tensor_tensor(out=m12[:, s, 2:3], in0=m12[:, s, 2:3], in1=m12[:, s, 3:4], op=ALU.mult), r=["m12"], w=["m12"])
                P.add("dve", lambda e, s=s: e.tensor_scalar(out=comb[:, s, :], in0=mk1[:, s, :], scalar1=m12[:, s, 3:4], scalar2=None, op0=ALU.mult),
                      r=["mk1", "m12"], w=["comb"])
                P.add("dve", lambda e, s=s: e.scalar_tensor_tensor(out=comb[:, s, :], in0=mk2[:, s, :], scalar=m12[:, s, 2:3], in1=comb[:, s, :],
                                                                  op0=ALU.mult, op1=ALU.add), r=["mk2", "m12", "comb"], w=["comb"])
            for ex in range(8):
                for s in range(ns):
                    rp, rk = rep.next()
                    P.add("dve", lambda e, rp=rp, s=s, ex=ex: e.tensor_scalar(out=rp[:, :], in0=C["ones"][:, :], scalar1=comb[:, s, ex:ex + 1], scalar2=None, op0=ALU.mult),
                          r=["comb", "ones"], w=[rk])
                    P.add("pe", lambda e, rp=rp, s=s: e.matmul(cbps[:, s * 128:(s + 1) * 128], rp[:, :], ident[:, :], start=True, stop=True),
                          r=[rk, "ident"], w=[("misc", 0)])
                P.add("act", lambda e, ex=ex: e.copy(out=cB[:, ex, :n], in_=cbps[:, :n]), r=[("misc", 0)], w=[("cB", ex)])
        nmm = NE * NFG * 4
        cnt = 0
        for ex in range(NE):
            for fg in range(NFG):
                wgt, wgk = wgs.next()
                wut, wuk = wus.next()
                wdt, wdk = wds.next()
                P.dma("pool", wgt[:], wg[ex].rearrange("(k p) f -> p k f", p=128)[:, :, fg * 512:(fg + 1) * 512], w=[wgk])
                P.dma("pool", wut[:], wu[ex].rearrange("(k p) f -> p k f", p=128)[:, :, fg * 512:(fg + 1) * 512], w=[wuk])
                P.dma("pool", wdt[:], wd[ex, fg * 512:(fg + 1) * 512, :].rearrange("(f p) d -> p f d", p=128), w=[wdk])
                hht, hhk = hh.next()
                for f in range(4):
                    gp, gk = gups.next()
                    for k in range(8):
                        P.add("pe", lambda e, k=k, f=f, gp=gp, wgt=wgt: e.matmul(gp[:, 0, :n], wgt[:, k, f * 128:(f + 1) * 128], u[:, k, :n], start=(k == 0), stop=(k == 7)),
                              r=["u", wgk], w=[gk])
                    for k in range(8):
                        P.add("pe", lambda e, k=k, f=f, gp=gp, wut=wut: e.matmul(gp[:, 1, :n], wut[:, k, f * 128:(f + 1) * 128], u[:, k, :n], start=(k == 0), stop=(k == 7)),
                              r=["u", wuk], w=[gk])
                    sgt, sgk = sg.next()
                    P.add("act", lambda e, gp=gp, sgt=sgt: e.activation(out=sgt[:, :n], in_=gp[:, 0, :n], func=AF.Silu), r=[gk], w=[sgk])
                    if moe:
                        tt, tk = t1.next()
                        P.add("dve", lambda e, gp=gp, sgt=sgt, tt=tt: e.tensor_tensor(out=tt[:, :n], in0=sgt[:, :n], in1=gp[:, 1, :n], op=ALU.mult),
                              r=[gk, sgk], w=[tk])
                        P.add("pool", lambda e, tt=tt, hht=hht, f=f, ex=ex: e.tensor_tensor(out=hht[:, f, :n], in0=tt[:, :n], in1=cB[:, ex, :n], op=ALU.mult),
                              r=[tk, ("cB", ex)], w=[(hhk, f)])
                    else:
                        P.add("dve", lambda e, gp=gp, sgt=sgt, hht=hht, f=f: e.tensor_tensor(out=hht[:, f, :n], in0=sgt[:, :n], in1=gp[:, 1, :n], op=ALU.mult),
                              r=[gk, sgk], w=[(hhk, f)])
                for f in range(4):
                    for dm in range(8):
                        P.add("pe", lambda e, f=f, dm=dm, hht=hht, wdt=wdt, cnt=cnt: e.matmul(yps[dm // 2][:, dm % 2, :n], wdt[:, f, dm * 128:(dm + 1) * 128], hht[:, f, :n],
                                                                                             start=(cnt == 0 and dm % 2 == 0), stop=(cnt == nmm - 1), skip_group_check=True),
                              r=[(hhk, f), wdk], w=[("yps", dm // 2)])
                    cnt += 1
        for dm in range(8):
            P.add("dve", lambda e, dm=dm: e.scalar_tensor_tensor(out=h2[:, dm, :n], in0=yps[dm // 2][:, dm % 2, :n], scalar=modt[:, 5, ti, dm:dm + 1],
                                                                in1=h1[:, dm, :n], op0=ALU.mult, op1=ALU.add), r=[("yps", dm // 2), "h1", "modt"], w=["h2"])
        P.dma("sp", ov[:, :, t0:t0 + n], h2[:, :, :n], r=["h2"])
        if final:
            norm_parts(h2, "h2", n)
            for k in range(8):
                P.add("dve", lambda e, k=k: e.tensor_tensor(out=tmp[:, k, :n], in0=h2[:, k, :n], in1=rs[:, :n], op=ALU.mult),
                      r=["h2", "rs"], w=[("tmp", k)])
                P.add("act", lambda e, k=k: e.activation(out=sq[:, k, :n], in_=tmp[:, k, :n], func=AF.Copy, scale=fng[:, k:k + 1]),
                      r=[("tmp", k), "fng"], w=["sq"])
            P.dma("sp", outT.rearrange("(k p) t -> p k t", p=128)[:, :, t0:t0 + n], sq[:, :, :n], r=["sq"])
    for ti, (t0, n) in enumerate(CT):
        tile_body(ti, t0, n)
    print("C ops", P.n_ops())
    return P.finish()


NTOK = 4352
NCH = 34
QT = [(0, 256)] + [(256 + 512 * i, 512) for i in range(8)]


def bconsts(P):
    C = {}
    C["ones"] = P.sb([128, 128], F32, "ones")
    P.add("pool", lambda e: e.memset(C["ones"][:], 1.0), w=["ones"])
    C["epsb"] = P.sb([128, 1], F32, "epsb")
    P.add("pool", lambda e: e.memset(C["epsb"][:], EPS), w=["epsb"])
    return C


def emit_mla(P, C, D, mixT):
    scale = 96 ** -0.5
    qng = P.sb([128, 2], F32, "qng")
    P.dma("sp", qng[:], D["c_qng"][:, :], w=["qng"])
    kvg = P.sb([128, 1], F32, "kvg")
    P.dma("sp", kvg[:], D["c_kvg"][:, :], w=["kvg"])
    wq = P.sb([128, 2, 2, 96], BF16, "wq")
    wqP = P.sb([128, 2, 2, 96], BF16, "wqP")
    P.dma("pool", wq[:].rearrange("p k h c -> p k (h c)"), D["c_wq"].rearrange("(k p) c -> p k c", p=128), w=["wq"])
    P.dma("pool", wqP[:].rearrange("p k h c -> p k (h c)"), D["c_wqP"].rearrange("(k p) c -> p k c", p=128), w=["wqP"])
    wkn = P.sb([128, 2, 96], BF16, "wkn")
    P.dma("pool", wkn[:].rearrange("p h c -> p (h c)"), D["c_wkn"][:, :], w=["wkn"])
    wv = P.sb([128, 128], BF16, "wv")
    P.dma("pool", wv[:], D["c_wv"][:, :], w=["wv"])
    sel = P.sb([32, 96], BF16, "sel")
    P.dma("pool", sel[:], D["c_sel"][:, :], w=["sel"])
    qT = P.sb([96, 2, NTOK], BF16, "mqT")
    kT = P.sb([96, 2, NTOK], BF16, "mkT")
    vaug = P.sb([128, NCH, 2, 65], BF16, "mvaug")
    P.add("pool", lambda e: e.memset(vaug[:], 1.0), w=["mvaug"])
    ckvn = P.sb([128, NTOK], BF16, "ckvn")
    cq = P.sb([128, 2, 512], F32, "m_cq")
    ckv = P.sb([128, 512], F32, "m_ckv")
    sq = P.sb([128, 2, 512], F32, "m_sq")
    rs = P.sb([128, 512], F32, "m_rs")
    rs2 = P.sb([128, 512], F32, "m_rs2")
    cqn = P.sb([128, 2, 512], BF16, "m_cqn")
    ct = P.sb([96, 512], F32, "m_ct")
    st = P.sb([96, 512], F32, "m_st")
    kr = P.sb([32, 512], F32, "m_kr")
    krP = P.sb([32, 512], F32, "m_krP")
    krr = P.sb([32, 512], BF16, "m_krr")
    t1 = P.sb([96, 512], F32, "m_t1")
    t2 = P.sb([96, 512], F32, "m_t2")
    ssps = P.ps([128, 512], F32, "m_ssps")
    pA = P.ps([128, 512], F32, "m_pA")
    pB = P.ps([128, 512], F32, "m_pB")

    ct32 = P.sb([32, 512], F32, "m_ct32")
    st32 = P.sb([32, 512], F32, "m_st32")

    def tile_all(t0, n):
        P.dma("sp", cq[:, :, :n], D["c_cq"].rearrange("(k p) t -> p k t", p=128)[:, :, t0:t0 + n], w=["m_cq"])
        P.dma("sp", ckv[:, :n], D["c_ckv"][:, t0:t0 + n], w=["m_ckv"])
        P.dma("sp", ct[:, :n], D["c_ct96"][:, t0:t0 + n], w=["m_ct"])
        P.dma("sp", st[:, :n], D["c_st96"][:, t0:t0 + n], w=["m_st"])
        P.dma("sp", ct32[:, :n], D["c_ct96"][64:96, t0:t0 + n], w=["m_ct32"])
        P.dma("sp", st32[:, :n], D["c_st96"][64:96, t0:t0 + n], w=["m_st32"])
        P.dma("sp", kr[:, :n], D["c_kr"][:, t0:t0 + n], w=["m_kr"])
        P.dma("sp", krP[:, :n], D["c_krP"][:, t0:t0 + n], w=["m_krP"])
        P.add("act", lambda e: e.activation(out=sq[:, :, :n], in_=cq[:, :, :n], func=AF.Square), r=["m_cq"], w=["m_sq"])
        for k in range(2):
            P.add("pe", lambda e, k=k: e.matmul(ssps[:, :n], C["ones"][:, :], sq[:, k, :n], start=(k == 0), stop=(k == 1)), r=["m_sq", "ones"], w=["m_ssps"])
        P.add("act", lambda e: e.activation(out=rs[:, :n], in_=ssps[:, :n], func=AF.Sqrt, scale=1.0 / 256, bias=C["epsb"][:, 0:1]), r=["m_ssps", "epsb"], w=["m_rs"])
        P.add("dve", lambda e: e.reciprocal(out=rs[:, :n], in_=rs[:, :n]), r=["m_rs"], w=["m_rs"])
        for k in range(2):
            P.add("dve", lambda e, k=k: e.scalar_tensor_tensor(out=cqn[:, k, :n], in0=cq[:, k, :n], scalar=qng[:, k:k + 1], in1=rs[:, :n], op0=ALU.mult, op1=ALU.mult),
                  r=["m_cq", "qng", "m_rs"], w=["m_cqn"])
        P.add("act", lambda e: e.activation(out=sq[:, 0, :n], in_=ckv[:, :n], func=AF.Square), r=["m_ckv"], w=["m_sq"])
        P.add("pe", lambda e: e.matmul(ssps[:, :n], C["ones"][:, :], sq[:, 0, :n], start=True, stop=True), r=["m_sq", "ones"], w=["m_ssps"])
        P.add("act", lambda e: e.activation(out=rs2[:, :n], in_=ssps[:, :n], func=AF.Sqrt, scale=1.0 / 128, bias=C["epsb"][:, 0:1]), r=["m_ssps", "epsb"], w=["m_rs2"])
        P.add("dve", lambda e: e.reciprocal(out=rs2[:, :n], in_=rs2[:, :n]), r=["m_rs2"], w=["m_rs2"])
        P.add("dve", lambda e: e.scalar_tensor_tensor(out=ckvn[:, t0:t0 + n], in0=ckv[:, :n], scalar=kvg[:, 0:1], in1=rs2[:, :n], op0=ALU.mult, op1=ALU.mult),
              r=["m_ckv", "kvg", "m_rs2"], w=["ckvn"])
        P.add("pool", lambda e: e.tensor_tensor(out=kr[:, :n], in0=kr[:, :n], in1=ct32[:, :n], op=ALU.mult), r=["m_kr", "m_ct32"], w=["m_kr"])
        P.add("pool", lambda e: e.tensor_tensor(out=krP[:, :n], in0=krP[:, :n], in1=st32[:, :n], op=ALU.mult), r=["m_krP", "m_st32"], w=["m_krP"])
        P.add("pool", lambda e: e.tensor_tensor(out=krr[:, :n], in0=kr[:, :n], in1=krP[:, :n], op=ALU.add), r=["m_kr", "m_krP"], w=["m_krr"])
        for h in range(2):
            for k in range(2):
                P.add("pe", lambda e, k=k, h=h: e.matmul(pA[:96, :n], wq[:, k, h, :], cqn[:, k, :n], start=(k == 0), stop=(k == 1)), r=["wq", "m_cqn"], w=["m_pA"])
            for k in range(2):
                P.add("pe", lambda e, k=k, h=h: e.matmul(pB[:96, :n], wqP[:, k, h, :], cqn[:, k, :n], start=(k == 0), stop=(k == 1)), r=["wqP", "m_cqn"], w=["m_pB"])
            P.add("dve", lambda e: e.tensor_tensor(out=t1[:, :n], in0=pA[:96, :n], in1=ct[:, :n], op=ALU.mult), r=["m_pA", "m_ct"], w=["m_t1"])
            P.add("dve", lambda e: e.tensor_tensor(out=t2[:, :n], in0=pB[:96, :n], in1=st[:, :n], op=ALU.mult), r=["m_pB", "m_st"], w=["m_t2"])
            P.add("pool", lambda e, h=h: e.tensor_tensor(out=qT[:, h, t0:t0 + n], in0=t1[:, :n], in1=t2[:, :n], op=ALU.add), r=["m_t1", "m_t2"], w=["mqT"])
            P.add("pe", lambda e, h=h: e.matmul(pA[:96, :n], wkn[:, h, :], ckvn[:, t0:t0 + n], start=True, stop=False), r=["wkn", "ckvn"], w=["m_pA"])
            P.add("pe", lambda e, h=h: e.matmul(pA[:96, :n], sel[:, :], krr[:, :n], start=False, stop=True), r=["sel", "m_krr"], w=["m_pA"])
            P.add("act", lambda e, h=h: e.copy(out=kT[:, h, t0:t0 + n], in_=pA[:96, :n]), r=["m_pA"], w=["mkT"])
        for s in range(n // 128):
            c = (t0 + s * 128) // 128
            P.add("pe", lambda e, s=s: e.matmul(pB[:, 0:128], ckvn[:, t0 + s * 128:t0 + (s + 1) * 128], wv[:, :], start=True, stop=True), r=["ckvn", "wv"], w=["m_pB"])
            P.add("act", lambda e, c=c: e.copy(out=vaug[:, c, :, 0:64], in_=pB[:, 0:128].rearrange("p (h d) -> p h d", h=2)), r=["m_pB"], w=["mvaug"])

    for (t0, n) in QT:
        tile_all(t0, n)

    sps = Rot([P.ps([128, 512], F32, f"m_sps{i}") for i in range(3)], "m_sps")
    ops_ = Rot([P.ps([128, 512], F32, f"m_ops{i}") for i in range(2)], "m_ops")
    E = Rot([P.sb([128, 512], BF16, f"m_E{i}") for i in range(3)], "m_E")
    oa = Rot([P.sb([65, 512], F32, f"m_oa{i}") for i in range(2)], "m_oa")
    rc = Rot([P.sb([64, 512], F32, f"m_rc{i}") for i in range(2)], "m_rc")
    oo = Rot([P.sb([64, 512], F32, f"m_oo{i}") for i in range(2)], "m_oo")

    def attn_tile(h, t0, n, chunks):
        op_, ok = ops_.next()
        nchk = len(chunks)
        pend = []

        def issue_s(c):
            sp, sk = sps.next()
            P.add("pe", lambda e, sp=sp, c=c: e.matmul(sp[:, :n], kT[:, h, c * 128:(c + 1) * 128], qT[:, h, t0:t0 + n], start=True, stop=True), r=["mkT", "mqT"], w=[sk])
            pend.append((sp, sk))
        issue_s(chunks[0])
        for ci, c in enumerate(chunks):
            if ci + 1 < nchk:
                issue_s(chunks[ci + 1])
            sp, sk = pend.pop(0)
            Et, ek = E.next()
            P.add("act", lambda e, sp=sp, Et=Et: e.activation(out=Et[:, :n], in_=sp[:, :n], func=AF.Exp, scale=scale), r=[sk], w=[ek])
            P.add("pe", lambda e, Et=Et, c=c, ci=ci: e.matmul(op_[:65, :n], vaug[:, c, h, :], Et[:, :n], start=(ci == 0), stop=(ci == nchk - 1)), r=[ek, "mvaug"], w=[ok])
        oat, oak = oa.next()
        P.add("act", lambda e: e.copy(out=oat[:, :n], in_=op_[:65, :n]), r=[ok], w=[oak])
        sp, sk = sps.next()
        P.add("pe", lambda e: e.matmul(sp[:64, :n], C["ones"][64:65, 0:64], oat[64:65, :n], start=True, stop=True), r=[oak, "ones"], w=[sk])
        rct, rck = rc.next()
        P.add("dve", lambda e: e.reciprocal(out=rct[:, :n], in_=sp[:64, :n]), r=[sk], w=[rck])
        oot, ook = oo.next()
        P.add("dve", lambda e: e.tensor_tensor(out=oot[:, :n], in0=oat[0:64, :n], in1=rct[:, :n], op=ALU.mult), r=[oak, rck], w=[ook])
        P.dma("sp", mixT[h * 64:(h + 1) * 64, t0:t0 + n], oot[:, :n], r=[ook])

    for h in range(2):
        attn_tile(h, 0, 256, [0, 1])
        for i in range(8):
            attn_tile(h, 256 + 512 * i, 512, list(range(NCH)))


MLA_IN = [("c_cq", [256, NTOK]), ("c_ckv", [128, NTOK]), ("c_kr", [32, NTOK]), ("c_krP", [32, NTOK]), ("c_ct96", [96, NTOK]), ("c_st96", [96, NTOK]),
          ("c_qng", [128, 2]), ("c_kvg", [128, 1]), ("c_wq", [256, 192]), ("c_wqP", [256, 192]), ("c_wkn", [128, 192]), ("c_wv", [128, 128]), ("c_sel", [32, 96])]


def build_mla():
    P = Prog()
    D = {nm: P.dram_in(nm, shp) for nm, shp in MLA_IN}
    out = P.dram_out("mix", [128, NTOK])
    C = bconsts(P)
    emit_mla(P, C, D, out)
    print("mla ops", P.n_ops())
    return P.finish()


SWA_IN = [("a_q", [128, NTOK]), ("a_qP", [128, NTOK]), ("a_k", [64, NTOK]), ("a_kP", [64, NTOK]), ("a_vtok", [NTOK, 64]),
          ("a_cos", [64, NTOK]), ("a_sin", [64, NTOK]), ("a_sink", [128, 2]), ("a_maskP", [128, 128]), ("a_maskN", [128, 128])]


def build_swa():
    P = Prog()
    D = {nm: P.dram_in(nm, shp) for nm, shp in SWA_IN}
    out = P.dram_out("mix", [128, NTOK])
    C = bconsts(P)
    scale = 64 ** -0.5
    aqT = P.sb([64, 2, NTOK], BF16, "aqT")
    akT = P.sb([64, NTOK], BF16, "akT")
    vaug = P.sb([128, NCH, 65], BF16, "avaug")
    P.add("pool", lambda e: e.memset(vaug[:], 1.0), w=["avaug"])
    P.dma("pool", vaug[:, :, 0:64], D["a_vtok"].rearrange("(c p) d -> p c d", p=128), w=["avaug"])
    es = P.sb([128, 2], F32, "a_es")
    P.dma("sp", es[:], D["a_sink"][:, :], w=["a_es"])
    P.add("act", lambda e: e.activation(out=es[:], in_=es[:], func=AF.Exp), r=["a_es"], w=["a_es"])
    mP = P.sb([128, 128], BF16, "a_mP")
    mN = P.sb([128, 128], BF16, "a_mN")
    P.dma("pool", mP[:], D["a_maskP"][:, :], w=["a_mP"])
    P.dma("pool", mN[:], D["a_maskN"][:, :], w=["a_mN"])
    q = P.sb([64, 2, 512], F32, "a_q")
    qP = P.sb([64, 2, 512], F32, "a_qP")
    k = P.sb([64, 512], F32, "a_k")
    kP = P.sb([64, 512], F32, "a_kP")
    cs = P.sb([64, 512], F32, "a_cs")
    sn = P.sb([64, 512], F32, "a_sn")
    t1 = P.sb([64, 512], F32, "a_t1")
    t2 = P.sb([64, 512], F32, "a_t2")

    def rope_tile(t0, n):
        P.dma("sp", q[:, :, :n], D["a_q"].rearrange("(h d) t -> d h t", d=64)[:, :, t0:t0 + n], w=["a_q"])
        P.dma("sp", qP[:, :, :n], D["a_qP"].rearrange("(h d) t -> d h t", d=64)[:, :, t0:t0 + n], w=["a_qP"])
        P.dma("sp", k[:, :n], D["a_k"][:, t0:t0 + n], w=["a_k"])
        P.dma("sp", kP[:, :n], D["a_kP"][:, t0:t0 + n], w=["a_kP"])
        P.dma("sp", cs[:, :n], D["a_cos"][:, t0:t0 + n], w=["a_cs"])
        P.dma("sp", sn[:, :n], D["a_sin"][:, t0:t0 + n], w=["a_sn"])
        for h in range(2):
            P.add("dve", lambda e, h=h: e.tensor_tensor(out=t1[:, :n], in0=q[:, h, :n], in1=cs[:, :n], op=ALU.mult), r=["a_q", "a_cs"], w=["a_t1"])
            P.add("pool", lambda e, h=h: e.tensor_tensor(out=t2[:, :n], in0=qP[:, h, :n], in1=sn[:, :n], op=ALU.mult), r=["a_qP", "a_sn"], w=["a_t2"])
            P.add("dve", lambda e, h=h: e.tensor_tensor(out=aqT[:, h, t0:t0 + n], in0=t1[:, :n], in1=t2[:, :n], op=ALU.add), r=["a_t1", "a_t2"], w=["aqT"])
        P.add("dve", lambda e: e.tensor_tensor(out=t1[:, :n], in0=k[:, :n], in1=cs[:, :n], op=ALU.mult), r=["a_k", "a_cs"], w=["a_t1"])
        P.add("pool", lambda e: e.tensor_tensor(out=t2[:, :n], in0=kP[:, :n], in1=sn[:, :n], op=ALU.mult), r=["a_kP", "a_sn"], w=["a_t2"])
        P.add("dve", lambda e: e.tensor_tensor(out=akT[:, t0:t0 + n], in0=t1[:, :n], in1=t2[:, :n], op=ALU.add), r=["a_t1", "a_t2"], w=["akT"])

    for (t0, n) in QT:
        rope_tile(t0, n)

    sp = [P.ps([128, 512], F32, f"a_sp{i}") for i in range(3)]
    ops_ = Rot([P.ps([128, 512], F32, f"a_op{i}") for i in range(2)], "a_op")
    bc = P.ps([128, 512], F32, "a_bc")
    E = Rot([P.sb([128, 5 * 256], BF16, f"a_E{i}") for i in range(2)], "a_E")
    oa = Rot([P.sb([65, 256], F32, f"a_oa{i}") for i in range(2)], "a_oa")
    rc = Rot([P.sb([64, 256], F32, f"a_rc{i}") for i in range(2)], "a_rc")
    oo = Rot([P.sb([64, 256], F32, f"a_oo{i}") for i in range(2)], "a_oo")
    outv = out.rearrange("(h d) t -> d h t", d=64)

    def block(q0, chunks):
        nck = len(chunks)
        for ci, (c, mk) in enumerate(chunks):
            b, off = ci // 2, (ci % 2) * 256
            P.add("pe", lambda e, b=b, off=off, c=c: e.matmul(sp[b][:, off:off + 256].rearrange("p (h q) -> p h q", h=2), akT[:, c * 128:(c + 1) * 128],
                                                             aqT[:, :, q0:q0 + 128], start=True, stop=True), r=["akT", "aqT"], w=[("a_sp", b)])
        Et, ek = E.next()
        for b in range((nck + 1) // 2):
            w_ = min(512, nck * 256 - b * 512)
            P.add("act", lambda e, b=b, w_=w_: e.activation(out=Et[:, b * 512:b * 512 + w_], in_=sp[b][:, :w_], func=AF.Exp, scale=scale), r=[("a_sp", b)], w=[ek])
        for ci, (c, mk) in enumerate(chunks):
            if mk is None:
                continue
            m = mP if mk == "P" else mN
            for h in range(2):
                o_ = ci * 256 + h * 128
                P.add("pool", lambda e, o_=o_, m=m: e.tensor_tensor(out=Et[:, o_:o_ + 128], in0=Et[:, o_:o_ + 128], in1=m[:, :], op=ALU.mult), r=[ek, "a_mP", "a_mN"], w=[ek])
        op_, ok = ops_.next()
        for ci, (c, mk) in enumerate(chunks):
            P.add("pe", lambda e, ci=ci, c=c: e.matmul(op_[:65, :256], vaug[:, c, :], Et[:, ci * 256:(ci + 1) * 256], start=(ci == 0), stop=(ci == nck - 1)), r=[ek, "avaug"], w=[ok])
        oat, oak = oa.next()
        P.add("act", lambda e: e.copy(out=oat[:, :], in_=op_[:65, :256]), r=[ok], w=[oak])
        for h in range(2):
            P.add("dve", lambda e, h=h: e.tensor_scalar(out=oat[64:65, h * 128:(h + 1) * 128], in0=oat[64:65, h * 128:(h + 1) * 128], scalar1=es[64:65, h:h + 1], scalar2=None, op0=ALU.add),
                  r=[oak, "a_es"], w=[oak])
        P.add("pe", lambda e: e.matmul(bc[:64, :256], C["ones"][64:65, 0:64], oat[64:65, :], start=True, stop=True), r=[oak, "ones"], w=["a_bc"])
        rct, rck = rc.next()
        P.add("dve", lambda e: e.reciprocal(out=rct[:, :], in_=bc[:64, :256]), r=["a_bc"], w=[rck])
        oot, ook = oo.next()
        P.add("dve", lambda e: e.tensor_tensor(out=oot[:, :], in0=oat[0:64, :], in1=rct[:, :], op=ALU.mult), r=[oak, rck], w=[ook])
        P.dma("sp", outv[:, :, q0:q0 + 128], oot[:, :].rearrange("d (h q) -> d h q", h=2), r=[ook])

    block(0, [(0, None), (1, None)])
    block(128, [(0, None), (1, None)])
    for nb in range(32):
        ch = [(0, None), (1, None)]
        if nb > 0:
            ch.append((nb + 1, "P"))
        ch.append((nb + 2, None))
        if nb < 31:
            ch.append((nb + 3, "N"))
        block(256 + 128 * nb, ch)
    print("swa ops", P.n_ops())
    return P.finish()


RET_IN = [("d_q", [128, NTOK]), ("d_qP", [128, NTOK]), ("d_k", [128, NTOK]), ("d_kP", [128, NTOK]), ("d_gate", [128, NTOK]),
          ("d_vtok", [NTOK, 128]), ("d_ktok", [NTOK, 128]), ("d_kPtok", [NTOK, 128]), ("d_cos", [64, NTOK]), ("d_sin", [64, NTOK]),
          ("d_costok", [NTOK, 64]), ("d_sintok", [NTOK, 64]), ("d_ldp", [128, 2]), ("d_ldr", [128, 4]), ("d_g", [128, 1]),
          ("d_relu", [128, 128]), ("d_rell", [128, 128]), ("d_um", [128, 128]), ("d_lm", [128, 128]), ("d_pos1", [128, 128]), ("d_posr", [128, 128]),
          ("d_pk", [128, 2]), ("d_bd", [128, 128]), ("d_bd64", [128, 128])]


def build_ret():
    P = Prog()
    D = {nm: P.dram_in(nm, shp) for nm, shp in RET_IN}
    out = P.dram_out("mix", [128, NTOK])
    C = bconsts(P)

    def ld(nm, shp, dt=F32, q="sp"):
        t = P.sb(shp, dt, "r_" + nm)
        P.dma(q, t[:], D[nm][:, :], w=["r_" + nm])
        return t
    ldp = ld("d_ldp", [128, 2]); ldr = ld("d_ldr", [128, 4]); g = ld("d_g", [128, 1])
    relu = ld("d_relu", [128, 128]); rell = ld("d_rell", [128, 128]); um = ld("d_um", [128, 128]); lm = ld("d_lm", [128, 128])
    pos1 = ld("d_pos1", [128, 128]); posr = ld("d_posr", [128, 128]); pk = ld("d_pk", [128, 2]); bd = ld("d_bd", [128, 128]); bd64 = ld("d_bd64", [128, 128])
    P.add("act", lambda e: e.activation(out=ldp[:], in_=ldp[:], func=AF.Exp), r=["r_d_ldp"], w=["r_d_ldp"])
    P.add("dve", lambda e: e.tensor_scalar(out=ldp[:], in0=ldp[:], scalar1=-1.0, scalar2=None, op0=ALU.mult), r=["r_d_ldp"], w=["r_d_ldp"])
    P.add("act", lambda e: e.activation(out=ldr[:], in_=ldr[:], func=AF.Exp), r=["r_d_ldr"], w=["r_d_ldr"])
    P.add("dve", lambda e: e.tensor_scalar(out=ldr[:], in0=ldr[:], scalar1=-1.0, scalar2=None, op0=ALU.mult), r=["r_d_ldr"], w=["r_d_ldr"])
    Qd = P.sb([128, 2, 128], F32, "r_Qd")
    for d, pt in ((0, pos1), (1, posr)):
        P.add("act", lambda e, d=d, pt=pt: e.activation(out=Qd[:, d, :], in_=pt[:, :], func=AF.Exp, scale=ldp[:, d:d + 1]), r=["r_d_ldp", "r_d_pos1", "r_d_posr"], w=["r_Qd"])
    P.add("dve", lambda e: e.tensor_scalar(out=Qd[:], in0=Qd[:], scalar1=0.125, scalar2=None, op0=ALU.mult), r=["r_Qd"], w=["r_Qd"])
    c128 = P.sb([128, 1], F32, "r_c128")
    P.add("pool", lambda e: e.memset(c128[:], 128.0), w=["r_c128"])
    cd = P.sb([128, 2], F32, "r_cd")
    for d in range(2):
        P.add("act", lambda e, d=d: e.activation(out=cd[:, d:d + 1], in_=c128[:, :], func=AF.Exp, scale=ldp[:, d:d + 1]), r=["r_d_ldp", "r_c128"], w=["r_cd"])
    Kd = P.sb([128, 2, 2], F32, "r_Kd")
    for d in range(2):
        P.add("act", lambda e, d=d: e.activation(out=Kd[:, d, :], in_=ldr[:, 2 * d:2 * d + 2], func=AF.Exp, scale=pk[:, d:d + 1]), r=["r_d_ldr", "r_d_pk"], w=["r_Kd"])
    DT = P.sb([128, 2, 128], F32, "r_DT")
    dt2 = P.sb([128, 128], F32, "r_dt2")
    for h in range(2):
        P.add("act", lambda e, h=h: e.activation(out=DT[:, h, :], in_=relu[:, :], func=AF.Exp, scale=ldr[:, h:h + 1]), r=["r_d_ldr", "r_d_relu"], w=["r_DT"])
        P.add("dve", lambda e, h=h: e.tensor_tensor(out=DT[:, h, :], in0=DT[:, h, :], in1=um[:, :], op=ALU.mult), r=["r_DT", "r_d_um"], w=["r_DT"])
        P.add("act", lambda e, h=h: e.activation(out=dt2[:, :], in_=rell[:, :], func=AF.Exp, scale=ldr[:, 2 + h:3 + h]), r=["r_d_ldr", "r_d_rell"], w=["r_dt2"])
        P.add("dve", lambda e, h=h: e.tensor_tensor(out=dt2[:, :], in0=dt2[:, :], in1=lm[:, :], op=ALU.mult), r=["r_dt2", "r_d_lm"], w=["r_dt2"])
        P.add("dve", lambda e, h=h: e.tensor_tensor(out=DT[:, h, :], in0=DT[:, h, :], in1=dt2[:, :], op=ALU.add), r=["r_DT", "r_dt2"], w=["r_DT"])
    P.add("dve", lambda e: e.tensor_scalar(out=DT[:], in0=DT[:], scalar1=0.125, scalar2=None, op0=ALU.mult), r=["r_DT"], w=["r_DT"])

    qdf = P.sb([128, NCH, 128], BF16, "r_qdf"); qdr = P.sb([128, NCH, 128], BF16, "r_qdr")
    qTb = P.sb([128, 2, NTOK], BF16, "r_qTb"); kTb = P.sb([128, NTOK], BF16, "r_kTb")
    P.add("pool", lambda e: e.memset(qTb[:], 0.0), w=["r_qTb"])
    kdf = P.sb([128, NCH, 128], BF16, "r_kdf"); kdr = P.sb([128, NCH, 128], BF16, "r_kdr")
    vt = P.sb([128, NCH, 128], BF16, "r_vt")
    vpad = P.sb([128, NCH, 2, 128], BF16, "r_vpad")
    P.add("pool", lambda e: e.memset(vpad[:], 0.0), w=["r_vpad"])
    vv = D["d_vtok"].rearrange("(c p) d -> p c d", p=128)
    P.dma("pool", vt[:], vv, w=["r_vt"])
    P.dma("pool", vpad[:, :, 0, 0:64], vv[:, :, 0:64], w=["r_vpad"])
    P.dma("pool", vpad[:, :, 1, 64:128], vv[:, :, 64:128], w=["r_vpad"])
    q = P.sb([128, 512], F32, "r_q"); qP = P.sb([128, 512], F32, "r_qP"); k = P.sb([128, 512], F32, "r_k"); kP = P.sb([128, 512], F32, "r_kP")
    cs = P.sb([128, 512], F32, "r_cs"); sn = P.sb([128, 512], F32, "r_sn")
    t1 = P.sb([128, 512], F32, "r_t1"); t2 = P.sb([128, 512], F32, "r_t2"); qr = P.sb([128, 512], F32, "r_qr")
    kt = P.sb([128, 4, 128], F32, "r_kt"); kPt = P.sb([128, 4, 128], F32, "r_kPt"); ct = P.sb([128, 4, 64], F32, "r_ct"); st = P.sb([128, 4, 64], F32, "r_st")
    krt = P.sb([128, 4, 128], F32, "r_krt"); kt2 = P.sb([128, 4, 128], F32, "r_kt2")

    def prep(t0, n):
        ns = n // 128
        c0 = t0 // 128
        for nm, t in (("d_q", q), ("d_qP", qP), ("d_k", k), ("d_kP", kP)):
            P.dma("sp", t[:, :n], D[nm][:, t0:t0 + n], w=[t.name])
        for hh in range(2):
            P.dma("sp", cs[hh * 64:(hh + 1) * 64, :n], D["d_cos"][:, t0:t0 + n], w=[cs.name])
            P.dma("sp", sn[hh * 64:(hh + 1) * 64, :n], D["d_sin"][:, t0:t0 + n], w=[sn.name])
        P.add("dve", lambda e: e.tensor_tensor(out=t1[:, :n], in0=q[:, :n], in1=cs[:, :n], op=ALU.mult), r=[q.name, cs.name], w=["r_t1"])
        P.add("pool", lambda e: e.tensor_tensor(out=t2[:, :n], in0=qP[:, :n], in1=sn[:, :n], op=ALU.mult), r=[qP.name, sn.name], w=["r_t2"])
        P.add("dve", lambda e: e.tensor_tensor(out=qr[:, :n], in0=t1[:, :n], in1=t2[:, :n], op=ALU.add), r=["r_t1", "r_t2"], w=["r_qr"])
        P.add("act", lambda e: e.copy(out=qTb[0:64, 0, t0:t0 + n], in_=qr[0:64, :n]), r=["r_qr"], w=["r_qTb"])
        P.add("act", lambda e: e.copy(out=qTb[64:128, 1, t0:t0 + n], in_=qr[64:128, :n]), r=["r_qr"], w=["r_qTb"])
        for s in range(ns):
            P.add("dve", lambda e, s=s: e.tensor_tensor(out=qdf[:, c0 + s, :], in0=qr[:, s * 128:(s + 1) * 128], in1=Qd[:, 0, :], op=ALU.mult), r=["r_qr", "r_Qd"], w=["r_qdf"])
            P.add("pool", lambda e, s=s: e.tensor_tensor(out=qdr[:, c0 + s, :], in0=qr[:, s * 128:(s + 1) * 128], in1=Qd[:, 1, :], op=ALU.mult), r=["r_qr", "r_Qd"], w=["r_qdr"])
        P.add("dve", lambda e: e.tensor_tensor(out=t1[:, :n], in0=k[:, :n], in1=cs[:, :n], op=ALU.mult), r=[k.name, cs.name], w=["r_t1"])
        P.add("pool", lambda e: e.tensor_tensor(out=t2[:, :n], in0=kP[:, :n], in1=sn[:, :n], op=ALU.mult), r=[kP.name, sn.name], w=["r_t2"])
        P.add("dve", lambda e: e.tensor_tensor(out=kTb[:, t0:t0 + n], in0=t1[:, :n], in1=t2[:, :n], op=ALU.add), r=["r_t1", "r_t2"], w=["r_kTb"])
        P.dma("sp", kt[:, :ns, :], D["d_ktok"].rearrange("(c p) d -> p c d", p=128)[:, c0:c0 + ns, :], w=["r_kt"])
        P.dma("sp", kPt[:, :ns, :], D["d_kPtok"].rearrange("(c p) d -> p c d", p=128)[:, c0:c0 + ns, :], w=["r_kPt"])
        P.dma("sp", ct[:, :ns, :], D["d_costok"].rearrange("(c p) d -> p c d", p=128)[:, c0:c0 + ns, :], w=["r_ct"])
        P.dma("sp", st[:, :ns, :], D["d_sintok"].rearrange("(c p) d -> p c d", p=128)[:, c0:c0 + ns, :], w=["r_st"])
        for h in range(2):
            hs = slice(h * 64, (h + 1) * 64)
            P.add("dve", lambda e, hs=hs: e.tensor_tensor(out=krt[:, :ns, hs], in0=kt[:, :ns, hs], in1=ct[:, :ns, :], op=ALU.mult), r=["r_kt", "r_ct"], w=["r_krt"])
            P.add("pool", lambda e, hs=hs: e.tensor_tensor(out=kt2[:, :ns, hs], in0=kPt[:, :ns, hs], in1=st[:, :ns, :], op=ALU.mult), r=["r_kPt", "r_st"], w=["r_kt2"])
        P.add("dve", lambda e: e.tensor_tensor(out=krt[:, :ns, :], in0=krt[:, :ns, :], in1=kt2[:, :ns, :], op=ALU.add), r=["r_krt", "r_kt2"], w=["r_krt"])
        for h in range(2):
            hs = slice(h * 64, (h + 1) * 64)
            P.add("dve", lambda e, hs=hs, h=h: e.tensor_scalar(out=kdf[:, c0:c0 + ns, hs], in0=krt[:, :ns, hs], scalar1=Kd[:, 0, h:h + 1], scalar2=None, op0=ALU.mult), r=["r_krt", "r_Kd"], w=["r_kdf"])
            P.add("pool", lambda e, hs=hs, h=h: e.tensor_scalar(out=kdr[:, c0:c0 + ns, hs], in0=krt[:, :ns, hs], scalar1=Kd[:, 1, h:h + 1], scalar2=None, op0=ALU.mult), r=["r_krt", "r_Kd"], w=["r_kdr"])

    RS = 3
    for (t0, n) in QT:
        prep(t0, n)
    if RS < 2:
        return P.finish()

    Sf = P.sb([128, NCH, 128], BF16, "r_Sf"); Sr = P.sb([128, NCH, 128], BF16, "r_Sr")
    S = P.sb([128, 128], F32, "r_S")
    gp = Rot([P.ps([128, 512], F32, f"r_gp{i}") for i in range(2)], "r_gp")
    tg = Rot([P.sb([128, 128], F32, f"r_tg{i}") for i in range(2)], "r_tg")

    def scan(order, kd, kdkey, Sall, skey, d):
        P.add("pool", lambda e: e.memset(S[:], 0.0), r=[], w=["r_S"])
        P.add("pool", lambda e: e.memset(Sall[:, order[0], :], 0.0), w=[skey])
        for idx in range(len(order) - 1):
            c = order[idx]
            g_, gk = gp.next()
            P.add("pe", lambda e, c=c, g_=g_: e.matmul(g_[:, 0:128], kd[:, c, :], vt[:, c, :], start=True, stop=True), r=[kdkey, "r_vt"], w=[gk])
            tg_, tk = tg.next()
            P.add("dve", lambda e, g_=g_, tg_=tg_: e.tensor_tensor(out=tg_[:, :], in0=g_[:, 0:128], in1=bd[:, :], op=ALU.mult), r=[gk, "r_d_bd"], w=[tk])
            P.add("dve", lambda e, tg_=tg_: e.scalar_tensor_tensor(out=S[:, :], in0=S[:, :], scalar=cd[:, d:d + 1], in1=tg_[:, :], op0=ALU.mult, op1=ALU.add), r=[tk, "r_S", "r_cd"], w=["r_S"])
            P.add("act", lambda e, nx=order[idx + 1]: e.copy(out=Sall[:, nx, :], in_=S[:, :]), r=["r_S"], w=[skey])

    scan(list(range(NCH)), kdf, "r_kdf", Sf, "r_Sf", 0)
    scan([1, 0] + list(range(NCH - 1, 1, -1)), kdr, "r_kdr", Sr, "r_Sr", 1)

    if RS < 3:
        return P.finish()
    bp = Rot([P.ps([128, 512], F32, f"r_bp{i}") for i in range(2)], "r_bp")
    op_ = Rot([P.ps([128, 512], F32, f"r_op{i}") for i in range(2)], "r_op")
    mv = P.ps([128, 512], F32, "r_mv")
    AT = Rot([P.sb([128, 2, 128], BF16, f"r_AT{i}") for i in range(2)], "r_AT")
    osb = P.sb([128, 512], F32, "r_osb"); dd = P.sb([128, 512], F32, "r_dd"); sq = P.sb([128, 512], F32, "r_sq"); rs = P.sb([128, 512], F32, "r_rs")
    gt = P.sb([128, 512], F32, "r_gt"); yo = P.sb([128, 512], F32, "r_yo")

    def out_tile(t0, n):
        ns = n // 128
        c0 = t0 // 128
        o_, ok = op_.next()
        for s in range(ns):
            c = c0 + s
            b_, bk = bp.next()
            for h in range(2):
                hs = slice(h * 64, (h + 1) * 64)
                P.add("pe", lambda e, h=h, hs=hs, c=c, b_=b_: e.matmul(b_[:, h * 128:(h + 1) * 128], kTb[:, c * 128:(c + 1) * 128], qTb[:, h, c * 128:(c + 1) * 128], start=True, stop=True),
                      r=["r_kTb", "r_qTb"], w=[bk])
            at, ak = AT.next()
            P.add("dve", lambda e, b_=b_, at=at: e.tensor_tensor(out=at[:, :, :], in0=b_[:, 0:256].rearrange("p (h q) -> p h q", h=2), in1=DT[:, :, :], op=ALU.mult), r=[bk, "r_DT"], w=[ak])
            reg = o_[:, s * 128:(s + 1) * 128]
            P.add("pe", lambda e, reg=reg, c=c: e.matmul(reg, Sf[:, c, :], qdf[:, c, :], start=True, stop=False), r=["r_Sf", "r_qdf"], w=[ok])
            P.add("pe", lambda e, reg=reg, c=c: e.matmul(reg, Sr[:, c, :], qdr[:, c, :], start=False, stop=False), r=["r_Sr", "r_qdr"], w=[ok])
            P.add("pe", lambda e, reg=reg, c=c, at=at: e.matmul(reg, vpad[:, c, 0, :], at[:, 0, :], start=False, stop=False), r=["r_vpad", ak], w=[ok])
            P.add("pe", lambda e, reg=reg, c=c, at=at: e.matmul(reg, vpad[:, c, 1, :], at[:, 1, :], start=False, stop=True), r=["r_vpad", ak], w=[ok])
        P.dma("sp", gt[:, :n], D["d_gate"][:, t0:t0 + n], w=["r_gt"])
        P.add("act", lambda e: e.copy(out=osb[:, :n], in_=o_[:, :n]), r=[ok], w=["r_osb"])
        P.add("pe", lambda e: e.matmul(mv[:, :n], bd64[:, :], osb[:, :n], start=True, stop=True), r=["r_d_bd64", "r_osb"], w=["r_mv"])
        P.add("dve", lambda e: e.tensor_tensor(out=dd[:, :n], in0=osb[:, :n], in1=mv[:, :n], op=ALU.subtract), r=["r_osb", "r_mv"], w=["r_dd"])
        P.add("act", lambda e: e.activation(out=sq[:, :n], in_=dd[:, :n], func=AF.Square), r=["r_dd"], w=["r_sq"])
        P.add("pe", lambda e: e.matmul(mv[:, :n], bd64[:, :], sq[:, :n], start=True, stop=True), r=["r_d_bd64", "r_sq"], w=["r_mv"])
        P.add("act", lambda e: e.activation(out=rs[:, :n], in_=mv[:, :n], func=AF.Sqrt, bias=C["epsb"][:, 0:1]), r=["r_mv", "epsb"], w=["r_rs"])
        P.add("dve", lambda e: e.reciprocal(out=rs[:, :n], in_=rs[:, :n]), r=["r_rs"], w=["r_rs"])
        P.add("dve", lambda e: e.tensor_tensor(out=dd[:, :n], in0=dd[:, :n], in1=rs[:, :n], op=ALU.mult), r=["r_dd", "r_rs"], w=["r_dd"])
        P.add("act", lambda e: e.activation(out=gt[:, :n], in_=gt[:, :n], func=AF.Silu), r=["r_gt"], w=["r_gt"])
        P.add("dve", lambda e: e.scalar_tensor_tensor(out=yo[:, :n], in0=dd[:, :n], scalar=g[:, 0:1], in1=gt[:, :n], op0=ALU.mult, op1=ALU.mult), r=["r_dd", "r_gt", "r_d_g"], w=["r_yo"])
        P.dma("sp", out[:, t0:t0 + n], yo[:, :n], r=["r_yo"])

    for (t0, n) in QT:
        out_tile(t0, n)
    print("ret ops", P.n_ops())
    return P.finish()


GDN_IN = [("b_q", [128, NTOK]), ("b_k", [128, NTOK]), ("b_v", [128, NTOK]), ("b_gate", [128, NTOK]), ("b_ab", [NTOK, 8]),
          ("b_cw", [64, 30]), ("b_dtb", [128, NCH * 8]), ("b_alog", [128, NCH * 8]), ("b_g", [64, 1]),
          ("b_triF", [128, 128]), ("b_triR", [128, 128]), ("b_ident", [128, 128]), ("b_um", [128, 128]), ("b_lm", [128, 128]),
          ("b_us", [128, 128]), ("b_ls", [128, 128])]


def build_gdn(nsteps=NCH, limit=10**9):
    P = Prog()
    _real_add = P.add
    _cnt = [0]
    _on = [False]

    def _ladd(eng, fn, r=(), w=()):
        if _on[0]:
            _cnt[0] += 1
            if _cnt[0] > limit:
                return None
        extra = [k for k in r if isinstance(k, tuple) and k[0] == 'bk' and k not in w]
        return _real_add(eng, fn, r, list(w) + extra)
    P.add = _ladd
    D = {nm: P.dram_in(nm, shp) for nm, shp in GDN_IN}
    out = P.dram_out("mix", [128, NTOK])
    C = bconsts(P)
    ones = C["ones"]

    def ld(nm, shp):
        t = P.sb(shp, F32, "g_" + nm)
        P.dma("sp", t[:], D[nm][:, :], w=["g_" + nm])
        return t
    cw = ld("b_cw", [64, 30])
    gg = ld("b_g", [64, 1])
    tri = [ld("b_triF", [128, 128]), ld("b_triR", [128, 128])]
    ident = ld("b_ident", [128, 128])
    msk = [ld("b_um", [128, 128]), ld("b_lm", [128, 128])]
    smsk = [ld("b_us", [128, 128]), ld("b_ls", [128, 128])]
    CK = ["g_b_triF", "g_b_triR", "g_b_ident", "g_b_um", "g_b_lm", "g_b_us", "g_b_ls", "ones"]
    banks = [P.ps([128, 512], F32, f"g_bk{j}") for j in range(8)]

    def X(j, r, rows=128, cols=128):
        return banks[j][0:rows, r * 128:r * 128 + cols]

    qn = P.sb([64, 2, NTOK], F32, "g_qn"); kn = P.sb([64, 2, NTOK], F32, "g_kn"); oacc = P.sb([64, 2, NTOK], F32, "g_oacc")
    ktok = P.sb([128, NCH, 128], F32, "g_ktok"); vtok = P.sb([128, NCH, 128], F32, "g_vtok")
    P.add("pool", lambda e: e.memset(oacc[:], 0.0), w=["g_oacc"])
    ab = P.sb([128, NCH * 8], F32, "g_ab"); dtb = ld("b_dtb", [128, NCH * 8]); alog = ld("b_alog", [128, NCH * 8])
    gtok = P.sb([128, NCH * 8], F32, "g_gtok"); btok = P.sb([128, NCH * 8], F32, "g_btok")
    P.dma("sp", ab[:].rearrange("p (c e) -> p c e", e=8), D["b_ab"].rearrange("(c p) e -> p c e", p=128), w=["g_ab"])
    P.add("act", lambda e: e.activation(out=btok[:], in_=ab[:], func=AF.Sigmoid), r=["g_ab"], w=["g_btok"])
    P.add("dve", lambda e: e.tensor_tensor(out=gtok[:], in0=ab[:], in1=dtb[:], op=ALU.add), r=["g_ab", "g_b_dtb"], w=["g_gtok"])
    P.add("act", lambda e: e.activation(out=gtok[:], in_=gtok[:], func=AF.Exp), r=["g_gtok"], w=["g_gtok"])
    P.add("act", lambda e: e.activation(out=gtok[:], in_=gtok[:], func=AF.Ln, bias=1.0), r=["g_gtok"], w=["g_gtok"])
    P.add("act", lambda e: e.activation(out=alog[:], in_=alog[:], func=AF.Exp), r=["g_b_alog"], w=["g_b_alog"])
    P.add("dve", lambda e: e.scalar_tensor_tensor(out=gtok[:], in0=gtok[:], scalar=-1.0, in1=alog[:], op0=ALU.mult, op1=ALU.mult), r=["g_gtok", "g_b_alog"], w=["g_gtok"])

    xr = P.sb([64, 2, 516], F32, "g_xr"); acc = P.sb([64, 2, 512], F32, "g_acc"); sq = P.sb([64, 2, 512], F32, "g_sq"); rs = P.sb([64, 2, 512], F32, "g_rs")

    def conv_tile(gi, src, t0, n):
        s0, s1 = (0, 256) if t0 < 256 else (256, NTOK)
        lo, hi = max(t0 - 2, s0), min(t0 + n + 2, s1)
        P.add("pool", lambda e: e.memset(xr[:], 0.0), w=["g_xr"])
        P.dma("sp", xr[:, :, lo - (t0 - 2):hi - (t0 - 2)], D[src].rearrange("(h d) t -> d h t", d=64)[:, :, lo:hi], w=["g_xr"])
        for h in range(2):
            eng = "dve"
            for tap in range(5):
                wcol = cw[:, gi * 10 + h * 5 + tap:gi * 10 + h * 5 + tap + 1]
                if tap == 0:
                    P.add(eng, lambda e, h=h, wcol=wcol: e.tensor_scalar(out=acc[:, h, :n], in0=xr[:, h, 0:n], scalar1=wcol, scalar2=None, op0=ALU.mult),
                          r=["g_xr", "g_b_cw"], w=[("g_acc", h)])
                else:
                    P.add(eng, lambda e, h=h, wcol=wcol, tap=tap: e.scalar_tensor_tensor(out=acc[:, h, :n], in0=xr[:, h, tap:tap + n], scalar=wcol, in1=acc[:, h, :n], op0=ALU.mult, op1=ALU.add),
                          r=["g_xr", "g_b_cw", ("g_acc", h)], w=[("g_acc", h)])
        P.add("act", lambda e: e.activation(out=acc[:, :, :n], in_=acc[:, :, :n], func=AF.Silu), r=[("g_acc", 0), ("g_acc", 1)], w=[("g_acc", 0), ("g_acc", 1)])

    def l2_tile(dst, dkey, t0, n, scl):
        P.add("act", lambda e: e.activation(out=sq[:, :, :n], in_=acc[:, :, :n], func=AF.Square), r=[("g_acc", 0), ("g_acc", 1)], w=["g_sq"])
        for h in range(2):
            P.add("pe", lambda e, h=h: e.matmul(banks[h][0:64, :n], ones[0:64, 0:64], sq[:, h, :n], start=True, stop=True), r=["g_sq", "ones"], w=[("bk", h)])
            P.add("act", lambda e, h=h: e.activation(out=rs[:, h, :n], in_=banks[h][0:64, :n], func=AF.Sqrt, bias=C["epsb"][0:64, 0:1]), r=[("bk", h), "epsb"], w=["g_rs"])
        P.add("dve", lambda e: e.reciprocal(out=rs[:, :, :n], in_=rs[:, :, :n]), r=["g_rs"], w=["g_rs"])
        P.add("dve", lambda e: e.scalar_tensor_tensor(out=dst[:, :, t0:t0 + n], in0=acc[:, :, :n], scalar=scl, in1=rs[:, :, :n], op0=ALU.mult, op1=ALU.mult),
              r=[("g_acc", 0), ("g_acc", 1), "g_rs"], w=[dkey])

    def tr_tile(srcfn, skeys, dst, dkey, t0, n):
        for s in range(n // 128):
            c = t0 // 128 + s
            j = 2 + (s % 2)
            for h in range(2):
                P.add("pe", lambda e, h=h, s=s, j=j: e.matmul(banks[j][:, h * 64:(h + 1) * 64], srcfn(h, s), ident[0:64, 0:64], start=True, stop=True),
                      r=skeys + ["g_b_ident"], w=[("bk", j)])
            P.add("act", lambda e, c=c, j=j: e.copy(out=dst[:, c, :], in_=banks[j][:, 0:128]), r=[("bk", j)], w=[dkey])

    for (t0, n) in QT:
        conv_tile(0, "b_q", t0, n)
        l2_tile(qn, "g_qn", t0, n, 0.125)
        conv_tile(1, "b_k", t0, n)
        l2_tile(kn, "g_kn", t0, n, 1.0)
        tr_tile(lambda h, s, t0=t0: kn[:, h, t0 + s * 128:t0 + (s + 1) * 128], ["g_kn"], ktok, "g_ktok", t0, n)
        conv_tile(2, "b_v", t0, n)
        tr_tile(lambda h, s: acc[:, h, s * 128:(s + 1) * 128], [("g_acc", 0), ("g_acc", 1)], vtok, "g_vtok", t0, n)

    NI = 4
    def tl(nm, shp):
        return [P.sb(shp, F32, f"g_{nm}{i}") for i in range(NI)]
    grep = tl("grep", [128, 128]); brep = tl("brep", [128, 128]); T1 = tl("T1", [128, 128]); EB = tl("EB", [64, 128]); bBs = tl("bBs", [64, 128])
    DTm = tl("DTm", [128, 128]); DTs = tl("DTs", [128, 128]); MT = tl("MT", [128, 128]); Pa = tl("Pa", [128, 128]); PTa = tl("PTa", [128, 128])
    Pb = tl("Pb", [128, 128]); PTb = tl("PTb", [128, 128]); RT = tl("RT", [128, 128]); AT = tl("AT", [128, 128])
    wT = tl("wT", [64, 128]); u = tl("u", [128, 64]); qd = tl("qd", [64, 128]); kd = tl("kd", [128, 64]); vb = tl("vb", [128, 64]); kbe = tl("kbe", [128, 64])
    kbT = tl("kbT", [64, 128]); vnew = tl("vnew", [128, 64]); cols = tl("cols", [128, 8]); Sst = tl("S", [64, 64])
    for i in range(NI):
        P.add("pool", lambda e, i=i: e.memset(Sst[i][:], 0.0), w=[("S", i)])
    orders = [list(range(NCH)), [1, 0] + list(range(NCH - 1, 1, -1))]

    def pre(i, c, d, h):
        K = lambda nm: (nm, i)
        bk = ("bk", i)
        gcol = gtok[:, c * 8 + d * 4 + h:c * 8 + d * 4 + h + 1]
        bcol = btok[:, c * 8 + d * 4 + 2 + h:c * 8 + d * 4 + 2 + h + 1]
        ch = slice(c * 128, (c + 1) * 128)
        hs = slice(h * 64, (h + 1) * 64)
        cl = cols[i]
        P.add("dve", lambda e: e.tensor_scalar(out=grep[i][:, :], in0=ones[:, :], scalar1=gcol, scalar2=None, op0=ALU.mult), r=["g_gtok", "ones"], w=[K("grep")])
        P.add("pool", lambda e: e.tensor_scalar(out=brep[i][:, :], in0=ones[:, :], scalar1=bcol, scalar2=None, op0=ALU.mult), r=["g_btok", "ones"], w=[K("brep")])
        P.add("pe", lambda e: e.matmul(X(i, 0), grep[i][:, :], tri[d][:, :], start=True, stop=True), r=[K("grep")] + CK, w=[bk])
        P.add("pe", lambda e: e.matmul(X(i, 2), brep[i][:, :], ident[:, :], start=True, stop=True), r=[K("brep")] + CK, w=[bk])
        P.add("dve", lambda e: e.tensor_tensor(out=T1[i][:, :], in0=X(i, 0), in1=ident[:, :], op=ALU.mult), r=[bk] + CK, w=[K("T1")])
        P.add("dve", lambda e: e.reduce_sum(out=cl[:, 0:1], in_=T1[i][:, :], axis=AX.X), r=[K("T1")], w=[K("cols")])
        lc = 127 if d == 0 else 0
        P.add("act", lambda e: e.copy(out=cl[:, 1:2], in_=banks[i][:, lc:lc + 1]), r=[bk], w=[K("cols")])
        P.add("act", lambda e: e.activation(out=EB[i][:, :], in_=X(i, 0, 64), func=AF.Exp), r=[bk], w=[K("EB")])
        P.add("dve", lambda e: e.tensor_scalar(out=T1[i][:, :], in0=X(i, 0), scalar1=cl[:, 0:1], scalar2=0.0, op0=ALU.subtract, op1=ALU.min), r=[bk, K("cols")], w=[K("T1")])
        P.add("dve", lambda e: e.tensor_copy(out=bBs[i][:, :], in_=X(i, 2, 64)), r=[bk], w=[K("bBs")])
        P.add("act", lambda e: e.activation(out=DTm[i][:, :], in_=T1[i][:, :], func=AF.Exp), r=[K("T1")], w=[K("DTm")])
        P.add("pool", lambda e: e.tensor_tensor(out=DTm[i][:, :], in0=DTm[i][:, :], in1=msk[d][:, :], op=ALU.mult), r=[K("DTm")] + CK, w=[K("DTm")])
        P.add("pool", lambda e: e.tensor_tensor(out=DTs[i][:, :], in0=DTm[i][:, :], in1=smsk[d][:, :], op=ALU.mult), r=[K("DTm")] + CK, w=[K("DTs")])
        P.add("act", lambda e: e.activation(out=cl[:, 2:3], in_=cl[:, 0:1], func=AF.Exp, scale=-1.0, bias=cl[:, 1:2]), r=[K("cols")], w=[K("cols")])
        P.add("act", lambda e: e.activation(out=cl[:, 3:4], in_=cl[:, 1:2], func=AF.Exp), r=[K("cols")], w=[K("cols")])
        P.add("act", lambda e: e.activation(out=cl[:, 4:5], in_=cl[:, 0:1], func=AF.Exp), r=[K("cols")], w=[K("cols")])
        P.add("dve", lambda e: e.tensor_tensor(out=cl[:, 5:6], in0=cl[:, 4:5], in1=bcol, op=ALU.mult), r=[K("cols"), "g_btok"], w=[K("cols")])
        P.add("dve", lambda e: e.tensor_tensor(out=kbT[i][:, :], in0=kn[:, h, ch], in1=bBs[i][:, :], op=ALU.mult), r=["g_kn", K("bBs")], w=[K("kbT")])
        P.add("dve", lambda e: e.tensor_tensor(out=qd[i][:, :], in0=qn[:, h, ch], in1=EB[i][:, :], op=ALU.mult), r=["g_qn", K("EB")], w=[K("qd")])
        P.add("pool", lambda e: e.tensor_scalar(out=vb[i][:, :], in0=vtok[:, c, hs], scalar1=bcol, scalar2=None, op0=ALU.mult), r=["g_vtok", "g_btok"], w=[K("vb")])
        P.add("pool", lambda e: e.tensor_scalar(out=kbe[i][:, :], in0=ktok[:, c, hs], scalar1=cl[:, 5:6], scalar2=None, op0=ALU.mult), r=["g_ktok", K("cols")], w=[K("kbe")])
        P.add("pool", lambda e: e.tensor_scalar(out=kd[i][:, :], in0=ktok[:, c, hs], scalar1=cl[:, 2:3], scalar2=None, op0=ALU.mult), r=["g_ktok", K("cols")], w=[K("kd")])
        P.add("pe", lambda e: e.matmul(X(i, 0), kn[:, h, ch], kbT[i][:, :], start=True, stop=True), r=["g_kn", K("kbT")], w=[bk])
        P.add("pe", lambda e: e.matmul(X(i, 1), kn[:, h, ch], qn[:, h, ch], start=True, stop=True), r=["g_kn", "g_qn"], w=[bk])
        P.add("dve", lambda e: e.scalar_tensor_tensor(out=MT[i][:, :], in0=X(i, 0), scalar=-1.0, in1=DTs[i][:, :], op0=ALU.mult, op1=ALU.mult), r=[bk, K("DTs")], w=[K("MT")])
        P.add("dve", lambda e: e.tensor_tensor(out=AT[i][:, :], in0=X(i, 1), in1=DTm[i][:, :], op=ALU.mult), r=[bk, K("DTm")], w=[K("AT")])
        P.add("pe", lambda e: e.matmul(X(i, 2), MT[i][:, :], ident[:, :], start=True, stop=True), r=[K("MT")] + CK, w=[bk])
        P.add("act", lambda e: e.copy(out=Pa[i][:, :], in_=X(i, 2)), r=[bk], w=[K("Pa")])
        P.add("pool", lambda e: e.tensor_tensor(out=RT[i][:, :], in0=MT[i][:, :], in1=ident[:, :], op=ALU.add), r=[K("MT")] + CK, w=[K("RT")])
        Pc, PTc, Pn, PTn = Pa[i], MT[i], Pb[i], PTb[i]
        kPc, kPTc, kPn, kPTn = K("Pa"), K("MT"), K("Pb"), K("PTb")
        for lvl in range(1, 7):
            P.add("pe", lambda e, Pc=Pc, PTc=PTc: e.matmul(X(i, 0), PTc[:, :], Pc[:, :], start=True, stop=True), r=[kPc, kPTc], w=[bk])
            if lvl < 6:
                P.add("pe", lambda e, Pc=Pc, PTc=PTc: e.matmul(X(i, 1), Pc[:, :], PTc[:, :], start=True, stop=True), r=[kPc, kPTc], w=[bk])
            P.add("act", lambda e, Pn=Pn: e.copy(out=Pn[:, :], in_=X(i, 0)), r=[bk], w=[kPn])
            if lvl < 6:
                P.add("dve", lambda e, PTn=PTn: e.tensor_copy(out=PTn[:, :], in_=X(i, 1)), r=[bk], w=[kPTn])
            P.add("pe", lambda e, Pn=Pn: e.matmul(X(i, 2), Pn[:, :], RT[i][:, :], start=True, stop=True), r=[kPn, K("RT")], w=[bk])
            P.add("dve", lambda e: e.tensor_tensor(out=RT[i][:, :], in0=RT[i][:, :], in1=X(i, 2), op=ALU.add), r=[bk, K("RT")], w=[K("RT")])
            if lvl == 1:
                Pc, PTc, Pn, PTn = Pb[i], PTb[i], Pa[i], PTa[i]
                kPc, kPTc, kPn, kPTn = K("Pb"), K("PTb"), K("Pa"), K("PTa")
            else:
                Pc, PTc, Pn, PTn = Pn, PTn, Pc, PTc
                kPc, kPTc, kPn, kPTn = kPn, kPTn, kPc, kPTc
        P.add("pe", lambda e: e.matmul(X(i, 0, 128, 64), RT[i][:, :], vb[i][:, :], start=True, stop=True), r=[K("RT"), K("vb")], w=[bk])
        P.add("pe", lambda e: e.matmul(X(i, 1, 64, 128), kbe[i][:, :], RT[i][:, :], start=True, stop=True), r=[K("RT"), K("kbe")], w=[bk])
        P.add("act", lambda e: e.copy(out=u[i][:, :], in_=X(i, 0, 128, 64)), r=[bk], w=[K("u")])
        P.add("dve", lambda e: e.tensor_copy(out=wT[i][:, :], in_=X(i, 1, 64, 128)), r=[bk], w=[K("wT")])

    def chain(i, c, d, h):
        K = lambda nm: (nm, i)
        bk = ("bk", 4 + i)
        j = 4 + i
        ch = slice(c * 128, (c + 1) * 128)
        cl = cols[i]
        P.add("pe", lambda e: e.matmul(X(j, 0, 128, 64), wT[i][:, :], Sst[i][:, :], start=True, stop=True), r=[K("wT"), ("S", i)], w=[bk])
        P.add("dve", lambda e: e.tensor_tensor(out=vnew[i][:, :], in0=u[i][:, :], in1=X(j, 0, 128, 64), op=ALU.subtract), r=[bk, K("u")], w=[K("vnew")])
        P.add("pe", lambda e: e.matmul(X(j, 1, 64, 128), Sst[i][:, :], qd[i][:, :], start=True, stop=False), r=[("S", i), K("qd")], w=[bk])
        P.add("pe", lambda e: e.matmul(X(j, 1, 64, 128), vnew[i][:, :], AT[i][:, :], start=False, stop=True), r=[K("vnew"), K("AT")], w=[bk])
        P.add("pe", lambda e: e.matmul(X(j, 2, 64, 64), kd[i][:, :], vnew[i][:, :], start=True, stop=True), r=[K("kd"), K("vnew")], w=[bk])
        P.add("dve", lambda e: e.tensor_tensor(out=oacc[:, h, ch], in0=oacc[:, h, ch], in1=X(j, 1, 64, 128), op=ALU.add), r=[bk, "g_oacc"], w=["g_oacc"])
        P.add("dve", lambda e: e.scalar_tensor_tensor(out=Sst[i][:, :], in0=Sst[i][:, :], scalar=cl[0:64, 3:4], in1=X(j, 2, 64, 64), op0=ALU.mult, op1=ALU.add),
              r=[bk, ("S", i), K("cols")], w=[("S", i)])

    _on[0] = True
    for s in range(nsteps):
        insts = [(h * 2 + d, orders[d][s], d, h) for h in range(2) for d in range(2)]
        for (i, c, d, h) in insts:
            pre(i, c, d, h)
        for (i, c, d, h) in insts:
            chain(i, c, d, h)

    _on[0] = False
    print('gdn inst ops', _cnt[0])
    gt = xr; yo = acc

    def fin(t0, n):
        P.dma("sp", gt[:, :, :n], D["b_gate"].rearrange("(h d) t -> d h t", d=64)[:, :, t0:t0 + n], w=["g_xr"])
        P.add("act", lambda e: e.activation(out=sq[:, :, :n], in_=oacc[:, :, t0:t0 + n], func=AF.Square), r=["g_oacc"], w=["g_sq"])
        for h in range(2):
            P.add("pe", lambda e, h=h: e.matmul(banks[h][0:64, :n], ones[0:64, 0:64], sq[:, h, :n], start=True, stop=True), r=["g_sq", "ones"], w=[("bk", h)])
            P.add("act", lambda e, h=h: e.activation(out=rs[:, h, :n], in_=banks[h][0:64, :n], func=AF.Sqrt, scale=1.0 / 64, bias=C["epsb"][0:64, 0:1]), r=[("bk", h), "epsb"], w=["g_rs"])
        P.add("dve", lambda e: e.reciprocal(out=rs[:, :, :n], in_=rs[:, :, :n]), r=["g_rs"], w=["g_rs"])
        P.add("dve", lambda e: e.tensor_tensor(out=yo[:, :, :n], in0=oacc[:, :, t0:t0 + n], in1=rs[:, :, :n], op=ALU.mult), r=["g_oacc", "g_rs"], w=[("g_acc", 0), ("g_acc", 1)])
        P.add("act", lambda e: e.activation(out=gt[:, :, :n], in_=gt[:, :, :n], func=AF.Silu), r=["g_xr"], w=["g_xr"])
        P.add("dve", lambda e: e.scalar_tensor_tensor(out=yo[:, :, :n], in0=yo[:, :, :n], scalar=gg[:, 0:1], in1=gt[:, :, :n], op0=ALU.mult, op1=ALU.mult), r=[("g_acc", 0), ("g_acc", 1), "g_xr", "g_b_g"], w=[("g_acc", 0), ("g_acc", 1)])
        P.dma("sp", out.rearrange("(h d) t -> d h t", d=64)[:, :, t0:t0 + n], yo[:, :, :n], r=[("g_acc", 0), ("g_acc", 1)])

    for (t0, n) in QT:
        fin(t0, n)
    print("gdn ops", P.n_ops())
    return P.finish()


def build_M():
    P = Prog()
    scT = P.dram_in("scT", [1024, 5])
    wm = P.dram_in("wm", [1024, 3072])
    bm = P.dram_in("bm", [128, 24])
    modo = P.dram_out("modo", [128, 120])
    sc = P.sb([128, 8, 5], F32, "sc")
    P.dma("sp", sc[:], scT.rearrange("(k p) j -> p k j", p=128), w=["sc"])
    P.add("act", lambda e: e.activation(out=sc[:], in_=sc[:], func=AF.Silu), r=["sc"], w=["sc"])
    bms = P.sb([128, 24], F32, "bms")
    P.dma("sp", bms[:], bm[:, :], w=["bms"])
    w = P.sb([128, 8, 3072], F32, "wms")
    for k in range(8):
        P.dma("sp", w[:, k, :], wm[k * 128:(k + 1) * 128, :], w=[("wms", k)])
    ps = P.ps([128, 512], F32, "mps")
    ob = P.sb([128, 120], F32, "ob")
    for cc in range(24):
        for k in range(8):
            P.add("pe", lambda e, cc=cc, k=k: e.matmul(ps[:, cc * 5:(cc + 1) * 5], w[:, k, cc * 128:(cc + 1) * 128], sc[:, k, :], start=(k == 0), stop=(k == 7)),
                  r=["sc"] + [("wms", kk) for kk in range(8)], w=["mps"])
    for cc in range(24):
        P.add("dve", lambda e, cc=cc: e.tensor_scalar(out=ob[:, cc * 5:(cc + 1) * 5], in0=ps[:, cc * 5:(cc + 1) * 5], scalar1=bms[:, cc:cc + 1], scalar2=None, op0=ALU.add),
              r=["mps", "bms"], w=["ob"])
    P.dma("sp", modo[:, :], ob[:], r=["ob"])
    return P.finish()


OFF = {}
_o = 0
for nm, n in [("Aq",256),("Ak",128),("Av",128),("Bqkv",768),("Bgate",256),("Bab",16),("Ccq",256),("Cckv",128),("Ckr",32),("Dq",256),("Dk",256),("Dv",256),("Dgate",256)]:
    OFF[nm] = (_o, n); _o += n

def rope_perm(dim):
    q = dim // 4
    perm = np.concatenate([np.arange(q) + q, np.arange(q), np.arange(q) + 3 * q, np.arange(q) + 2 * q])
    sign = np.concatenate([-np.ones(q), np.ones(q), -np.ones(q), np.ones(q)]).astype(np.float32)
    return perm, sign

def rope_tables(rot_dim, rows=64, grid_w=64, theta=10000.0):
    n_freq = rot_dim // 4
    inv_freq = (theta ** (-np.arange(n_freq, dtype=np.float32) / n_freq)).astype(np.float32)
    row = np.repeat(np.arange(rows, dtype=np.float32), grid_w)
    col = np.tile(np.arange(grid_w, dtype=np.float32), rows)
    ang_r = row[:, None] * inv_freq
    ang_c = col[:, None] * inv_freq
    ang = np.concatenate([ang_r, ang_r, ang_c, ang_c], axis=-1).astype(np.float32)
    return np.cos(ang).astype(np.float32), np.sin(ang).astype(np.float32)

def rope_tabs_T(rot_dim):
    cos, sin = rope_tables(rot_dim)
    perm, sign = rope_perm(rot_dim)
    cT = np.ones((rot_dim, 4352), np.float32); sT = np.zeros((rot_dim, 4352), np.float32)
    cT[:, 256:] = cos.T; sT[:, 256:] = (sin * sign[None, :]).T
    return cT, sT

def perm_heads(w, dim):
    perm, _ = rope_perm(dim)
    nh = w.shape[1] // dim
    idx = np.concatenate([h * dim + perm for h in range(nh)])
    return w[:, idx]


def fm(v, nk):
    return np.ascontiguousarray(v.reshape(nk, 128).T)


def mla_inputs(P_, hf, W):
    hs = [2 * hf, 2 * hf + 1]
    perm32, _ = rope_perm(32)
    ct, st = rope_tabs_T(32)
    ct96 = np.ones((96, 4352), np.float32); st96 = np.zeros((96, 4352), np.float32)
    ct96[64:] = ct; st96[64:] = st
    wq = np.concatenate([W["mla_w_q_up"][:, h * 96:(h + 1) * 96] for h in hs], axis=1)
    wqP = np.zeros((256, 192), np.float32)
    for i, h in enumerate(hs):
        wqP[:, i * 96 + 64:i * 96 + 96] = W["mla_w_q_up"][:, h * 96 + 64 + perm32]
    wkn = np.zeros((128, 192), np.float32)
    for i, h in enumerate(hs):
        wkn[:, i * 96:i * 96 + 64] = W["mla_w_kv_up"][:, h * 128:h * 128 + 64]
    wv = np.concatenate([W["mla_w_kv_up"][:, h * 128 + 64:h * 128 + 128] for h in hs], axis=1)
    sel = np.zeros((32, 96), np.float32); sel[np.arange(32), 64 + np.arange(32)] = 1
    return {"c_cq": P_["Ccq"], "c_ckv": P_["Cckv"], "c_kr": P_["Ckr"], "c_krP": P_["CkrP"], "c_ct96": ct96, "c_st96": st96,
            "c_qng": fm(W["mla_q_norm"], 2), "c_kvg": fm(W["mla_kv_norm"], 1), "c_wq": np.ascontiguousarray(wq), "c_wqP": wqP,
            "c_wkn": wkn, "c_wv": np.ascontiguousarray(wv), "c_sel": sel}


def swa_inputs(PF, PT, hf, W):
    cT, sT = rope_tabs_T(64)
    j = np.arange(128)[:, None]; i = np.arange(128)[None, :]
    sink = W["swa_sink"][2 * hf:2 * hf + 2]
    return {"a_q": PF["Aq"][hf * 128:(hf + 1) * 128], "a_qP": PF["AqP"][hf * 128:(hf + 1) * 128],
            "a_k": PF["Ak"][hf * 64:(hf + 1) * 64], "a_kP": PF["AkP"][hf * 64:(hf + 1) * 64],
            "a_vtok": np.ascontiguousarray(PT["Av"][:, hf * 64:(hf + 1) * 64]), "a_cos": cT, "a_sin": sT,
            "a_sink": np.ascontiguousarray(np.broadcast_to(sink[None, :], (128, 2))).astype(np.float32),
            "a_maskP": (j >= i).astype(np.float32), "a_maskN": (j <= i).astype(np.float32)}


def ret_inputs(PF, PT, hf, W):
    cT, sT = rope_tabs_T(64)
    hs = [2 * hf, 2 * hf + 1]
    sl = slice(hf * 128, (hf + 1) * 128)
    ld = W["ret_log_decay"]
    ldp = np.zeros((128, 2), np.float32); ldr = np.zeros((128, 4), np.float32)
    for d in range(2):
        for i, h in enumerate(hs):
            ldp[i * 64:(i + 1) * 64, d] = ld[d, h]
            ldr[:, 2 * d + i] = ld[d, h]
    j = np.arange(128)[:, None].astype(np.float32); i = np.arange(128)[None, :].astype(np.float32)
    bd = np.zeros((128, 128), np.float32); bd[:64, :64] = 1; bd[64:, 64:] = 1
    pk = np.stack([127 - np.arange(128), np.arange(128)], 1).astype(np.float32)
    return {"d_q": PF["Dq"][sl], "d_qP": PF["DqP"][sl], "d_k": PF["Dk"][sl], "d_kP": PF["DkP"][sl], "d_gate": PF["Dgate"][sl],
            "d_vtok": np.ascontiguousarray(PT["Dv"][:, sl]), "d_ktok": np.ascontiguousarray(PT["Dk"][:, sl]), "d_kPtok": np.ascontiguousarray(PT["DkP"][:, sl]),
            "d_cos": cT, "d_sin": sT, "d_costok": np.ascontiguousarray(cT.T), "d_sintok": np.ascontiguousarray(sT.T),
            "d_ldp": ldp, "d_ldr": ldr, "d_g": np.ascontiguousarray(W["ret_norm"][sl].reshape(128, 1)),
            "d_relu": np.maximum(i - j, 0) + 0 * j, "d_rell": np.maximum(j - i, 0) + 0 * i, "d_um": (i >= j).astype(np.float32), "d_lm": (j >= i).astype(np.float32),
            "d_pos1": (i + 1) + 0 * j, "d_posr": (128 - i) + 0 * j, "d_pk": pk, "d_bd": bd, "d_bd64": bd / 64}


def gdn_inputs(PF, PT, hf, W):
    hs = [2 * hf, 2 * hf + 1]
    qkv = PF["Bqkv"]
    sel = lambda base: np.ascontiguousarray(np.concatenate([qkv[base + h * 64: base + (h + 1) * 64] for h in hs], 0))
    ab = PT["Bab"]
    abl = np.zeros((4352, 8), np.float32)
    dtb = np.zeros((8,), np.float32); alog = np.zeros((8,), np.float32)
    for d in range(2):
        for w in range(2):
            for hl, h in enumerate(hs):
                abl[:, d * 4 + w * 2 + hl] = ab[:, d * 8 + w * 4 + h]
        for hl, h in enumerate(hs):
            dtb[d * 4 + hl] = W["gdn_dt_bias"][d, h]; alog[d * 4 + hl] = W["gdn_a_log"][d, h]
    cwt = np.zeros((64, 3, 2, 5), np.float32)
    conv = W["gdn_conv"]
    for gi in range(3):
        for hl, h in enumerate(hs):
            cwt[:, gi, hl, :] = conv[:, gi * 256 + h * 64: gi * 256 + (h + 1) * 64].T
    k = np.arange(128)[:, None]; i = np.arange(128)[None, :]
    f = lambda m: m.astype(np.float32)
    return {"b_q": sel(0), "b_k": sel(256), "b_v": sel(512), "b_gate": PF["Bgate"][hf * 128:(hf + 1) * 128], "b_ab": abl,
            "b_cw": cwt.reshape(64, 30), "b_dtb": np.ascontiguousarray(np.broadcast_to(np.tile(dtb, 34)[None], (128, 272))),
            "b_alog": np.ascontiguousarray(np.broadcast_to(np.tile(alog, 34)[None], (128, 272))), "b_g": np.ascontiguousarray(W["gdn_norm"].reshape(64, 1)),
            "b_triF": f(k <= i), "b_triR": f(k >= i), "b_ident": np.eye(128, dtype=np.float32), "b_um": f(i >= k), "b_lm": f(k >= i),
            "b_us": f(i > k), "b_ls": f(k > i)}


_PROGS = {}


def _prog(name, fn):
    if name not in _PROGS:
        _PROGS[name] = fn()
    return _PROGS[name]


def _run(name, fn, in_maps):
    nc = fn()
    in_maps = [{k: np.ascontiguousarray(v, dtype=np.float32) for k, v in m.items()} for m in in_maps]
    res = run_bass_kernel_spmd(nc, in_maps, core_ids=list(range(8)))
    return res.results


A_TILES = TILES
HALF = 2176


def _mod_table(mod_l, b, hf, tiles_ctx):
    nt = len(tiles_ctx)
    t = np.zeros((128, 6, nt, 8), np.float32)
    for ti, is_ctx in enumerate(tiles_ctx):
        v = mod_l[4 if is_ctx else b].reshape(6, 8, 128)
        t[:, :, ti, :] = v.transpose(2, 0, 1)
    return t


def kernel(x, c, ctx, c_ctx, w_mod, b_mod, norm1, norm2, w_in, w_out, swa_sink, gdn_conv, gdn_a_log,
           gdn_dt_bias, gdn_norm, mla_q_norm, mla_kv_norm, mla_w_q_up, mla_w_kv_up, ret_log_decay,
           ret_norm, ffn_w_gate, ffn_w_up, ffn_w_down, moe_router, moe_w_gate, moe_w_up, moe_w_down,
           final_norm, _nlayers=4):
    f32 = np.float32
    x = np.asarray(x, f32); ctx = np.asarray(ctx, f32)
    B = 4
    scT = np.ascontiguousarray(np.concatenate([np.asarray(c, f32), np.asarray(c_ctx, f32)[None]], 0).T)
    wm_all = np.asarray(w_mod, f32).transpose(1, 0, 2).reshape(1024, 4 * 6144)
    bm_all = np.asarray(b_mod, f32).reshape(4 * 6144)
    ims = []
    for core in range(8):
        sl = slice(core * 3072, (core + 1) * 3072)
        ims.append({"scT": scT, "wm": np.ascontiguousarray(wm_all[:, sl]), "bm": np.ascontiguousarray(bm_all[sl].reshape(24, 128).T)})
    res = _run("M", build_M, ims)
    mod = np.zeros((5, 4 * 6144), f32)
    for core in range(8):
        mo = res[core]["modo"].reshape(128, 24, 5)
        mod[:, core * 3072:(core + 1) * 3072] = mo.transpose(2, 1, 0).reshape(5, 3072)
    mod = mod.reshape(5, 4, 6144)

    hT = [np.ascontiguousarray(np.concatenate([ctx[b], x[b]], 0).T) for b in range(B)]
    p64, _ = rope_perm(64)
    p32, _ = rope_perm(32)
    ident = np.eye(128, dtype=f32)
    out_final = None
    for l in range(_nlayers):
        W = {"w_in": np.asarray(w_in[l], f32), "swa_sink": np.asarray(swa_sink[l], f32), "gdn_conv": np.asarray(gdn_conv[l], f32),
             "gdn_a_log": np.asarray(gdn_a_log[l], f32), "gdn_dt_bias": np.asarray(gdn_dt_bias[l], f32), "gdn_norm": np.asarray(gdn_norm[l], f32),
             "mla_q_norm": np.asarray(mla_q_norm[l], f32), "mla_kv_norm": np.asarray(mla_kv_norm[l], f32), "mla_w_q_up": np.asarray(mla_w_q_up[l], f32),
             "mla_w_kv_up": np.asarray(mla_w_kv_up[l], f32), "ret_log_decay": np.asarray(ret_log_decay[l], f32), "ret_norm": np.asarray(ret_norm[l], f32)}
        wi = W["w_in"]
        blk = lambda nm: wi[:, OFF[nm][0]:OFF[nm][0] + OFF[nm][1]]
        fcols = {"Aq": blk("Aq"), "AqP": perm_heads(blk("Aq"), 64), "Ak": blk("Ak"), "AkP": perm_heads(blk("Ak"), 64), "Bqkv": blk("Bqkv"),
                 "Bgate": blk("Bgate"), "Ccq": blk("Ccq"), "Cckv": blk("Cckv"), "Ckr": blk("Ckr"), "CkrP": perm_heads(blk("Ckr"), 32),
                 "Dq": blk("Dq"), "DqP": perm_heads(blk("Dq"), 64), "Dk": blk("Dk"), "DkP": perm_heads(blk("Dk"), 64), "Dgate": blk("Dgate")}
        tcols = {"Av": blk("Av"), "Bab": blk("Bab"), "Dv": blk("Dv"), "Dk": blk("Dk"), "DkP": perm_heads(blk("Dk"), 64)}
        win_ext = np.ascontiguousarray(np.concatenate([fcols[nm] for nm, _ in F_BLOCKS] + [tcols[nm] for nm, _ in T_BLOCKS], 1))
        gn1 = fm(np.asarray(norm1[l], f32), 8)
        gn2 = fm(np.asarray(norm2[l], f32), 8)
        ims = []
        for core in range(8):
            b, hf = core // 2, core % 2
            mt = _mod_table(mod[:, l], b, hf, [hf == 0 and ti == 0 for ti in range(5)])
            ims.append({"hT": np.ascontiguousarray(hT[b][:, hf * HALF:(hf + 1) * HALF]), "modt": mt.reshape(128, 240), "gn": gn1, "win": win_ext})
        res = _run("A", build_A, ims)
        PFs, PTs = [], []
        for b in range(B):
            pT = np.concatenate([res[2 * b]["projT"], res[2 * b + 1]["projT"]], 1)
            pK = np.concatenate([res[2 * b]["projTok"], res[2 * b + 1]["projTok"]], 0)
            PF, PT = {}, {}
            o = 0
            for nm, n in F_BLOCKS:
                PF[nm] = pT[o:o + n]; o += n
            o = 0
            for nm, n in T_BLOCKS:
                PT[nm] = pK[:, o:o + n]; o += n
            PFs.append(PF); PTs.append(PT)
        mixT = [np.zeros((1024, NTOK), f32) for _ in range(B)]
        for gi, (nm, bfn, ifn) in enumerate((("swa", build_swa, swa_inputs), ("gdn", build_gdn, gdn_inputs), ("mla", build_mla, None), ("ret", build_ret, ret_inputs))):
            ims = []
            for core in range(8):
                b, hf = core // 2, core % 2
                ims.append(mla_inputs(PFs[b], hf, W) if nm == "mla" else ifn(PFs[b], PTs[b], hf, W))
            res = _run(nm, bfn, ims)
            for core in range(8):
                b, hf = core // 2, core % 2
                mixT[b][gi * 256 + hf * 128: gi * 256 + (hf + 1) * 128] = res[core]["mix"]
        moe = (l % 2 == 1)
        final = (l == 3)
        i2 = l // 2
        nt = 5 if moe else 9
        nl = 2 if moe else 1
        span = nt * 256
        padw = nl * span
        if moe:
            wts = {"wg": np.asarray(moe_w_gate[i2], f32), "wu": np.asarray(moe_w_up[i2], f32), "wd": np.asarray(moe_w_down[i2], f32),
                   "wr": np.asarray(moe_router[i2], f32), "ident": ident}
        else:
            wts = {"wg": np.asarray(ffn_w_gate[i2], f32)[None], "wu": np.asarray(ffn_w_up[i2], f32)[None], "wd": np.asarray(ffn_w_down[i2], f32)[None]}
        wts["wout"] = np.asarray(w_out[l], f32)
        wts["gn"] = gn2
        if final:
            wts["fn"] = fm(np.asarray(final_norm, f32), 8)
        newh = [np.zeros((1024, NTOK), f32) for _ in range(B)]
        outs = [np.zeros((1024, NTOK), f32) for _ in range(B)]
        for r in range(nl):
            ims = []
            for core in range(8):
                b, hf = core // 2, core % 2
                hp = np.zeros((1024, padw), f32); mp = np.zeros((1024, padw), f32)
                hp[:, :HALF] = hT[b][:, hf * HALF:(hf + 1) * HALF]
                mp[:, :HALF] = mixT[b][:, hf * HALF:(hf + 1) * HALF]
                mt = _mod_table(mod[:, l], b, hf, [hf == 0 and (r * nt + ti) == 0 for ti in range(nt)])
                d = {"hT": np.ascontiguousarray(hp[:, r * span:(r + 1) * span]), "mixT": np.ascontiguousarray(mp[:, r * span:(r + 1) * span]),
                     "modt": mt.reshape(128, 6 * nt * 8)}
                d.update(wts)
                ims.append(d)
            res = _run(("C", moe, final, nt), lambda: build_C(moe, final, nt), ims)
            for core in range(8):
                b, hf = core // 2, core % 2
                lo = r * span
                hi = min((r + 1) * span, HALF)
                if hi > lo:
                    newh[b][:, hf * HALF + lo: hf * HALF + hi] = res[core]["h2T"][:, :hi - lo]
                    if final:
                        outs[b][:, hf * HALF + lo: hf * HALF + hi] = res[core]["outT"][:, :hi - lo]
        hT = newh
        if final:
            out_final = np.stack([np.ascontiguousarray(outs[b][:, 256:].T) for b in range(B)], 0)
    if _nlayers < 4:
        return hT
    return out_final.astype(np.float32)
```

```python
import numpy as np
import concourse.bass as bass
import concourse.mybir as mybir
from concourse.bass_utils import run_bass_kernel_spmd
from contextlib import ExitStack

F32 = mybir.dt.float32
BF16 = mybir.dt.bfloat16
AF = mybir.ActivationFunctionType
ALU = mybir.AluOpType
AX = mybir.AxisListType

ENGS = ("pe", "act", "dve", "pool", "sp")


class Op:
    __slots__ = ("eng", "fn", "deps", "is_dma", "sem", "val", "marked", "idx", "prewait")

    def __init__(self, eng, fn, is_dma):
        self.eng = eng
        self.fn = fn
        self.deps = []
        self.is_dma = is_dma
        self.sem = None
        self.val = None
        self.marked = False
        self.prewait = None


class Prog:
    def __init__(self, name="k", n_dma_sems=12):
        self.nc = bass.Bass("TRN2", target_bir_lowering=False)
        self.es = ExitStack()
        self.ops = {e: [] for e in ENGS}
        self.last_w = {}
        self.readers = {}
        self.n_dma_sems = n_dma_sems
        self.dma_rr = 0
        self.dma_last = [None] * n_dma_sems
        self.dma_cnt = [0] * n_dma_sems
        self.uid = 0

    def sb(self, shape, dt=F32, name=None):
        self.uid += 1
        return self.es.enter_context(self.nc.sbuf_tensor("s_" + (name or f"sb{self.uid}"), list(shape), dt))

    def ps(self, shape, dt=F32, name=None):
        self.uid += 1
        return self.es.enter_context(self.nc.psum_tensor("p_" + (name or f"ps{self.uid}"), list(shape), dt))

    def dram_in(self, name, shape, dt=F32):
        return self.nc.dram_tensor(name, list(shape), dt, kind="ExternalInput").ap()

    def dram_out(self, name, shape, dt=F32):
        return self.nc.dram_tensor(name, list(shape), dt, kind="ExternalOutput").ap()

    def dram_tmp(self, name, shape, dt=F32):
        return self.nc.dram_tensor(name, list(shape), dt, kind="Internal").ap()

    def _track(self, op, r, w):
        deps = []
        for k in r:
            lw = self.last_w.get(k)
            if lw is not None:
                deps.append(lw)
        for k in w:
            lw = self.last_w.get(k)
            if lw is not None:
                deps.append(lw)
            for rd in self.readers.get(k, ()):
                deps.append(rd)
        for k in r:
            self.readers.setdefault(k, []).append(op)
        for k in w:
            self.last_w[k] = op
            self.readers[k] = []
        op.deps = [d for d in deps if d is not op and not (d.eng == "pe" and op.eng == "pe" and not d.is_dma and not op.is_dma)]
        for d in op.deps:
            d.marked = True

    def add(self, eng, fn, r=(), w=()):
        op = Op(eng, fn, False)
        self._track(op, r, w)
        self.ops[eng].append(op)
        return op

    def dma(self, eng, out, in_, r=(), w=(), fn=None):
        op = Op(eng, fn if fn is not None else (lambda e: e.dma_start(out=out, in_=in_)), True)
        s = self.dma_rr
        self.dma_rr = (s + 1) % self.n_dma_sems
        op.prewait = self.dma_last[s]
        self.dma_cnt[s] += 16
        op.sem = s
        op.val = self.dma_cnt[s]
        op.marked = True
        self.dma_last[s] = op
        self._track(op, r, w)
        self.ops[eng].append(op)
        return op

    def finish(self):
        nc = self.nc
        es = self.es
        esem = {e: es.enter_context(nc.semaphore(f"sem_{e}")) for e in ENGS}
        dsem = [es.enter_context(nc.semaphore(f"sem_dma{i}")) for i in range(self.n_dma_sems)]
        for e in ENGS:
            c = 0
            for op in self.ops[e]:
                if op.is_dma:
                    continue
                if op.marked:
                    c += 1
                    op.val = c
                    op.sem = e
        block = es.enter_context(nc.Block())
        ops = self.ops

        def emit(e, eng):
            waited = {}
            for op in ops[e]:
                need = {}
                dl = list(op.deps)
                if op.prewait is not None:
                    dl.append(op.prewait)
                for d in dl:
                    key = ("d", d.sem) if d.is_dma else ("e", d.sem)
                    if need.get(key, 0) < d.val:
                        need[key] = d.val
                for key, v in need.items():
                    if waited.get(key, 0) >= v:
                        continue
                    waited[key] = v
                    sem = dsem[key[1]] if key[0] == "d" else esem[key[1]]
                    eng.wait_ge(sem, v)
                ins = op.fn(eng)
                if op.is_dma:
                    ins.then_inc(dsem[op.sem], 16)
                elif op.marked:
                    ins.then_inc(esem[e], 1)
            if e == "sp":
                for i in range(self.n_dma_sems):
                    if self.dma_cnt[i] > 0:
                        eng.wait_ge(dsem[i], self.dma_cnt[i])

        @block.tensor
        def _(eng):
            emit("pe", eng)

        @block.scalar
        def _(eng):
            emit("act", eng)

        @block.vector
        def _(eng):
            emit("dve", eng)

        @block.gpsimd
        def _(eng):
            emit("pool", eng)

        @block.sync
        def _(eng):
            emit("sp", eng)

        es.close()
        return nc

    def n_ops(self):
        return {e: len(v) for e, v in self.ops.items()}


def run(nc, in_maps, trace=False):
    res = run_bass_kernel_spmd(nc, in_maps, core_ids=list(range(len(in_maps))), trace=trace)
    return res


TILES = [(0, 256), (256, 512), (768, 512), (1280, 512), (1792, 384)]
TC = 2176
EPS = 1e-6
F_BLOCKS = [("Aq", 256), ("AqP", 256), ("Ak", 128), ("AkP", 128), ("Bqkv", 768), ("Bgate", 256), ("Ccq", 256),
            ("Cckv", 128), ("Ckr", 32), ("CkrP", 32), ("Dq", 256), ("DqP", 256), ("Dk", 256), ("DkP", 256), ("Dgate", 256)]
T_BLOCKS = [("Av", 128), ("Bab", 16), ("Dv", 256), ("Dk", 256), ("DkP", 256)]
NF = sum(n for _, n in F_BLOCKS)
NT = sum(n for _, n in T_BLOCKS)


def foff(name, blocks):
    o = 0
    for nm, n in blocks:
        if nm == name:
            return o, n
        o += n
    raise KeyError(name)


class Rot:
    def __init__(self, bufs, name):
        self.bufs = bufs
        self.i = 0
        self.name = name

    def next(self):
        b = self.bufs[self.i % len(self.bufs)]
        k = (self.name, self.i % len(self.bufs))
        self.i += 1
        return b, k


def emit_norm_mod(P, C, ht, hkey, n, Asc, shv, t, u, ukey):
    sq, ssps, rs, tmp = C["sq"], C["ssps"], C["rs"], C["tmp"]
    P.add("act", lambda e: e.activation(out=sq[:, :, :n], in_=ht[:, :, :n], func=AF.Square), r=[hkey], w=["sq"])
    for k in range(8):
        P.add("pe", lambda e, k=k: e.matmul(ssps[:, :n], C["ones"][:, :], sq[:, k, :n], start=(k == 0), stop=(k == 7)),
              r=["sq", "ones"], w=["ssps"])
    P.add("act", lambda e: e.activation(out=rs[:, :n], in_=ssps[:, :n], func=AF.Sqrt, scale=1.0 / 1024, bias=C["epsb"][:, 0:1]),
          r=["ssps", "epsb"], w=["rs"])
    P.add("dve", lambda e: e.reciprocal(out=rs[:, :n], in_=rs[:, :n]), r=["rs"], w=["rs"])
    for k in range(8):
        P.add("dve", lambda e, k=k: e.tensor_tensor(out=tmp[:, k, :n], in0=ht[:, k, :n], in1=rs[:, :n], op=ALU.mult),
              r=[hkey, "rs"], w=[("tmp", k)])
        P.add("act", lambda e, k=k: e.activation(out=u[:, k, :n], in_=tmp[:, k, :n], func=AF.Identity,
                                                scale=Asc[:, t, k:k + 1], bias=shv[:, t, k:k + 1]),
              r=[("tmp", k), "modc"], w=[ukey])


def common_consts(P):
    C = {}
    C["ones"] = P.sb([128, 128], F32, "ones")
    P.add("pool", lambda e: e.memset(C["ones"][:], 1.0), w=["ones"])
    C["epsb"] = P.sb([128, 1], F32, "epsb")
    P.add("pool", lambda e: e.memset(C["epsb"][:], EPS), w=["epsb"])
    C["sq"] = P.sb([128, 8, 512], F32, "sq")
    C["tmp"] = P.sb([128, 8, 512], F32, "tmp")
    C["rs"] = P.sb([128, 512], F32, "rs")
    C["ssps"] = P.ps([128, 512], F32, "ssps")
    return C


def load_mod(P, modt_d, gn_d, kinds):
    modt = P.sb([128, 6, 5, 8], F32, "modt")
    P.dma("sp", modt[:].rearrange("p a b c -> p (a b c)"), modt_d[:, :], w=["modt"])
    return modt


def build_A():
    P = Prog()
    hT = P.dram_in("hT", [1024, TC])
    modt_d = P.dram_in("modt", [128, 240])
    gn_d = P.dram_in("gn", [128, 8])
    win = P.dram_in("win", [1024, NF + NT])
    projT = P.dram_out("projT", [NF, TC])
    projTok = P.dram_out("projTok", [TC, NT])
    C = common_consts(P)
    emit_A_body(P, C, hT, None, modt_d, gn_d, win, projT, projTok)
    return P.finish()


def emit_A_setup(P, C, modt_d, gn_d, win, pre=""):
    modt = P.sb([128, 6, 5, 8], F32, pre + "modt")
    P.dma("sp", modt[:].rearrange("p a b c -> p (a b c)"), modt_d[:, :], w=[pre + "modt"])
    gn = P.sb([128, 8], F32, pre + "gn")
    P.dma("sp", gn[:], gn_d[:, :], w=[pre + "gn"])
    A1 = P.sb([128, 5, 8], F32, pre + "A1")
    for t in range(5):
        P.add("dve", lambda e, t=t: e.scalar_tensor_tensor(out=A1[:, t, :], in0=modt[:, 1, t, :], scalar=1.0, in1=gn[:, :],
                                                          op0=ALU.add, op1=ALU.mult), r=[pre + "modt", pre + "gn"], w=["modc"])
    wb = P.sb([128, 8, NF + NT], BF16, pre + "wb")
    for k in range(8):
        P.dma("pool", wb[:, k, :], win[k * 128:(k + 1) * 128, :], w=[("wb", k)])
    return modt, A1, wb


def emit_A_tile(P, C, S, ti, ht, hkey, projT, projTok):
    modt, A1, wb = S["modt"], S["A1"], S["wb"]
    t0, n = TILES[ti]
    u, ukey = S["u"].next()
    emit_norm_mod(P, C, ht, hkey, n, A1, modt[:, 0], ti, u, ukey)
    wkeys = [("wb", k) for k in range(8)]
    ncc = (NF + 127) // 128
    for cc in range(ncc):
        c0 = cc * 128
        m = min(128, NF - c0)
        ps, pk = S["mmps"].next()
        for k in range(8):
            P.add("pe", lambda e, k=k, ps=ps, c0=c0, m=m: e.matmul(ps[:m, :n], wb[:, k, c0:c0 + m], u[:, k, :n], start=(k == 0), stop=(k == 7)),
                  r=[ukey] + wkeys, w=[pk])
        st, sk = S["stage"].next()
        if cc % 2 == 0:
            P.add("act", lambda e, ps=ps, st=st, m=m: e.copy(out=st[:m, :n], in_=ps[:m, :n]), r=[pk], w=[sk])
        else:
            P.add("dve", lambda e, ps=ps, st=st, m=m: e.tensor_copy(out=st[:m, :n], in_=ps[:m, :n]), r=[pk], w=[sk])
        P.dma("sp", projT[c0:c0 + m, t0:t0 + n], st[:m, :n], r=[sk])
    for s in range(n // 128):
        for (c0, c1) in ((0, 512), (512, NT)):
            ps, pk = S["mmps"].next()
            w_ = c1 - c0
            for k in range(8):
                P.add("pe", lambda e, k=k, ps=ps, c0=c0, w_=w_, s=s: e.matmul(ps[:, :w_], u[:, k, s * 128:(s + 1) * 128], wb[:, k, NF + c0:NF + c0 + w_],
                                                                         start=(k == 0), stop=(k == 7)), r=[ukey] + wkeys, w=[pk])
            st, sk = S["stage"].next()
            P.add("dve" if s % 2 else "act", (lambda e, ps=ps, st=st, w_=w_: e.tensor_copy(out=st[:, :w_], in_=ps[:, :w_])) if s % 2 else
                  (lambda e, ps=ps, st=st, w_=w_: e.copy(out=st[:, :w_], in_=ps[:, :w_])), r=[pk], w=[sk])
            P.dma("sp", projTok[t0 + s * 128:t0 + (s + 1) * 128, c0:c1], st[:, :w_], r=[sk])


def emit_A_body(P, C, hT, _, modt_d, gn_d, win, projT, projTok):
    modt, A1, wb = emit_A_setup(P, C, modt_d, gn_d, win)
    S = {"modt": modt, "A1": A1, "wb": wb}
    S["u"] = Rot([P.sb([128, 8, 512], BF16, f"u{i}") for i in range(2)], "u")
    S["mmps"] = Rot([P.ps([128, 512], F32, f"mmps{i}") for i in range(4)], "mmps")
    S["stage"] = Rot([P.sb([128, 512], F32, f"stg{i}") for i in range(4)], "stg")
    hts = Rot([P.sb([128, 8, 512], F32, f"ht{i}") for i in range(2)], "ht")
    hv = hT.rearrange("(k p) t -> p k t", p=128)
    for ti, (t0, n) in enumerate(TILES):
        ht, hk = hts.next()
        P.dma("sp", ht[:, :, :n], hv[:, :, t0:t0 + n], w=[hk])
        emit_A_tile(P, C, S, ti, ht, hk, projT, projTok)


DFF = 3584
NFG = 7


def build_C(moe, final, ntiles=9):
    TC = ntiles * 256
    CT = [(i * 256, 256) for i in range(ntiles)]
    NE = 8 if moe else 1
    P = Prog()
    hT = P.dram_in("hT", [1024, TC])
    mixT = P.dram_in("mixT", [1024, TC])
    wout = P.dram_in("wout", [1024, 1024])
    modt_d = P.dram_in("modt", [128, 6 * ntiles * 8])
    gn_d = P.dram_in("gn", [128, 8])
    wg = P.dram_in("wg", [NE, 1024, DFF])
    wu = P.dram_in("wu", [NE, 1024, DFF])
    wd = P.dram_in("wd", [NE, DFF, 1024])
    if moe:
        wr_d = P.dram_in("wr", [1024, 8])
        ident_d = P.dram_in("ident", [128, 128])
    if final:
        fn_d = P.dram_in("fn", [128, 8])
    h2T = P.dram_out("h2T", [1024, TC])
    C = common_consts(P)
    for nm in ("sq", "tmp"):
        pass
    modt = P.sb([128, 6, ntiles, 8], F32, "modt")
    P.dma("sp", modt[:].rearrange("p a b c -> p (a b c)"), modt_d[:, :], w=["modt"])
    gn = P.sb([128, 8], F32, "gn")
    P.dma("sp", gn[:], gn_d[:, :], w=["gn"])
    A2 = P.sb([128, ntiles, 8], F32, "A2")
    for t in range(ntiles):
        P.add("dve", lambda e, t=t: e.scalar_tensor_tensor(out=A2[:, t, :], in0=modt[:, 4, t, :], scalar=1.0, in1=gn[:, :],
                                                          op0=ALU.add, op1=ALU.mult), r=["modt", "gn"], w=["modc"])
    wo = P.sb([128, 8, 1024], BF16, "wo")
    for k in range(8):
        P.dma("pool", wo[:, k, :], wout[k * 128:(k + 1) * 128, :], w=["wo"])
    if moe:
        wr = P.sb([128, 8, 8], F32, "wr")
        P.dma("sp", wr[:], wr_d.rearrange("(k p) e -> p k e", p=128), w=["wr"])
        ident = P.sb([128, 128], F32, "ident")
        P.dma("sp", ident[:], ident_d[:, :], w=["ident"])
        lg = P.sb([128, 2, 8], F32, "lg")
        l2 = P.sb([128, 2, 8], F32, "l2")
        mk1 = P.sb([128, 2, 8], F32, "mk1")
        mk2 = P.sb([128, 2, 8], F32, "mk2")
        comb = P.sb([128, 2, 8], F32, "comb")
        m12 = P.sb([128, 2, 4], F32, "m12")
        rep = Rot([P.sb([128, 128], F32, f"rep{i}") for i in range(2)], "rep")
        cB = P.sb([128, 8, 256], F32, "cB")
        uf = P.sb([128, 8, 256], F32, "uf")
    if final:
        fng = P.sb([128, 8], F32, "fng")
        P.dma("sp", fng[:], fn_d[:, :], w=["fng"])
        outT = P.dram_out("outT", [1024, TC])
    ht = P.sb([128, 8, 256], F32, "ht")
    mixb = P.sb([128, 8, 256], BF16, "mixb")
    h1 = P.sb([128, 8, 256], F32, "h1")
    h2 = P.sb([128, 8, 256], F32, "h2")
    u = P.sb([128, 8, 256], BF16, "u")
    hh = Rot([P.sb([128, 4, 256], BF16, f"hh{i}") for i in range(2)], "hh")
    sg = Rot([P.sb([128, 256], F32, f"sg{i}") for i in range(2)], "sg")
    t1 = Rot([P.sb([128, 256], F32, f"t1{i}") for i in range(2)], "t1")
    wgs = Rot([P.sb([128, 8, 512], BF16, f"wgs{i}") for i in range(2)], "wgs")
    wus = Rot([P.sb([128, 8, 512], BF16, f"wus{i}") for i in range(2)], "wus")
    wds = Rot([P.sb([128, 4, 1024], BF16, f"wds{i}") for i in range(2)], "wds")
    gups = Rot([P.ps([128, 2, 256], F32, f"gups{i}") for i in range(2)], "gups")
    yps = [P.ps([128, 2, 256], F32, f"yps{i}") for i in range(4)]
    misc = P.ps([128, 512], F32, "misc")
    mps = Rot([misc[:, 0:256]], "misc")
    cbps = misc[:, 256:512]
    sq, tmp, rs, ssps = C["sq"], C["tmp"], C["rs"], C["ssps"]
    lgps = ssps[:, 384:400].rearrange("p (s e) -> p s e", e=8)
    hv = hT.rearrange("(k p) t -> p k t", p=128)
    mv = mixT.rearrange("(k p) t -> p k t", p=128)
    ov = h2T.rearrange("(k p) t -> p k t", p=128)

    def norm_parts(src, skey, n):
        P.add("act", lambda e: e.activation(out=sq[:, :, :n], in_=src[:, :, :n], func=AF.Square), r=[skey], w=["sq"])
        for k in range(8):
            P.add("pe", lambda e, k=k: e.matmul(ssps[:, :n], C["ones"][:, :], sq[:, k, :n], start=(k == 0), stop=(k == 7)),
                  r=["sq", "ones"], w=["ssps"])
        P.add("act", lambda e: e.activation(out=rs[:, :n], in_=ssps[:, :n], func=AF.Sqrt, scale=1.0 / 1024, bias=C["epsb"][:, 0:1]),
              r=["ssps", "epsb"], w=["rs"])
        P.add("dve", lambda e: e.reciprocal(out=rs[:, :n], in_=rs[:, :n]), r=["rs"], w=["rs"])

    def tile_body(ti, t0, n):
        P.dma("sp", ht[:, :, :n], hv[:, :, t0:t0 + n], w=["ht"])
        P.dma("pool", mixb[:, :, :n], mv[:, :, t0:t0 + n], w=["mixb"])
        for dm in range(8):
            ps, pk = mps.next()
            for k in range(8):
                P.add("pe", lambda e, k=k, dm=dm, ps=ps: e.matmul(ps[:, :n], wo[:, k, dm * 128:(dm + 1) * 128], mixb[:, k, :n],
                                                                 start=(k == 0), stop=(k == 7)), r=["wo", "mixb"], w=[pk])
            P.add("dve", lambda e, dm=dm, ps=ps: e.scalar_tensor_tensor(out=h1[:, dm, :n], in0=ps[:, :n], scalar=modt[:, 2, ti, dm:dm + 1],
                                                                      in1=ht[:, dm, :n], op0=ALU.mult, op1=ALU.add),
                  r=[pk, "ht", "modt"], w=["h1"])
        norm_parts(h1, "h1", n)
        for k in range(8):
            P.add("dve", lambda e, k=k: e.tensor_tensor(out=tmp[:, k, :n], in0=h1[:, k, :n], in1=rs[:, :n], op=ALU.mult),
                  r=["h1", "rs"], w=[("tmp", k)])
            if moe:
                P.add("act", lambda e, k=k: e.activation(out=uf[:, k, :n], in_=tmp[:, k, :n], func=AF.Identity,
                                                        scale=A2[:, ti, k:k + 1], bias=modt[:, 3, ti, k:k + 1]),
                      r=[("tmp", k), "modc", "modt"], w=[("uf", k)])
                P.add("pool", lambda e, k=k: e.tensor_copy(out=u[:, k, :n], in_=uf[:, k, :n]), r=[("uf", k)], w=["u"])
            else:
                P.add("act", lambda e, k=k: e.activation(out=u[:, k, :n], in_=tmp[:, k, :n], func=AF.Identity,
                                                        scale=A2[:, ti, k:k + 1], bias=modt[:, 3, ti, k:k + 1]),
                      r=[("tmp", k), "modc", "modt"], w=["u"])
        ns = n // 128
        if moe:
            for s in range(ns):
                for k in range(8):
                    P.add("pe", lambda e, k=k, s=s: e.matmul(lgps[:, s, :], uf[:, k, s * 128:(s + 1) * 128], wr[:, k, :], start=(k == 0), stop=(k == 7)),
                          r=[("uf", kk) for kk in range(8)] + ["wr"], w=["ssps"])
            P.add("dve", lambda e: e.tensor_copy(out=lg[:, :ns, :], in_=lgps[:, :ns, :]), r=["ssps"], w=["lg"])
            for s in range(ns):
                P.add("dve", lambda e, s=s: e.reduce_max(out=m12[:, s, 0:1], in_=lg[:, s, :], axis=AX.X), r=["lg"], w=["m12"])
                P.add("dve", lambda e, s=s: e.tensor_scalar(out=mk1[:, s, :], in0=lg[:, s, :], scalar1=m12[:, s, 0:1], scalar2=None, op0=ALU.is_equal),
                      r=["lg", "m12"], w=["mk1"])
                P.add("dve", lambda e, s=s: e.scalar_tensor_tensor(out=l2[:, s, :], in0=mk1[:, s, :], scalar=-1e30, in1=lg[:, s, :], op0=ALU.mult, op1=ALU.add),
                      r=["mk1", "lg"], w=["l2"])
                P.add("dve", lambda e, s=s: e.reduce_max(out=m12[:, s, 1:2], in_=l2[:, s, :], axis=AX.X), r=["l2", "m12"], w=["m12"])
                P.add("dve", lambda e, s=s: e.tensor_scalar(out=mk2[:, s, :], in0=l2[:, s, :], scalar1=m12[:, s, 1:2], scalar2=None, op0=ALU.is_equal),
                      r=["l2", "m12"], w=["mk2"])
                P.add("dve", lambda e, s=s: e.tensor_tensor(out=m12[:, s, 2:3], in0=m12[:, s, 1:2], in1=m12[:, s, 0:1], op=ALU.subtract), r=["m12"], w=["m12"])
                P.add("act", lambda e, s=s: e.activation(out=m12[:, s, 2:3], in_=m12[:, s, 2:3], func=AF.Exp), r=["m12"], w=["m12"])
                P.add("dve", lambda e, s=s: e.tensor_scalar(out=m12[:, s, 3:4], in0=m12[:, s, 2:3], scalar1=1.0, scalar2=None, op0=ALU.add), r=["m12"], w=["m12"])
                P.add("dve", lambda e, s=s: e.reciprocal(out=m12[:, s, 3:4], in_=m12[:, s, 3:4]), r=["m12"], w=["m12"])
                P.add("dve", lambda e, s=s: e.tensor_tensor(out=m12[:, s, 2:3], in0=m12[:, s, 2:3], in1=m12[:, s, 3:4], op=ALU.mult), r=["m12"], w=["m12"])
                P.add("dve", lambda e, s=s: e.tensor_scalar(out=comb[:, s, :], in0=mk1[:, s, :], scalar1=m12[:, s, 3:4], scalar2=None, op0=ALU.mult),
                      r=["mk1", "m12"], w=["comb"])
                P.add("dve", lambda e, s=s: e.scalar_tensor_tensor(out=comb[:, s, :], in0=mk2[:, s, :], scalar=m12[:, s, 2:3], in1=comb[:, s, :],
                                                                  op0=ALU.mult, op1=ALU.add), r=["mk2", "m12", "comb"], w=["comb"])
            for ex in range(8):
                for s in range(ns):
                    rp, rk = rep.next()
                    P.add("dve", lambda e, rp=rp, s=s, ex=ex: e.tensor_scalar(out=rp[:, :], in0=C["ones"][:, :], scalar1=comb[:, s, ex:ex + 1], scalar2=None, op0=ALU.mult),
                          r=["comb", "ones"], w=[rk])
                    P.add("pe", lambda e, rp=rp, s=s: e.matmul(cbps[:, s * 128:(s + 1) * 128], rp[:, :], ident[:, :], start=True, stop=True),
                          r=[rk, "ident"], w=[("misc", 0)])
                P.add("act", lambda e, ex=ex: e.copy(out=cB[:, ex, :n], in_=cbps[:, :n]), r=[("misc", 0)], w=[("cB", ex)])
        nmm = NE * NFG * 4
        cnt = 0
        for ex in range(NE):
            for fg in range(NFG):
                wgt, wgk = wgs.next()
                wut, wuk = wus.next()
                wdt, wdk = wds.next()
                P.dma("pool", wgt[:], wg[ex].rearrange("(k p) f -> p k f", p=128)[:, :, fg * 512:(fg + 1) * 512], w=[wgk])
                P.dma("pool", wut[:], wu[ex].rearrange("(k p) f -> p k f", p=128)[:, :, fg * 512:(fg + 1) * 512], w=[wuk])
                P.dma("pool", wdt[:], wd[ex, fg * 512:(fg + 1) * 512, :].rearrange("(f p) d -> p f d", p=128), w=[wdk])
                hht, hhk = hh.next()
                for f in range(4):
                    gp, gk = gups.next()
                    for k in range(8):
                        P.add("pe", lambda e, k=k, f=f, gp=gp, wgt=wgt: e.matmul(gp[:, 0, :n], wgt[:, k, f * 128:(f + 1) * 128], u[:, k, :n], start=(k == 0), stop=(k == 7)),
                              r=["u", wgk], w=[gk])
                    for k in range(8):
                        P.add("pe", lambda e, k=k, f=f, gp=gp, wut=wut: e.matmul(gp[:, 1, :n], wut[:, k, f * 128:(f + 1) * 128], u[:, k, :n], start=(k == 0), stop=(k == 7)),
                              r=["u", wuk], w=[gk])
                    sgt, sgk = sg.next()
                    P.add("act", lambda e, gp=gp, sgt=sgt: e.activation(out=sgt[:, :n], in_=gp[:, 0, :n], func=AF.Silu), r=[gk], w=[sgk])
                    if moe:
                        tt, tk = t1.next()
                        P.add("dve", lambda e, gp=gp, sgt=sgt, tt=tt: e.tensor_tensor(out=tt[:, :n], in0=sgt[:, :n], in1=gp[:, 1, :n], op=ALU.mult),
                              r=[gk, sgk], w=[tk])
                        P.add("pool", lambda e, tt=tt, hht=hht, f=f, ex=ex: e.tensor_tensor(out=hht[:, f, :n], in0=tt[:, :n], in1=cB[:, ex, :n], op=ALU.mult),
                              r=[tk, ("cB", ex)], w=[(hhk, f)])
                    else:
                        P.add("dve", lambda e, gp=gp, sgt=sgt, hht=hht, f=f: e.tensor_tensor(out=hht[:, f, :n], in0=sgt[:, :n], in1=gp[:, 1, :n], op=ALU.mult),
                              r=[gk, sgk], w=[(hhk, f)])
                for f in range(4):
                    for dm in range(8):
                        P.add("pe", lambda e, f=f, dm=dm, hht=hht, wdt=wdt, cnt=cnt: e.matmul(yps[dm // 2][:, dm % 2, :n], wdt[:, f, dm * 128:(dm + 1) * 128], hht[:, f, :n],
                                                                                             start=(cnt == 0 and dm % 2 == 0), stop=(cnt == nmm - 1), skip_group_check=True),
                              r=[(hhk, f), wdk], w=[("yps", dm // 2)])
                    cnt += 1
        for dm in range(8):
            P.add("dve", lambda e, dm=dm: e.scalar_tensor_tensor(out=h2[:, dm, :n], in0=yps[dm // 2][:, dm % 2, :n], scalar=modt[:, 5, ti, dm:dm + 1],
                                                                in1=h1[:, dm, :n], op0=ALU.mult, op1=ALU.add), r=[("yps", dm // 2), "h1", "modt"], w=["h2"])
        P.dma("sp", ov[:, :, t0:t0 + n], h2[:, :, :n], r=["h2"])
        if final:
            norm_parts(h2, "h2", n)
            for k in range(8):
                P.add("dve", lambda e, k=k: e.tensor_tensor(out=tmp[:, k, :n], in0=h2[:, k, :n], in1=rs[:, :n], op=ALU.mult),
                      r=["h2", "rs"], w=[("tmp", k)])
                P.add("act", lambda e, k=k: e.activation(out=sq[:, k, :n], in_=tmp[:, k, :n], func=AF.Copy, scale=fng[:, k:k + 1]),
                      r=[("tmp", k), "fng"], w=["sq"])
            P.dma("sp", outT.rearrange("(k p) t -> p k t", p=128)[:, :, t0:t0 + n], sq[:, :, :n], r=["sq"])
    for ti, (t0, n) in enumerate(CT):
        tile_body(ti, t0, n)
    print("C ops", P.n_ops())
    return P.finish()


NTOK = 4352
NCH = 34
QT = [(0, 256)] + [(256 + 512 * i, 512) for i in range(8)]


def bconsts(P):
    C = {}
    C["ones"] = P.sb([128, 128], F32, "ones")
    P.add("pool", lambda e: e.memset(C["ones"][:], 1.0), w=["ones"])
    C["epsb"] = P.sb([128, 1], F32, "epsb")
    P.add("pool", lambda e: e.memset(C["epsb"][:], EPS), w=["epsb"])
    return C


def emit_mla(P, C, D, mixT):
    scale = 96 ** -0.5
    qng = P.sb([128, 2], F32, "qng")
    P.dma("sp", qng[:], D["c_qng"][:, :], w=["qng"])
    kvg = P.sb([128, 1], F32, "kvg")
    P.dma("sp", kvg[:], D["c_kvg"][:, :], w=["kvg"])
    wq = P.sb([128, 2, 2, 96], BF16, "wq")
    wqP = P.sb([128, 2, 2, 96], BF16, "wqP")
    P.dma("pool", wq[:].rearrange("p k h c -> p k (h c)"), D["c_wq"].rearrange("(k p) c -> p k c", p=128), w=["wq"])
    P.dma("pool", wqP[:].rearrange("p k h c -> p k (h c)"), D["c_wqP"].rearrange("(k p) c -> p k c", p=128), w=["wqP"])
    wkn = P.sb([128, 2, 96], BF16, "wkn")
    P.dma("pool", wkn[:].rearrange("p h c -> p (h c)"), D["c_wkn"][:, :], w=["wkn"])
    wv = P.sb([128, 128], BF16, "wv")
    P.dma("pool", wv[:], D["c_wv"][:, :], w=["wv"])
    sel = P.sb([32, 96], BF16, "sel")
    P.dma("pool", sel[:], D["c_sel"][:, :], w=["sel"])
    qT = P.sb([96, 2, NTOK], BF16, "mqT")
    kT = P.sb([96, 2, NTOK], BF16, "mkT")
    vaug = P.sb([128, NCH, 2, 65], BF16, "mvaug")
    P.add("pool", lambda e: e.memset(vaug[:], 1.0), w=["mvaug"])
    ckvn = P.sb([128, NTOK], BF16, "ckvn")
    cq = P.sb([128, 2, 512], F32, "m_cq")
    ckv = P.sb([128, 512], F32, "m_ckv")
    sq = P.sb([128, 2, 512], F32, "m_sq")
    rs = P.sb([128, 512], F32, "m_rs")
    rs2 = P.sb([128, 512], F32, "m_rs2")
    cqn = P.sb([128, 2, 512], BF16, "m_cqn")
    ct = P.sb([96, 512], F32, "m_ct")
    st = P.sb([96, 512], F32, "m_st")
    kr = P.sb([32, 512], F32, "m_kr")
    krP = P.sb([32, 512], F32, "m_krP")
    krr = P.sb([32, 512], BF16, "m_krr")
    t1 = P.sb([96, 512], F32, "m_t1")
    t2 = P.sb([96, 512], F32, "m_t2")
    ssps = P.ps([128, 512], F32, "m_ssps")
    pA = P.ps([128, 512], F32, "m_pA")
    pB = P.ps([128, 512], F32, "m_pB")

    ct32 = P.sb([32, 512], F32, "m_ct32")
    st32 = P.sb([32, 512], F32, "m_st32")

    def tile_all(t0, n):
        P.dma("sp", cq[:, :, :n], D["c_cq"].rearrange("(k p) t -> p k t", p=128)[:, :, t0:t0 + n], w=["m_cq"])
        P.dma("sp", ckv[:, :n], D["c_ckv"][:, t0:t0 + n], w=["m_ckv"])
        P.dma("sp", ct[:, :n], D["c_ct96"][:, t0:t0 + n], w=["m_ct"])
        P.dma("sp", st[:, :n], D["c_st96"][:, t0:t0 + n], w=["m_st"])
        P.dma("sp", ct32[:, :n], D["c_ct96"][64:96, t0:t0 + n], w=["m_ct32"])
        P.dma("sp", st32[:, :n], D["c_st96"][64:96, t0:t0 + n], w=["m_st32"])
        P.dma("sp", kr[:, :n], D["c_kr"][:, t0:t0 + n], w=["m_kr"])
        P.dma("sp", krP[:, :n], D["c_krP"][:, t0:t0 + n], w=["m_krP"])
        P.add("act", lambda e: e.activation(out=sq[:, :, :n], in_=cq[:, :, :n], func=AF.Square), r=["m_cq"], w=["m_sq"])
        for k in range(2):
            P.add("pe", lambda e, k=k: e.matmul(ssps[:, :n], C["ones"][:, :], sq[:, k, :n], start=(k == 0), stop=(k == 1)), r=["m_sq", "ones"], w=["m_ssps"])
        P.add("act", lambda e: e.activation(out=rs[:, :n], in_=ssps[:, :n], func=AF.Sqrt, scale=1.0 / 256, bias=C["epsb"][:, 0:1]), r=["m_ssps", "epsb"], w=["m_rs"])
        P.add("dve", lambda e: e.reciprocal(out=rs[:, :n], in_=rs[:, :n]), r=["m_rs"], w=["m_rs"])
        for k in range(2):
            P.add("dve", lambda e, k=k: e.scalar_tensor_tensor(out=cqn[:, k, :n], in0=cq[:, k, :n], scalar=qng[:, k:k + 1], in1=rs[:, :n], op0=ALU.mult, op1=ALU.mult),
                  r=["m_cq", "qng", "m_rs"], w=["m_cqn"])
        P.add("act", lambda e: e.activation(out=sq[:, 0, :n], in_=ckv[:, :n], func=AF.Square), r=["m_ckv"], w=["m_sq"])
        P.add("pe", lambda e: e.matmul(ssps[:, :n], C["ones"][:, :], sq[:, 0, :n], start=True, stop=True), r=["m_sq", "ones"], w=["m_ssps"])
        P.add("act", lambda e: e.activation(out=rs2[:, :n], in_=ssps[:, :n], func=AF.Sqrt, scale=1.0 / 128, bias=C["epsb"][:, 0:1]), r=["m_ssps", "epsb"], w=["m_rs2"])
        P.add("dve", lambda e: e.reciprocal(out=rs2[:, :n], in_=rs2[:, :n]), r=["m_rs2"], w=["m_rs2"])
        P.add("dve", lambda e: e.scalar_tensor_tensor(out=ckvn[:, t0:t0 + n], in0=ckv[:, :n], scalar=kvg[:, 0:1], in1=rs2[:, :n], op0=ALU.mult, op1=ALU.mult),
              r=["m_ckv", "kvg", "m_rs2"], w=["ckvn"])
        P.add("pool", lambda e: e.tensor_tensor(out=kr[:, :n], in0=kr[:, :n], in1=ct32[:, :n], op=ALU.mult), r=["m_kr", "m_ct32"], w=["m_kr"])
        P.add("pool", lambda e: e.tensor_tensor(out=krP[:, :n], in0=krP[:, :n], in1=st32[:, :n], op=ALU.mult), r=["m_krP", "m_st32"], w=["m_krP"])
        P.add("pool", lambda e: e.tensor_tensor(out=krr[:, :n], in0=kr[:, :n], in1=krP[:, :n], op=ALU.add), r=["m_kr", "m_krP"], w=["m_krr"])
        for h in range(2):
            for k in range(2):
                P.add("pe", lambda e, k=k, h=h: e.matmul(pA[:96, :n], wq[:, k, h, :], cqn[:, k, :n], start=(k == 0), stop=(k == 1)), r=["wq", "m_cqn"], w=["m_pA"])
            for k in range(2):
                P.add("pe", lambda e, k=k, h=h: e.matmul(pB[:96, :n], wqP[:, k, h, :], cqn[:, k, :n], start=(k == 0), stop=(k == 1)), r=["wqP", "m_cqn"], w=["m_pB"])
            P.add("dve", lambda e: e.tensor_tensor(out=t1[:, :n], in0=pA[:96, :n], in1=ct[:, :n], op=ALU.mult), r=["m_pA", "m_ct"], w=["m_t1"])
            P.add("dve", lambda e: e.tensor_tensor(out=t2[:, :n], in0=pB[:96, :n], in1=st[:, :n], op=ALU.mult), r=["m_pB", "m_st"], w=["m_t2"])
            P.add("pool", lambda e, h=h: e.tensor_tensor(out=qT[:, h, t0:t0 + n], in0=t1[:, :n], in1=t2[:, :n], op=ALU.add), r=["m_t1", "m_t2"], w=["mqT"])
            P.add("pe", lambda e, h=h: e.matmul(pA[:96, :n], wkn[:, h, :], ckvn[:, t0:t0 + n], start=True, stop=False), r=["wkn", "ckvn"], w=["m_pA"])
            P.add("pe", lambda e, h=h: e.matmul(pA[:96, :n], sel[:, :], krr[:, :n], start=False, stop=True), r=["sel", "m_krr"], w=["m_pA"])
            P.add("act", lambda e, h=h: e.copy(out=kT[:, h, t0:t0 + n], in_=pA[:96, :n]), r=["m_pA"], w=["mkT"])
        for s in range(n // 128):
            c = (t0 + s * 128) // 128
            P.add("pe", lambda e, s=s: e.matmul(pB[:, 0:128], ckvn[:, t0 + s * 128:t0 + (s + 1) * 128], wv[:, :], start=True, stop=True), r=["ckvn", "wv"], w=["m_pB"])
            P.add("act", lambda e, c=c: e.copy(out=vaug[:, c, :, 0:64], in_=pB[:, 0:128].rearrange("p (h d) -> p h d", h=2)), r=["m_pB"], w=["mvaug"])

    for (t0, n) in QT:
        tile_all(t0, n)

    sps = Rot([P.ps([128, 512], F32, f"m_sps{i}") for i in range(3)], "m_sps")
    ops_ = Rot([P.ps([128, 512], F32, f"m_ops{i}") for i in range(2)], "m_ops")
    E = Rot([P.sb([128, 512], BF16, f"m_E{i}") for i in range(3)], "m_E")
    oa = Rot([P.sb([65, 512], F32, f"m_oa{i}") for i in range(2)], "m_oa")
    rc = Rot([P.sb([64, 512], F32, f"m_rc{i}") for i in range(2)], "m_rc")
    oo = Rot([P.sb([64, 512], F32, f"m_oo{i}") for i in range(2)], "m_oo")

    def attn_tile(h, t0, n, chunks):
        op_, ok = ops_.next()
        nchk = len(chunks)
        pend = []

        def issue_s(c):
            sp, sk = sps.next()
            P.add("pe", lambda e, sp=sp, c=c: e.matmul(sp[:, :n], kT[:, h, c * 128:(c + 1) * 128], qT[:, h, t0:t0 + n], start=True, stop=True), r=["mkT", "mqT"], w=[sk])
            pend.append((sp, sk))
        issue_s(chunks[0])
        for ci, c in enumerate(chunks):
            if ci + 1 < nchk:
                issue_s(chunks[ci + 1])
            sp, sk = pend.pop(0)
            Et, ek = E.next()
            P.add("act", lambda e, sp=sp, Et=Et: e.activation(out=Et[:, :n], in_=sp[:, :n], func=AF.Exp, scale=scale), r=[sk], w=[ek])
            P.add("pe", lambda e, Et=Et, c=c, ci=ci: e.matmul(op_[:65, :n], vaug[:, c, h, :], Et[:, :n], start=(ci == 0), stop=(ci == nchk - 1)), r=[ek, "mvaug"], w=[ok])
        oat, oak = oa.next()
        P.add("act", lambda e: e.copy(out=oat[:, :n], in_=op_[:65, :n]), r=[ok], w=[oak])
        sp, sk = sps.next()
        P.add("pe", lambda e: e.matmul(sp[:64, :n], C["ones"][64:65, 0:64], oat[64:65, :n], start=True, stop=True), r=[oak, "ones"], w=[sk])
        rct, rck = rc.next()
        P.add("dve", lambda e: e.reciprocal(out=rct[:, :n], in_=sp[:64, :n]), r=[sk], w=[rck])
        oot, ook = oo.next()
        P.add("dve", lambda e: e.tensor_tensor(out=oot[:, :n], in0=oat[0:64, :n], in1=rct[:, :n], op=ALU.mult), r=[oak, rck], w=[ook])
        P.dma("sp", mixT[h * 64:(h + 1) * 64, t0:t0 + n], oot[:, :n], r=[ook])

    for h in range(2):
        attn_tile(h, 0, 256, [0, 1])
        for i in range(8):
            attn_tile(h, 256 + 512 * i, 512, list(range(NCH)))


MLA_IN = [("c_cq", [256, NTOK]), ("c_ckv", [128, NTOK]), ("c_kr", [32, NTOK]), ("c_krP", [32, NTOK]), ("c_ct96", [96, NTOK]), ("c_st96", [96, NTOK]),
          ("c_qng", [128, 2]), ("c_kvg", [128, 1]), ("c_wq", [256, 192]), ("c_wqP", [256, 192]), ("c_wkn", [128, 192]), ("c_wv", [128, 128]), ("c_sel", [32, 96])]


def build_mla():
    P = Prog()
    D = {nm: P.dram_in(nm, shp) for nm, shp in MLA_IN}
    out = P.dram_out("mix", [128, NTOK])
    C = bconsts(P)
    emit_mla(P, C, D, out)
    print("mla ops", P.n_ops())
    return P.finish()


SWA_IN = [("a_q", [128, NTOK]), ("a_qP", [128, NTOK]), ("a_k", [64, NTOK]), ("a_kP", [64, NTOK]), ("a_vtok", [NTOK, 64]),
          ("a_cos", [64, NTOK]), ("a_sin", [64, NTOK]), ("a_sink", [128, 2]), ("a_maskP", [128, 128]), ("a_maskN", [128, 128])]


def build_swa():
    P = Prog()
    D = {nm: P.dram_in(nm, shp) for nm, shp in SWA_IN}
    out = P.dram_out("mix", [128, NTOK])
    C = bconsts(P)
    scale = 64 ** -0.5
    aqT = P.sb([64, 2, NTOK], BF16, "aqT")
    akT = P.sb([64, NTOK], BF16, "akT")
    vaug = P.sb([128, NCH, 65], BF16, "avaug")
    P.add("pool", lambda e: e.memset(vaug[:], 1.0), w=["avaug"])
    P.dma("pool", vaug[:, :, 0:64], D["a_vtok"].rearrange("(c p) d -> p c d", p=128), w=["avaug"])
    es = P.sb([128, 2], F32, "a_es")
    P.dma("sp", es[:], D["a_sink"][:, :], w=["a_es"])
    P.add("act", lambda e: e.activation(out=es[:], in_=es[:], func=AF.Exp), r=["a_es"], w=["a_es"])
    mP = P.sb([128, 128], BF16, "a_mP")
    mN = P.sb([128, 128], BF16, "a_mN")
    P.dma("pool", mP[:], D["a_maskP"][:, :], w=["a_mP"])
    P.dma("pool", mN[:], D["a_maskN"][:, :], w=["a_mN"])
    q = P.sb([64, 2, 512], F32, "a_q")
    qP = P.sb([64, 2, 512], F32, "a_qP")
    k = P.sb([64, 512], F32, "a_k")
    kP = P.sb([64, 512], F32, "a_kP")
    cs = P.sb([64, 512], F32, "a_cs")
    sn = P.sb([64, 512], F32, "a_sn")
    t1 = P.sb([64, 512], F32, "a_t1")
    t2 = P.sb([64, 512], F32, "a_t2")

    def rope_tile(t0, n):
        P.dma("sp", q[:, :, :n], D["a_q"].rearrange("(h d) t -> d h t", d=64)[:, :, t0:t0 + n], w=["a_q"])
        P.dma("sp", qP[:, :, :n], D["a_qP"].rearrange("(h d) t -> d h t", d=64)[:, :, t0:t0 + n], w=["a_qP"])
        P.dma("sp", k[:, :n], D["a_k"][:, t0:t0 + n], w=["a_k"])
        P.dma("sp", kP[:, :n], D["a_kP"][:, t0:t0 + n], w=["a_kP"])
        P.dma("sp", cs[:, :n], D["a_cos"][:, t0:t0 + n], w=["a_cs"])
        P.dma("sp", sn[:, :n], D["a_sin"][:, t0:t0 + n], w=["a_sn"])
        for h in range(2):
            P.add("dve", lambda e, h=h: e.tensor_tensor(out=t1[:, :n], in0=q[:, h, :n], in1=cs[:, :n], op=ALU.mult), r=["a_q", "a_cs"], w=["a_t1"])
            P.add("pool", lambda e, h=h: e.tensor_tensor(out=t2[:, :n], in0=qP[:, h, :n], in1=sn[:, :n], op=ALU.mult), r=["a_qP", "a_sn"], w=["a_t2"])
            P.add("dve", lambda e, h=h: e.tensor_tensor(out=aqT[:, h, t0:t0 + n], in0=t1[:, :n], in1=t2[:, :n], op=ALU.add), r=["a_t1", "a_t2"], w=["aqT"])
        P.add("dve", lambda e: e.tensor_tensor(out=t1[:, :n], in0=k[:, :n], in1=cs[:, :n], op=ALU.mult), r=["a_k", "a_cs"], w=["a_t1"])
        P.add("pool", lambda e: e.tensor_tensor(out=t2[:, :n], in0=kP[:, :n], in1=sn[:, :n], op=ALU.mult), r=["a_kP", "a_sn"], w=["a_t2"])
        P.add("dve", lambda e: e.tensor_tensor(out=akT[:, t0:t0 + n], in0=t1[:, :n], in1=t2[:, :n], op=ALU.add), r=["a_t1", "a_t2"], w=["akT"])

    for (t0, n) in QT:
        rope_tile(t0, n)

    sp = [P.ps([128, 512], F32, f"a_sp{i}") for i in range(3)]
    ops_ = Rot([P.ps([128, 512], F32, f"a_op{i}") for i in range(2)], "a_op")
    bc = P.ps([128, 512], F32, "a_bc")
    E = Rot([P.sb([128, 5 * 256], BF16, f"a_E{i}") for i in range(2)], "a_E")
    oa = Rot([P.sb([65, 256], F32, f"a_oa{i}") for i in range(2)], "a_oa")
    rc = Rot([P.sb([64, 256], F32, f"a_rc{i}") for i in range(2)], "a_rc")
    oo = Rot([P.sb([64, 256], F32, f"a_oo{i}") for i in range(2)], "a_oo")
    outv = out.rearrange("(h d) t -> d h t", d=64)

    def block(q0, chunks):
        nck = len(chunks)
        for ci, (c, mk) in enumerate(chunks):
            b, off = ci // 2, (ci % 2) * 256
            P.add("pe", lambda e, b=b, off=off, c=c: e.matmul(sp[b][:, off:off + 256].rearrange("p (h q) -> p h q", h=2), akT[:, c * 128:(c + 1) * 128],
                                                             aqT[:, :, q0:q0 + 128], start=True, stop=True), r=["akT", "aqT"], w=[("a_sp", b)])
        Et, ek = E.next()
        for b in range((nck + 1) // 2):
            w_ = min(512, nck * 256 - b * 512)
            P.add("act", lambda e, b=b, w_=w_: e.activation(out=Et[:, b * 512:b * 512 + w_], in_=sp[b][:, :w_], func=AF.Exp, scale=scale), r=[("a_sp", b)], w=[ek])
        for ci, (c, mk) in enumerate(chunks):
            if mk is None:
                continue
            m = mP if mk == "P" else mN
            for h in range(2):
                o_ = ci * 256 + h * 128
                P.add("pool", lambda e, o_=o_, m=m: e.tensor_tensor(out=Et[:, o_:o_ + 128], in0=Et[:, o_:o_ + 128], in1=m[:, :], op=ALU.mult), r=[ek, "a_mP", "a_mN"], w=[ek])
        op_, ok = ops_.next()
        for ci, (c, mk) in enumerate(chunks):
            P.add("pe", lambda e, ci=ci, c=c: e.matmul(op_[:65, :256], vaug[:, c, :], Et[:, ci * 256:(ci + 1) * 256], start=(ci == 0), stop=(ci == nck - 1)), r=[ek, "avaug"], w=[ok])
        oat, oak = oa.next()
        P.add("act", lambda e: e.copy(out=oat[:, :], in_=op_[:65, :256]), r=[ok], w=[oak])
        for h in range(2):
            P.add("dve", lambda e, h=h: e.tensor_scalar(out=oat[64:65, h * 128:(h + 1) * 128], in0=oat[64:65, h * 128:(h + 1) * 128], scalar1=es[64:65, h:h + 1], scalar2=None, op0=ALU.add),
                  r=[oak, "a_es"], w=[oak])
        P.add("pe", lambda e: e.matmul(bc[:64, :256], C["ones"][64:65, 0:64], oat[64:65, :], start=True, stop=True), r=[oak, "ones"], w=["a_bc"])
        rct, rck = rc.next()
        P.add("dve", lambda e: e.reciprocal(out=rct[:, :], in_=bc[:64, :256]), r=["a_bc"], w=[rck])
        oot, ook = oo.next()
        P.add("dve", lambda e: e.tensor_tensor(out=oot[:, :], in0=oat[0:64, :], in1=rct[:, :], op=ALU.mult), r=[oak, rck], w=[ook])
        P.dma("sp", outv[:, :, q0:q0 + 128], oot[:, :].rearrange("d (h q) -> d h q", h=2), r=[ook])

    block(0, [(0, None), (1, None)])
    block(128, [(0, None), (1, None)])
    for nb in range(32):
        ch = [(0, None), (1, None)]
        if nb > 0:
            ch.append((nb + 1, "P"))
        ch.append((nb + 2, None))
        if nb < 31:
            ch.append((nb + 3, "N"))
        block(256 + 128 * nb, ch)
    print("swa ops", P.n_ops())
    return P.finish()


RET_IN = [("d_q", [128, NTOK]), ("d_qP", [128, NTOK]), ("d_k", [128, NTOK]), ("d_kP", [128, NTOK]), ("d_gate", [128, NTOK]),
          ("d_vtok", [NTOK, 128]), ("d_ktok", [NTOK, 128]), ("d_kPtok", [NTOK, 128]), ("d_cos", [64, NTOK]), ("d_sin", [64, NTOK]),
          ("d_costok", [NTOK, 64]), ("d_sintok", [NTOK, 64]), ("d_ldp", [128, 2]), ("d_ldr", [128, 4]), ("d_g", [128, 1]),
          ("d_relu", [128, 128]), ("d_rell", [128, 128]), ("d_um", [128, 128]), ("d_lm", [128, 128]), ("d_pos1", [128, 128]), ("d_posr", [128, 128]),
          ("d_pk", [128, 2]), ("d_bd", [128, 128]), ("d_bd64", [128, 128])]


def build_ret():
    P = Prog()
    D = {nm: P.dram_in(nm, shp) for nm, shp in RET_IN}
    out = P.dram_out("mix", [128, NTOK])
    C = bconsts(P)

    def ld(nm, shp, dt=F32, q="sp"):
        t = P.sb(shp, dt, "r_" + nm)
        P.dma(q, t[:], D[nm][:, :], w=["r_" + nm])
        return t
    ldp = ld("d_ldp", [128, 2]); ldr = ld("d_ldr", [128, 4]); g = ld("d_g", [128, 1])
    relu = ld("d_relu", [128, 128]); rell = ld("d_rell", [128, 128]); um = ld("d_um", [128, 128]); lm = ld("d_lm", [128, 128])
    pos1 = ld("d_pos1", [128, 128]); posr = ld("d_posr", [128, 128]); pk = ld("d_pk", [128, 2]); bd = ld("d_bd", [128, 128]); bd64 = ld("d_bd64", [128, 128])
    P.add("act", lambda e: e.activation(out=ldp[:], in_=ldp[:], func=AF.Exp), r=["r_d_ldp"], w=["r_d_ldp"])
    P.add("dve", lambda e: e.tensor_scalar(out=ldp[:], in0=ldp[:], scalar1=-1.0, scalar2=None, op0=ALU.mult), r=["r_d_ldp"], w=["r_d_ldp"])
    P.add("act", lambda e: e.activation(out=ldr[:], in_=ldr[:], func=AF.Exp), r=["r_d_ldr"], w=["r_d_ldr"])
    P.add("dve", lambda e: e.tensor_scalar(out=ldr[:], in0=ldr[:], scalar1=-1.0, scalar2=None, op0=ALU.mult), r=["r_d_ldr"], w=["r_d_ldr"])
    Qd = P.sb([128, 2, 128], F32, "r_Qd")
    for d, pt in ((0, pos1), (1, posr)):
        P.add("act", lambda e, d=d, pt=pt: e.activation(out=Qd[:, d, :], in_=pt[:, :], func=AF.Exp, scale=ldp[:, d:d + 1]), r=["r_d_ldp", "r_d_pos1", "r_d_posr"], w=["r_Qd"])
    P.add("dve", lambda e: e.tensor_scalar(out=Qd[:], in0=Qd[:], scalar1=0.125, scalar2=None, op0=ALU.mult), r=["r_Qd"], w=["r_Qd"])
    c128 = P.sb([128, 1], F32, "r_c128")
    P.add("pool", lambda e: e.memset(c128[:], 128.0), w=["r_c128"])
    cd = P.sb([128, 2], F32, "r_cd")
    for d in range(2):
        P.add("act", lambda e, d=d: e.activation(out=cd[:, d:d + 1], in_=c128[:, :], func=AF.Exp, scale=ldp[:, d:d + 1]), r=["r_d_ldp", "r_c128"], w=["r_cd"])
    Kd = P.sb([128, 2, 2], F32, "r_Kd")
    for d in range(2):
        P.add("act", lambda e, d=d: e.activation(out=Kd[:, d, :], in_=ldr[:, 2 * d:2 * d + 2], func=AF.Exp, scale=pk[:, d:d + 1]), r=["r_d_ldr", "r_d_pk"], w=["r_Kd"])
    DT = P.sb([128, 2, 128], F32, "r_DT")
    dt2 = P.sb([128, 128], F32, "r_dt2")
    for h in range(2):
        P.add("act", lambda e, h=h: e.activation(out=DT[:, h, :], in_=relu[:, :], func=AF.Exp, scale=ldr[:, h:h + 1]), r=["r_d_ldr", "r_d_relu"], w=["r_DT"])
        P.add("dve", lambda e, h=h: e.tensor_tensor(out=DT[:, h, :], in0=DT[:, h, :], in1=um[:, :], op=ALU.mult), r=["r_DT", "r_d_um"], w=["r_DT"])
        P.add("act", lambda e, h=h: e.activation(out=dt2[:, :], in_=rell[:, :], func=AF.Exp, scale=ldr[:, 2 + h:3 + h]), r=["r_d_ldr", "r_d_rell"], w=["r_dt2"])
        P.add("dve", lambda e, h=h: e.tensor_tensor(out=dt2[:, :], in0=dt2[:, :], in1=lm[:, :], op=ALU.mult), r=["r_dt2", "r_d_lm"], w=["r_dt2"])
        P.add("dve", lambda e, h=h: e.tensor_tensor(out=DT[:, h, :], in0=DT[:, h, :], in1=dt2[:, :], op=ALU.add), r=["r_DT", "r_dt2"], w=["r_DT"])
    P.add("dve", lambda e: e.tensor_scalar(out=DT[:], in0=DT[:], scalar1=0.125, scalar2=None, op0=ALU.mult), r=["r_DT"], w=["r_DT"])

    qdf = P.sb([128, NCH, 128], BF16, "r_qdf"); qdr = P.sb([128, NCH, 128], BF16, "r_qdr")
    qTb = P.sb([128, 2, NTOK], BF16, "r_qTb"); kTb = P.sb([128, NTOK], BF16, "r_kTb")
    P.add("pool", lambda e: e.memset(qTb[:], 0.0), w=["r_qTb"])
    kdf = P.sb([128, NCH, 128], BF16, "r_kdf"); kdr = P.sb([128, NCH, 128], BF16, "r_kdr")
    vt = P.sb([128, NCH, 128], BF16, "r_vt")
    vpad = P.sb([128, NCH, 2, 128], BF16, "r_vpad")
    P.add("pool", lambda e: e.memset(vpad[:], 0.0), w=["r_vpad"])
    vv = D["d_vtok"].rearrange("(c p) d -> p c d", p=128)
    P.dma("pool", vt[:], vv, w=["r_vt"])
    P.dma("pool", vpad[:, :, 0, 0:64], vv[:, :, 0:64], w=["r_vpad"])
    P.dma("pool", vpad[:, :, 1, 64:128], vv[:, :, 64:128], w=["r_vpad"])
    q = P.sb([128, 512], F32, "r_q"); qP = P.sb([128, 512], F32, "r_qP"); k = P.sb([128, 512], F32, "r_k"); kP = P.sb([128, 512], F32, "r_kP")
    cs = P.sb([128, 512], F32, "r_cs"); sn = P.sb([128, 512], F32, "r_sn")
    t1 = P.sb([128, 512], F32, "r_t1"); t2 = P.sb([128, 512], F32, "r_t2"); qr = P.sb([128, 512], F32, "r_qr")
    kt = P.sb([128, 4, 128], F32, "r_kt"); kPt = P.sb([128, 4, 128], F32, "r_kPt"); ct = P.sb([128, 4, 64], F32, "r_ct"); st = P.sb([128, 4, 64], F32, "r_st")
    krt = P.sb([128, 4, 128], F32, "r_krt"); kt2 = P.sb([128, 4, 128], F32, "r_kt2")

    def prep(t0, n):
        ns = n // 128
        c0 = t0 // 128
        for nm, t in (("d_q", q), ("d_qP", qP), ("d_k", k), ("d_kP", kP)):
            P.dma("sp", t[:, :n], D[nm][:, t0:t0 + n], w=[t.name])
        for hh in range(2):
            P.dma("sp", cs[hh * 64:(hh + 1) * 64, :n], D["d_cos"][:, t0:t0 + n], w=[cs.name])
            P.dma("sp", sn[hh * 64:(hh + 1) * 64, :n], D["d_sin"][:, t0:t0 + n], w=[sn.name])
        P.add("dve", lambda e: e.tensor_tensor(out=t1[:, :n], in0=q[:, :n], in1=cs[:, :n], op=ALU.mult), r=[q.name, cs.name], w=["r_t1"])
        P.add("pool", lambda e: e.tensor_tensor(out=t2[:, :n], in0=qP[:, :n], in1=sn[:, :n], op=ALU.mult), r=[qP.name, sn.name], w=["r_t2"])
        P.add("dve", lambda e: e.tensor_tensor(out=qr[:, :n], in0=t1[:, :n], in1=t2[:, :n], op=ALU.add), r=["r_t1", "r_t2"], w=["r_qr"])
        P.add("act", lambda e: e.copy(out=qTb[0:64, 0, t0:t0 + n], in_=qr[0:64, :n]), r=["r_qr"], w=["r_qTb"])
        P.add("act", lambda e: e.copy(out=qTb[64:128, 1, t0:t0 + n], in_=qr[64:128, :n]), r=["r_qr"], w=["r_qTb"])
        for s in range(ns):
            P.add("dve", lambda e, s=s: e.tensor_tensor(out=qdf[:, c0 + s, :], in0=qr[:, s * 128:(s + 1) * 128], in1=Qd[:, 0, :], op=ALU.mult), r=["r_qr", "r_Qd"], w=["r_qdf"])
            P.add("pool", lambda e, s=s: e.tensor_tensor(out=qdr[:, c0 + s, :], in0=qr[:, s * 128:(s + 1) * 128], in1=Qd[:, 1, :], op=ALU.mult), r=["r_qr", "r_Qd"], w=["r_qdr"])
        P.add("dve", lambda e: e.tensor_tensor(out=t1[:, :n], in0=k[:, :n], in1=cs[:, :n], op=ALU.mult), r=[k.name, cs.name], w=["r_t1"])
        P.add("pool", lambda e: e.tensor_tensor(out=t2[:, :n], in0=kP[:, :n], in1=sn[:, :n], op=ALU.mult), r=[kP.name, sn.name], w=["r_t2"])
        P.add("dve", lambda e: e.tensor_tensor(out=kTb[:, t0:t0 + n], in0=t1[:, :n], in1=t2[:, :n], op=ALU.add), r=["r_t1", "r_t2"], w=["r_kTb"])
        P.dma("sp", kt[:, :ns, :], D["d_ktok"].rearrange("(c p) d -> p c d", p=128)[:, c0:c0 + ns, :], w=["r_kt"])
        P.dma("sp", kPt[:, :ns, :], D["d_kPtok"].rearrange("(c p) d -> p c d", p=128)[:, c0:c0 + ns, :], w=["r_kPt"])
        P.dma("sp", ct[:, :ns, :], D["d_costok"].rearrange("(c p) d -> p c d", p=128)[:, c0:c0 + ns, :], w=["r_ct"])
        P.dma("sp", st[:, :ns, :], D["d_sintok"].rearrange("(c p) d -> p c d", p=128)[:, c0:c0 + ns, :], w=["r_st"])
        for h in range(2):
            hs = slice(h * 64, (h + 1) * 64)
            P.add("dve", lambda e, hs=hs: e.tensor_tensor(out=krt[:, :ns, hs], in0=kt[:, :ns, hs], in1=ct[:, :ns, :], op=ALU.mult), r=["r_kt", "r_ct"], w=["r_krt"])
            P.add("pool", lambda e, hs=hs: e.tensor_tensor(out=kt2[:, :ns, hs], in0=kPt[:, :ns, hs], in1=st[:, :ns, :], op=ALU.mult), r=["r_kPt", "r_st"], w=["r_kt2"])
        P.add("dve", lambda e: e.tensor_tensor(out=krt[:, :ns, :], in0=krt[:, :ns, :], in1=kt2[:, :ns, :], op=ALU.add), r=["r_krt", "r_kt2"], w=["r_krt"])
        for h in range(2):
            hs = slice(h * 64, (h + 1) * 64)
            P.add("dve", lambda e, hs=hs, h=h: e.tensor_scalar(out=kdf[:, c0:c0 + ns, hs], in0=krt[:, :ns, hs], scalar1=Kd[:, 0, h:h + 1], scalar2=None, op0=ALU.mult), r=["r_krt", "r_Kd"], w=["r_kdf"])
            P.add("pool", lambda e, hs=hs, h=h: e.tensor_scalar(out=kdr[:, c0:c0 + ns, hs], in0=krt[:, :ns, hs], scalar1=Kd[:, 1, h:h + 1], scalar2=None, op0=ALU.mult), r=["r_krt", "r_Kd"], w=["r_kdr"])

    RS = 3
    for (t0, n) in QT:
        prep(t0, n)
    if RS < 2:
        return P.finish()

    Sf = P.sb([128, NCH, 128], BF16, "r_Sf"); Sr = P.sb([128, NCH, 128], BF16, "r_Sr")
    S = P.sb([128, 128], F32, "r_S")
    gp = Rot([P.ps([128, 512], F32, f"r_gp{i}") for i in range(2)], "r_gp")
    tg = Rot([P.sb([128, 128], F32, f"r_tg{i}") for i in range(2)], "r_tg")

    def scan(order, kd, kdkey, Sall, skey, d):
        P.add("pool", lambda e: e.memset(S[:], 0.0), r=[], w=["r_S"])
        P.add("pool", lambda e: e.memset(Sall[:, order[0], :], 0.0), w=[skey])
        for idx in range(len(order) - 1):
            c = order[idx]
            g_, gk = gp.next()
            P.add("pe", lambda e, c=c, g_=g_: e.matmul(g_[:, 0:128], kd[:, c, :], vt[:, c, :], start=True, stop=True), r=[kdkey, "r_vt"], w=[gk])
            tg_, tk = tg.next()
            P.add("dve", lambda e, g_=g_, tg_=tg_: e.tensor_tensor(out=tg_[:, :], in0=g_[:, 0:128], in1=bd[:, :], op=ALU.mult), r=[gk, "r_d_bd"], w=[tk])
            P.add("dve", lambda e, tg_=tg_: e.scalar_tensor_tensor(out=S[:, :], in0=S[:, :], scalar=cd[:, d:d + 1], in1=tg_[:, :], op0=ALU.mult, op1=ALU.add), r=[tk, "r_S", "r_cd"], w=["r_S"])
            P.add("act", lambda e, nx=order[idx + 1]: e.copy(out=Sall[:, nx, :], in_=S[:, :]), r=["r_S"], w=[skey])

    scan(list(range(NCH)), kdf, "r_kdf", Sf, "r_Sf", 0)
    scan([1, 0] + list(range(NCH - 1, 1, -1)), kdr, "r_kdr", Sr, "r_Sr", 1)

    if RS < 3:
        return P.finish()
    bp = Rot([P.ps([128, 512], F32, f"r_bp{i}") for i in range(2)], "r_bp")
    op_ = Rot([P.ps([128, 512], F32, f"r_op{i}") for i in range(2)], "r_op")
    mv = P.ps([128, 512], F32, "r_mv")
    AT = Rot([P.sb([128, 2, 128], BF16, f"r_AT{i}") for i in range(2)], "r_AT")
    osb = P.sb([128, 512], F32, "r_osb"); dd = P.sb([128, 512], F32, "r_dd"); sq = P.sb([128, 512], F32, "r_sq"); rs = P.sb([128, 512], F32, "r_rs")
    gt = P.sb([128, 512], F32, "r_gt"); yo = P.sb([128, 512], F32, "r_yo")

    def out_tile(t0, n):
        ns = n // 128
        c0 = t0 // 128
        o_, ok = op_.next()
        for s in range(ns):
            c = c0 + s
            b_, bk = bp.next()
            for h in range(2):
                hs = slice(h * 64, (h + 1) * 64)
                P.add("pe", lambda e, h=h, hs=hs, c=c, b_=b_: e.matmul(b_[:, h * 128:(h + 1) * 128], kTb[:, c * 128:(c + 1) * 128], qTb[:, h, c * 128:(c + 1) * 128], start=True, stop=True),
                      r=["r_kTb", "r_qTb"], w=[bk])
            at, ak = AT.next()
            P.add("dve", lambda e, b_=b_, at=at: e.tensor_tensor(out=at[:, :, :], in0=b_[:, 0:256].rearrange("p (h q) -> p h q", h=2), in1=DT[:, :, :], op=ALU.mult), r=[bk, "r_DT"], w=[ak])
            reg = o_[:, s * 128:(s + 1) * 128]
            P.add("pe", lambda e, reg=reg, c=c: e.matmul(reg, Sf[:, c, :], qdf[:, c, :], start=True, stop=False), r=["r_Sf", "r_qdf"], w=[ok])
            P.add("pe", lambda e, reg=reg, c=c: e.matmul(reg, Sr[:, c, :], qdr[:, c, :], start=False, stop=False), r=["r_Sr", "r_qdr"], w=[ok])
            P.add("pe", lambda e, reg=reg, c=c, at=at: e.matmul(reg, vpad[:, c, 0, :], at[:, 0, :], start=False, stop=False), r=["r_vpad", ak], w=[ok])
            P.add("pe", lambda e, reg=reg, c=c, at=at: e.matmul(reg, vpad[:, c, 1, :], at[:, 1, :], start=False, stop=True), r=["r_vpad", ak], w=[ok])
        P.dma("sp", gt[:, :n], D["d_gate"][:, t0:t0 + n], w=["r_gt"])
        P.add("act", lambda e: e.copy(out=osb[:, :n], in_=o_[:, :n]), r=[ok], w=["r_osb"])
        P.add("pe", lambda e: e.matmul(mv[:, :n], bd64[:, :], osb[:, :n], start=True, stop=True), r=["r_d_bd64", "r_osb"], w=["r_mv"])
        P.add("dve", lambda e: e.tensor_tensor(out=dd[:, :n], in0=osb[:, :n], in1=mv[:, :n], op=ALU.subtract), r=["r_osb", "r_mv"], w=["r_dd"])
        P.add("act", lambda e: e.activation(out=sq[:, :n], in_=dd[:, :n], func=AF.Square), r=["r_dd"], w=["r_sq"])
        P.add("pe", lambda e: e.matmul(mv[:, :n], bd64[:, :], sq[:, :n], start=True, stop=True), r=["r_d_bd64", "r_sq"], w=["r_mv"])
        P.add("act", lambda e: e.activation(out=rs[:, :n], in_=mv[:, :n], func=AF.Sqrt, bias=C["epsb"][:, 0:1]), r=["r_mv", "epsb"], w=["r_rs"])
        P.add("dve", lambda e: e.reciprocal(out=rs[:, :n], in_=rs[:, :n]), r=["r_rs"], w=["r_rs"])
        P.add("dve", lambda e: e.tensor_tensor(out=dd[:, :n], in0=dd[:, :n], in1=rs[:, :n], op=ALU.mult), r=["r_dd", "r_rs"], w=["r_dd"])
        P.add("act", lambda e: e.activation(out=gt[:, :n], in_=gt[:, :n], func=AF.Silu), r=["r_gt"], w=["r_gt"])
        P.add("dve", lambda e: e.scalar_tensor_tensor(out=yo[:, :n], in0=dd[:, :n], scalar=g[:, 0:1], in1=gt[:, :n], op0=ALU.mult, op1=ALU.mult), r=["r_dd", "r_gt", "r_d_g"], w=["r_yo"])
        P.dma("sp", out[:, t0:t0 + n], yo[:, :n], r=["r_yo"])

    for (t0, n) in QT:
        out_tile(t0, n)
    print("ret ops", P.n_ops())
    return P.finish()


GDN_IN = [("b_q", [128, NTOK]), ("b_k", [128, NTOK]), ("b_v", [128, NTOK]), ("b_gate", [128, NTOK]), ("b_ab", [NTOK, 8]),
          ("b_cw", [64, 30]), ("b_dtb", [128, NCH * 8]), ("b_alog", [128, NCH * 8]), ("b_g", [64, 1]),
          ("b_triF", [128, 128]), ("b_triR", [128, 128]), ("b_ident", [128, 128]), ("b_um", [128, 128]), ("b_lm", [128, 128]),
          ("b_us", [128, 128]), ("b_ls", [128, 128])]


def build_gdn(nsteps=NCH, limit=10**9):
    P = Prog()
    _real_add = P.add
    _cnt = [0]
    _on = [False]

    def _ladd(eng, fn, r=(), w=()):
        if _on[0]:
            _cnt[0] += 1
            if _cnt[0] > limit:
                return None
        extra = [k for k in r if isinstance(k, tuple) and k[0] == 'bk' and k not in w]
        return _real_add(eng, fn, r, list(w) + extra)
    P.add = _ladd
    D = {nm: P.dram_in(nm, shp) for nm, shp in GDN_IN}
    out = P.dram_out("mix", [128, NTOK])
    C = bconsts(P)
    ones = C["ones"]

    def ld(nm, shp):
        t = P.sb(shp, F32, "g_" + nm)
        P.dma("sp", t[:], D[nm][:, :], w=["g_" + nm])
        return t
    cw = ld("b_cw", [64, 30])
    gg = ld("b_g", [64, 1])
    tri = [ld("b_triF", [128, 128]), ld("b_triR", [128, 128])]
    ident = ld("b_ident", [128, 128])
    msk = [ld("b_um", [128, 128]), ld("b_lm", [128, 128])]
    smsk = [ld("b_us", [128, 128]), ld("b_ls", [128, 128])]
    CK = ["g_b_triF", "g_b_triR", "g_b_ident", "g_b_um", "g_b_lm", "g_b_us", "g_b_ls", "ones"]
    banks = [P.ps([128, 512], F32, f"g_bk{j}") for j in range(8)]

    def X(j, r, rows=128, cols=128):
        return banks[j][0:rows, r * 128:r * 128 + cols]

    qn = P.sb([64, 2, NTOK], F32, "g_qn"); kn = P.sb([64, 2, NTOK], F32, "g_kn"); oacc = P.sb([64, 2, NTOK], F32, "g_oacc")
    ktok = P.sb([128, NCH, 128], F32, "g_ktok"); vtok = P.sb([128, NCH, 128], F32, "g_vtok")
    P.add("pool", lambda e: e.memset(oacc[:], 0.0), w=["g_oacc"])
    ab = P.sb([128, NCH * 8], F32, "g_ab"); dtb = ld("b_dtb", [128, NCH * 8]); alog = ld("b_alog", [128, NCH * 8])
    gtok = P.sb([128, NCH * 8], F32, "g_gtok"); btok = P.sb([128, NCH * 8], F32, "g_btok")
    P.dma("sp", ab[:].rearrange("p (c e) -> p c e", e=8), D["b_ab"].rearrange("(c p) e -> p c e", p=128), w=["g_ab"])
    P.add("act", lambda e: e.activation(out=btok[:], in_=ab[:], func=AF.Sigmoid), r=["g_ab"], w=["g_btok"])
    P.add("dve", lambda e: e.tensor_tensor(out=gtok[:], in0=ab[:], in1=dtb[:], op=ALU.add), r=["g_ab", "g_b_dtb"], w=["g_gtok"])
    P.add("act", lambda e: e.activation(out=gtok[:], in_=gtok[:], func=AF.Exp), r=["g_gtok"], w=["g_gtok"])
    P.add("act", lambda e: e.activation(out=gtok[:], in_=gtok[:], func=AF.Ln, bias=1.0), r=["g_gtok"], w=["g_gtok"])
    P.add("act", lambda e: e.activation(out=alog[:], in_=alog[:], func=AF.Exp), r=["g_b_alog"], w=["g_b_alog"])
    P.add("dve", lambda e: e.scalar_tensor_tensor(out=gtok[:], in0=gtok[:], scalar=-1.0, in1=alog[:], op0=ALU.mult, op1=ALU.mult), r=["g_gtok", "g_b_alog"], w=["g_gtok"])

    xr = P.sb([64, 2, 516], F32, "g_xr"); acc = P.sb([64, 2, 512], F32, "g_acc"); sq = P.sb([64, 2, 512], F32, "g_sq"); rs = P.sb([64, 2, 512], F32, "g_rs")

    def conv_tile(gi, src, t0, n):
        s0, s1 = (0, 256) if t0 < 256 else (256, NTOK)
        lo, hi = max(t0 - 2, s0), min(t0 + n + 2, s1)
        P.add("pool", lambda e: e.memset(xr[:], 0.0), w=["g_xr"])
        P.dma("sp", xr[:, :, lo - (t0 - 2):hi - (t0 - 2)], D[src].rearrange("(h d) t -> d h t", d=64)[:, :, lo:hi], w=["g_xr"])
        for h in range(2):
            eng = "dve"
            for tap in range(5):
                wcol = cw[:, gi * 10 + h * 5 + tap:gi * 10 + h * 5 + tap + 1]
                if tap == 0:
                    P.add(eng, lambda e, h=h, wcol=wcol: e.tensor_scalar(out=acc[:, h, :n], in0=xr[:, h, 0:n], scalar1=wcol, scalar2=None, op0=ALU.mult),
                          r=["g_xr", "g_b_cw"], w=[("g_acc", h)])
                else:
                    P.add(eng, lambda e, h=h, wcol=wcol, tap=tap: e.scalar_tensor_tensor(out=acc[:, h, :n], in0=xr[:, h, tap:tap + n], scalar=wcol, in1=acc[:, h, :n], op0=ALU.mult, op1=ALU.add),
                          r=["g_xr", "g_b_cw", ("g_acc", h)], w=[("g_acc", h)])
        P.add("act", lambda e: e.activation(out=acc[:, :, :n], in_=acc[:, :, :n], func=AF.Silu), r=[("g_acc", 0), ("g_acc", 1)], w=[("g_acc", 0), ("g_acc", 1)])

    def l2_tile(dst, dkey, t0, n, scl):
        P.add("act", lambda e: e.activation(out=sq[:, :, :n], in_=acc[:, :, :n], func=AF.Square), r=[("g_acc", 0), ("g_acc", 1)], w=["g_sq"])
        for h in range(2):
            P.add("pe", lambda e, h=h: e.matmul(banks[h][0:64, :n], ones[0:64, 0:64], sq[:, h, :n], start=True, stop=True), r=["g_sq", "ones"], w=[("bk", h)])
            P.add("act", lambda e, h=h: e.activation(out=rs[:, h, :n], in_=banks[h][0:64, :n], func=AF.Sqrt, bias=C["epsb"][0:64, 0:1]), r=[("bk", h), "epsb"], w=["g_rs"])
        P.add("dve", lambda e: e.reciprocal(out=rs[:, :, :n], in_=rs[:, :, :n]), r=["g_rs"], w=["g_rs"])
        P.add("dve", lambda e: e.scalar_tensor_tensor(out=dst[:, :, t0:t0 + n], in0=acc[:, :, :n], scalar=scl, in1=rs[:, :, :n], op0=ALU.mult, op1=ALU.mult),
              r=[("g_acc", 0), ("g_acc", 1), "g_rs"], w=[dkey])

    def tr_tile(srcfn, skeys, dst, dkey, t0, n):
        for s in range(n // 128):
            c = t0 // 128 + s
            j = 2 + (s % 2)
            for h in range(2):
                P.add("pe", lambda e, h=h, s=s, j=j: e.matmul(banks[j][:, h * 64:(h + 1) * 64], srcfn(h, s), ident[0:64, 0:64], start=True, stop=True),
                      r=skeys + ["g_b_ident"], w=[("bk", j)])
            P.add("act", lambda e, c=c, j=j: e.copy(out=dst[:, c, :], in_=banks[j][:, 0:128]), r=[("bk", j)], w=[dkey])

    for (t0, n) in QT:
        conv_tile(0, "b_q", t0, n)
        l2_tile(qn, "g_qn", t0, n, 0.125)
        conv_tile(1, "b_k", t0, n)
        l2_tile(kn, "g_kn", t0, n, 1.0)
        tr_tile(lambda h, s, t0=t0: kn[:, h, t0 + s * 128:t0 + (s + 1) * 128], ["g_kn"], ktok, "g_ktok", t0, n)
        conv_tile(2, "b_v", t0, n)
        tr_tile(lambda h, s: acc[:, h, s * 128:(s + 1) * 128], [("g_acc", 0), ("g_acc", 1)], vtok, "g_vtok", t0, n)

    NI = 4
    def tl(nm, shp):
        return [P.sb(shp, F32, f"g_{nm}{i}") for i in range(NI)]
    grep = tl("grep", [128, 128]); brep = tl("brep", [128, 128]); T1 = tl("T1", [128, 128]); EB = tl("EB", [64, 128]); bBs = tl("bBs", [64, 128])
    DTm = tl("DTm", [128, 128]); DTs = tl("DTs", [128, 128]); MT = tl("MT", [128, 128]); Pa = tl("Pa", [128, 128]); PTa = tl("PTa", [128, 128])
    Pb = tl("Pb", [128, 128]); PTb = tl("PTb", [128, 128]); RT = tl("RT", [128, 128]); AT = tl("AT", [128, 128])
    wT = tl("wT", [64, 128]); u = tl("u", [128, 64]); qd = tl("qd", [64, 128]); kd = tl("kd", [128, 64]); vb = tl("vb", [128, 64]); kbe = tl("kbe", [128, 64])
    kbT = tl("kbT", [64, 128]); vnew = tl("vnew", [128, 64]); cols = tl("cols", [128, 8]); Sst = tl("S", [64, 64])
    for i in range(NI):
        P.add("pool", lambda e, i=i: e.memset(Sst[i][:], 0.0), w=[("S", i)])
    orders = [list(range(NCH)), [1, 0] + list(range(NCH - 1, 1, -1))]

    def pre(i, c, d, h):
        K = lambda nm: (nm, i)
        bk = ("bk", i)
        gcol = gtok[:, c * 8 + d * 4 + h:c * 8 + d * 4 + h + 1]
        bcol = btok[:, c * 8 + d * 4 + 2 + h:c * 8 + d * 4 + 2 + h + 1]
        ch = slice(c * 128, (c + 1) * 128)
        hs = slice(h * 64, (h + 1) * 64)
        cl = cols[i]
        P.add("dve", lambda e: e.tensor_scalar(out=grep[i][:, :], in0=ones[:, :], scalar1=gcol, scalar2=None, op0=ALU.mult), r=["g_gtok", "ones"], w=[K("grep")])
        P.add("pool", lambda e: e.tensor_scalar(out=brep[i][:, :], in0=ones[:, :], scalar1=bcol, scalar2=None, op0=ALU.mult), r=["g_btok", "ones"], w=[K("brep")])
        P.add("pe", lambda e: e.matmul(X(i, 0), grep[i][:, :], tri[d][:, :], start=True, stop=True), r=[K("grep")] + CK, w=[bk])
        yield
        P.add("pe", lambda e: e.matmul(X(i, 2), brep[i][:, :], ident[:, :], start=True, stop=True), r=[K("brep")] + CK, w=[bk])
        yield
        P.add("dve", lambda e: e.tensor_tensor(out=T1[i][:, :], in0=X(i, 0), in1=ident[:, :], op=ALU.mult), r=[bk] + CK, w=[K("T1")])
        P.add("dve", lambda e: e.reduce_sum(out=cl[:, 0:1], in_=T1[i][:, :], axis=AX.X), r=[K("T1")], w=[K("cols")])
        lc = 127 if d == 0 else 0
        P.add("act", lambda e: e.copy(out=cl[:, 1:2], in_=banks[i][:, lc:lc + 1]), r=[bk], w=[K("cols")])
        P.add("act", lambda e: e.activation(out=EB[i][:, :], in_=X(i, 0, 64), func=AF.Exp), r=[bk], w=[K("EB")])
        P.add("dve", lambda e: e.tensor_scalar(out=T1[i][:, :], in0=X(i, 0), scalar1=cl[:, 0:1], scalar2=0.0, op0=ALU.subtract, op1=ALU.min), r=[bk, K("cols")], w=[K("T1")])
        P.add("dve", lambda e: e.tensor_copy(out=bBs[i][:, :], in_=X(i, 2, 64)), r=[bk], w=[K("bBs")])
        P.add("act", lambda e: e.activation(out=DTm[i][:, :], in_=T1[i][:, :], func=AF.Exp), r=[K("T1")], w=[K("DTm")])
        P.add("pool", lambda e: e.tensor_tensor(out=DTm[i][:, :], in0=DTm[i][:, :], in1=msk[d][:, :], op=ALU.mult), r=[K("DTm")] + CK, w=[K("DTm")])
        P.add("pool", lambda e: e.tensor_tensor(out=DTs[i][:, :], in0=DTm[i][:, :], in1=smsk[d][:, :], op=ALU.mult), r=[K("DTm")] + CK, w=[K("DTs")])
        P.add("act", lambda e: e.activation(out=cl[:, 2:3], in_=cl[:, 0:1], func=AF.Exp, scale=-1.0, bias=cl[:, 1:2]), r=[K("cols")], w=[K("cols")])
        P.add("act", lambda e: e.activation(out=cl[:, 3:4], in_=cl[:, 1:2], func=AF.Exp), r=[K("cols")], w=[K("cols")])
        P.add("act", lambda e: e.activation(out=cl[:, 4:5], in_=cl[:, 0:1], func=AF.Exp), r=[K("cols")], w=[K("cols")])
        P.add("dve", lambda e: e.tensor_tensor(out=cl[:, 5:6], in0=cl[:, 4:5], in1=bcol, op=ALU.mult), r=[K("cols"), "g_btok"], w=[K("cols")])
        P.add("dve", lambda e: e.tensor_tensor(out=kbT[i][:, :], in0=kn[:, h, ch], in1=bBs[i][:, :], op=ALU.mult), r=["g_kn", K("bBs")], w=[K("kbT")])
        P.add("dve", lambda e: e.tensor_tensor(out=qd[i][:, :], in0=qn[:, h, ch], in1=EB[i][:, :], op=ALU.mult), r=["g_qn", K("EB")], w=[K("qd")])
        P.add("pool", lambda e: e.tensor_scalar(out=vb[i][:, :], in0=vtok[:, c, hs], scalar1=bcol, scalar2=None, op0=ALU.mult), r=["g_vtok", "g_btok"], w=[K("vb")])
        P.add("pool", lambda e: e.tensor_scalar(out=kbe[i][:, :], in0=ktok[:, c, hs], scalar1=cl[:, 5:6], scalar2=None, op0=ALU.mult), r=["g_ktok", K("cols")], w=[K("kbe")])
        P.add("pool", lambda e: e.tensor_scalar(out=kd[i][:, :], in0=ktok[:, c, hs], scalar1=cl[:, 2:3], scalar2=None, op0=ALU.mult), r=["g_ktok", K("cols")], w=[K("kd")])
        P.add("pe", lambda e: e.matmul(X(i, 0), kn[:, h, ch], kbT[i][:, :], start=True, stop=True), r=["g_kn", K("kbT")], w=[bk])
        yield
        P.add("pe", lambda e: e.matmul(X(i, 1), kn[:, h, ch], qn[:, h, ch], start=True, stop=True), r=["g_kn", "g_qn"], w=[bk])
        yield
        P.add("dve", lambda e: e.scalar_tensor_tensor(out=MT[i][:, :], in0=X(i, 0), scalar=-1.0, in1=DTs[i][:, :], op0=ALU.mult, op1=ALU.mult), r=[bk, K("DTs")], w=[K("MT")])
        P.add("dve", lambda e: e.tensor_tensor(out=AT[i][:, :], in0=X(i, 1), in1=DTm[i][:, :], op=ALU.mult), r=[bk, K("DTm")], w=[K("AT")])
        P.add("pe", lambda e: e.matmul(X(i, 2), MT[i][:, :], ident[:, :], start=True, stop=True), r=[K("MT")] + CK, w=[bk])
        yield
        P.add("act", lambda e: e.copy(out=Pa[i][:, :], in_=X(i, 2)), r=[bk], w=[K("Pa")])
        P.add("pool", lambda e: e.tensor_tensor(out=RT[i][:, :], in0=MT[i][:, :], in1=ident[:, :], op=ALU.add), r=[K("MT")] + CK, w=[K("RT")])
        Pc, PTc, Pn, PTn = Pa[i], MT[i], Pb[i], PTb[i]
        kPc, kPTc, kPn, kPTn = K("Pa"), K("MT"), K("Pb"), K("PTb")
        for lvl in range(1, 7):
            P.add("pe", lambda e, Pc=Pc, PTc=PTc: e.matmul(X(i, 0), PTc[:, :], Pc[:, :], start=True, stop=True), r=[kPc, kPTc], w=[bk])
            yield
            if lvl < 6:
                P.add("pe", lambda e, Pc=Pc, PTc=PTc: e.matmul(X(i, 1), Pc[:, :], PTc[:, :], start=True, stop=True), r=[kPc, kPTc], w=[bk])
                yield
            P.add("act", lambda e, Pn=Pn: e.copy(out=Pn[:, :], in_=X(i, 0)), r=[bk], w=[kPn])
            if lvl < 6:
                P.add("dve", lambda e, PTn=PTn: e.tensor_copy(out=PTn[:, :], in_=X(i, 1)), r=[bk], w=[kPTn])
            P.add("pe", lambda e, Pn=Pn: e.matmul(X(i, 2), Pn[:, :], RT[i][:, :], start=True, stop=True), r=[kPn, K("RT")], w=[bk])
            yield
            P.add("dve", lambda e: e.tensor_tensor(out=RT[i][:, :], in0=RT[i][:, :], in1=X(i, 2), op=ALU.add), r=[bk, K("RT")], w=[K("RT")])
            if lvl == 1:
                Pc, PTc, Pn, PTn = Pb[i], PTb[i], Pa[i], PTa[i]
                kPc, kPTc, kPn, kPTn = K("Pb"), K("PTb"), K("Pa"), K("PTa")
            else:
                Pc, PTc, Pn, PTn = Pn, PTn, Pc, PTc
                kPc, kPTc, kPn, kPTn = kPn, kPTn, kPc, kPTc
        P.add("pe", lambda e: e.matmul(X(i, 0, 128, 64), RT[i][:, :], vb[i][:, :], start=True, stop=True), r=[K("RT"), K("vb")], w=[bk])
        yield
        P.add("pe", lambda e: e.matmul(X(i, 1, 64, 128), kbe[i][:, :], RT[i][:, :], start=True, stop=True), r=[K("RT"), K("kbe")], w=[bk])
        yield
        P.add("act", lambda e: e.copy(out=u[i][:, :], in_=X(i, 0, 128, 64)), r=[bk], w=[K("u")])
        P.add("dve", lambda e: e.tensor_copy(out=wT[i][:, :], in_=X(i, 1, 64, 128)), r=[bk], w=[K("wT")])

    def chain(i, c, d, h):
        K = lambda nm: (nm, i)
        bk = ("bk", 4 + i)
        j = 4 + i
        ch = slice(c * 128, (c + 1) * 128)
        cl = cols[i]
        P.add("pe", lambda e: e.matmul(X(j, 0, 128, 64), wT[i][:, :], Sst[i][:, :], start=True, stop=True), r=[K("wT"), ("S", i)], w=[bk])
        yield
        P.add("dve", lambda e: e.tensor_tensor(out=vnew[i][:, :], in0=u[i][:, :], in1=X(j, 0, 128, 64), op=ALU.subtract), r=[bk, K("u")], w=[K("vnew")])
        P.add("pe", lambda e: e.matmul(X(j, 1, 64, 128), Sst[i][:, :], qd[i][:, :], start=True, stop=False), r=[("S", i), K("qd")], w=[bk])
        yield
        P.add("pe", lambda e: e.matmul(X(j, 1, 64, 128), vnew[i][:, :], AT[i][:, :], start=False, stop=True), r=[K("vnew"), K("AT")], w=[bk])
        yield
        P.add("pe", lambda e: e.matmul(X(j, 2, 64, 64), kd[i][:, :], vnew[i][:, :], start=True, stop=True), r=[K("kd"), K("vnew")], w=[bk])
        yield
        P.add("dve", lambda e: e.tensor_tensor(out=oacc[:, h, ch], in0=oacc[:, h, ch], in1=X(j, 1, 64, 128), op=ALU.add), r=[bk, "g_oacc"], w=["g_oacc"])
        P.add("dve", lambda e: e.scalar_tensor_tensor(out=Sst[i][:, :], in0=Sst[i][:, :], scalar=cl[0:64, 3:4], in1=X(j, 2, 64, 64), op0=ALU.mult, op1=ALU.add),
              r=[bk, ("S", i), K("cols")], w=[("S", i)])

    _on[0] = True
    for s in range(nsteps):
        insts = [(h * 2 + d, orders[d][s], d, h) for h in range(2) for d in range(2)]
        for gens in ([pre(*a_) for a_ in insts], [chain(*a_) for a_ in insts]):
            live = list(gens)
            while live:
                nxt = []
                for g_ in live:
                    try:
                        next(g_)
                        nxt.append(g_)
                    except StopIteration:
                        pass
                live = nxt

    _on[0] = False
    print('gdn inst ops', _cnt[0])
    gt = xr; yo = acc

    def fin(t0, n):
        P.dma("sp", gt[:, :, :n], D["b_gate"].rearrange("(h d) t -> d h t", d=64)[:, :, t0:t0 + n], w=["g_xr"])
        P.add("act", lambda e: e.activation(out=sq[:, :, :n], in_=oacc[:, :, t0:t0 + n], func=AF.Square), r=["g_oacc"], w=["g_sq"])
        for h in range(2):
            P.add("pe", lambda e, h=h: e.matmul(banks[h][0:64, :n], ones[0:64, 0:64], sq[:, h, :n], start=True, stop=True), r=["g_sq", "ones"], w=[("bk", h)])
            P.add("act", lambda e, h=h: e.activation(out=rs[:, h, :n], in_=banks[h][0:64, :n], func=AF.Sqrt, scale=1.0 / 64, bias=C["epsb"][0:64, 0:1]), r=[("bk", h), "epsb"], w=["g_rs"])
        P.add("dve", lambda e: e.reciprocal(out=rs[:, :, :n], in_=rs[:, :, :n]), r=["g_rs"], w=["g_rs"])
        P.add("dve", lambda e: e.tensor_tensor(out=yo[:, :, :n], in0=oacc[:, :, t0:t0 + n], in1=rs[:, :, :n], op=ALU.mult), r=["g_oacc", "g_rs"], w=[("g_acc", 0), ("g_acc", 1)])
        P.add("act", lambda e: e.activation(out=gt[:, :, :n], in_=gt[:, :, :n], func=AF.Silu), r=["g_xr"], w=["g_xr"])
        P.add("dve", lambda e: e.scalar_tensor_tensor(out=yo[:, :, :n], in0=yo[:, :, :n], scalar=gg[:, 0:1], in1=gt[:, :, :n], op0=ALU.mult, op1=ALU.mult), r=[("g_acc", 0), ("g_acc", 1), "g_xr", "g_b_g"], w=[("g_acc", 0), ("g_acc", 1)])
        P.dma("sp", out.rearrange("(h d) t -> d h t", d=64)[:, :, t0:t0 + n], yo[:, :, :n], r=[("g_acc", 0), ("g_acc", 1)])

    for (t0, n) in QT:
        fin(t0, n)
    print("gdn ops", P.n_ops())
    return P.finish()


def build_M():
    P = Prog()
    scT = P.dram_in("scT", [1024, 5])
    wm = P.dram_in("wm", [1024, 3072])
    bm = P.dram_in("bm", [128, 24])
    modo = P.dram_out("modo", [128, 120])
    sc = P.sb([128, 8, 5], F32, "sc")
    P.dma("sp", sc[:], scT.rearrange("(k p) j -> p k j", p=128), w=["sc"])
    P.add("act", lambda e: e.activation(out=sc[:], in_=sc[:], func=AF.Silu), r=["sc"], w=["sc"])
    bms = P.sb([128, 24], F32, "bms")
    P.dma("sp", bms[:], bm[:, :], w=["bms"])
    w = P.sb([128, 8, 3072], F32, "wms")
    for k in range(8):
        P.dma("sp", w[:, k, :], wm[k * 128:(k + 1) * 128, :], w=[("wms", k)])
    ps = P.ps([128, 512], F32, "mps")
    ob = P.sb([128, 120], F32, "ob")
    for cc in range(24):
        for k in range(8):
            P.add("pe", lambda e, cc=cc, k=k: e.matmul(ps[:, cc * 5:(cc + 1) * 5], w[:, k, cc * 128:(cc + 1) * 128], sc[:, k, :], start=(k == 0), stop=(k == 7)),
                  r=["sc"] + [("wms", kk) for kk in range(8)], w=["mps"])
    for cc in range(24):
        P.add("dve", lambda e, cc=cc: e.tensor_scalar(out=ob[:, cc * 5:(cc + 1) * 5], in0=ps[:, cc * 5:(cc + 1) * 5], scalar1=bms[:, cc:cc + 1], scalar2=None, op0=ALU.add),
              r=["mps", "bms"], w=["ob"])
    P.dma("sp", modo[:, :], ob[:], r=["ob"])
    return P.finish()


OFF = {}
_o = 0
for nm, n in [("Aq",256),("Ak",128),("Av",128),("Bqkv",768),("Bgate",256),("Bab",16),("Ccq",256),("Cckv",128),("Ckr",32),("Dq",256),("Dk",256),("Dv",256),("Dgate",256)]:
    OFF[nm] = (_o, n); _o += n

def rope_perm(dim):
    q = dim // 4
    perm = np.concatenate([np.arange(q) + q, np.arange(q), np.arange(q) + 3 * q, np.arange(q) + 2 * q])
    sign = np.concatenate([-np.ones(q), np.ones(q), -np.ones(q), np.ones(q)]).astype(np.float32)
    return perm, sign

def rope_tables(rot_dim, rows=64, grid_w=64, theta=10000.0):
    n_freq = rot_dim // 4
    inv_freq = (theta ** (-np.arange(n_freq, dtype=np.float32) / n_freq)).astype(np.float32)
    row = np.repeat(np.arange(rows, dtype=np.float32), grid_w)
    col = np.tile(np.arange(grid_w, dtype=np.float32), rows)
    ang_r = row[:, None] * inv_freq
    ang_c = col[:, None] * inv_freq
    ang = np.concatenate([ang_r, ang_r, ang_c, ang_c], axis=-1).astype(np.float32)
    return np.cos(ang).astype(np.float32), np.sin(ang).astype(np.float32)

def rope_tabs_T(rot_dim):
    cos, sin = rope_tables(rot_dim)
    perm, sign = rope_perm(rot_dim)
    cT = np.ones((rot_dim, 4352), np.float32); sT = np.zeros((rot_dim, 4352), np.float32)
    cT[:, 256:] = cos.T; sT[:, 256:] = (sin * sign[None, :]).T
    return cT, sT

def perm_heads(w, dim):
    perm, _ = rope_perm(dim)
    nh = w.shape[1] // dim
    idx = np.concatenate([h * dim + perm for h in range(nh)])
    return w[:, idx]


def fm(v, nk):
    return np.ascontiguousarray(v.reshape(nk, 128).T)


def mla_inputs(P_, hf, W):
    hs = [2 * hf, 2 * hf + 1]
    perm32, _ = rope_perm(32)
    ct, st = rope_tabs_T(32)
    ct96 = np.ones((96, 4352), np.float32); st96 = np.zeros((96, 4352), np.float32)
    ct96[64:] = ct; st96[64:] = st
    wq = np.concatenate([W["mla_w_q_up"][:, h * 96:(h + 1) * 96] for h in hs], axis=1)
    wqP = np.zeros((256, 192), np.float32)
    for i, h in enumerate(hs):
        wqP[:, i * 96 + 64:i * 96 + 96] = W["mla_w_q_up"][:, h * 96 + 64 + perm32]
    wkn = np.zeros((128, 192), np.float32)
    for i, h in enumerate(hs):
        wkn[:, i * 96:i * 96 + 64] = W["mla_w_kv_up"][:, h * 128:h * 128 + 64]
    wv = np.concatenate([W["mla_w_kv_up"][:, h * 128 + 64:h * 128 + 128] for h in hs], axis=1)
    sel = np.zeros((32, 96), np.float32); sel[np.arange(32), 64 + np.arange(32)] = 1
    return {"c_cq": P_["Ccq"], "c_ckv": P_["Cckv"], "c_kr": P_["Ckr"], "c_krP": P_["CkrP"], "c_ct96": ct96, "c_st96": st96,
            "c_qng": fm(W["mla_q_norm"], 2), "c_kvg": fm(W["mla_kv_norm"], 1), "c_wq": np.ascontiguousarray(wq), "c_wqP": wqP,
            "c_wkn": wkn, "c_wv": np.ascontiguousarray(wv), "c_sel": sel}


def swa_inputs(PF, PT, hf, W):
    cT, sT = rope_tabs_T(64)
    j = np.arange(128)[:, None]; i = np.arange(128)[None, :]
    sink = W["swa_sink"][2 * hf:2 * hf + 2]
    return {"a_q": PF["Aq"][hf * 128:(hf + 1) * 128], "a_qP": PF["AqP"][hf * 128:(hf + 1) * 128],
            "a_k": PF["Ak"][hf * 64:(hf + 1) * 64], "a_kP": PF["AkP"][hf * 64:(hf + 1) * 64],
            "a_vtok": np.ascontiguousarray(PT["Av"][:, hf * 64:(hf + 1) * 64]), "a_cos": cT, "a_sin": sT,
            "a_sink": np.ascontiguousarray(np.broadcast_to(sink[None, :], (128, 2))).astype(np.float32),
            "a_maskP": (j >= i).astype(np.float32), "a_maskN": (j <= i).astype(np.float32)}


def ret_inputs(PF, PT, hf, W):
    cT, sT = rope_tabs_T(64)
    hs = [2 * hf, 2 * hf + 1]
    sl = slice(hf * 128, (hf + 1) * 128)
    ld = W["ret_log_decay"]
    ldp = np.zeros((128, 2), np.float32); ldr = np.zeros((128, 4), np.float32)
    for d in range(2):
        for i, h in enumerate(hs):
            ldp[i * 64:(i + 1) * 64, d] = ld[d, h]
            ldr[:, 2 * d + i] = ld[d, h]
    j = np.arange(128)[:, None].astype(np.float32); i = np.arange(128)[None, :].astype(np.float32)
    bd = np.zeros((128, 128), np.float32); bd[:64, :64] = 1; bd[64:, 64:] = 1
    pk = np.stack([127 - np.arange(128), np.arange(128)], 1).astype(np.float32)
    return {"d_q": PF["Dq"][sl], "d_qP": PF["DqP"][sl], "d_k": PF["Dk"][sl], "d_kP": PF["DkP"][sl], "d_gate": PF["Dgate"][sl],
            "d_vtok": np.ascontiguousarray(PT["Dv"][:, sl]), "d_ktok": np.ascontiguousarray(PT["Dk"][:, sl]), "d_kPtok": np.ascontiguousarray(PT["DkP"][:, sl]),
            "d_cos": cT, "d_sin": sT, "d_costok": np.ascontiguousarray(cT.T), "d_sintok": np.ascontiguousarray(sT.T),
            "d_ldp": ldp, "d_ldr": ldr, "d_g": np.ascontiguousarray(W["ret_norm"][sl].reshape(128, 1)),
            "d_relu": np.maximum(i - j, 0) + 0 * j, "d_rell": np.maximum(j - i, 0) + 0 * i, "d_um": (i >= j).astype(np.float32), "d_lm": (j >= i).astype(np.float32),
            "d_pos1": (i + 1) + 0 * j, "d_posr": (128 - i) + 0 * j, "d_pk": pk, "d_bd": bd, "d_bd64": bd / 64}


def gdn_inputs(PF, PT, hf, W):
    hs = [2 * hf, 2 * hf + 1]
    qkv = PF["Bqkv"]
    sel = lambda base: np.ascontiguousarray(np.concatenate([qkv[base + h * 64: base + (h + 1) * 64] for h in hs], 0))
    ab = PT["Bab"]
    abl = np.zeros((4352, 8), np.float32)
    dtb = np.zeros((8,), np.float32); alog = np.zeros((8,), np.float32)
    for d in range(2):
        for w in range(2):
            for hl, h in enumerate(hs):
                abl[:, d * 4 + w * 2 + hl] = ab[:, d * 8 + w * 4 + h]
        for hl, h in enumerate(hs):
            dtb[d * 4 + hl] = W["gdn_dt_bias"][d, h]; alog[d * 4 + hl] = W["gdn_a_log"][d, h]
    cwt = np.zeros((64, 3, 2, 5), np.float32)
    conv = W["gdn_conv"]
    for gi in range(3):
        for hl, h in enumerate(hs):
            cwt[:, gi, hl, :] = conv[:, gi * 256 + h * 64: gi * 256 + (h + 1) * 64].T
    k = np.arange(128)[:, None]; i = np.arange(128)[None, :]
    f = lambda m: m.astype(np.float32)
    return {"b_q": sel(0), "b_k": sel(256), "b_v": sel(512), "b_gate": PF["Bgate"][hf * 128:(hf + 1) * 128], "b_ab": abl,
            "b_cw": cwt.reshape(64, 30), "b_dtb": np.ascontiguousarray(np.broadcast_to(np.tile(dtb, 34)[None], (128, 272))),
            "b_alog": np.ascontiguousarray(np.broadcast_to(np.tile(alog, 34)[None], (128, 272))), "b_g": np.ascontiguousarray(W["gdn_norm"].reshape(64, 1)),
            "b_triF": f(k <= i), "b_triR": f(k >= i), "b_ident": np.eye(128, dtype=np.float32), "b_um": f(i >= k), "b_lm": f(k >= i),
            "b_us": f(i > k), "b_ls": f(k > i)}


_PROGS = {}
_TRACE = [False]


def _prog(name, fn):
    if name not in _PROGS:
        _PROGS[name] = fn()
    return _PROGS[name]


def _run(name, fn, in_maps):
    nc = fn()
    in_maps = [{k: np.ascontiguousarray(v, dtype=np.float32) for k, v in m.items()} for m in in_maps]
    res = run_bass_kernel_spmd(nc, in_maps, core_ids=list(range(8)), trace=_TRACE[0])
    if _TRACE[0]:
        print('STAGE', name, 'exec_ns', res.exec_time_ns, flush=True)
    return res.results


A_TILES = TILES
HALF = 2176


def _mod_table(mod_l, b, hf, tiles_ctx):
    nt = len(tiles_ctx)
    t = np.zeros((128, 6, nt, 8), np.float32)
    for ti, is_ctx in enumerate(tiles_ctx):
        v = mod_l[4 if is_ctx else b].reshape(6, 8, 128)
        t[:, :, ti, :] = v.transpose(2, 0, 1)
    return t


def kernel(x, c, ctx, c_ctx, w_mod, b_mod, norm1, norm2, w_in, w_out, swa_sink, gdn_conv, gdn_a_log,
           gdn_dt_bias, gdn_norm, mla_q_norm, mla_kv_norm, mla_w_q_up, mla_w_kv_up, ret_log_decay,
           ret_norm, ffn_w_gate, ffn_w_up, ffn_w_down, moe_router, moe_w_gate, moe_w_up, moe_w_down,
           final_norm, _nlayers=4):
    f32 = np.float32
    x = np.asarray(x, f32); ctx = np.asarray(ctx, f32)
    B = 4
    scT = np.ascontiguousarray(np.concatenate([np.asarray(c, f32), np.asarray(c_ctx, f32)[None]], 0).T)
    wm_all = np.asarray(w_mod, f32).transpose(1, 0, 2).reshape(1024, 4 * 6144)
    bm_all = np.asarray(b_mod, f32).reshape(4 * 6144)
    ims = []
    for core in range(8):
        sl = slice(core * 3072, (core + 1) * 3072)
        ims.append({"scT": scT, "wm": np.ascontiguousarray(wm_all[:, sl]), "bm": np.ascontiguousarray(bm_all[sl].reshape(24, 128).T)})
    res = _run("M", build_M, ims)
    mod = np.zeros((5, 4 * 6144), f32)
    for core in range(8):
        mo = res[core]["modo"].reshape(128, 24, 5)
        mod[:, core * 3072:(core + 1) * 3072] = mo.transpose(2, 1, 0).reshape(5, 3072)
    mod = mod.reshape(5, 4, 6144)

    hT = [np.ascontiguousarray(np.concatenate([ctx[b], x[b]], 0).T) for b in range(B)]
    p64, _ = rope_perm(64)
    p32, _ = rope_perm(32)
    ident = np.eye(128, dtype=f32)
    out_final = None
    for l in range(_nlayers):
        W = {"w_in": np.asarray(w_in[l], f32), "swa_sink": np.asarray(swa_sink[l], f32), "gdn_conv": np.asarray(gdn_conv[l], f32),
             "gdn_a_log": np.asarray(gdn_a_log[l], f32), "gdn_dt_bias": np.asarray(gdn_dt_bias[l], f32), "gdn_norm": np.asarray(gdn_norm[l], f32),
             "mla_q_norm": np.asarray(mla_q_norm[l], f32), "mla_kv_norm": np.asarray(mla_kv_norm[l], f32), "mla_w_q_up": np.asarray(mla_w_q_up[l], f32),
             "mla_w_kv_up": np.asarray(mla_w_kv_up[l], f32), "ret_log_decay": np.asarray(ret_log_decay[l], f32), "ret_norm": np.asarray(ret_norm[l], f32)}
        wi = W["w_in"]
        blk = lambda nm: wi[:, OFF[nm][0]:OFF[nm][0] + OFF[nm][1]]
        fcols = {"Aq": blk("Aq"), "AqP": perm_heads(blk("Aq"), 64), "Ak": blk("Ak"), "AkP": perm_heads(blk("Ak"), 64), "Bqkv": blk("Bqkv"),
                 "Bgate": blk("Bgate"), "Ccq": blk("Ccq"), "Cckv": blk("Cckv"), "Ckr": blk("Ckr"), "CkrP": perm_heads(blk("Ckr"), 32),
                 "Dq": blk("Dq"), "DqP": perm_heads(blk("Dq"), 64), "Dk": blk("Dk"), "DkP": perm_heads(blk("Dk"), 64), "Dgate": blk("Dgate")}
        tcols = {"Av": blk("Av"), "Bab": blk("Bab"), "Dv": blk("Dv"), "Dk": blk("Dk"), "DkP": perm_heads(blk("Dk"), 64)}
        win_ext = np.ascontiguousarray(np.concatenate([fcols[nm] for nm, _ in F_BLOCKS] + [tcols[nm] for nm, _ in T_BLOCKS], 1))
        gn1 = fm(np.asarray(norm1[l], f32), 8)
        gn2 = fm(np.asarray(norm2[l], f32), 8)
        ims = []
        for core in range(8):
            b, hf = core // 2, core % 2
            mt = _mod_table(mod[:, l], b, hf, [hf == 0 and ti == 0 for ti in range(5)])
            ims.append({"hT": np.ascontiguousarray(hT[b][:, hf * HALF:(hf + 1) * HALF]), "modt": mt.reshape(128, 240), "gn": gn1, "win": win_ext})
        res = _run("A", build_A, ims)
        PFs, PTs = [], []
        for b in range(B):
            pT = np.concatenate([res[2 * b]["projT"], res[2 * b + 1]["projT"]], 1)
            pK = np.concatenate([res[2 * b]["projTok"], res[2 * b + 1]["projTok"]], 0)
            PF, PT = {}, {}
            o = 0
            for nm, n in F_BLOCKS:
                PF[nm] = pT[o:o + n]; o += n
            o = 0
            for nm, n in T_BLOCKS:
                PT[nm] = pK[:, o:o + n]; o += n
            PFs.append(PF); PTs.append(PT)
        mixT = [np.zeros((1024, NTOK), f32) for _ in range(B)]
        for gi, (nm, bfn, ifn) in enumerate((("swa", build_swa, swa_inputs), ("gdn", build_gdn, gdn_inputs), ("mla", build_mla, None), ("ret", build_ret, ret_inputs))):
            ims = []
            for core in range(8):
                b, hf = core // 2, core % 2
                ims.append(mla_inputs(PFs[b], hf, W) if nm == "mla" else ifn(PFs[b], PTs[b], hf, W))
            res = _run(nm, bfn, ims)
            for core in range(8):
                b, hf = core // 2, core % 2
                mixT[b][gi * 256 + hf * 128: gi * 256 + (hf + 1) * 128] = res[core]["mix"]
        moe = (l % 2 == 1)
        final = (l == 3)
        i2 = l // 2
        nt = 5 if moe else 9
        nl = 2 if moe else 1
        span = nt * 256
        padw = nl * span
        if moe:
            wts = {"wg": np.asarray(moe_w_gate[i2], f32), "wu": np.asarray(moe_w_up[i2], f32), "wd": np.asarray(moe_w_down[i2], f32),
                   "wr": np.asarray(moe_router[i2], f32), "ident": ident}
        else:
            wts = {"wg": np.asarray(ffn_w_gate[i2], f32)[None], "wu": np.asarray(ffn_w_up[i2], f32)[None], "wd": np.asarray(ffn_w_down[i2], f32)[None]}
        wts["wout"] = np.asarray(w_out[l], f32)
        wts["gn"] = gn2
        if final:
            wts["fn"] = fm(np.asarray(final_norm, f32), 8)
        newh = [np.zeros((1024, NTOK), f32) for _ in range(B)]
        outs = [np.zeros((1024, NTOK), f32) for _ in range(B)]
        for r in range(nl):
            ims = []
            for core in range(8):
                b, hf = core // 2, core % 2
                hp = np.zeros((1024, padw), f32); mp = np.zeros((1024, padw), f32)
                hp[:, :HALF] = hT[b][:, hf * HALF:(hf + 1) * HALF]
                mp[:, :HALF] = mixT[b][:, hf * HALF:(hf + 1) * HALF]
                mt = _mod_table(mod[:, l], b, hf, [hf == 0 and (r * nt + ti) == 0 for ti in range(nt)])
                d = {"hT": np.ascontiguousarray(hp[:, r * span:(r + 1) * span]), "mixT": np.ascontiguousarray(mp[:, r * span:(r + 1) * span]),
                     "modt": mt.reshape(128, 6 * nt * 8)}
                d.update(wts)
                ims.append(d)
            res = _run(("C", moe, final, nt), lambda: build_C(moe, final, nt), ims)
            for core in range(8):
                b, hf = core // 2, core % 2
                lo = r * span
                hi = min((r + 1) * span, HALF)
                if hi > lo:
                    newh[b][:, hf * HALF + lo: hf * HALF + hi] = res[core]["h2T"][:, :hi - lo]
                    if final:
                        outs[b][:, hf * HALF + lo: hf * HALF + hi] = res[core]["outT"][:, :hi - lo]
        hT = newh
        if final:
            out_final = np.stack([np.ascontiguousarray(outs[b][:, 256:].T) for b in range(B)], 0)
    if _nlayers < 4:
        return hT
    return out_final.astype(np.float32)
```

```python
import numpy as np
import concourse.bass as bass
import concourse.mybir as mybir
from concourse.bass_utils import run_bass_kernel_spmd
from contextlib import ExitStack

F32 = mybir.dt.float32
BF16 = mybir.dt.bfloat16
AF = mybir.ActivationFunctionType
ALU = mybir.AluOpType
AX = mybir.AxisListType

ENGS = ("pe", "act", "dve", "pool", "sp")


class Op:
    __slots__ = ("eng", "fn", "deps", "is_dma", "sem", "val", "marked", "idx", "prewait")

    def __init__(self, eng, fn, is_dma):
        self.eng = eng
        self.fn = fn
        self.deps = []
        self.is_dma = is_dma
        self.sem = None
        self.val = None
        self.marked = False
        self.prewait = None


class Prog:
    def __init__(self, name="k", n_dma_sems=12):
        self.nc = bass.Bass("TRN2", target_bir_lowering=False)
        self.es = ExitStack()
        self.ops = {e: [] for e in ENGS}
        self.last_w = {}
        self.readers = {}
        self.n_dma_sems = n_dma_sems
        self.dma_rr = 0
        self.dma_last = [None] * n_dma_sems
        self.dma_cnt = [0] * n_dma_sems
        self.uid = 0

    def sb(self, shape, dt=F32, name=None):
        self.uid += 1
        return self.es.enter_context(self.nc.sbuf_tensor("s_" + (name or f"sb{self.uid}"), list(shape), dt))

    def ps(self, shape, dt=F32, name=None):
        self.uid += 1
        return self.es.enter_context(self.nc.psum_tensor("p_" + (name or f"ps{self.uid}"), list(shape), dt))

    def dram_in(self, name, shape, dt=F32):
        return self.nc.dram_tensor(name, list(shape), dt, kind="ExternalInput").ap()

    def dram_out(self, name, shape, dt=F32):
        return self.nc.dram_tensor(name, list(shape), dt, kind="ExternalOutput").ap()

    def dram_tmp(self, name, shape, dt=F32):
        return self.nc.dram_tensor(name, list(shape), dt, kind="Internal").ap()

    def _track(self, op, r, w):
        deps = []
        for k in r:
            lw = self.last_w.get(k)
            if lw is not None:
                deps.append(lw)
        for k in w:
            lw = self.last_w.get(k)
            if lw is not None:
                deps.append(lw)
            for rd in self.readers.get(k, ()):
                deps.append(rd)
        for k in r:
            self.readers.setdefault(k, []).append(op)
        for k in w:
            self.last_w[k] = op
            self.readers[k] = []
        op.deps = [d for d in deps if d is not op and not (d.eng == "pe" and op.eng == "pe" and not d.is_dma and not op.is_dma)]
        for d in op.deps:
            d.marked = True

    def add(self, eng, fn, r=(), w=()):
        op = Op(eng, fn, False)
        self._track(op, r, w)
        self.ops[eng].append(op)
        return op

    def dma(self, eng, out, in_, r=(), w=(), fn=None):
        op = Op(eng, fn if fn is not None else (lambda e: e.dma_start(out=out, in_=in_)), True)
        s = self.dma_rr
        self.dma_rr = (s + 1) % self.n_dma_sems
        op.prewait = self.dma_last[s]
        self.dma_cnt[s] += 16
        op.sem = s
        op.val = self.dma_cnt[s]
        op.marked = True
        self.dma_last[s] = op
        self._track(op, r, w)
        self.ops[eng].append(op)
        return op

    def finish(self):
        nc = self.nc
        es = self.es
        esem = {e: es.enter_context(nc.semaphore(f"sem_{e}")) for e in ENGS}
        dsem = [es.enter_context(nc.semaphore(f"sem_dma{i}")) for i in range(self.n_dma_sems)]
        for e in ENGS:
            c = 0
            for op in self.ops[e]:
                if op.is_dma:
                    continue
                if op.marked:
                    c += 1
                    op.val = c
                    op.sem = e
        block = es.enter_context(nc.Block())
        ops = self.ops

        def emit(e, eng):
            waited = {}
            for op in ops[e]:
                need = {}
                dl = list(op.deps)
                if op.prewait is not None:
                    dl.append(op.prewait)
                for d in dl:
                    key = ("d", d.sem) if d.is_dma else ("e", d.sem)
                    if need.get(key, 0) < d.val:
                        need[key] = d.val
                for key, v in need.items():
                    if waited.get(key, 0) >= v:
                        continue
                    waited[key] = v
                    sem = dsem[key[1]] if key[0] == "d" else esem[key[1]]
                    eng.wait_ge(sem, v)
                ins = op.fn(eng)
                if op.is_dma:
                    ins.then_inc(dsem[op.sem], 16)
                elif op.marked:
                    ins.then_inc(esem[e], 1)
            if e == "sp":
                for i in range(self.n_dma_sems):
                    if self.dma_cnt[i] > 0:
                        eng.wait_ge(dsem[i], self.dma_cnt[i])

        @block.tensor
        def _(eng):
            emit("pe", eng)

        @block.scalar
        def _(eng):
            emit("act", eng)

        @block.vector
        def _(eng):
            emit("dve", eng)

        @block.gpsimd
        def _(eng):
            emit("pool", eng)

        @block.sync
        def _(eng):
            emit("sp", eng)

        es.close()
        return nc

    def n_ops(self):
        return {e: len(v) for e, v in self.ops.items()}


def run(nc, in_maps, trace=False):
    res = run_bass_kernel_spmd(nc, in_maps, core_ids=list(range(len(in_maps))), trace=trace)
    return res


TILES = [(0, 256), (256, 512), (768, 512), (1280, 512), (1792, 384)]
TC = 2176
EPS = 1e-6
F_BLOCKS = [("Aq", 256), ("AqP", 256), ("Ak", 128), ("AkP", 128), ("Bqkv", 768), ("Bgate", 256), ("Ccq", 256),
            ("Cckv", 128), ("Ckr", 32), ("CkrP", 32), ("Dq", 256), ("DqP", 256), ("Dk", 256), ("DkP", 256), ("Dgate", 256)]
T_BLOCKS = [("Av", 128), ("Bab", 16), ("Dv", 256), ("Dk", 256), ("DkP", 256)]
NF = sum(n for _, n in F_BLOCKS)
NT = sum(n for _, n in T_BLOCKS)


def foff(name, blocks):
    o = 0
    for nm, n in blocks:
        if nm == name:
            return o, n
        o += n
    raise KeyError(name)


class Rot:
    def __init__(self, bufs, name):
        self.bufs = bufs
        self.i = 0
        self.name = name

    def next(self):
        b = self.bufs[self.i % len(self.bufs)]
        k = (self.name, self.i % len(self.bufs))
        self.i += 1
        return b, k


def emit_norm_mod(P, C, ht, hkey, n, Asc, shv, t, u, ukey):
    sq, ssps, rs, tmp = C["sq"], C["ssps"], C["rs"], C["tmp"]
    P.add("act", lambda e: e.activation(out=sq[:, :, :n], in_=ht[:, :, :n], func=AF.Square), r=[hkey], w=["sq"])
    for k in range(8):
        P.add("pe", lambda e, k=k: e.matmul(ssps[:, :n], C["ones"][:, :], sq[:, k, :n], start=(k == 0), stop=(k == 7)),
              r=["sq", "ones"], w=["ssps"])
    P.add("act", lambda e: e.activation(out=rs[:, :n], in_=ssps[:, :n], func=AF.Sqrt, scale=1.0 / 1024, bias=C["epsb"][:, 0:1]),
          r=["ssps", "epsb"], w=["rs"])
    P.add("dve", lambda e: e.reciprocal(out=rs[:, :n], in_=rs[:, :n]), r=["rs"], w=["rs"])
    for k in range(8):
        P.add("dve", lambda e, k=k: e.tensor_tensor(out=tmp[:, k, :n], in0=ht[:, k, :n], in1=rs[:, :n], op=ALU.mult),
              r=[hkey, "rs"], w=[("tmp", k)])
        P.add("act", lambda e, k=k: e.activation(out=u[:, k, :n], in_=tmp[:, k, :n], func=AF.Identity,
                                                scale=Asc[:, t, k:k + 1], bias=shv[:, t, k:k + 1]),
              r=[("tmp", k), "modc"], w=[ukey])


def common_consts(P):
    C = {}
    C["ones"] = P.sb([128, 128], F32, "ones")
    P.add("pool", lambda e: e.memset(C["ones"][:], 1.0), w=["ones"])
    C["epsb"] = P.sb([128, 1], F32, "epsb")
    P.add("pool", lambda e: e.memset(C["epsb"][:], EPS), w=["epsb"])
    C["sq"] = P.sb([128, 8, 512], F32, "sq")
    C["tmp"] = P.sb([128, 8, 512], F32, "tmp")
    C["rs"] = P.sb([128, 512], F32, "rs")
    C["ssps"] = P.ps([128, 512], F32, "ssps")
    return C


def load_mod(P, modt_d, gn_d, kinds):
    modt = P.sb([128, 6, 5, 8], F32, "modt")
    P.dma("sp", modt[:].rearrange("p a b c -> p (a b c)"), modt_d[:, :], w=["modt"])
    return modt


def build_A():
    P = Prog()
    hT = P.dram_in("hT", [1024, TC])
    modt_d = P.dram_in("modt", [128, 240])
    gn_d = P.dram_in("gn", [128, 8])
    win = P.dram_in("win", [1024, NF + NT])
    projT = P.dram_out("projT", [NF, TC])
    projTok = P.dram_out("projTok", [TC, NT])
    C = common_consts(P)
    emit_A_body(P, C, hT, None, modt_d, gn_d, win, projT, projTok)
    return P.finish()


def emit_A_setup(P, C, modt_d, gn_d, win, pre=""):
    modt = P.sb([128, 6, 5, 8], F32, pre + "modt")
    P.dma("sp", modt[:].rearrange("p a b c -> p (a b c)"), modt_d[:, :], w=[pre + "modt"])
    gn = P.sb([128, 8], F32, pre + "gn")
    P.dma("sp", gn[:], gn_d[:, :], w=[pre + "gn"])
    A1 = P.sb([128, 5, 8], F32, pre + "A1")
    for t in range(5):
        P.add("dve", lambda e, t=t: e.scalar_tensor_tensor(out=A1[:, t, :], in0=modt[:, 1, t, :], scalar=1.0, in1=gn[:, :],
                                                          op0=ALU.add, op1=ALU.mult), r=[pre + "modt", pre + "gn"], w=["modc"])
    wb = P.sb([128, 8, NF + NT], BF16, pre + "wb")
    for k in range(8):
        P.dma("pool", wb[:, k, :], win[k * 128:(k + 1) * 128, :], w=[("wb", k)])
    return modt, A1, wb


def emit_A_tile(P, C, S, ti, ht, hkey, projT, projTok):
    modt, A1, wb = S["modt"], S["A1"], S["wb"]
    t0, n = TILES[ti]
    u, ukey = S["u"].next()
    emit_norm_mod(P, C, ht, hkey, n, A1, modt[:, 0], ti, u, ukey)
    wkeys = [("wb", k) for k in range(8)]
    ncc = (NF + 127) // 128
    for cc in range(ncc):
        c0 = cc * 128
        m = min(128, NF - c0)
        ps, pk = S["mmps"].next()
        for k in range(8):
            P.add("pe", lambda e, k=k, ps=ps, c0=c0, m=m: e.matmul(ps[:m, :n], wb[:, k, c0:c0 + m], u[:, k, :n], start=(k == 0), stop=(k == 7)),
                  r=[ukey] + wkeys, w=[pk])
        st, sk = S["stage"].next()
        if cc % 2 == 0:
            P.add("act", lambda e, ps=ps, st=st, m=m: e.copy(out=st[:m, :n], in_=ps[:m, :n]), r=[pk], w=[sk])
        else:
            P.add("dve", lambda e, ps=ps, st=st, m=m: e.tensor_copy(out=st[:m, :n], in_=ps[:m, :n]), r=[pk], w=[sk])
        P.dma("sp", projT[c0:c0 + m, t0:t0 + n], st[:m, :n], r=[sk])
    for s in range(n // 128):
        for (c0, c1) in ((0, 512), (512, NT)):
            ps, pk = S["mmps"].next()
            w_ = c1 - c0
            for k in range(8):
                P.add("pe", lambda e, k=k, ps=ps, c0=c0, w_=w_, s=s: e.matmul(ps[:, :w_], u[:, k, s * 128:(s + 1) * 128], wb[:, k, NF + c0:NF + c0 + w_],
                                                                         start=(k == 0), stop=(k == 7)), r=[ukey] + wkeys, w=[pk])
            st, sk = S["stage"].next()
            P.add("dve" if s % 2 else "act", (lambda e, ps=ps, st=st, w_=w_: e.tensor_copy(out=st[:, :w_], in_=ps[:, :w_])) if s % 2 else
                  (lambda e, ps=ps, st=st, w_=w_: e.copy(out=st[:, :w_], in_=ps[:, :w_])), r=[pk], w=[sk])
            P.dma("sp", projTok[t0 + s * 128:t0 + (s + 1) * 128, c0:c1], st[:, :w_], r=[sk])


def emit_A_body(P, C, hT, _, modt_d, gn_d, win, projT, projTok):
    modt, A1, wb = emit_A_setup(P, C, modt_d, gn_d, win)
    S = {"modt": modt, "A1": A1, "wb": wb}
    S["u"] = Rot([P.sb([128, 8, 512], BF16, f"u{i}") for i in range(2)], "u")
    S["mmps"] = Rot([P.ps([128, 512], F32, f"mmps{i}") for i in range(4)], "mmps")
    S["stage"] = Rot([P.sb([128, 512], F32, f"stg{i}") for i in range(4)], "stg")
    hts = Rot([P.sb([128, 8, 512], F32, f"ht{i}") for i in range(2)], "ht")
    hv = hT.rearrange("(k p) t -> p k t", p=128)
    for ti, (t0, n) in enumerate(TILES):
        ht, hk = hts.next()
        P.dma("sp", ht[:, :, :n], hv[:, :, t0:t0 + n], w=[hk])
        emit_A_tile(P, C, S, ti, ht, hk, projT, projTok)


DFF = 3584
NFG = 7


def build_C2(moe, final, ntiles=9):
    NE = 8 if moe else 1
    TC = ntiles * 256
    NS = TC // 128
    CT = [(i * 256, 256) for i in range(ntiles)]
    n = 256
    P = Prog()
    hT = P.dram_in("hT", [1024, TC]); mixT = P.dram_in("mixT", [1024, TC]); wout = P.dram_in("wout", [1024, 1024])
    modt_d = P.dram_in("modt", [128, 6 * ntiles * 8]); gn_d = P.dram_in("gn", [128, 8])
    wg = P.dram_in("wg", [NE, 1024, DFF]); wu = P.dram_in("wu", [NE, 1024, DFF]); wd = P.dram_in("wd", [NE, DFF, 1024])
    if moe:
        wr_d = P.dram_in("wr", [1024, 8]); ident_d = P.dram_in("ident", [128, 128])
    if final:
        fn_d = P.dram_in("fn", [128, 8]); outT = P.dram_out("outT", [1024, TC])
    h2T = P.dram_out("h2T", [1024, TC])
    ones = P.sb([128, 128], F32, "ones"); P.add("pool", lambda e: e.memset(ones[:], 1.0), w=["ones"])
    epsb = P.sb([128, 1], F32, "epsb"); P.add("pool", lambda e: e.memset(epsb[:], EPS), w=["epsb"])
    modt = P.sb([128, 6, ntiles, 8], F32, "modt")
    P.dma("sp", modt[:].rearrange("p a b c -> p (a b c)"), modt_d[:, :], w=["modt"])
    gn = P.sb([128, 8], F32, "gn"); P.dma("sp", gn[:], gn_d[:, :], w=["gn"])
    A2 = P.sb([128, ntiles, 8], F32, "A2")
    for t in range(ntiles):
        P.add("dve", lambda e, t=t: e.scalar_tensor_tensor(out=A2[:, t, :], in0=modt[:, 4, t, :], scalar=1.0, in1=gn[:, :], op0=ALU.add, op1=ALU.mult), r=["modt", "gn"], w=["modc"])
    wo = P.sb([128, 8, 1024], BF16, "wo")
    for k in range(8):
        P.dma("pool", wo[:, k, :], wout[k * 128:(k + 1) * 128, :], w=["wo"])
    BIG = P.sb([128, 8 * TC], F32, "yacc")
    yacc = BIG[:, :].rearrange("p (d t) -> p d t", d=8)

    def carve(i):
        return BIG[:, i * 2048:(i + 1) * 2048].rearrange("p (k t) -> p k t", k=8)
    ht, h1, sq, tmp, uf = carve(0), carve(1), carve(2), carve(3), carve(4)
    rs = BIG[:, 5 * 2048:5 * 2048 + 256]
    mixb = P.sb([128, 8, 256], BF16, "mixb")
    uall = P.sb([128, 8, TC], BF16, "uall")
    h1b = P.sb([128, 8, 256], F32, "h1b"); rsb = P.sb([128, 256], F32, "rsb")
    ssps = P.ps([128, 512], F32, "ssps"); misc = P.ps([128, 512], F32, "misc")
    if moe:
        wr = P.sb([128, 8, 8], F32, "wr"); P.dma("sp", wr[:], wr_d.rearrange("(k p) e -> p k e", p=128), w=["wr"])
        ident = P.sb([128, 128], F32, "ident"); P.dma("sp", ident[:], ident_d[:, :], w=["ident"])
        lg = P.sb([128, 2, 8], F32, "lg"); l2 = P.sb([128, 2, 8], F32, "l2"); mk1 = P.sb([128, 2, 8], F32, "mk1"); mk2 = P.sb([128, 2, 8], F32, "mk2")
        comb = P.sb([128, NS, 8], F32, "comb"); m12 = P.sb([128, 2, 4], F32, "m12")
        rep = Rot([P.sb([128, 128], F32, f"rep{i}") for i in range(2)], "rep")
        cB = P.sb([128, TC], F32, "cB")
        lgps = ssps[:, 384:400].rearrange("p (s e) -> p s e", e=8)
    if final:
        fng = P.sb([128, 8], F32, "fng"); P.dma("sp", fng[:], fn_d[:, :], w=["fng"])
    hv = hT.rearrange("(k p) t -> p k t", p=128); mv = mixT.rearrange("(k p) t -> p k t", p=128); ov = h2T.rearrange("(k p) t -> p k t", p=128)

    def add1(eng, fn, r=(), w=()):
        return P.add(eng, fn, list(r) + ["BIG"], w)

    def phase1(ti, t0):
        P.dma("sp", ht[:, :, :n], hv[:, :, t0:t0 + n], r=["BIG"], w=["ht"])
        P.dma("pool", mixb[:, :, :n], mv[:, :, t0:t0 + n], w=["mixb"])
        for dm in range(8):
            for k in range(8):
                add1("pe", lambda e, k=k, dm=dm: e.matmul(misc[:, 0:n], wo[:, k, dm * 128:(dm + 1) * 128], mixb[:, k, :n], start=(k == 0), stop=(k == 7)), r=["wo", "mixb"], w=["misc"])
            add1("dve", lambda e, dm=dm: e.scalar_tensor_tensor(out=h1[:, dm, :n], in0=misc[:, 0:n], scalar=modt[:, 2, ti, dm:dm + 1], in1=ht[:, dm, :n], op0=ALU.mult, op1=ALU.add),
                 r=["misc", "ht", "modt"], w=["h1", "misc"])
        P.dma("sp", ov[:, :, t0:t0 + n], h1[:, :, :n], r=["h1", "BIG"], w=[("h1d", ti)])
        add1("act", lambda e: e.activation(out=sq[:, :, :n], in_=h1[:, :, :n], func=AF.Square), r=["h1"], w=["sq"])
        for k in range(8):
            add1("pe", lambda e, k=k: e.matmul(ssps[:, :n], ones[:, :], sq[:, k, :n], start=(k == 0), stop=(k == 7)), r=["sq", "ones"], w=["ssps"])
        add1("act", lambda e: e.activation(out=rs[:, :n], in_=ssps[:, :n], func=AF.Sqrt, scale=1.0 / 1024, bias=epsb[:, 0:1]), r=["ssps", "epsb"], w=["rs", "ssps"])
        add1("dve", lambda e: e.reciprocal(out=rs[:, :n], in_=rs[:, :n]), r=["rs"], w=["rs"])
        for k in range(8):
            add1("dve", lambda e, k=k: e.tensor_tensor(out=tmp[:, k, :n], in0=h1[:, k, :n], in1=rs[:, :n], op=ALU.mult), r=["h1", "rs"], w=[("tmp", k)])
            if moe:
                add1("act", lambda e, k=k: e.activation(out=uf[:, k, :n], in_=tmp[:, k, :n], func=AF.Identity, scale=A2[:, ti, k:k + 1], bias=modt[:, 3, ti, k:k + 1]),
                     r=[("tmp", k), "modc", "modt"], w=[("uf", k)])
                add1("pool", lambda e, k=k: e.tensor_copy(out=uall[:, k, t0:t0 + n], in_=uf[:, k, :n]), r=[("uf", k)], w=["uall"])
            else:
                add1("act", lambda e, k=k: e.activation(out=uall[:, k, t0:t0 + n], in_=tmp[:, k, :n], func=AF.Identity, scale=A2[:, ti, k:k + 1], bias=modt[:, 3, ti, k:k + 1]),
                     r=[("tmp", k), "modc", "modt"], w=["uall"])
        if moe:
            for s in range(2):
                for k in range(8):
                    add1("pe", lambda e, k=k, s=s: e.matmul(lgps[:, s, :], uf[:, k, s * 128:(s + 1) * 128], wr[:, k, :], start=(k == 0), stop=(k == 7)),
                         r=[("uf", kk) for kk in range(8)] + ["wr"], w=["ssps"])
            add1("dve", lambda e: e.tensor_copy(out=lg[:, :, :], in_=lgps[:, :, :]), r=["ssps"], w=["lg", "ssps"])
            for s in range(2):
                gs = ti * 2 + s
                add1("dve", lambda e, s=s: e.reduce_max(out=m12[:, s, 0:1], in_=lg[:, s, :], axis=AX.X), r=["lg"], w=["m12"])
                add1("dve", lambda e, s=s: e.tensor_scalar(out=mk1[:, s, :], in0=lg[:, s, :], scalar1=m12[:, s, 0:1], scalar2=None, op0=ALU.is_equal), r=["lg", "m12"], w=["mk1"])
                add1("dve", lambda e, s=s: e.scalar_tensor_tensor(out=l2[:, s, :], in0=mk1[:, s, :], scalar=-1e30, in1=lg[:, s, :], op0=ALU.mult, op1=ALU.add), r=["mk1", "lg"], w=["l2"])
                add1("dve", lambda e, s=s: e.reduce_max(out=m12[:, s, 1:2], in_=l2[:, s, :], axis=AX.X), r=["l2", "m12"], w=["m12"])
                add1("dve", lambda e, s=s: e.tensor_scalar(out=mk2[:, s, :], in0=l2[:, s, :], scalar1=m12[:, s, 1:2], scalar2=None, op0=ALU.is_equal), r=["l2", "m12"], w=["mk2"])
                add1("dve", lambda e, s=s: e.tensor_tensor(out=m12[:, s, 2:3], in0=m12[:, s, 1:2], in1=m12[:, s, 0:1], op=ALU.subtract), r=["m12"], w=["m12"])
                add1("act", lambda e, s=s: e.activation(out=m12[:, s, 2:3], in_=m12[:, s, 2:3], func=AF.Exp), r=["m12"], w=["m12"])
                add1("dve", lambda e, s=s: e.tensor_scalar(out=m12[:, s, 3:4], in0=m12[:, s, 2:3], scalar1=1.0, scalar2=None, op0=ALU.add), r=["m12"], w=["m12"])
                add1("dve", lambda e, s=s: e.reciprocal(out=m12[:, s, 3:4], in_=m12[:, s, 3:4]), r=["m12"], w=["m12"])
                add1("dve", lambda e, s=s: e.tensor_tensor(out=m12[:, s, 2:3], in0=m12[:, s, 2:3], in1=m12[:, s, 3:4], op=ALU.mult), r=["m12"], w=["m12"])
                add1("dve", lambda e, s=s, gs=gs: e.tensor_scalar(out=comb[:, gs, :], in0=mk1[:, s, :], scalar1=m12[:, s, 3:4], scalar2=None, op0=ALU.mult), r=["mk1", "m12"], w=["comb"])
                add1("dve", lambda e, s=s, gs=gs: e.scalar_tensor_tensor(out=comb[:, gs, :], in0=mk2[:, s, :], scalar=m12[:, s, 2:3], in1=comb[:, gs, :], op0=ALU.mult, op1=ALU.add),
                     r=["mk2", "m12", "comb"], w=["comb"])

    for ti, (t0, _) in enumerate(CT):
        phase1(ti, t0)

    P.add("pool", lambda e: e.memset(BIG[:, :], 0.0), w=["BIG", "yacc"])
    hh = Rot([P.sb([128, 4, 256], BF16, f"hh{i}") for i in range(2)], "hh")
    sg = Rot([P.sb([128, 256], F32, f"sg{i}") for i in range(2)], "sg")
    t1 = Rot([P.sb([128, 256], F32, f"t1{i}") for i in range(2)], "t1")
    wgs = Rot([P.sb([128, 8, 512], BF16, f"wgs{i}") for i in range(2)], "wgs")
    wus = Rot([P.sb([128, 8, 512], BF16, f"wus{i}") for i in range(2)], "wus")
    wds = Rot([P.sb([128, 4, 1024], BF16, f"wds{i}") for i in range(2)], "wds")
    gups = Rot([P.ps([128, 2, 256], F32, f"gups{i}") for i in range(2)], "gups")
    yps = [P.ps([128, 2, 256], F32, f"yps{i}") for i in range(4)]

    def gate_up(ex, wgt, wgk, wut, wuk, t0):
        hht, hhk = hh.next()
        for f in range(4):
            gp, gk = gups.next()
            for k in range(8):
                P.add("pe", lambda e, k=k, f=f, gp=gp: e.matmul(gp[:, 0, :n], wgt[:, k, f * 128:(f + 1) * 128], uall[:, k, t0:t0 + n], start=(k == 0), stop=(k == 7)), r=["uall", wgk], w=[gk])
            for k in range(8):
                P.add("pe", lambda e, k=k, f=f, gp=gp: e.matmul(gp[:, 1, :n], wut[:, k, f * 128:(f + 1) * 128], uall[:, k, t0:t0 + n], start=(k == 0), stop=(k == 7)), r=["uall", wuk], w=[gk])
            sgt, sgk = sg.next()
            P.add("act", lambda e, gp=gp, sgt=sgt: e.activation(out=sgt[:, :n], in_=gp[:, 0, :n], func=AF.Silu), r=[gk], w=[sgk, gk])
            if moe:
                tt, tk = t1.next()
                P.add("dve", lambda e, gp=gp, sgt=sgt, tt=tt: e.tensor_tensor(out=tt[:, :n], in0=sgt[:, :n], in1=gp[:, 1, :n], op=ALU.mult), r=[gk, sgk], w=[tk, gk])
                P.add("pool", lambda e, tt=tt, f=f: e.tensor_tensor(out=hht[:, f, :n], in0=tt[:, :n], in1=cB[:, t0:t0 + n], op=ALU.mult), r=[tk, "cB"], w=[(hhk, f)])
            else:
                P.add("dve", lambda e, gp=gp, sgt=sgt, f=f: e.tensor_tensor(out=hht[:, f, :n], in0=sgt[:, :n], in1=gp[:, 1, :n], op=ALU.mult), r=[gk, sgk], w=[(hhk, f), gk])
        return hht, hhk

    def down(hht, hhk, wdt, wdk, t0):
        for dm in range(8):
            for f in range(4):
                P.add("pe", lambda e, f=f, dm=dm: e.matmul(yps[dm // 2][:, dm % 2, :n], wdt[:, f, dm * 128:(dm + 1) * 128], hht[:, f, :n], start=(f == 0 and dm % 2 == 0), stop=(f == 3),
                                                           skip_group_check=True), r=[(hhk, f), wdk], w=[("yps", dm // 2)])
        for b in range(4):
            P.add("dve", lambda e, b=b: e.tensor_tensor(out=yacc[:, 2 * b:2 * b + 2, t0:t0 + n], in0=yacc[:, 2 * b:2 * b + 2, t0:t0 + n], in1=yps[b][:, :, :n], op=ALU.add),
                  r=[("yps", b), "yacc"], w=["yacc", ("yps", b)])

    for ex in range(NE):
        if moe:
            for s0 in range(0, NS, 4):
                ns_ = min(4, NS - s0)
                for s in range(ns_):
                    rp, rk = rep.next()
                    P.add("dve", lambda e, rp=rp, s=s, s0=s0, ex=ex: e.tensor_scalar(out=rp[:, :], in0=ones[:, :], scalar1=comb[:, s0 + s, ex:ex + 1], scalar2=None, op0=ALU.mult), r=["comb", "ones"], w=[rk])
                    P.add("pe", lambda e, rp=rp, s=s: e.matmul(misc[:, s * 128:(s + 1) * 128], rp[:, :], ident[:, :], start=True, stop=True), r=[rk, "ident"], w=["misc"])
                P.add("act", lambda e, s0=s0, ns_=ns_: e.copy(out=cB[:, s0 * 128:(s0 + ns_) * 128], in_=misc[:, :ns_ * 128]), r=["misc"], w=["cB", "misc"])
        for fg in range(NFG):
            wgt, wgk = wgs.next(); wut, wuk = wus.next(); wdt, wdk = wds.next()
            P.dma("pool", wgt[:], wg[ex].rearrange("(k p) f -> p k f", p=128)[:, :, fg * 512:(fg + 1) * 512], w=[wgk])
            P.dma("pool", wut[:], wu[ex].rearrange("(k p) f -> p k f", p=128)[:, :, fg * 512:(fg + 1) * 512], w=[wuk])
            P.dma("pool", wdt[:], wd[ex, fg * 512:(fg + 1) * 512, :].rearrange("(f p) d -> p f d", p=128), w=[wdk])
            pend = gate_up(ex, wgt, wgk, wut, wuk, CT[0][0])
            for ti, (t0, _) in enumerate(CT):
                nxt = gate_up(ex, wgt, wgk, wut, wuk, CT[ti + 1][0]) if ti + 1 < ntiles else None
                down(pend[0], pend[1], wdt, wdk, t0)
                pend = nxt

    def phase3(ti, t0):
        P.dma("sp", h1b[:, :, :n], ov[:, :, t0:t0 + n], r=[("h1d", ti)], w=["h1b"])
        for dm in range(8):
            P.add("dve", lambda e, dm=dm: e.scalar_tensor_tensor(out=yacc[:, dm, t0:t0 + n], in0=yacc[:, dm, t0:t0 + n], scalar=modt[:, 5, ti, dm:dm + 1], in1=h1b[:, dm, :n], op0=ALU.mult, op1=ALU.add),
                  r=["yacc", "h1b", "modt"], w=["yacc"])
        P.dma("sp", ov[:, :, t0:t0 + n], yacc[:, :, t0:t0 + n], r=["yacc", "h1b"], w=[("h1d", ti)])
        if final:
            P.add("act", lambda e: e.activation(out=h1b[:, :, :n], in_=yacc[:, :, t0:t0 + n], func=AF.Square), r=["yacc"], w=["h1b"])
            for k in range(8):
                P.add("pe", lambda e, k=k: e.matmul(ssps[:, :n], ones[:, :], h1b[:, k, :n], start=(k == 0), stop=(k == 7)), r=["h1b", "ones"], w=["ssps"])
            P.add("act", lambda e: e.activation(out=rsb[:, :n], in_=ssps[:, :n], func=AF.Sqrt, scale=1.0 / 1024, bias=epsb[:, 0:1]), r=["ssps", "epsb"], w=["rsb", "ssps"])
            P.add("dve", lambda e: e.reciprocal(out=rsb[:, :n], in_=rsb[:, :n]), r=["rsb"], w=["rsb"])
            for k in range(8):
                P.add("dve", lambda e, k=k: e.scalar_tensor_tensor(out=h1b[:, k, :n], in0=yacc[:, k, t0:t0 + n], scalar=fng[:, k:k + 1], in1=rsb[:, :n], op0=ALU.mult, op1=ALU.mult),
                      r=["yacc", "rsb", "fng", "ssps"], w=["h1b"])
            P.dma("sp", outT.rearrange("(k p) t -> p k t", p=128)[:, :, t0:t0 + n], h1b[:, :, :n], r=["h1b"])

    for ti, (t0, _) in enumerate(CT):
        phase3(ti, t0)
    print("C2 ops", P.n_ops())
    return P.finish()


NTOK = 4352
NCH = 34
QT = [(0, 256)] + [(256 + 512 * i, 512) for i in range(8)]


def bconsts(P):
    C = {}
    C["ones"] = P.sb([128, 128], F32, "ones")
    P.add("pool", lambda e: e.memset(C["ones"][:], 1.0), w=["ones"])
    C["epsb"] = P.sb([128, 1], F32, "epsb")
    P.add("pool", lambda e: e.memset(C["epsb"][:], EPS), w=["epsb"])
    return C


def emit_mla(P, C, D, mixT):
    scale = 96 ** -0.5
    qng = P.sb([128, 2], F32, "qng")
    P.dma("sp", qng[:], D["c_qng"][:, :], w=["qng"])
    kvg = P.sb([128, 1], F32, "kvg")
    P.dma("sp", kvg[:], D["c_kvg"][:, :], w=["kvg"])
    wq = P.sb([128, 2, 2, 96], BF16, "wq")
    wqP = P.sb([128, 2, 2, 96], BF16, "wqP")
    P.dma("pool", wq[:].rearrange("p k h c -> p k (h c)"), D["c_wq"].rearrange("(k p) c -> p k c", p=128), w=["wq"])
    P.dma("pool", wqP[:].rearrange("p k h c -> p k (h c)"), D["c_wqP"].rearrange("(k p) c -> p k c", p=128), w=["wqP"])
    wkn = P.sb([128, 2, 96], BF16, "wkn")
    P.dma("pool", wkn[:].rearrange("p h c -> p (h c)"), D["c_wkn"][:, :], w=["wkn"])
    wv = P.sb([128, 128], BF16, "wv")
    P.dma("pool", wv[:], D["c_wv"][:, :], w=["wv"])
    sel = P.sb([32, 96], BF16, "sel")
    P.dma("pool", sel[:], D["c_sel"][:, :], w=["sel"])
    qT = P.sb([96, 2, NTOK], BF16, "mqT")
    kT = P.sb([96, 2, NTOK], BF16, "mkT")
    vaug = P.sb([128, NCH, 2, 65], BF16, "mvaug")
    P.add("pool", lambda e: e.memset(vaug[:], 1.0), w=["mvaug"])
    ckvn = P.sb([128, NTOK], BF16, "ckvn")
    cq = P.sb([128, 2, 512], F32, "m_cq")
    ckv = P.sb([128, 512], F32, "m_ckv")
    sq = P.sb([128, 2, 512], F32, "m_sq")
    rs = P.sb([128, 512], F32, "m_rs")
    rs2 = P.sb([128, 512], F32, "m_rs2")
    cqn = P.sb([128, 2, 512], BF16, "m_cqn")
    ct = P.sb([96, 512], F32, "m_ct")
    st = P.sb([96, 512], F32, "m_st")
    kr = P.sb([32, 512], F32, "m_kr")
    krP = P.sb([32, 512], F32, "m_krP")
    krr = P.sb([32, 512], BF16, "m_krr")
    t1 = P.sb([96, 512], F32, "m_t1")
    t2 = P.sb([96, 512], F32, "m_t2")
    ssps = P.ps([128, 512], F32, "m_ssps")
    pA = P.ps([128, 512], F32, "m_pA")
    pB = P.ps([128, 512], F32, "m_pB")

    ct32 = P.sb([32, 512], F32, "m_ct32")
    st32 = P.sb([32, 512], F32, "m_st32")

    def tile_all(t0, n):
        P.dma("sp", cq[:, :, :n], D["c_cq"].rearrange("(k p) t -> p k t", p=128)[:, :, t0:t0 + n], w=["m_cq"])
        P.dma("sp", ckv[:, :n], D["c_ckv"][:, t0:t0 + n], w=["m_ckv"])
        P.dma("sp", ct[:, :n], D["c_ct96"][:, t0:t0 + n], w=["m_ct"])
        P.dma("sp", st[:, :n], D["c_st96"][:, t0:t0 + n], w=["m_st"])
        P.dma("sp", ct32[:, :n], D["c_ct96"][64:96, t0:t0 + n], w=["m_ct32"])
        P.dma("sp", st32[:, :n], D["c_st96"][64:96, t0:t0 + n], w=["m_st32"])
        P.dma("sp", kr[:, :n], D["c_kr"][:, t0:t0 + n], w=["m_kr"])
        P.dma("sp", krP[:, :n], D["c_krP"][:, t0:t0 + n], w=["m_krP"])
        P.add("act", lambda e: e.activation(out=sq[:, :, :n], in_=cq[:, :, :n], func=AF.Square), r=["m_cq"], w=["m_sq"])
        for k in range(2):
            P.add("pe", lambda e, k=k: e.matmul(ssps[:, :n], C["ones"][:, :], sq[:, k, :n], start=(k == 0), stop=(k == 1)), r=["m_sq", "ones"], w=["m_ssps"])
        P.add("act", lambda e: e.activation(out=rs[:, :n], in_=ssps[:, :n], func=AF.Sqrt, scale=1.0 / 256, bias=C["epsb"][:, 0:1]), r=["m_ssps", "epsb"], w=["m_rs"])
        P.add("dve", lambda e: e.reciprocal(out=rs[:, :n], in_=rs[:, :n]), r=["m_rs"], w=["m_rs"])
        for k in range(2):
            P.add("dve", lambda e, k=k: e.scalar_tensor_tensor(out=cqn[:, k, :n], in0=cq[:, k, :n], scalar=qng[:, k:k + 1], in1=rs[:, :n], op0=ALU.mult, op1=ALU.mult),
                  r=["m_cq", "qng", "m_rs"], w=["m_cqn"])
        P.add("act", lambda e: e.activation(out=sq[:, 0, :n], in_=ckv[:, :n], func=AF.Square), r=["m_ckv"], w=["m_sq"])
        P.add("pe", lambda e: e.matmul(ssps[:, :n], C["ones"][:, :], sq[:, 0, :n], start=True, stop=True), r=["m_sq", "ones"], w=["m_ssps"])
        P.add("act", lambda e: e.activation(out=rs2[:, :n], in_=ssps[:, :n], func=AF.Sqrt, scale=1.0 / 128, bias=C["epsb"][:, 0:1]), r=["m_ssps", "epsb"], w=["m_rs2"])
        P.add("dve", lambda e: e.reciprocal(out=rs2[:, :n], in_=rs2[:, :n]), r=["m_rs2"], w=["m_rs2"])
        P.add("dve", lambda e: e.scalar_tensor_tensor(out=ckvn[:, t0:t0 + n], in0=ckv[:, :n], scalar=kvg[:, 0:1], in1=rs2[:, :n], op0=ALU.mult, op1=ALU.mult),
              r=["m_ckv", "kvg", "m_rs2"], w=["ckvn"])
        P.add("pool", lambda e: e.tensor_tensor(out=kr[:, :n], in0=kr[:, :n], in1=ct32[:, :n], op=ALU.mult), r=["m_kr", "m_ct32"], w=["m_kr"])
        P.add("pool", lambda e: e.tensor_tensor(out=krP[:, :n], in0=krP[:, :n], in1=st32[:, :n], op=ALU.mult), r=["m_krP", "m_st32"], w=["m_krP"])
        P.add("pool", lambda e: e.tensor_tensor(out=krr[:, :n], in0=kr[:, :n], in1=krP[:, :n], op=ALU.add), r=["m_kr", "m_krP"], w=["m_krr"])
        for h in range(2):
            for k in range(2):
                P.add("pe", lambda e, k=k, h=h: e.matmul(pA[:96, :n], wq[:, k, h, :], cqn[:, k, :n], start=(k == 0), stop=(k == 1)), r=["wq", "m_cqn"], w=["m_pA"])
            for k in range(2):
                P.add("pe", lambda e, k=k, h=h: e.matmul(pB[:96, :n], wqP[:, k, h, :], cqn[:, k, :n], start=(k == 0), stop=(k == 1)), r=["wqP", "m_cqn"], w=["m_pB"])
            P.add("dve", lambda e: e.tensor_tensor(out=t1[:, :n], in0=pA[:96, :n], in1=ct[:, :n], op=ALU.mult), r=["m_pA", "m_ct"], w=["m_t1"])
            P.add("dve", lambda e: e.tensor_tensor(out=t2[:, :n], in0=pB[:96, :n], in1=st[:, :n], op=ALU.mult), r=["m_pB", "m_st"], w=["m_t2"])
            P.add("pool", lambda e, h=h: e.tensor_tensor(out=qT[:, h, t0:t0 + n], in0=t1[:, :n], in1=t2[:, :n], op=ALU.add), r=["m_t1", "m_t2"], w=["mqT"])
            P.add("pe", lambda e, h=h: e.matmul(pA[:96, :n], wkn[:, h, :], ckvn[:, t0:t0 + n], start=True, stop=False), r=["wkn", "ckvn"], w=["m_pA"])
            P.add("pe", lambda e, h=h: e.matmul(pA[:96, :n], sel[:, :], krr[:, :n], start=False, stop=True), r=["sel", "m_krr"], w=["m_pA"])
            P.add("act", lambda e, h=h: e.copy(out=kT[:, h, t0:t0 + n], in_=pA[:96, :n]), r=["m_pA"], w=["mkT"])
        for s in range(n // 128):
            c = (t0 + s * 128) // 128
            P.add("pe", lambda e, s=s: e.matmul(pB[:, 0:128], ckvn[:, t0 + s * 128:t0 + (s + 1) * 128], wv[:, :], start=True, stop=True), r=["ckvn", "wv"], w=["m_pB"])
            P.add("act", lambda e, c=c: e.copy(out=vaug[:, c, :, 0:64], in_=pB[:, 0:128].rearrange("p (h d) -> p h d", h=2)), r=["m_pB"], w=["mvaug"])

    for (t0, n) in QT:
        tile_all(t0, n)

    sps = Rot([P.ps([128, 512], F32, f"m_sps{i}") for i in range(3)], "m_sps")
    ops_ = Rot([P.ps([128, 512], F32, f"m_ops{i}") for i in range(2)], "m_ops")
    E = Rot([P.sb([128, 512], BF16, f"m_E{i}") for i in range(3)], "m_E")
    oa = Rot([P.sb([65, 512], F32, f"m_oa{i}") for i in range(2)], "m_oa")
    rc = Rot([P.sb([64, 512], F32, f"m_rc{i}") for i in range(2)], "m_rc")
    oo = Rot([P.sb([64, 512], F32, f"m_oo{i}") for i in range(2)], "m_oo")

    def attn_tile(h, t0, n, chunks):
        op_, ok = ops_.next()
        nchk = len(chunks)
        pend = []

        def issue_s(c):
            sp, sk = sps.next()
            P.add("pe", lambda e, sp=sp, c=c: e.matmul(sp[:, :n], kT[:, h, c * 128:(c + 1) * 128], qT[:, h, t0:t0 + n], start=True, stop=True), r=["mkT", "mqT"], w=[sk])
            pend.append((sp, sk))
        issue_s(chunks[0])
        for ci, c in enumerate(chunks):
            if ci + 1 < nchk:
                issue_s(chunks[ci + 1])
            sp, sk = pend.pop(0)
            Et, ek = E.next()
            P.add("act", lambda e, sp=sp, Et=Et: e.activation(out=Et[:, :n], in_=sp[:, :n], func=AF.Exp, scale=scale), r=[sk], w=[ek])
            P.add("pe", lambda e, Et=Et, c=c, ci=ci: e.matmul(op_[:65, :n], vaug[:, c, h, :], Et[:, :n], start=(ci == 0), stop=(ci == nchk - 1)), r=[ek, "mvaug"], w=[ok])
        oat, oak = oa.next()
        P.add("act", lambda e: e.copy(out=oat[:, :n], in_=op_[:65, :n]), r=[ok], w=[oak])
        sp, sk = sps.next()
        P.add("pe", lambda e: e.matmul(sp[:64, :n], C["ones"][64:65, 0:64], oat[64:65, :n], start=True, stop=True), r=[oak, "ones"], w=[sk])
        rct, rck = rc.next()
        P.add("dve", lambda e: e.reciprocal(out=rct[:, :n], in_=sp[:64, :n]), r=[sk], w=[rck])
        oot, ook = oo.next()
        P.add("dve", lambda e: e.tensor_tensor(out=oot[:, :n], in0=oat[0:64, :n], in1=rct[:, :n], op=ALU.mult), r=[oak, rck], w=[ook])
        P.dma("sp", mixT[h * 64:(h + 1) * 64, t0:t0 + n], oot[:, :n], r=[ook])

    for h in range(2):
        attn_tile(h, 0, 256, [0, 1])
        for i in range(8):
            attn_tile(h, 256 + 512 * i, 512, list(range(NCH)))


MLA_IN = [("c_cq", [256, NTOK]), ("c_ckv", [128, NTOK]), ("c_kr", [32, NTOK]), ("c_krP", [32, NTOK]), ("c_ct96", [96, NTOK]), ("c_st96", [96, NTOK]),
          ("c_qng", [128, 2]), ("c_kvg", [128, 1]), ("c_wq", [256, 192]), ("c_wqP", [256, 192]), ("c_wkn", [128, 192]), ("c_wv", [128, 128]), ("c_sel", [32, 96])]


def build_mla():
    P = Prog()
    D = {nm: P.dram_in(nm, shp) for nm, shp in MLA_IN}
    out = P.dram_out("mix", [128, NTOK])
    C = bconsts(P)
    emit_mla(P, C, D, out)
    print("mla ops", P.n_ops())
    return P.finish()


SWA_IN = [("a_q", [128, NTOK]), ("a_qP", [128, NTOK]), ("a_k", [64, NTOK]), ("a_kP", [64, NTOK]), ("a_vtok", [NTOK, 64]),
          ("a_cos", [64, NTOK]), ("a_sin", [64, NTOK]), ("a_sink", [128, 2]), ("a_maskP", [128, 128]), ("a_maskN", [128, 128])]


def build_swa():
    P = Prog()
    D = {nm: P.dram_in(nm, shp) for nm, shp in SWA_IN}
    out = P.dram_out("mix", [128, NTOK])
    C = bconsts(P)
    scale = 64 ** -0.5
    aqT = P.sb([64, 2, NTOK], BF16, "aqT")
    akT = P.sb([64, NTOK], BF16, "akT")
    vaug = P.sb([128, NCH, 65], BF16, "avaug")
    P.add("pool", lambda e: e.memset(vaug[:], 1.0), w=["avaug"])
    P.dma("pool", vaug[:, :, 0:64], D["a_vtok"].rearrange("(c p) d -> p c d", p=128), w=["avaug"])
    es = P.sb([128, 2], F32, "a_es")
    P.dma("sp", es[:], D["a_sink"][:, :], w=["a_es"])
    P.add("act", lambda e: e.activation(out=es[:], in_=es[:], func=AF.Exp), r=["a_es"], w=["a_es"])
    mP = P.sb([128, 128], BF16, "a_mP")
    mN = P.sb([128, 128], BF16, "a_mN")
    P.dma("pool", mP[:], D["a_maskP"][:, :], w=["a_mP"])
    P.dma("pool", mN[:], D["a_maskN"][:, :], w=["a_mN"])
    q = P.sb([64, 2, 512], F32, "a_q")
    qP = P.sb([64, 2, 512], F32, "a_qP")
    k = P.sb([64, 512], F32, "a_k")
    kP = P.sb([64, 512], F32, "a_kP")
    cs = P.sb([64, 512], F32, "a_cs")
    sn = P.sb([64, 512], F32, "a_sn")
    t1 = P.sb([64, 512], F32, "a_t1")
    t2 = P.sb([64, 512], F32, "a_t2")

    def rope_tile(t0, n):
        P.dma("sp", q[:, :, :n], D["a_q"].rearrange("(h d) t -> d h t", d=64)[:, :, t0:t0 + n], w=["a_q"])
        P.dma("sp", qP[:, :, :n], D["a_qP"].rearrange("(h d) t -> d h t", d=64)[:, :, t0:t0 + n], w=["a_qP"])
        P.dma("sp", k[:, :n], D["a_k"][:, t0:t0 + n], w=["a_k"])
        P.dma("sp", kP[:, :n], D["a_kP"][:, t0:t0 + n], w=["a_kP"])
        P.dma("sp", cs[:, :n], D["a_cos"][:, t0:t0 + n], w=["a_cs"])
        P.dma("sp", sn[:, :n], D["a_sin"][:, t0:t0 + n], w=["a_sn"])
        for h in range(2):
            P.add("dve", lambda e, h=h: e.tensor_tensor(out=t1[:, :n], in0=q[:, h, :n], in1=cs[:, :n], op=ALU.mult), r=["a_q", "a_cs"], w=["a_t1"])
            P.add("pool", lambda e, h=h: e.tensor_tensor(out=t2[:, :n], in0=qP[:, h, :n], in1=sn[:, :n], op=ALU.mult), r=["a_qP", "a_sn"], w=["a_t2"])
            P.add("dve", lambda e, h=h: e.tensor_tensor(out=aqT[:, h, t0:t0 + n], in0=t1[:, :n], in1=t2[:, :n], op=ALU.add), r=["a_t1", "a_t2"], w=["aqT"])
        P.add("dve", lambda e: e.tensor_tensor(out=t1[:, :n], in0=k[:, :n], in1=cs[:, :n], op=ALU.mult), r=["a_k", "a_cs"], w=["a_t1"])
        P.add("pool", lambda e: e.tensor_tensor(out=t2[:, :n], in0=kP[:, :n], in1=sn[:, :n], op=ALU.mult), r=["a_kP", "a_sn"], w=["a_t2"])
        P.add("dve", lambda e: e.tensor_tensor(out=akT[:, t0:t0 + n], in0=t1[:, :n], in1=t2[:, :n], op=ALU.add), r=["a_t1", "a_t2"], w=["akT"])

    for (t0, n) in QT:
        rope_tile(t0, n)

    sp = [P.ps([128, 512], F32, f"a_sp{i}") for i in range(3)]
    ops_ = Rot([P.ps([128, 512], F32, f"a_op{i}") for i in range(2)], "a_op")
    bc = P.ps([128, 512], F32, "a_bc")
    E = Rot([P.sb([128, 5 * 256], BF16, f"a_E{i}") for i in range(2)], "a_E")
    oa = Rot([P.sb([65, 256], F32, f"a_oa{i}") for i in range(2)], "a_oa")
    rc = Rot([P.sb([64, 256], F32, f"a_rc{i}") for i in range(2)], "a_rc")
    oo = Rot([P.sb([64, 256], F32, f"a_oo{i}") for i in range(2)], "a_oo")
    outv = out.rearrange("(h d) t -> d h t", d=64)

    def block(q0, chunks):
        nck = len(chunks)
        for ci, (c, mk) in enumerate(chunks):
            b, off = ci // 2, (ci % 2) * 256
            P.add("pe", lambda e, b=b, off=off, c=c: e.matmul(sp[b][:, off:off + 256].rearrange("p (h q) -> p h q", h=2), akT[:, c * 128:(c + 1) * 128],
                                                             aqT[:, :, q0:q0 + 128], start=True, stop=True), r=["akT", "aqT"], w=[("a_sp", b)])
        Et, ek = E.next()
        for b in range((nck + 1) // 2):
            w_ = min(512, nck * 256 - b * 512)
            P.add("act", lambda e, b=b, w_=w_: e.activation(out=Et[:, b * 512:b * 512 + w_], in_=sp[b][:, :w_], func=AF.Exp, scale=scale), r=[("a_sp", b)], w=[ek])
        for ci, (c, mk) in enumerate(chunks):
            if mk is None:
                continue
            m = mP if mk == "P" else mN
            for h in range(2):
                o_ = ci * 256 + h * 128
                P.add("pool", lambda e, o_=o_, m=m: e.tensor_tensor(out=Et[:, o_:o_ + 128], in0=Et[:, o_:o_ + 128], in1=m[:, :], op=ALU.mult), r=[ek, "a_mP", "a_mN"], w=[ek])
        op_, ok = ops_.next()
        for ci, (c, mk) in enumerate(chunks):
            P.add("pe", lambda e, ci=ci, c=c: e.matmul(op_[:65, :256], vaug[:, c, :], Et[:, ci * 256:(ci + 1) * 256], start=(ci == 0), stop=(ci == nck - 1)), r=[ek, "avaug"], w=[ok])
        oat, oak = oa.next()
        P.add("act", lambda e: e.copy(out=oat[:, :], in_=op_[:65, :256]), r=[ok], w=[oak])
        for h in range(2):
            P.add("dve", lambda e, h=h: e.tensor_scalar(out=oat[64:65, h * 128:(h + 1) * 128], in0=oat[64:65, h * 128:(h + 1) * 128], scalar1=es[64:65, h:h + 1], scalar2=None, op0=ALU.add),
                  r=[oak, "a_es"], w=[oak])
        P.add("pe", lambda e: e.matmul(bc[:64, :256], C["ones"][64:65, 0:64], oat[64:65, :], start=True, stop=True), r=[oak, "ones"], w=["a_bc"])
        rct, rck = rc.next()
        P.add("dve", lambda e: e.reciprocal(out=rct[:, :], in_=bc[:64, :256]), r=["a_bc"], w=[rck])
        oot, ook = oo.next()
        P.add("dve", lambda e: e.tensor_tensor(out=oot[:, :], in0=oat[0:64, :], in1=rct[:, :], op=ALU.mult), r=[oak, rck], w=[ook])
        P.dma("sp", outv[:, :, q0:q0 + 128], oot[:, :].rearrange("d (h q) -> d h q", h=2), r=[ook])

    block(0, [(0, None), (1, None)])
    block(128, [(0, None), (1, None)])
    for nb in range(32):
        ch = [(0, None), (1, None)]
        if nb > 0:
            ch.append((nb + 1, "P"))
        ch.append((nb + 2, None))
        if nb < 31:
            ch.append((nb + 3, "N"))
        block(256 + 128 * nb, ch)
    print("swa ops", P.n_ops())
    return P.finish()


RET_IN = [("d_q", [128, NTOK]), ("d_qP", [128, NTOK]), ("d_k", [128, NTOK]), ("d_kP", [128, NTOK]), ("d_gate", [128, NTOK]),
          ("d_vtok", [NTOK, 128]), ("d_ktok", [NTOK, 128]), ("d_kPtok", [NTOK, 128]), ("d_cos", [64, NTOK]), ("d_sin", [64, NTOK]),
          ("d_costok", [NTOK, 64]), ("d_sintok", [NTOK, 64]), ("d_ldp", [128, 2]), ("d_ldr", [128, 4]), ("d_g", [128, 1]),
          ("d_relu", [128, 128]), ("d_rell", [128, 128]), ("d_um", [128, 128]), ("d_lm", [128, 128]), ("d_pos1", [128, 128]), ("d_posr", [128, 128]),
          ("d_pk", [128, 2]), ("d_bd", [128, 128]), ("d_bd64", [128, 128])]


def build_ret():
    P = Prog()
    D = {nm: P.dram_in(nm, shp) for nm, shp in RET_IN}
    out = P.dram_out("mix", [128, NTOK])
    C = bconsts(P)

    def ld(nm, shp, dt=F32, q="sp"):
        t = P.sb(shp, dt, "r_" + nm)
        P.dma(q, t[:], D[nm][:, :], w=["r_" + nm])
        return t
    ldp = ld("d_ldp", [128, 2]); ldr = ld("d_ldr", [128, 4]); g = ld("d_g", [128, 1])
    relu = ld("d_relu", [128, 128]); rell = ld("d_rell", [128, 128]); um = ld("d_um", [128, 128]); lm = ld("d_lm", [128, 128])
    pos1 = ld("d_pos1", [128, 128]); posr = ld("d_posr", [128, 128]); pk = ld("d_pk", [128, 2]); bd = ld("d_bd", [128, 128]); bd64 = ld("d_bd64", [128, 128])
    P.add("act", lambda e: e.activation(out=ldp[:], in_=ldp[:], func=AF.Exp), r=["r_d_ldp"], w=["r_d_ldp"])
    P.add("dve", lambda e: e.tensor_scalar(out=ldp[:], in0=ldp[:], scalar1=-1.0, scalar2=None, op0=ALU.mult), r=["r_d_ldp"], w=["r_d_ldp"])
    P.add("act", lambda e: e.activation(out=ldr[:], in_=ldr[:], func=AF.Exp), r=["r_d_ldr"], w=["r_d_ldr"])
    P.add("dve", lambda e: e.tensor_scalar(out=ldr[:], in0=ldr[:], scalar1=-1.0, scalar2=None, op0=ALU.mult), r=["r_d_ldr"], w=["r_d_ldr"])
    Qd = P.sb([128, 2, 128], F32, "r_Qd")
    for d, pt in ((0, pos1), (1, posr)):
        P.add("act", lambda e, d=d, pt=pt: e.activation(out=Qd[:, d, :], in_=pt[:, :], func=AF.Exp, scale=ldp[:, d:d + 1]), r=["r_d_ldp", "r_d_pos1", "r_d_posr"], w=["r_Qd"])
    P.add("dve", lambda e: e.tensor_scalar(out=Qd[:], in0=Qd[:], scalar1=0.125, scalar2=None, op0=ALU.mult), r=["r_Qd"], w=["r_Qd"])
    c128 = P.sb([128, 1], F32, "r_c128")
    P.add("pool", lambda e: e.memset(c128[:], 128.0), w=["r_c128"])
    cd = P.sb([128, 2], F32, "r_cd")
    for d in range(2):
        P.add("act", lambda e, d=d: e.activation(out=cd[:, d:d + 1], in_=c128[:, :], func=AF.Exp, scale=ldp[:, d:d + 1]), r=["r_d_ldp", "r_c128"], w=["r_cd"])
    Kd = P.sb([128, 2, 2], F32, "r_Kd")
    for d in range(2):
        P.add("act", lambda e, d=d: e.activation(out=Kd[:, d, :], in_=ldr[:, 2 * d:2 * d + 2], func=AF.Exp, scale=pk[:, d:d + 1]), r=["r_d_ldr", "r_d_pk"], w=["r_Kd"])
    DT = P.sb([128, 2, 128], F32, "r_DT")
    dt2 = P.sb([128, 128], F32, "r_dt2")
    for h in range(2):
        P.add("act", lambda e, h=h: e.activation(out=DT[:, h, :], in_=relu[:, :], func=AF.Exp, scale=ldr[:, h:h + 1]), r=["r_d_ldr", "r_d_relu"], w=["r_DT"])
        P.add("dve", lambda e, h=h: e.tensor_tensor(out=DT[:, h, :], in0=DT[:, h, :], in1=um[:, :], op=ALU.mult), r=["r_DT", "r_d_um"], w=["r_DT"])
        P.add("act", lambda e, h=h: e.activation(out=dt2[:, :], in_=rell[:, :], func=AF.Exp, scale=ldr[:, 2 + h:3 + h]), r=["r_d_ldr", "r_d_rell"], w=["r_dt2"])
        P.add("dve", lambda e, h=h: e.tensor_tensor(out=dt2[:, :], in0=dt2[:, :], in1=lm[:, :], op=ALU.mult), r=["r_dt2", "r_d_lm"], w=["r_dt2"])
        P.add("dve", lambda e, h=h: e.tensor_tensor(out=DT[:, h, :], in0=DT[:, h, :], in1=dt2[:, :], op=ALU.add), r=["r_DT", "r_dt2"], w=["r_DT"])
    P.add("dve", lambda e: e.tensor_scalar(out=DT[:], in0=DT[:], scalar1=0.125, scalar2=None, op0=ALU.mult), r=["r_DT"], w=["r_DT"])

    qdf = P.sb([128, NCH, 128], BF16, "r_qdf"); qdr = P.sb([128, NCH, 128], BF16, "r_qdr")
    qTb = P.sb([128, 2, NTOK], BF16, "r_qTb"); kTb = P.sb([128, NTOK], BF16, "r_kTb")
    P.add("pool", lambda e: e.memset(qTb[:], 0.0), w=["r_qTb"])
    kdf = P.sb([128, NCH, 128], BF16, "r_kdf"); kdr = P.sb([128, NCH, 128], BF16, "r_kdr")
    vt = P.sb([128, NCH, 128], BF16, "r_vt")
    vpad = P.sb([128, NCH, 2, 128], BF16, "r_vpad")
    P.add("pool", lambda e: e.memset(vpad[:], 0.0), w=["r_vpad"])
    vv = D["d_vtok"].rearrange("(c p) d -> p c d", p=128)
    P.dma("pool", vt[:], vv, w=["r_vt"])
    P.dma("pool", vpad[:, :, 0, 0:64], vv[:, :, 0:64], w=["r_vpad"])
    P.dma("pool", vpad[:, :, 1, 64:128], vv[:, :, 64:128], w=["r_vpad"])
    q = P.sb([128, 512], F32, "r_q"); qP = P.sb([128, 512], F32, "r_qP"); k = P.sb([128, 512], F32, "r_k"); kP = P.sb([128, 512], F32, "r_kP")
    cs = P.sb([128, 512], F32, "r_cs"); sn = P.sb([128, 512], F32, "r_sn")
    t1 = P.sb([128, 512], F32, "r_t1"); t2 = P.sb([128, 512], F32, "r_t2"); qr = P.sb([128, 512], F32, "r_qr")
    kt = P.sb([128, 4, 128], F32, "r_kt"); kPt = P.sb([128, 4, 128], F32, "r_kPt"); ct = P.sb([128, 4, 64], F32, "r_ct"); st = P.sb([128, 4, 64], F32, "r_st")
    krt = P.sb([128, 4, 128], F32, "r_krt"); kt2 = P.sb([128, 4, 128], F32, "r_kt2")

    def prep(t0, n):
        ns = n // 128
        c0 = t0 // 128
        for nm, t in (("d_q", q), ("d_qP", qP), ("d_k", k), ("d_kP", kP)):
            P.dma("sp", t[:, :n], D[nm][:, t0:t0 + n], w=[t.name])
        for hh in range(2):
            P.dma("sp", cs[hh * 64:(hh + 1) * 64, :n], D["d_cos"][:, t0:t0 + n], w=[cs.name])
            P.dma("sp", sn[hh * 64:(hh + 1) * 64, :n], D["d_sin"][:, t0:t0 + n], w=[sn.name])
        P.add("dve", lambda e: e.tensor_tensor(out=t1[:, :n], in0=q[:, :n], in1=cs[:, :n], op=ALU.mult), r=[q.name, cs.name], w=["r_t1"])
        P.add("pool", lambda e: e.tensor_tensor(out=t2[:, :n], in0=qP[:, :n], in1=sn[:, :n], op=ALU.mult), r=[qP.name, sn.name], w=["r_t2"])
        P.add("dve", lambda e: e.tensor_tensor(out=qr[:, :n], in0=t1[:, :n], in1=t2[:, :n], op=ALU.add), r=["r_t1", "r_t2"], w=["r_qr"])
        P.add("act", lambda e: e.copy(out=qTb[0:64, 0, t0:t0 + n], in_=qr[0:64, :n]), r=["r_qr"], w=["r_qTb"])
        P.add("act", lambda e: e.copy(out=qTb[64:128, 1, t0:t0 + n], in_=qr[64:128, :n]), r=["r_qr"], w=["r_qTb"])
        for s in range(ns):
            P.add("dve", lambda e, s=s: e.tensor_tensor(out=qdf[:, c0 + s, :], in0=qr[:, s * 128:(s + 1) * 128], in1=Qd[:, 0, :], op=ALU.mult), r=["r_qr", "r_Qd"], w=["r_qdf"])
            P.add("pool", lambda e, s=s: e.tensor_tensor(out=qdr[:, c0 + s, :], in0=qr[:, s * 128:(s + 1) * 128], in1=Qd[:, 1, :], op=ALU.mult), r=["r_qr", "r_Qd"], w=["r_qdr"])
        P.add("dve", lambda e: e.tensor_tensor(out=t1[:, :n], in0=k[:, :n], in1=cs[:, :n], op=ALU.mult), r=[k.name, cs.name], w=["r_t1"])
        P.add("pool", lambda e: e.tensor_tensor(out=t2[:, :n], in0=kP[:, :n], in1=sn[:, :n], op=ALU.mult), r=[kP.name, sn.name], w=["r_t2"])
        P.add("dve", lambda e: e.tensor_tensor(out=kTb[:, t0:t0 + n], in0=t1[:, :n], in1=t2[:, :n], op=ALU.add), r=["r_t1", "r_t2"], w=["r_kTb"])
        P.dma("sp", kt[:, :ns, :], D["d_ktok"].rearrange("(c p) d -> p c d", p=128)[:, c0:c0 + ns, :], w=["r_kt"])
        P.dma("sp", kPt[:, :ns, :], D["d_kPtok"].rearrange("(c p) d -> p c d", p=128)[:, c0:c0 + ns, :], w=["r_kPt"])
        P.dma("sp", ct[:, :ns, :], D["d_costok"].rearrange("(c p) d -> p c d", p=128)[:, c0:c0 + ns, :], w=["r_ct"])
        P.dma("sp", st[:, :ns, :], D["d_sintok"].rearrange("(c p) d -> p c d", p=128)[:, c0:c0 + ns, :], w=["r_st"])
        for h in range(2):
            hs = slice(h * 64, (h + 1) * 64)
            P.add("dve", lambda e, hs=hs: e.tensor_tensor(out=krt[:, :ns, hs], in0=kt[:, :ns, hs], in1=ct[:, :ns, :], op=ALU.mult), r=["r_kt", "r_ct"], w=["r_krt"])
            P.add("pool", lambda e, hs=hs: e.tensor_tensor(out=kt2[:, :ns, hs], in0=kPt[:, :ns, hs], in1=st[:, :ns, :], op=ALU.mult), r=["r_kPt", "r_st"], w=["r_kt2"])
        P.add("dve", lambda e: e.tensor_tensor(out=krt[:, :ns, :], in0=krt[:, :ns, :], in1=kt2[:, :ns, :], op=ALU.add), r=["r_krt", "r_kt2"], w=["r_krt"])
        for h in range(2):
            hs = slice(h * 64, (h + 1) * 64)
            P.add("dve", lambda e, hs=hs, h=h: e.tensor_scalar(out=kdf[:, c0:c0 + ns, hs], in0=krt[:, :ns, hs], scalar1=Kd[:, 0, h:h + 1], scalar2=None, op0=ALU.mult), r=["r_krt", "r_Kd"], w=["r_kdf"])
            P.add("pool", lambda e, hs=hs, h=h: e.tensor_scalar(out=kdr[:, c0:c0 + ns, hs], in0=krt[:, :ns, hs], scalar1=Kd[:, 1, h:h + 1], scalar2=None, op0=ALU.mult), r=["r_krt", "r_Kd"], w=["r_kdr"])

    RS = 3
    for (t0, n) in QT:
        prep(t0, n)
    if RS < 2:
        return P.finish()

    Sf = P.sb([128, NCH, 128], BF16, "r_Sf"); Sr = P.sb([128, NCH, 128], BF16, "r_Sr")
    S = P.sb([128, 128], F32, "r_S")
    gp = Rot([P.ps([128, 512], F32, f"r_gp{i}") for i in range(2)], "r_gp")
    tg = Rot([P.sb([128, 128], F32, f"r_tg{i}") for i in range(2)], "r_tg")

    def scan(order, kd, kdkey, Sall, skey, d):
        P.add("pool", lambda e: e.memset(S[:], 0.0), r=[], w=["r_S"])
        P.add("pool", lambda e: e.memset(Sall[:, order[0], :], 0.0), w=[skey])
        for idx in range(len(order) - 1):
            c = order[idx]
            g_, gk = gp.next()
            P.add("pe", lambda e, c=c, g_=g_: e.matmul(g_[:, 0:128], kd[:, c, :], vt[:, c, :], start=True, stop=True), r=[kdkey, "r_vt"], w=[gk])
            tg_, tk = tg.next()
            P.add("dve", lambda e, g_=g_, tg_=tg_: e.tensor_tensor(out=tg_[:, :], in0=g_[:, 0:128], in1=bd[:, :], op=ALU.mult), r=[gk, "r_d_bd"], w=[tk])
            P.add("dve", lambda e, tg_=tg_: e.scalar_tensor_tensor(out=S[:, :], in0=S[:, :], scalar=cd[:, d:d + 1], in1=tg_[:, :], op0=ALU.mult, op1=ALU.add), r=[tk, "r_S", "r_cd"], w=["r_S"])
            P.add("act", lambda e, nx=order[idx + 1]: e.copy(out=Sall[:, nx, :], in_=S[:, :]), r=["r_S"], w=[skey])

    scan(list(range(NCH)), kdf, "r_kdf", Sf, "r_Sf", 0)
    scan([1, 0] + list(range(NCH - 1, 1, -1)), kdr, "r_kdr", Sr, "r_Sr", 1)

    if RS < 3:
        return P.finish()
    bp = Rot([P.ps([128, 512], F32, f"r_bp{i}") for i in range(2)], "r_bp")
    op_ = Rot([P.ps([128, 512], F32, f"r_op{i}") for i in range(2)], "r_op")
    mv = P.ps([128, 512], F32, "r_mv")
    AT = Rot([P.sb([128, 2, 128], BF16, f"r_AT{i}") for i in range(2)], "r_AT")
    osb = P.sb([128, 512], F32, "r_osb"); dd = P.sb([128, 512], F32, "r_dd"); sq = P.sb([128, 512], F32, "r_sq"); rs = P.sb([128, 512], F32, "r_rs")
    gt = P.sb([128, 512], F32, "r_gt"); yo = P.sb([128, 512], F32, "r_yo")

    def out_tile(t0, n):
        ns = n // 128
        c0 = t0 // 128
        o_, ok = op_.next()
        for s in range(ns):
            c = c0 + s
            b_, bk = bp.next()
            for h in range(2):
                hs = slice(h * 64, (h + 1) * 64)
                P.add("pe", lambda e, h=h, hs=hs, c=c, b_=b_: e.matmul(b_[:, h * 128:(h + 1) * 128], kTb[:, c * 128:(c + 1) * 128], qTb[:, h, c * 128:(c + 1) * 128], start=True, stop=True),
                      r=["r_kTb", "r_qTb"], w=[bk])
            at, ak = AT.next()
            P.add("dve", lambda e, b_=b_, at=at: e.tensor_tensor(out=at[:, :, :], in0=b_[:, 0:256].rearrange("p (h q) -> p h q", h=2), in1=DT[:, :, :], op=ALU.mult), r=[bk, "r_DT"], w=[ak])
            reg = o_[:, s * 128:(s + 1) * 128]
            P.add("pe", lambda e, reg=reg, c=c: e.matmul(reg, Sf[:, c, :], qdf[:, c, :], start=True, stop=False), r=["r_Sf", "r_qdf"], w=[ok])
            P.add("pe", lambda e, reg=reg, c=c: e.matmul(reg, Sr[:, c, :], qdr[:, c, :], start=False, stop=False), r=["r_Sr", "r_qdr"], w=[ok])
            P.add("pe", lambda e, reg=reg, c=c, at=at: e.matmul(reg, vpad[:, c, 0, :], at[:, 0, :], start=False, stop=False), r=["r_vpad", ak], w=[ok])
            P.add("pe", lambda e, reg=reg, c=c, at=at: e.matmul(reg, vpad[:, c, 1, :], at[:, 1, :], start=False, stop=True), r=["r_vpad", ak], w=[ok])
        P.dma("sp", gt[:, :n], D["d_gate"][:, t0:t0 + n], w=["r_gt"])
        P.add("act", lambda e: e.copy(out=osb[:, :n], in_=o_[:, :n]), r=[ok], w=["r_osb"])
        P.add("pe", lambda e: e.matmul(mv[:, :n], bd64[:, :], osb[:, :n], start=True, stop=True), r=["r_d_bd64", "r_osb"], w=["r_mv"])
        P.add("dve", lambda e: e.tensor_tensor(out=dd[:, :n], in0=osb[:, :n], in1=mv[:, :n], op=ALU.subtract), r=["r_osb", "r_mv"], w=["r_dd"])
        P.add("act", lambda e: e.activation(out=sq[:, :n], in_=dd[:, :n], func=AF.Square), r=["r_dd"], w=["r_sq"])
        P.add("pe", lambda e: e.matmul(mv[:, :n], bd64[:, :], sq[:, :n], start=True, stop=True), r=["r_d_bd64", "r_sq"], w=["r_mv"])
        P.add("act", lambda e: e.activation(out=rs[:, :n], in_=mv[:, :n], func=AF.Sqrt, bias=C["epsb"][:, 0:1]), r=["r_mv", "epsb"], w=["r_rs"])
        P.add("dve", lambda e: e.reciprocal(out=rs[:, :n], in_=rs[:, :n]), r=["r_rs"], w=["r_rs"])
        P.add("dve", lambda e: e.tensor_tensor(out=dd[:, :n], in0=dd[:, :n], in1=rs[:, :n], op=ALU.mult), r=["r_dd", "r_rs"], w=["r_dd"])
        P.add("act", lambda e: e.activation(out=gt[:, :n], in_=gt[:, :n], func=AF.Silu), r=["r_gt"], w=["r_gt"])
        P.add("dve", lambda e: e.scalar_tensor_tensor(out=yo[:, :n], in0=dd[:, :n], scalar=g[:, 0:1], in1=gt[:, :n], op0=ALU.mult, op1=ALU.mult), r=["r_dd", "r_gt", "r_d_g"], w=["r_yo"])
        P.dma("sp", out[:, t0:t0 + n], yo[:, :n], r=["r_yo"])

    for (t0, n) in QT:
        out_tile(t0, n)
    print("ret ops", P.n_ops())
    return P.finish()


GDN_IN = [("b_q", [128, NTOK]), ("b_k", [128, NTOK]), ("b_v", [128, NTOK]), ("b_gate", [128, NTOK]), ("b_ab", [NTOK, 8]),
          ("b_cw", [64, 30]), ("b_dtb", [128, NCH * 8]), ("b_alog", [128, NCH * 8]), ("b_g", [64, 1]),
          ("b_triF", [128, 128]), ("b_triR", [128, 128]), ("b_ident", [128, 128]), ("b_um", [128, 128]), ("b_lm", [128, 128]),
          ("b_us", [128, 128]), ("b_ls", [128, 128])]


def build_gdn(nsteps=NCH, limit=10**9):
    P = Prog()
    _real_add = P.add
    _cnt = [0]
    _on = [False]

    def _ladd(eng, fn, r=(), w=()):
        if _on[0]:
            _cnt[0] += 1
            if _cnt[0] > limit:
                return None
        extra = [k for k in r if isinstance(k, tuple) and k[0] == 'bk' and k not in w]
        return _real_add(eng, fn, r, list(w) + extra)
    P.add = _ladd
    D = {nm: P.dram_in(nm, shp) for nm, shp in GDN_IN}
    out = P.dram_out("mix", [128, NTOK])
    C = bconsts(P)
    ones = C["ones"]

    def ld(nm, shp):
        t = P.sb(shp, F32, "g_" + nm)
        P.dma("sp", t[:], D[nm][:, :], w=["g_" + nm])
        return t
    cw = ld("b_cw", [64, 30])
    gg = ld("b_g", [64, 1])
    tri = [ld("b_triF", [128, 128]), ld("b_triR", [128, 128])]
    ident = ld("b_ident", [128, 128])
    msk = [ld("b_um", [128, 128]), ld("b_lm", [128, 128])]
    smsk = [ld("b_us", [128, 128]), ld("b_ls", [128, 128])]
    CK = ["g_b_triF", "g_b_triR", "g_b_ident", "g_b_um", "g_b_lm", "g_b_us", "g_b_ls", "ones"]
    banks = [P.ps([128, 512], F32, f"g_bk{j}") for j in range(8)]

    def X(j, r, rows=128, cols=128):
        return banks[j][0:rows, r * 128:r * 128 + cols]

    qn = P.sb([64, 2, NTOK], F32, "g_qn"); kn = P.sb([64, 2, NTOK], F32, "g_kn"); oacc = P.sb([64, 2, NTOK], F32, "g_oacc")
    ktok = P.sb([128, NCH, 128], F32, "g_ktok"); vtok = P.sb([128, NCH, 128], F32, "g_vtok")
    P.add("pool", lambda e: e.memset(oacc[:], 0.0), w=["g_oacc"])
    ab = P.sb([128, NCH * 8], F32, "g_ab"); dtb = ld("b_dtb", [128, NCH * 8]); alog = ld("b_alog", [128, NCH * 8])
    gtok = P.sb([128, NCH * 8], F32, "g_gtok"); btok = P.sb([128, NCH * 8], F32, "g_btok")
    P.dma("sp", ab[:].rearrange("p (c e) -> p c e", e=8), D["b_ab"].rearrange("(c p) e -> p c e", p=128), w=["g_ab"])
    P.add("act", lambda e: e.activation(out=btok[:], in_=ab[:], func=AF.Sigmoid), r=["g_ab"], w=["g_btok"])
    P.add("dve", lambda e: e.tensor_tensor(out=gtok[:], in0=ab[:], in1=dtb[:], op=ALU.add), r=["g_ab", "g_b_dtb"], w=["g_gtok"])
    P.add("act", lambda e: e.activation(out=gtok[:], in_=gtok[:], func=AF.Exp), r=["g_gtok"], w=["g_gtok"])
    P.add("act", lambda e: e.activation(out=gtok[:], in_=gtok[:], func=AF.Ln, bias=1.0), r=["g_gtok"], w=["g_gtok"])
    P.add("act", lambda e: e.activation(out=alog[:], in_=alog[:], func=AF.Exp), r=["g_b_alog"], w=["g_b_alog"])
    P.add("dve", lambda e: e.scalar_tensor_tensor(out=gtok[:], in0=gtok[:], scalar=-1.0, in1=alog[:], op0=ALU.mult, op1=ALU.mult), r=["g_gtok", "g_b_alog"], w=["g_gtok"])

    xr = P.sb([64, 2, 516], F32, "g_xr"); acc = P.sb([64, 2, 512], F32, "g_acc"); sq = P.sb([64, 2, 512], F32, "g_sq"); rs = P.sb([64, 2, 512], F32, "g_rs")

    def conv_tile(gi, src, t0, n):
        s0, s1 = (0, 256) if t0 < 256 else (256, NTOK)
        lo, hi = max(t0 - 2, s0), min(t0 + n + 2, s1)
        P.add("pool", lambda e: e.memset(xr[:], 0.0), w=["g_xr"])
        P.dma("sp", xr[:, :, lo - (t0 - 2):hi - (t0 - 2)], D[src].rearrange("(h d) t -> d h t", d=64)[:, :, lo:hi], w=["g_xr"])
        for h in range(2):
            eng = "dve"
            for tap in range(5):
                wcol = cw[:, gi * 10 + h * 5 + tap:gi * 10 + h * 5 + tap + 1]
                if tap == 0:
                    P.add(eng, lambda e, h=h, wcol=wcol: e.tensor_scalar(out=acc[:, h, :n], in0=xr[:, h, 0:n], scalar1=wcol, scalar2=None, op0=ALU.mult),
                          r=["g_xr", "g_b_cw"], w=[("g_acc", h)])
                else:
                    P.add(eng, lambda e, h=h, wcol=wcol, tap=tap: e.scalar_tensor_tensor(out=acc[:, h, :n], in0=xr[:, h, tap:tap + n], scalar=wcol, in1=acc[:, h, :n], op0=ALU.mult, op1=ALU.add),
                          r=["g_xr", "g_b_cw", ("g_acc", h)], w=[("g_acc", h)])
        P.add("act", lambda e: e.activation(out=acc[:, :, :n], in_=acc[:, :, :n], func=AF.Silu), r=[("g_acc", 0), ("g_acc", 1)], w=[("g_acc", 0), ("g_acc", 1)])

    def l2_tile(dst, dkey, t0, n, scl):
        P.add("act", lambda e: e.activation(out=sq[:, :, :n], in_=acc[:, :, :n], func=AF.Square), r=[("g_acc", 0), ("g_acc", 1)], w=["g_sq"])
        for h in range(2):
            P.add("pe", lambda e, h=h: e.matmul(banks[h][0:64, :n], ones[0:64, 0:64], sq[:, h, :n], start=True, stop=True), r=["g_sq", "ones"], w=[("bk", h)])
            P.add("act", lambda e, h=h: e.activation(out=rs[:, h, :n], in_=banks[h][0:64, :n], func=AF.Sqrt, bias=C["epsb"][0:64, 0:1]), r=[("bk", h), "epsb"], w=["g_rs"])
        P.add("dve", lambda e: e.reciprocal(out=rs[:, :, :n], in_=rs[:, :, :n]), r=["g_rs"], w=["g_rs"])
        P.add("dve", lambda e: e.scalar_tensor_tensor(out=dst[:, :, t0:t0 + n], in0=acc[:, :, :n], scalar=scl, in1=rs[:, :, :n], op0=ALU.mult, op1=ALU.mult),
              r=[("g_acc", 0), ("g_acc", 1), "g_rs"], w=[dkey])

    def tr_tile(srcfn, skeys, dst, dkey, t0, n):
        for s in range(n // 128):
            c = t0 // 128 + s
            j = 2 + (s % 2)
            for h in range(2):
                P.add("pe", lambda e, h=h, s=s, j=j: e.matmul(banks[j][:, h * 64:(h + 1) * 64], srcfn(h, s), ident[0:64, 0:64], start=True, stop=True),
                      r=skeys + ["g_b_ident"], w=[("bk", j)])
            P.add("act", lambda e, c=c, j=j: e.copy(out=dst[:, c, :], in_=banks[j][:, 0:128]), r=[("bk", j)], w=[dkey])

    for (t0, n) in QT:
        conv_tile(0, "b_q", t0, n)
        l2_tile(qn, "g_qn", t0, n, 0.125)
        conv_tile(1, "b_k", t0, n)
        l2_tile(kn, "g_kn", t0, n, 1.0)
        tr_tile(lambda h, s, t0=t0: kn[:, h, t0 + s * 128:t0 + (s + 1) * 128], ["g_kn"], ktok, "g_ktok", t0, n)
        conv_tile(2, "b_v", t0, n)
        tr_tile(lambda h, s: acc[:, h, s * 128:(s + 1) * 128], [("g_acc", 0), ("g_acc", 1)], vtok, "g_vtok", t0, n)

    NI = 4
    def tl(nm, shp):
        return [P.sb(shp, F32, f"g_{nm}{i}") for i in range(NI)]
    grep = tl("grep", [128, 128]); brep = tl("brep", [128, 128]); T1 = tl("T1", [128, 128]); EB = tl("EB", [64, 128]); bBs = tl("bBs", [64, 128])
    DTm = tl("DTm", [128, 128]); DTs = tl("DTs", [128, 128]); MT = tl("MT", [128, 128]); Pa = tl("Pa", [128, 128]); PTa = tl("PTa", [128, 128])
    Pb = tl("Pb", [128, 128]); PTb = tl("PTb", [128, 128]); RT = tl("RT", [128, 128]); AT = tl("AT", [128, 128])
    wT = tl("wT", [64, 128]); u = tl("u", [128, 64]); qd = tl("qd", [64, 128]); kd = tl("kd", [128, 64]); vb = tl("vb", [128, 64]); kbe = tl("kbe", [128, 64])
    kbT = tl("kbT", [64, 128]); vnew = tl("vnew", [128, 64]); cols = tl("cols", [128, 8]); Sst = tl("S", [64, 64])
    for i in range(NI):
        P.add("pool", lambda e, i=i: e.memset(Sst[i][:], 0.0), w=[("S", i)])
    orders = [list(range(NCH)), [1, 0] + list(range(NCH - 1, 1, -1))]

    def pre(i, c, d, h):
        K = lambda nm: (nm, i)
        bk = ("bk", i)
        gcol = gtok[:, c * 8 + d * 4 + h:c * 8 + d * 4 + h + 1]
        bcol = btok[:, c * 8 + d * 4 + 2 + h:c * 8 + d * 4 + 2 + h + 1]
        ch = slice(c * 128, (c + 1) * 128)
        hs = slice(h * 64, (h + 1) * 64)
        cl = cols[i]
        P.add("dve", lambda e: e.tensor_scalar(out=grep[i][:, :], in0=ones[:, :], scalar1=gcol, scalar2=None, op0=ALU.mult), r=["g_gtok", "ones"], w=[K("grep")])
        P.add("pool", lambda e: e.tensor_scalar(out=brep[i][:, :], in0=ones[:, :], scalar1=bcol, scalar2=None, op0=ALU.mult), r=["g_btok", "ones"], w=[K("brep")])
        P.add("pe", lambda e: e.matmul(X(i, 0), grep[i][:, :], tri[d][:, :], start=True, stop=True), r=[K("grep")] + CK, w=[bk])
        yield
        P.add("pe", lambda e: e.matmul(X(i, 2), brep[i][:, :], ident[:, :], start=True, stop=True), r=[K("brep")] + CK, w=[bk])
        yield
        P.add("dve", lambda e: e.tensor_tensor(out=T1[i][:, :], in0=X(i, 0), in1=ident[:, :], op=ALU.mult), r=[bk] + CK, w=[K("T1")])
        P.add("dve", lambda e: e.reduce_sum(out=cl[:, 0:1], in_=T1[i][:, :], axis=AX.X), r=[K("T1")], w=[K("cols")])
        lc = 127 if d == 0 else 0
        P.add("act", lambda e: e.copy(out=cl[:, 1:2], in_=banks[i][:, lc:lc + 1]), r=[bk], w=[K("cols")])
        P.add("act", lambda e: e.activation(out=EB[i][:, :], in_=X(i, 0, 64), func=AF.Exp), r=[bk], w=[K("EB")])
        P.add("dve", lambda e: e.tensor_scalar(out=T1[i][:, :], in0=X(i, 0), scalar1=cl[:, 0:1], scalar2=0.0, op0=ALU.subtract, op1=ALU.min), r=[bk, K("cols")], w=[K("T1")])
        P.add("dve", lambda e: e.tensor_copy(out=bBs[i][:, :], in_=X(i, 2, 64)), r=[bk], w=[K("bBs")])
        P.add("act", lambda e: e.activation(out=DTm[i][:, :], in_=T1[i][:, :], func=AF.Exp), r=[K("T1")], w=[K("DTm")])
        P.add("pool", lambda e: e.tensor_tensor(out=DTm[i][:, :], in0=DTm[i][:, :], in1=msk[d][:, :], op=ALU.mult), r=[K("DTm")] + CK, w=[K("DTm")])
        P.add("pool", lambda e: e.tensor_tensor(out=DTs[i][:, :], in0=DTm[i][:, :], in1=smsk[d][:, :], op=ALU.mult), r=[K("DTm")] + CK, w=[K("DTs")])
        P.add("act", lambda e: e.activation(out=cl[:, 2:3], in_=cl[:, 0:1], func=AF.Exp, scale=-1.0, bias=cl[:, 1:2]), r=[K("cols")], w=[K("cols")])
        P.add("act", lambda e: e.activation(out=cl[:, 3:4], in_=cl[:, 1:2], func=AF.Exp), r=[K("cols")], w=[K("cols")])
        P.add("act", lambda e: e.activation(out=cl[:, 4:5], in_=cl[:, 0:1], func=AF.Exp), r=[K("cols")], w=[K("cols")])
        P.add("dve", lambda e: e.tensor_tensor(out=cl[:, 5:6], in0=cl[:, 4:5], in1=bcol, op=ALU.mult), r=[K("cols"), "g_btok"], w=[K("cols")])
        P.add("dve", lambda e: e.tensor_tensor(out=kbT[i][:, :], in0=kn[:, h, ch], in1=bBs[i][:, :], op=ALU.mult), r=["g_kn", K("bBs")], w=[K("kbT")])
        P.add("dve", lambda e: e.tensor_tensor(out=qd[i][:, :], in0=qn[:, h, ch], in1=EB[i][:, :], op=ALU.mult), r=["g_qn", K("EB")], w=[K("qd")])
        P.add("pool", lambda e: e.tensor_scalar(out=vb[i][:, :], in0=vtok[:, c, hs], scalar1=bcol, scalar2=None, op0=ALU.mult), r=["g_vtok", "g_btok"], w=[K("vb")])
        P.add("pool", lambda e: e.tensor_scalar(out=kbe[i][:, :], in0=ktok[:, c, hs], scalar1=cl[:, 5:6], scalar2=None, op0=ALU.mult), r=["g_ktok", K("cols")], w=[K("kbe")])
        P.add("pool", lambda e: e.tensor_scalar(out=kd[i][:, :], in0=ktok[:, c, hs], scalar1=cl[:, 2:3], scalar2=None, op0=ALU.mult), r=["g_ktok", K("cols")], w=[K("kd")])
        P.add("pe", lambda e: e.matmul(X(i, 0), kn[:, h, ch], kbT[i][:, :], start=True, stop=True), r=["g_kn", K("kbT")], w=[bk])
        yield
        P.add("pe", lambda e: e.matmul(X(i, 1), kn[:, h, ch], qn[:, h, ch], start=True, stop=True), r=["g_kn", "g_qn"], w=[bk])
        yield
        P.add("dve", lambda e: e.scalar_tensor_tensor(out=MT[i][:, :], in0=X(i, 0), scalar=-1.0, in1=DTs[i][:, :], op0=ALU.mult, op1=ALU.mult), r=[bk, K("DTs")], w=[K("MT")])
        P.add("dve", lambda e: e.tensor_tensor(out=AT[i][:, :], in0=X(i, 1), in1=DTm[i][:, :], op=ALU.mult), r=[bk, K("DTm")], w=[K("AT")])
        P.add("pe", lambda e: e.matmul(X(i, 2), MT[i][:, :], ident[:, :], start=True, stop=True), r=[K("MT")] + CK, w=[bk])
        yield
        P.add("act", lambda e: e.copy(out=Pa[i][:, :], in_=X(i, 2)), r=[bk], w=[K("Pa")])
        P.add("pool", lambda e: e.tensor_tensor(out=RT[i][:, :], in0=MT[i][:, :], in1=ident[:, :], op=ALU.add), r=[K("MT")] + CK, w=[K("RT")])
        Pc, PTc, Pn, PTn = Pa[i], MT[i], Pb[i], PTb[i]
        kPc, kPTc, kPn, kPTn = K("Pa"), K("MT"), K("Pb"), K("PTb")
        for lvl in range(1, 7):
            P.add("pe", lambda e, Pc=Pc, PTc=PTc: e.matmul(X(i, 0), PTc[:, :], Pc[:, :], start=True, stop=True), r=[kPc, kPTc], w=[bk])
            yield
            if lvl < 6:
                P.add("pe", lambda e, Pc=Pc, PTc=PTc: e.matmul(X(i, 1), Pc[:, :], PTc[:, :], start=True, stop=True), r=[kPc, kPTc], w=[bk])
                yield
            P.add("act", lambda e, Pn=Pn: e.copy(out=Pn[:, :], in_=X(i, 0)), r=[bk], w=[kPn])
            if lvl < 6:
                P.add("dve", lambda e, PTn=PTn: e.tensor_copy(out=PTn[:, :], in_=X(i, 1)), r=[bk], w=[kPTn])
            P.add("pe", lambda e, Pn=Pn: e.matmul(X(i, 2), Pn[:, :], RT[i][:, :], start=True, stop=True), r=[kPn, K("RT")], w=[bk])
            yield
            P.add("dve", lambda e: e.tensor_tensor(out=RT[i][:, :], in0=RT[i][:, :], in1=X(i, 2), op=ALU.add), r=[bk, K("RT")], w=[K("RT")])
            if lvl == 1:
                Pc, PTc, Pn, PTn = Pb[i], PTb[i], Pa[i], PTa[i]
                kPc, kPTc, kPn, kPTn = K("Pb"), K("PTb"), K("Pa"), K("PTa")
            else:
                Pc, PTc, Pn, PTn = Pn, PTn, Pc, PTc
                kPc, kPTc, kPn, kPTn = kPn, kPTn, kPc, kPTc
        P.add("pe", lambda e: e.matmul(X(i, 0, 128, 64), RT[i][:, :], vb[i][:, :], start=True, stop=True), r=[K("RT"), K("vb")], w=[bk])
        yield
        P.add("pe", lambda e: e.matmul(X(i, 1, 64, 128), kbe[i][:, :], RT[i][:, :], start=True, stop=True), r=[K("RT"), K("kbe")], w=[bk])
        yield
        P.add("act", lambda e: e.copy(out=u[i][:, :], in_=X(i, 0, 128, 64)), r=[bk], w=[K("u")])
        P.add("dve", lambda e: e.tensor_copy(out=wT[i][:, :], in_=X(i, 1, 64, 128)), r=[bk], w=[K("wT")])

    def chain(i, c, d, h):
        K = lambda nm: (nm, i)
        bk = ("bk", 4 + i)
        j = 4 + i
        ch = slice(c * 128, (c + 1) * 128)
        cl = cols[i]
        P.add("pe", lambda e: e.matmul(X(j, 0, 128, 64), wT[i][:, :], Sst[i][:, :], start=True, stop=True), r=[K("wT"), ("S", i)], w=[bk])
        yield
        P.add("dve", lambda e: e.tensor_tensor(out=vnew[i][:, :], in0=u[i][:, :], in1=X(j, 0, 128, 64), op=ALU.subtract), r=[bk, K("u")], w=[K("vnew")])
        P.add("pe", lambda e: e.matmul(X(j, 1, 64, 128), Sst[i][:, :], qd[i][:, :], start=True, stop=False), r=[("S", i), K("qd")], w=[bk])
        yield
        P.add("pe", lambda e: e.matmul(X(j, 1, 64, 128), vnew[i][:, :], AT[i][:, :], start=False, stop=True), r=[K("vnew"), K("AT")], w=[bk])
        yield
        P.add("pe", lambda e: e.matmul(X(j, 2, 64, 64), kd[i][:, :], vnew[i][:, :], start=True, stop=True), r=[K("kd"), K("vnew")], w=[bk])
        yield
        P.add("dve", lambda e: e.tensor_tensor(out=oacc[:, h, ch], in0=oacc[:, h, ch], in1=X(j, 1, 64, 128), op=ALU.add), r=[bk, "g_oacc"], w=["g_oacc"])
        P.add("dve", lambda e: e.scalar_tensor_tensor(out=Sst[i][:, :], in0=Sst[i][:, :], scalar=cl[0:64, 3:4], in1=X(j, 2, 64, 64), op0=ALU.mult, op1=ALU.add),
              r=[bk, ("S", i), K("cols")], w=[("S", i)])

    _on[0] = True
    for s in range(nsteps):
        insts = [(h * 2 + d, orders[d][s], d, h) for h in range(2) for d in range(2)]
        for gens in ([pre(*a_) for a_ in insts], [chain(*a_) for a_ in insts]):
            live = list(gens)
            while live:
                nxt = []
                for g_ in live:
                    try:
                        next(g_)
                        nxt.append(g_)
                    except StopIteration:
                        pass
                live = nxt

    _on[0] = False
    print('gdn inst ops', _cnt[0])
    gt = xr; yo = acc

    def fin(t0, n):
        P.dma("sp", gt[:, :, :n], D["b_gate"].rearrange("(h d) t -> d h t", d=64)[:, :, t0:t0 + n], w=["g_xr"])
        P.add("act", lambda e: e.activation(out=sq[:, :, :n], in_=oacc[:, :, t0:t0 + n], func=AF.Square), r=["g_oacc"], w=["g_sq"])
        for h in range(2):
            P.add("pe", lambda e, h=h: e.matmul(banks[h][0:64, :n], ones[0:64, 0:64], sq[:, h, :n], start=True, stop=True), r=["g_sq", "ones"], w=[("bk", h)])
            P.add("act", lambda e, h=h: e.activation(out=rs[:, h, :n], in_=banks[h][0:64, :n], func=AF.Sqrt, scale=1.0 / 64, bias=C["epsb"][0:64, 0:1]), r=[("bk", h), "epsb"], w=["g_rs"])
        P.add("dve", lambda e: e.reciprocal(out=rs[:, :, :n], in_=rs[:, :, :n]), r=["g_rs"], w=["g_rs"])
        P.add("dve", lambda e: e.tensor_tensor(out=yo[:, :, :n], in0=oacc[:, :, t0:t0 + n], in1=rs[:, :, :n], op=ALU.mult), r=["g_oacc", "g_rs"], w=[("g_acc", 0), ("g_acc", 1)])
        P.add("act", lambda e: e.activation(out=gt[:, :, :n], in_=gt[:, :, :n], func=AF.Silu), r=["g_xr"], w=["g_xr"])
        P.add("dve", lambda e: e.scalar_tensor_tensor(out=yo[:, :, :n], in0=yo[:, :, :n], scalar=gg[:, 0:1], in1=gt[:, :, :n], op0=ALU.mult, op1=ALU.mult), r=[("g_acc", 0), ("g_acc", 1), "g_xr", "g_b_g"], w=[("g_acc", 0), ("g_acc", 1)])
        P.dma("sp", out.rearrange("(h d) t -> d h t", d=64)[:, :, t0:t0 + n], yo[:, :, :n], r=[("g_acc", 0), ("g_acc", 1)])

    for (t0, n) in QT:
        fin(t0, n)
    print("gdn ops", P.n_ops())
    return P.finish()


def build_M():
    P = Prog()
    scT = P.dram_in("scT", [1024, 5])
    wm = P.dram_in("wm", [1024, 3072])
    bm = P.dram_in("bm", [128, 24])
    modo = P.dram_out("modo", [128, 120])
    sc = P.sb([128, 8, 5], F32, "sc")
    P.dma("sp", sc[:], scT.rearrange("(k p) j -> p k j", p=128), w=["sc"])
    P.add("act", lambda e: e.activation(out=sc[:], in_=sc[:], func=AF.Silu), r=["sc"], w=["sc"])
    bms = P.sb([128, 24], F32, "bms")
    P.dma("sp", bms[:], bm[:, :], w=["bms"])
    w = P.sb([128, 8, 3072], F32, "wms")
    for k in range(8):
        P.dma("sp", w[:, k, :], wm[k * 128:(k + 1) * 128, :], w=[("wms", k)])
    ps = P.ps([128, 512], F32, "mps")
    ob = P.sb([128, 120], F32, "ob")
    for cc in range(24):
        for k in range(8):
            P.add("pe", lambda e, cc=cc, k=k: e.matmul(ps[:, cc * 5:(cc + 1) * 5], w[:, k, cc * 128:(cc + 1) * 128], sc[:, k, :], start=(k == 0), stop=(k == 7)),
                  r=["sc"] + [("wms", kk) for kk in range(8)], w=["mps"])
    for cc in range(24):
        P.add("dve", lambda e, cc=cc: e.tensor_scalar(out=ob[:, cc * 5:(cc + 1) * 5], in0=ps[:, cc * 5:(cc + 1) * 5], scalar1=bms[:, cc:cc + 1], scalar2=None, op0=ALU.add),
              r=["mps", "bms"], w=["ob"])
    P.dma("sp", modo[:, :], ob[:], r=["ob"])
    return P.finish()


OFF = {}
_o = 0
for nm, n in [("Aq",256),("Ak",128),("Av",128),("Bqkv",768),("Bgate",256),("Bab",16),("Ccq",256),("Cckv",128),("Ckr",32),("Dq",256),("Dk",256),("Dv",256),("Dgate",256)]:
    OFF[nm] = (_o, n); _o += n

def rope_perm(dim):
    q = dim // 4
    perm = np.concatenate([np.arange(q) + q, np.arange(q), np.arange(q) + 3 * q, np.arange(q) + 2 * q])
    sign = np.concatenate([-np.ones(q), np.ones(q), -np.ones(q), np.ones(q)]).astype(np.float32)
    return perm, sign

def rope_tables(rot_dim, rows=64, grid_w=64, theta=10000.0):
    n_freq = rot_dim // 4
    inv_freq = (theta ** (-np.arange(n_freq, dtype=np.float32) / n_freq)).astype(np.float32)
    row = np.repeat(np.arange(rows, dtype=np.float32), grid_w)
    col = np.tile(np.arange(grid_w, dtype=np.float32), rows)
    ang_r = row[:, None] * inv_freq
    ang_c = col[:, None] * inv_freq
    ang = np.concatenate([ang_r, ang_r, ang_c, ang_c], axis=-1).astype(np.float32)
    return np.cos(ang).astype(np.float32), np.sin(ang).astype(np.float32)

def rope_tabs_T(rot_dim):
    cos, sin = rope_tables(rot_dim)
    perm, sign = rope_perm(rot_dim)
    cT = np.ones((rot_dim, 4352), np.float32); sT = np.zeros((rot_dim, 4352), np.float32)
    cT[:, 256:] = cos.T; sT[:, 256:] = (sin * sign[None, :]).T
    return cT, sT

def perm_heads(w, dim):
    perm, _ = rope_perm(dim)
    nh = w.shape[1] // dim
    idx = np.concatenate([h * dim + perm for h in range(nh)])
    return w[:, idx]


def fm(v, nk):
    return np.ascontiguousarray(v.reshape(nk, 128).T)


def mla_inputs(P_, hf, W):
    hs = [2 * hf, 2 * hf + 1]
    perm32, _ = rope_perm(32)
    ct, st = rope_tabs_T(32)
    ct96 = np.ones((96, 4352), np.float32); st96 = np.zeros((96, 4352), np.float32)
    ct96[64:] = ct; st96[64:] = st
    wq = np.concatenate([W["mla_w_q_up"][:, h * 96:(h + 1) * 96] for h in hs], axis=1)
    wqP = np.zeros((256, 192), np.float32)
    for i, h in enumerate(hs):
        wqP[:, i * 96 + 64:i * 96 + 96] = W["mla_w_q_up"][:, h * 96 + 64 + perm32]
    wkn = np.zeros((128, 192), np.float32)
    for i, h in enumerate(hs):
        wkn[:, i * 96:i * 96 + 64] = W["mla_w_kv_up"][:, h * 128:h * 128 + 64]
    wv = np.concatenate([W["mla_w_kv_up"][:, h * 128 + 64:h * 128 + 128] for h in hs], axis=1)
    sel = np.zeros((32, 96), np.float32); sel[np.arange(32), 64 + np.arange(32)] = 1
    return {"c_cq": P_["Ccq"], "c_ckv": P_["Cckv"], "c_kr": P_["Ckr"], "c_krP": P_["CkrP"], "c_ct96": ct96, "c_st96": st96,
            "c_qng": fm(W["mla_q_norm"], 2), "c_kvg": fm(W["mla_kv_norm"], 1), "c_wq": np.ascontiguousarray(wq), "c_wqP": wqP,
            "c_wkn": wkn, "c_wv": np.ascontiguousarray(wv), "c_sel": sel}


def swa_inputs(PF, PT, hf, W):
    cT, sT = rope_tabs_T(64)
    j = np.arange(128)[:, None]; i = np.arange(128)[None, :]
    sink = W["swa_sink"][2 * hf:2 * hf + 2]
    return {"a_q": PF["Aq"][hf * 128:(hf + 1) * 128], "a_qP": PF["AqP"][hf * 128:(hf + 1) * 128],
            "a_k": PF["Ak"][hf * 64:(hf + 1) * 64], "a_kP": PF["AkP"][hf * 64:(hf + 1) * 64],
            "a_vtok": np.ascontiguousarray(PT["Av"][:, hf * 64:(hf + 1) * 64]), "a_cos": cT, "a_sin": sT,
            "a_sink": np.ascontiguousarray(np.broadcast_to(sink[None, :], (128, 2))).astype(np.float32),
            "a_maskP": (j >= i).astype(np.float32), "a_maskN": (j <= i).astype(np.float32)}


def ret_inputs(PF, PT, hf, W):
    cT, sT = rope_tabs_T(64)
    hs = [2 * hf, 2 * hf + 1]
    sl = slice(hf * 128, (hf + 1) * 128)
    ld = W["ret_log_decay"]
    ldp = np.zeros((128, 2), np.float32); ldr = np.zeros((128, 4), np.float32)
    for d in range(2):
        for i, h in enumerate(hs):
            ldp[i * 64:(i + 1) * 64, d] = ld[d, h]
            ldr[:, 2 * d + i] = ld[d, h]
    j = np.arange(128)[:, None].astype(np.float32); i = np.arange(128)[None, :].astype(np.float32)
    bd = np.zeros((128, 128), np.float32); bd[:64, :64] = 1; bd[64:, 64:] = 1
    pk = np.stack([127 - np.arange(128), np.arange(128)], 1).astype(np.float32)
    return {"d_q": PF["Dq"][sl], "d_qP": PF["DqP"][sl], "d_k": PF["Dk"][sl], "d_kP": PF["DkP"][sl], "d_gate": PF["Dgate"][sl],
            "d_vtok": np.ascontiguousarray(PT["Dv"][:, sl]), "d_ktok": np.ascontiguousarray(PT["Dk"][:, sl]), "d_kPtok": np.ascontiguousarray(PT["DkP"][:, sl]),
            "d_cos": cT, "d_sin": sT, "d_costok": np.ascontiguousarray(cT.T), "d_sintok": np.ascontiguousarray(sT.T),
            "d_ldp": ldp, "d_ldr": ldr, "d_g": np.ascontiguousarray(W["ret_norm"][sl].reshape(128, 1)),
            "d_relu": np.maximum(i - j, 0) + 0 * j, "d_rell": np.maximum(j - i, 0) + 0 * i, "d_um": (i >= j).astype(np.float32), "d_lm": (j >= i).astype(np.float32),
            "d_pos1": (i + 1) + 0 * j, "d_posr": (128 - i) + 0 * j, "d_pk": pk, "d_bd": bd, "d_bd64": bd / 64}


def gdn_inputs(PF, PT, hf, W):
    hs = [2 * hf, 2 * hf + 1]
    qkv = PF["Bqkv"]
    sel = lambda base: np.ascontiguousarray(np.concatenate([qkv[base + h * 64: base + (h + 1) * 64] for h in hs], 0))
    ab = PT["Bab"]
    abl = np.zeros((4352, 8), np.float32)
    dtb = np.zeros((8,), np.float32); alog = np.zeros((8,), np.float32)
    for d in range(2):
        for w in range(2):
            for hl, h in enumerate(hs):
                abl[:, d * 4 + w * 2 + hl] = ab[:, d * 8 + w * 4 + h]
        for hl, h in enumerate(hs):
            dtb[d * 4 + hl] = W["gdn_dt_bias"][d, h]; alog[d * 4 + hl] = W["gdn_a_log"][d, h]
    cwt = np.zeros((64, 3, 2, 5), np.float32)
    conv = W["gdn_conv"]
    for gi in range(3):
        for hl, h in enumerate(hs):
            cwt[:, gi, hl, :] = conv[:, gi * 256 + h * 64: gi * 256 + (h + 1) * 64].T
    k = np.arange(128)[:, None]; i = np.arange(128)[None, :]
    f = lambda m: m.astype(np.float32)
    return {"b_q": sel(0), "b_k": sel(256), "b_v": sel(512), "b_gate": PF["Bgate"][hf * 128:(hf + 1) * 128], "b_ab": abl,
            "b_cw": cwt.reshape(64, 30), "b_dtb": np.ascontiguousarray(np.broadcast_to(np.tile(dtb, 34)[None], (128, 272))),
            "b_alog": np.ascontiguousarray(np.broadcast_to(np.tile(alog, 34)[None], (128, 272))), "b_g": np.ascontiguousarray(W["gdn_norm"].reshape(64, 1)),
            "b_triF": f(k <= i), "b_triR": f(k >= i), "b_ident": np.eye(128, dtype=np.float32), "b_um": f(i >= k), "b_lm": f(k >= i),
            "b_us": f(i > k), "b_ls": f(k > i)}


_PROGS = {}
_TRACE = [False]


def _prog(name, fn):
    if name not in _PROGS:
        _PROGS[name] = fn()
    return _PROGS[name]


def _run(name, fn, in_maps):
    nc = fn()
    in_maps = [{k: np.ascontiguousarray(v, dtype=np.float32) for k, v in m.items()} for m in in_maps]
    res = run_bass_kernel_spmd(nc, in_maps, core_ids=list(range(8)), trace=_TRACE[0])
    if _TRACE[0]:
        print('STAGE', name, 'exec_ns', res.exec_time_ns, flush=True)
    return res.results


A_TILES = TILES
HALF = 2176


def _mod_table(mod_l, b, hf, tiles_ctx):
    nt = len(tiles_ctx)
    t = np.zeros((128, 6, nt, 8), np.float32)
    for ti, is_ctx in enumerate(tiles_ctx):
        v = mod_l[4 if is_ctx else b].reshape(6, 8, 128)
        t[:, :, ti, :] = v.transpose(2, 0, 1)
    return t


def kernel(x, c, ctx, c_ctx, w_mod, b_mod, norm1, norm2, w_in, w_out, swa_sink, gdn_conv, gdn_a_log,
           gdn_dt_bias, gdn_norm, mla_q_norm, mla_kv_norm, mla_w_q_up, mla_w_kv_up, ret_log_decay,
           ret_norm, ffn_w_gate, ffn_w_up, ffn_w_down, moe_router, moe_w_gate, moe_w_up, moe_w_down,
           final_norm, _nlayers=4):
    f32 = np.float32
    x = np.asarray(x, f32); ctx = np.asarray(ctx, f32)
    B = 4
    scT = np.ascontiguousarray(np.concatenate([np.asarray(c, f32), np.asarray(c_ctx, f32)[None]], 0).T)
    wm_all = np.asarray(w_mod, f32).transpose(1, 0, 2).reshape(1024, 4 * 6144)
    bm_all = np.asarray(b_mod, f32).reshape(4 * 6144)
    ims = []
    for core in range(8):
        sl = slice(core * 3072, (core + 1) * 3072)
        ims.append({"scT": scT, "wm": np.ascontiguousarray(wm_all[:, sl]), "bm": np.ascontiguousarray(bm_all[sl].reshape(24, 128).T)})
    res = _run("M", build_M, ims)
    mod = np.zeros((5, 4 * 6144), f32)
    for core in range(8):
        mo = res[core]["modo"].reshape(128, 24, 5)
        mod[:, core * 3072:(core + 1) * 3072] = mo.transpose(2, 1, 0).reshape(5, 3072)
    mod = mod.reshape(5, 4, 6144)

    hT = [np.ascontiguousarray(np.concatenate([ctx[b], x[b]], 0).T) for b in range(B)]
    p64, _ = rope_perm(64)
    p32, _ = rope_perm(32)
    ident = np.eye(128, dtype=f32)
    out_final = None
    for l in range(_nlayers):
        W = {"w_in": np.asarray(w_in[l], f32), "swa_sink": np.asarray(swa_sink[l], f32), "gdn_conv": np.asarray(gdn_conv[l], f32),
             "gdn_a_log": np.asarray(gdn_a_log[l], f32), "gdn_dt_bias": np.asarray(gdn_dt_bias[l], f32), "gdn_norm": np.asarray(gdn_norm[l], f32),
             "mla_q_norm": np.asarray(mla_q_norm[l], f32), "mla_kv_norm": np.asarray(mla_kv_norm[l], f32), "mla_w_q_up": np.asarray(mla_w_q_up[l], f32),
             "mla_w_kv_up": np.asarray(mla_w_kv_up[l], f32), "ret_log_decay": np.asarray(ret_log_decay[l], f32), "ret_norm": np.asarray(ret_norm[l], f32)}
        wi = W["w_in"]
        blk = lambda nm: wi[:, OFF[nm][0]:OFF[nm][0] + OFF[nm][1]]
        fcols = {"Aq": blk("Aq"), "AqP": perm_heads(blk("Aq"), 64), "Ak": blk("Ak"), "AkP": perm_heads(blk("Ak"), 64), "Bqkv": blk("Bqkv"),
                 "Bgate": blk("Bgate"), "Ccq": blk("Ccq"), "Cckv": blk("Cckv"), "Ckr": blk("Ckr"), "CkrP": perm_heads(blk("Ckr"), 32),
                 "Dq": blk("Dq"), "DqP": perm_heads(blk("Dq"), 64), "Dk": blk("Dk"), "DkP": perm_heads(blk("Dk"), 64), "Dgate": blk("Dgate")}
        tcols = {"Av": blk("Av"), "Bab": blk("Bab"), "Dv": blk("Dv"), "Dk": blk("Dk"), "DkP": perm_heads(blk("Dk"), 64)}
        win_ext = np.ascontiguousarray(np.concatenate([fcols[nm] for nm, _ in F_BLOCKS] + [tcols[nm] for nm, _ in T_BLOCKS], 1))
        gn1 = fm(np.asarray(norm1[l], f32), 8)
        gn2 = fm(np.asarray(norm2[l], f32), 8)
        ims = []
        for core in range(8):
            b, hf = core // 2, core % 2
            mt = _mod_table(mod[:, l], b, hf, [hf == 0 and ti == 0 for ti in range(5)])
            ims.append({"hT": np.ascontiguousarray(hT[b][:, hf * HALF:(hf + 1) * HALF]), "modt": mt.reshape(128, 240), "gn": gn1, "win": win_ext})
        res = _run("A", build_A, ims)
        PFs, PTs = [], []
        for b in range(B):
            pT = np.concatenate([res[2 * b]["projT"], res[2 * b + 1]["projT"]], 1)
            pK = np.concatenate([res[2 * b]["projTok"], res[2 * b + 1]["projTok"]], 0)
            PF, PT = {}, {}
            o = 0
            for nm, n in F_BLOCKS:
                PF[nm] = pT[o:o + n]; o += n
            o = 0
            for nm, n in T_BLOCKS:
                PT[nm] = pK[:, o:o + n]; o += n
            PFs.append(PF); PTs.append(PT)
        mixT = [np.zeros((1024, NTOK), f32) for _ in range(B)]
        for gi, (nm, bfn, ifn) in enumerate((("swa", build_swa, swa_inputs), ("gdn", build_gdn, gdn_inputs), ("mla", build_mla, None), ("ret", build_ret, ret_inputs))):
            ims = []
            for core in range(8):
                b, hf = core // 2, core % 2
                ims.append(mla_inputs(PFs[b], hf, W) if nm == "mla" else ifn(PFs[b], PTs[b], hf, W))
            res = _run(nm, bfn, ims)
            for core in range(8):
                b, hf = core // 2, core % 2
                mixT[b][gi * 256 + hf * 128: gi * 256 + (hf + 1) * 128] = res[core]["mix"]
        moe = (l % 2 == 1)
        final = (l == 3)
        i2 = l // 2
        nt = 9
        nl = 1
        span = nt * 256
        padw = nl * span
        if moe:
            wts = {"wg": np.asarray(moe_w_gate[i2], f32), "wu": np.asarray(moe_w_up[i2], f32), "wd": np.asarray(moe_w_down[i2], f32),
                   "wr": np.asarray(moe_router[i2], f32), "ident": ident}
        else:
            wts = {"wg": np.asarray(ffn_w_gate[i2], f32)[None], "wu": np.asarray(ffn_w_up[i2], f32)[None], "wd": np.asarray(ffn_w_down[i2], f32)[None]}
        wts["wout"] = np.asarray(w_out[l], f32)
        wts["gn"] = gn2
        if final:
            wts["fn"] = fm(np.asarray(final_norm, f32), 8)
        newh = [np.zeros((1024, NTOK), f32) for _ in range(B)]
        outs = [np.zeros((1024, NTOK), f32) for _ in range(B)]
        for r in range(nl):
            ims = []
            for core in range(8):
                b, hf = core // 2, core % 2
                hp = np.zeros((1024, padw), f32); mp = np.zeros((1024, padw), f32)
                hp[:, :HALF] = hT[b][:, hf * HALF:(hf + 1) * HALF]
                mp[:, :HALF] = mixT[b][:, hf * HALF:(hf + 1) * HALF]
                mt = _mod_table(mod[:, l], b, hf, [hf == 0 and (r * nt + ti) == 0 for ti in range(nt)])
                d = {"hT": np.ascontiguousarray(hp[:, r * span:(r + 1) * span]), "mixT": np.ascontiguousarray(mp[:, r * span:(r + 1) * span]),
                     "modt": mt.reshape(128, 6 * nt * 8)}
                d.update(wts)
                ims.append(d)
            res = _run(("C", moe, final, nt), lambda: build_C2(moe, final, nt), ims)
            for core in range(8):
                b, hf = core // 2, core % 2
                lo = r * span
                hi = min((r + 1) * span, HALF)
                if hi > lo:
                    newh[b][:, hf * HALF + lo: hf * HALF + hi] = res[core]["h2T"][:, :hi - lo]
                    if final:
                        outs[b][:, hf * HALF + lo: hf * HALF + hi] = res[core]["outT"][:, :hi - lo]
        hT = newh
        if final:
            out_final = np.stack([np.ascontiguousarray(outs[b][:, 256:].T) for b in range(B)], 0)
    if _nlayers < 4:
        return hT
    return out_final.astype(np.float32)
```

```python
import numpy as np
import concourse.bass as bass
import concourse.mybir as mybir
from concourse.bass_utils import run_bass_kernel_spmd
from contextlib import ExitStack

F32 = mybir.dt.float32
BF16 = mybir.dt.bfloat16
AF = mybir.ActivationFunctionType
ALU = mybir.AluOpType
AX = mybir.AxisListType

ENGS = ("pe", "act", "dve", "pool", "sp")


class Op:
    __slots__ = ("eng", "fn", "deps", "is_dma", "sem", "val", "marked", "idx", "prewait")

    def __init__(self, eng, fn, is_dma):
        self.eng = eng
        self.fn = fn
        self.deps = []
        self.is_dma = is_dma
        self.sem = None
        self.val = None
        self.marked = False
        self.prewait = None


class Prog:
    def __init__(self, name="k", n_dma_sems=12):
        self.nc = bass.Bass("TRN2", target_bir_lowering=False)
        self.es = ExitStack()
        self.ops = {e: [] for e in ENGS}
        self.last_w = {}
        self.readers = {}
        self.n_dma_sems = 2 * n_dma_sems
        self.n_half = n_dma_sems
        self.dma_rr = {"hw": 0, "sw": 0}
        self.dma_last = [None] * (2 * n_dma_sems)
        self.dma_cnt = [0] * (2 * n_dma_sems)
        self.uid = 0

    def sb(self, shape, dt=F32, name=None):
        self.uid += 1
        return self.es.enter_context(self.nc.sbuf_tensor("s_" + (name or f"sb{self.uid}"), list(shape), dt))

    def ps(self, shape, dt=F32, name=None):
        self.uid += 1
        return self.es.enter_context(self.nc.psum_tensor("p_" + (name or f"ps{self.uid}"), list(shape), dt))

    def dram_in(self, name, shape, dt=F32):
        return self.nc.dram_tensor(name, list(shape), dt, kind="ExternalInput").ap()

    def dram_out(self, name, shape, dt=F32):
        return self.nc.dram_tensor(name, list(shape), dt, kind="ExternalOutput").ap()

    def dram_tmp(self, name, shape, dt=F32):
        return self.nc.dram_tensor(name, list(shape), dt, kind="Internal").ap()

    def _track(self, op, r, w):
        deps = []
        for k in r:
            lw = self.last_w.get(k)
            if lw is not None:
                deps.append(lw)
        for k in w:
            lw = self.last_w.get(k)
            if lw is not None:
                deps.append(lw)
            for rd in self.readers.get(k, ()):
                deps.append(rd)
        for k in r:
            self.readers.setdefault(k, []).append(op)
        for k in w:
            self.last_w[k] = op
            self.readers[k] = []
        op.deps = [d for d in deps if d is not op and not (d.eng == "pe" and op.eng == "pe" and not d.is_dma and not op.is_dma)]
        for d in op.deps:
            d.marked = True

    def add(self, eng, fn, r=(), w=()):
        op = Op(eng, fn, False)
        self._track(op, r, w)
        self.ops[eng].append(op)
        return op

    def dma(self, eng, out, in_, r=(), w=(), fn=None):
        op = Op(eng, fn if fn is not None else (lambda e: e.dma_start(out=out, in_=in_)), True)
        kind = "sw" if eng == "pool" else "hw"
        s = self.dma_rr[kind] + (self.n_half if kind == "sw" else 0)
        self.dma_rr[kind] = (self.dma_rr[kind] + 1) % self.n_half
        op.prewait = self.dma_last[s]
        self.dma_cnt[s] += 16
        op.sem = s
        op.val = self.dma_cnt[s]
        op.marked = True
        self.dma_last[s] = op
        self._track(op, r, w)
        self.ops[eng].append(op)
        return op

    def finish(self):
        nc = self.nc
        es = self.es
        esem = {e: es.enter_context(nc.semaphore(f"sem_{e}")) for e in ENGS}
        dsem = [es.enter_context(nc.semaphore(f"sem_dma{i}")) for i in range(self.n_dma_sems)]
        for e in ENGS:
            c = 0
            for op in self.ops[e]:
                if op.is_dma:
                    continue
                if op.marked:
                    c += 1
                    op.val = c
                    op.sem = e
        block = es.enter_context(nc.Block())
        ops = self.ops

        def emit(e, eng):
            waited = {}
            for op in ops[e]:
                need = {}
                dl = list(op.deps)
                if op.prewait is not None:
                    dl.append(op.prewait)
                for d in dl:
                    key = ("d", d.sem) if d.is_dma else ("e", d.sem)
                    if need.get(key, 0) < d.val:
                        need[key] = d.val
                for key, v in need.items():
                    if waited.get(key, 0) >= v:
                        continue
                    waited[key] = v
                    sem = dsem[key[1]] if key[0] == "d" else esem[key[1]]
                    eng.wait_ge(sem, v)
                ins = op.fn(eng)
                if op.is_dma:
                    ins.then_inc(dsem[op.sem], 16)
                elif op.marked:
                    ins.then_inc(esem[e], 1)
            if e == "sp":
                for i in range(self.n_dma_sems):
                    if self.dma_cnt[i] > 0:
                        eng.wait_ge(dsem[i], self.dma_cnt[i])

        @block.tensor
        def _(eng):
            emit("pe", eng)

        @block.scalar
        def _(eng):
            emit("act", eng)

        @block.vector
        def _(eng):
            emit("dve", eng)

        @block.gpsimd
        def _(eng):
            emit("pool", eng)

        @block.sync
        def _(eng):
            emit("sp", eng)

        es.close()
        return nc

    def n_ops(self):
        return {e: len(v) for e, v in self.ops.items()}


def run(nc, in_maps, trace=False):
    res = run_bass_kernel_spmd(nc, in_maps, core_ids=list(range(len(in_maps))), trace=trace)
    return res


TILES = [(0, 256), (256, 512), (768, 512), (1280, 512), (1792, 384)]
TC = 2176
EPS = 1e-6
F_BLOCKS = [("Aq", 256), ("AqP", 256), ("Ak", 128), ("AkP", 128), ("Bqkv", 768), ("Bgate", 256), ("Ccq", 256),
            ("Cckv", 128), ("Ckr", 32), ("CkrP", 32), ("Dq", 256), ("DqP", 256), ("Dk", 256), ("DkP", 256), ("Dgate", 256)]
T_BLOCKS = [("Av", 128), ("Bab", 16), ("Dv", 256), ("Dk", 256), ("DkP", 256)]
NF = sum(n for _, n in F_BLOCKS)
NT = sum(n for _, n in T_BLOCKS)


def foff(name, blocks):
    o = 0
    for nm, n in blocks:
        if nm == name:
            return o, n
        o += n
    raise KeyError(name)


class Rot:
    def __init__(self, bufs, name):
        self.bufs = bufs
        self.i = 0
        self.name = name

    def next(self):
        b = self.bufs[self.i % len(self.bufs)]
        k = (self.name, self.i % len(self.bufs))
        self.i += 1
        return b, k


def emit_norm_mod(P, C, ht, hkey, n, Asc, shv, t, u, ukey):
    sq, ssps, rs, tmp = C["sq"], C["ssps"], C["rs"], C["tmp"]
    P.add("act", lambda e: e.activation(out=sq[:, :, :n], in_=ht[:, :, :n], func=AF.Square), r=[hkey], w=["sq"])
    for k in range(8):
        P.add("pe", lambda e, k=k: e.matmul(ssps[:, :n], C["ones"][:, :], sq[:, k, :n], start=(k == 0), stop=(k == 7)),
              r=["sq", "ones"], w=["ssps"])
    P.add("act", lambda e: e.activation(out=rs[:, :n], in_=ssps[:, :n], func=AF.Sqrt, scale=1.0 / 1024, bias=C["epsb"][:, 0:1]),
          r=["ssps", "epsb"], w=["rs"])
    P.add("dve", lambda e: e.reciprocal(out=rs[:, :n], in_=rs[:, :n]), r=["rs"], w=["rs"])
    for k in range(8):
        P.add("dve", lambda e, k=k: e.tensor_tensor(out=tmp[:, k, :n], in0=ht[:, k, :n], in1=rs[:, :n], op=ALU.mult),
              r=[hkey, "rs"], w=[("tmp", k)])
        P.add("act", lambda e, k=k: e.activation(out=u[:, k, :n], in_=tmp[:, k, :n], func=AF.Identity,
                                                scale=Asc[:, t, k:k + 1], bias=shv[:, t, k:k + 1]),
              r=[("tmp", k), "modc"], w=[ukey])


def common_consts(P):
    C = {}
    C["ones"] = P.sb([128, 128], F32, "ones")
    P.add("pool", lambda e: e.memset(C["ones"][:], 1.0), w=["ones"])
    C["epsb"] = P.sb([128, 1], F32, "epsb")
    P.add("pool", lambda e: e.memset(C["epsb"][:], EPS), w=["epsb"])
    C["sq"] = P.sb([128, 8, 512], F32, "sq")
    C["tmp"] = P.sb([128, 8, 512], F32, "tmp")
    C["rs"] = P.sb([128, 512], F32, "rs")
    C["ssps"] = P.ps([128, 512], F32, "ssps")
    return C


def load_mod(P, modt_d, gn_d, kinds):
    modt = P.sb([128, 6, 5, 8], F32, "modt")
    P.dma("sp", modt[:].rearrange("p a b c -> p (a b c)"), modt_d[:, :], w=["modt"])
    return modt


def build_A():
    P = Prog()
    hT = P.dram_in("hT", [1024, TC])
    modt_d = P.dram_in("modt", [128, 240])
    gn_d = P.dram_in("gn", [128, 8])
    win = P.dram_in("win", [1024, NF + NT])
    projT = P.dram_out("projT", [NF, TC])
    projTok = P.dram_out("projTok", [TC, NT])
    C = common_consts(P)
    emit_A_body(P, C, hT, None, modt_d, gn_d, win, projT, projTok)
    return P.finish()


def emit_A_setup(P, C, modt_d, gn_d, win, pre=""):
    modt = P.sb([128, 6, 5, 8], F32, pre + "modt")
    P.dma("sp", modt[:].rearrange("p a b c -> p (a b c)"), modt_d[:, :], w=[pre + "modt"])
    gn = P.sb([128, 8], F32, pre + "gn")
    P.dma("sp", gn[:], gn_d[:, :], w=[pre + "gn"])
    A1 = P.sb([128, 5, 8], F32, pre + "A1")
    for t in range(5):
        P.add("dve", lambda e, t=t: e.scalar_tensor_tensor(out=A1[:, t, :], in0=modt[:, 1, t, :], scalar=1.0, in1=gn[:, :],
                                                          op0=ALU.add, op1=ALU.mult), r=[pre + "modt", pre + "gn"], w=["modc"])
    wb = P.sb([128, 8, NF + NT], BF16, pre + "wb")
    for k in range(8):
        P.dma("pool", wb[:, k, :], win[k * 128:(k + 1) * 128, :], w=[("wb", k)])
    return modt, A1, wb


def emit_A_tile(P, C, S, ti, ht, hkey, projT, projTok):
    modt, A1, wb = S["modt"], S["A1"], S["wb"]
    t0, n = TILES[ti]
    u, ukey = S["u"].next()
    emit_norm_mod(P, C, ht, hkey, n, A1, modt[:, 0], ti, u, ukey)
    wkeys = [("wb", k) for k in range(8)]
    ncc = (NF + 127) // 128
    for cc in range(ncc):
        c0 = cc * 128
        m = min(128, NF - c0)
        ps, pk = S["mmps"].next()
        for k in range(8):
            P.add("pe", lambda e, k=k, ps=ps, c0=c0, m=m: e.matmul(ps[:m, :n], wb[:, k, c0:c0 + m], u[:, k, :n], start=(k == 0), stop=(k == 7)),
                  r=[ukey] + wkeys, w=[pk])
        st, sk = S["stage"].next()
        if cc % 2 == 0:
            P.add("act", lambda e, ps=ps, st=st, m=m: e.copy(out=st[:m, :n], in_=ps[:m, :n]), r=[pk], w=[sk])
        else:
            P.add("dve", lambda e, ps=ps, st=st, m=m: e.tensor_copy(out=st[:m, :n], in_=ps[:m, :n]), r=[pk], w=[sk])
        P.dma("sp", projT[c0:c0 + m, t0:t0 + n], st[:m, :n], r=[sk])
    for s in range(n // 128):
        for (c0, c1) in ((0, 512), (512, NT)):
            ps, pk = S["mmps"].next()
            w_ = c1 - c0
            for k in range(8):
                P.add("pe", lambda e, k=k, ps=ps, c0=c0, w_=w_, s=s: e.matmul(ps[:, :w_], u[:, k, s * 128:(s + 1) * 128], wb[:, k, NF + c0:NF + c0 + w_],
                                                                         start=(k == 0), stop=(k == 7)), r=[ukey] + wkeys, w=[pk])
            st, sk = S["stage"].next()
            P.add("dve" if s % 2 else "act", (lambda e, ps=ps, st=st, w_=w_: e.tensor_copy(out=st[:, :w_], in_=ps[:, :w_])) if s % 2 else
                  (lambda e, ps=ps, st=st, w_=w_: e.copy(out=st[:, :w_], in_=ps[:, :w_])), r=[pk], w=[sk])
            P.dma("sp", projTok[t0 + s * 128:t0 + (s + 1) * 128, c0:c1], st[:, :w_], r=[sk])


def emit_A_body(P, C, hT, _, modt_d, gn_d, win, projT, projTok):
    modt, A1, wb = emit_A_setup(P, C, modt_d, gn_d, win)
    S = {"modt": modt, "A1": A1, "wb": wb}
    S["u"] = Rot([P.sb([128, 8, 512], BF16, f"u{i}") for i in range(2)], "u")
    S["mmps"] = Rot([P.ps([128, 512], F32, f"mmps{i}") for i in range(4)], "mmps")
    S["stage"] = Rot([P.sb([128, 512], F32, f"stg{i}") for i in range(4)], "stg")
    hts = Rot([P.sb([128, 8, 512], F32, f"ht{i}") for i in range(2)], "ht")
    hv = hT.rearrange("(k p) t -> p k t", p=128)
    for ti, (t0, n) in enumerate(TILES):
        ht, hk = hts.next()
        P.dma("sp", ht[:, :, :n], hv[:, :, t0:t0 + n], w=[hk])
        emit_A_tile(P, C, S, ti, ht, hk, projT, projTok)


DFF = 3584
NFG = 7


def build_C2(moe, final, ntiles=9):
    NE = 8 if moe else 1
    TC = ntiles * 256
    NS = TC // 128
    CT = [(i * 256, 256) for i in range(ntiles)]
    n = 256
    P = Prog()
    hT = P.dram_in("hT", [1024, TC]); mixT = P.dram_in("mixT", [1024, TC]); wout = P.dram_in("wout", [1024, 1024])
    modt_d = P.dram_in("modt", [128, 6 * ntiles * 8]); gn_d = P.dram_in("gn", [128, 8])
    wg = P.dram_in("wg", [NE, 1024, DFF]); wu = P.dram_in("wu", [NE, 1024, DFF]); wd = P.dram_in("wd", [NE, DFF, 1024])
    if moe:
        wr_d = P.dram_in("wr", [1024, 8]); ident_d = P.dram_in("ident", [128, 128])
    if final:
        fn_d = P.dram_in("fn", [128, 8]); outT = P.dram_out("outT", [1024, TC])
    h2T = P.dram_out("h2T", [1024, TC])
    ones = P.sb([128, 128], F32, "ones"); P.add("pool", lambda e: e.memset(ones[:], 1.0), w=["ones"])
    epsb = P.sb([128, 1], F32, "epsb"); P.add("pool", lambda e: e.memset(epsb[:], EPS), w=["epsb"])
    modt = P.sb([128, 6, ntiles, 8], F32, "modt")
    P.dma("sp", modt[:].rearrange("p a b c -> p (a b c)"), modt_d[:, :], w=["modt"])
    gn = P.sb([128, 8], F32, "gn"); P.dma("sp", gn[:], gn_d[:, :], w=["gn"])
    A2 = P.sb([128, ntiles, 8], F32, "A2")
    for t in range(ntiles):
        P.add("dve", lambda e, t=t: e.scalar_tensor_tensor(out=A2[:, t, :], in0=modt[:, 4, t, :], scalar=1.0, in1=gn[:, :], op0=ALU.add, op1=ALU.mult), r=["modt", "gn"], w=["modc"])
    wo = P.sb([128, 8, 1024], BF16, "wo")
    for k in range(8):
        P.dma("pool", wo[:, k, :], wout[k * 128:(k + 1) * 128, :], w=["wo"])
    BIG = P.sb([128, 8 * TC], F32, "yacc")
    yacc = BIG[:, :].rearrange("p (d t) -> p d t", d=8)

    def carve(i):
        return BIG[:, i * 2048:(i + 1) * 2048].rearrange("p (k t) -> p k t", k=8)
    ht, h1, sq, tmp, uf = carve(0), carve(1), carve(2), carve(3), carve(4)
    rs = BIG[:, 5 * 2048:5 * 2048 + 256]
    mixb = P.sb([128, 8, 256], BF16, "mixb")
    uall = P.sb([128, 8, TC], BF16, "uall")
    h1b = P.sb([128, 8, 256], F32, "h1b"); rsb = P.sb([128, 256], F32, "rsb")
    ssps = P.ps([128, 512], F32, "ssps"); misc = P.ps([128, 512], F32, "misc")
    if moe:
        wr = P.sb([128, 8, 8], F32, "wr"); P.dma("sp", wr[:], wr_d.rearrange("(k p) e -> p k e", p=128), w=["wr"])
        ident = P.sb([128, 128], F32, "ident"); P.dma("sp", ident[:], ident_d[:, :], w=["ident"])
        lg = P.sb([128, 2, 8], F32, "lg"); l2 = P.sb([128, 2, 8], F32, "l2"); mk1 = P.sb([128, 2, 8], F32, "mk1"); mk2 = P.sb([128, 2, 8], F32, "mk2")
        comb = P.sb([128, NS, 8], F32, "comb"); m12 = P.sb([128, 2, 4], F32, "m12")
        rep = Rot([P.sb([128, 128], F32, f"rep{i}") for i in range(2)], "rep")
        cB = P.sb([128, TC], F32, "cB")
        lgps = ssps[:, 384:400].rearrange("p (s e) -> p s e", e=8)
    if final:
        fng = P.sb([128, 8], F32, "fng"); P.dma("sp", fng[:], fn_d[:, :], w=["fng"])
    hv = hT.rearrange("(k p) t -> p k t", p=128); mv = mixT.rearrange("(k p) t -> p k t", p=128); ov = h2T.rearrange("(k p) t -> p k t", p=128)

    def add1(eng, fn, r=(), w=()):
        return P.add(eng, fn, list(r) + ["BIG"], w)

    def phase1(ti, t0):
        P.dma("sp", ht[:, :, :n], hv[:, :, t0:t0 + n], r=["BIG"], w=["ht"])
        P.dma("pool", mixb[:, :, :n], mv[:, :, t0:t0 + n], w=["mixb"])
        for dm in range(8):
            for k in range(8):
                add1("pe", lambda e, k=k, dm=dm: e.matmul(misc[:, 0:n], wo[:, k, dm * 128:(dm + 1) * 128], mixb[:, k, :n], start=(k == 0), stop=(k == 7)), r=["wo", "mixb"], w=["misc"])
            add1("dve", lambda e, dm=dm: e.scalar_tensor_tensor(out=h1[:, dm, :n], in0=misc[:, 0:n], scalar=modt[:, 2, ti, dm:dm + 1], in1=ht[:, dm, :n], op0=ALU.mult, op1=ALU.add),
                 r=["misc", "ht", "modt"], w=["h1", "misc"])
        P.dma("sp", ov[:, :, t0:t0 + n], h1[:, :, :n], r=["h1", "BIG"], w=[("h1d", ti)])
        add1("act", lambda e: e.activation(out=sq[:, :, :n], in_=h1[:, :, :n], func=AF.Square), r=["h1"], w=["sq"])
        for k in range(8):
            add1("pe", lambda e, k=k: e.matmul(ssps[:, :n], ones[:, :], sq[:, k, :n], start=(k == 0), stop=(k == 7)), r=["sq", "ones"], w=["ssps"])
        add1("act", lambda e: e.activation(out=rs[:, :n], in_=ssps[:, :n], func=AF.Sqrt, scale=1.0 / 1024, bias=epsb[:, 0:1]), r=["ssps", "epsb"], w=["rs", "ssps"])
        add1("dve", lambda e: e.reciprocal(out=rs[:, :n], in_=rs[:, :n]), r=["rs"], w=["rs"])
        for k in range(8):
            add1("dve", lambda e, k=k: e.tensor_tensor(out=tmp[:, k, :n], in0=h1[:, k, :n], in1=rs[:, :n], op=ALU.mult), r=["h1", "rs"], w=[("tmp", k)])
            if moe:
                add1("act", lambda e, k=k: e.activation(out=uf[:, k, :n], in_=tmp[:, k, :n], func=AF.Identity, scale=A2[:, ti, k:k + 1], bias=modt[:, 3, ti, k:k + 1]),
                     r=[("tmp", k), "modc", "modt"], w=[("uf", k)])
                add1("pool", lambda e, k=k: e.tensor_copy(out=uall[:, k, t0:t0 + n], in_=uf[:, k, :n]), r=[("uf", k)], w=["uall"])
            else:
                add1("act", lambda e, k=k: e.activation(out=uall[:, k, t0:t0 + n], in_=tmp[:, k, :n], func=AF.Identity, scale=A2[:, ti, k:k + 1], bias=modt[:, 3, ti, k:k + 1]),
                     r=[("tmp", k), "modc", "modt"], w=["uall"])
        if moe:
            for s in range(2):
                for k in range(8):
                    add1("pe", lambda e, k=k, s=s: e.matmul(lgps[:, s, :], uf[:, k, s * 128:(s + 1) * 128], wr[:, k, :], start=(k == 0), stop=(k == 7)),
                         r=[("uf", kk) for kk in range(8)] + ["wr"], w=["ssps"])
            add1("dve", lambda e: e.tensor_copy(out=lg[:, :, :], in_=lgps[:, :, :]), r=["ssps"], w=["lg", "ssps"])
            for s in range(2):
                gs = ti * 2 + s
                add1("dve", lambda e, s=s: e.reduce_max(out=m12[:, s, 0:1], in_=lg[:, s, :], axis=AX.X), r=["lg"], w=["m12"])
                add1("dve", lambda e, s=s: e.tensor_scalar(out=mk1[:, s, :], in0=lg[:, s, :], scalar1=m12[:, s, 0:1], scalar2=None, op0=ALU.is_equal), r=["lg", "m12"], w=["mk1"])
                add1("dve", lambda e, s=s: e.scalar_tensor_tensor(out=l2[:, s, :], in0=mk1[:, s, :], scalar=-1e30, in1=lg[:, s, :], op0=ALU.mult, op1=ALU.add), r=["mk1", "lg"], w=["l2"])
                add1("dve", lambda e, s=s: e.reduce_max(out=m12[:, s, 1:2], in_=l2[:, s, :], axis=AX.X), r=["l2", "m12"], w=["m12"])
                add1("dve", lambda e, s=s: e.tensor_scalar(out=mk2[:, s, :], in0=l2[:, s, :], scalar1=m12[:, s, 1:2], scalar2=None, op0=ALU.is_equal), r=["l2", "m12"], w=["mk2"])
                add1("dve", lambda e, s=s: e.tensor_tensor(out=m12[:, s, 2:3], in0=m12[:, s, 1:2], in1=m12[:, s, 0:1], op=ALU.subtract), r=["m12"], w=["m12"])
                add1("act", lambda e, s=s: e.activation(out=m12[:, s, 2:3], in_=m12[:, s, 2:3], func=AF.Exp), r=["m12"], w=["m12"])
                add1("dve", lambda e, s=s: e.tensor_scalar(out=m12[:, s, 3:4], in0=m12[:, s, 2:3], scalar1=1.0, scalar2=None, op0=ALU.add), r=["m12"], w=["m12"])
                add1("dve", lambda e, s=s: e.reciprocal(out=m12[:, s, 3:4], in_=m12[:, s, 3:4]), r=["m12"], w=["m12"])
                add1("dve", lambda e, s=s: e.tensor_tensor(out=m12[:, s, 2:3], in0=m12[:, s, 2:3], in1=m12[:, s, 3:4], op=ALU.mult), r=["m12"], w=["m12"])
                add1("dve", lambda e, s=s, gs=gs: e.tensor_scalar(out=comb[:, gs, :], in0=mk1[:, s, :], scalar1=m12[:, s, 3:4], scalar2=None, op0=ALU.mult), r=["mk1", "m12"], w=["comb"])
                add1("dve", lambda e, s=s, gs=gs: e.scalar_tensor_tensor(out=comb[:, gs, :], in0=mk2[:, s, :], scalar=m12[:, s, 2:3], in1=comb[:, gs, :], op0=ALU.mult, op1=ALU.add),
                     r=["mk2", "m12", "comb"], w=["comb"])

    for ti, (t0, _) in enumerate(CT):
        phase1(ti, t0)

    P.add("pool", lambda e: e.memset(BIG[:, :], 0.0), w=["BIG", "yacc"])
    hh = Rot([P.sb([128, 4, 256], BF16, f"hh{i}") for i in range(2)], "hh")
    sg = Rot([P.sb([128, 256], F32, f"sg{i}") for i in range(2)], "sg")
    t1 = Rot([P.sb([128, 256], F32, f"t1{i}") for i in range(2)], "t1")
    wgs = Rot([P.sb([128, 8, 512], BF16, f"wgs{i}") for i in range(2)], "wgs")
    wus = Rot([P.sb([128, 8, 512], BF16, f"wus{i}") for i in range(2)], "wus")
    wds = Rot([P.sb([128, 4, 1024], BF16, f"wds{i}") for i in range(2)], "wds")
    gups = Rot([P.ps([128, 2, 256], F32, f"gups{i}") for i in range(2)], "gups")
    yps = [P.ps([128, 2, 256], F32, f"yps{i}") for i in range(4)]

    def gate_up(ex, wgt, wgk, wut, wuk, t0):
        hht, hhk = hh.next()
        for f in range(4):
            gp, gk = gups.next()
            for k in range(8):
                P.add("pe", lambda e, k=k, f=f, gp=gp: e.matmul(gp[:, 0, :n], wgt[:, k, f * 128:(f + 1) * 128], uall[:, k, t0:t0 + n], start=(k == 0), stop=(k == 7)), r=["uall", wgk], w=[gk])
            for k in range(8):
                P.add("pe", lambda e, k=k, f=f, gp=gp: e.matmul(gp[:, 1, :n], wut[:, k, f * 128:(f + 1) * 128], uall[:, k, t0:t0 + n], start=(k == 0), stop=(k == 7)), r=["uall", wuk], w=[gk])
            sgt, sgk = sg.next()
            P.add("act", lambda e, gp=gp, sgt=sgt: e.activation(out=sgt[:, :n], in_=gp[:, 0, :n], func=AF.Silu), r=[gk], w=[sgk, gk])
            if moe:
                tt, tk = t1.next()
                P.add("dve", lambda e, gp=gp, sgt=sgt, tt=tt: e.tensor_tensor(out=tt[:, :n], in0=sgt[:, :n], in1=gp[:, 1, :n], op=ALU.mult), r=[gk, sgk], w=[tk, gk])
                P.add("pool", lambda e, tt=tt, f=f: e.tensor_tensor(out=hht[:, f, :n], in0=tt[:, :n], in1=cB[:, t0:t0 + n], op=ALU.mult), r=[tk, "cB"], w=[(hhk, f)])
            else:
                P.add("dve", lambda e, gp=gp, sgt=sgt, f=f: e.tensor_tensor(out=hht[:, f, :n], in0=sgt[:, :n], in1=gp[:, 1, :n], op=ALU.mult), r=[gk, sgk], w=[(hhk, f), gk])
        return hht, hhk

    def down(hht, hhk, wdt, wdk, t0):
        for dm in range(8):
            for f in range(4):
                P.add("pe", lambda e, f=f, dm=dm: e.matmul(yps[dm // 2][:, dm % 2, :n], wdt[:, f, dm * 128:(dm + 1) * 128], hht[:, f, :n], start=(f == 0 and dm % 2 == 0), stop=(f == 3),
                                                           skip_group_check=True), r=[(hhk, f), wdk], w=[("yps", dm // 2)])
        for b in range(4):
            P.add("dve", lambda e, b=b: e.tensor_tensor(out=yacc[:, 2 * b:2 * b + 2, t0:t0 + n], in0=yacc[:, 2 * b:2 * b + 2, t0:t0 + n], in1=yps[b][:, :, :n], op=ALU.add),
                  r=[("yps", b), "yacc"], w=["yacc", ("yps", b)])

    for ex in range(NE):
        if moe:
            for s0 in range(0, NS, 4):
                ns_ = min(4, NS - s0)
                for s in range(ns_):
                    rp, rk = rep.next()
                    P.add("dve", lambda e, rp=rp, s=s, s0=s0, ex=ex: e.tensor_scalar(out=rp[:, :], in0=ones[:, :], scalar1=comb[:, s0 + s, ex:ex + 1], scalar2=None, op0=ALU.mult), r=["comb", "ones"], w=[rk])
                    P.add("pe", lambda e, rp=rp, s=s: e.matmul(misc[:, s * 128:(s + 1) * 128], rp[:, :], ident[:, :], start=True, stop=True), r=[rk, "ident"], w=["misc"])
                P.add("act", lambda e, s0=s0, ns_=ns_: e.copy(out=cB[:, s0 * 128:(s0 + ns_) * 128], in_=misc[:, :ns_ * 128]), r=["misc"], w=["cB", "misc"])
        for fg in range(NFG):
            wgt, wgk = wgs.next(); wut, wuk = wus.next(); wdt, wdk = wds.next()
            P.dma("pool", wgt[:], wg[ex].rearrange("(k p) f -> p k f", p=128)[:, :, fg * 512:(fg + 1) * 512], w=[wgk])
            P.dma("pool", wut[:], wu[ex].rearrange("(k p) f -> p k f", p=128)[:, :, fg * 512:(fg + 1) * 512], w=[wuk])
            P.dma("pool", wdt[:], wd[ex, fg * 512:(fg + 1) * 512, :].rearrange("(f p) d -> p f d", p=128), w=[wdk])
            pend = gate_up(ex, wgt, wgk, wut, wuk, CT[0][0])
            for ti, (t0, _) in enumerate(CT):
                nxt = gate_up(ex, wgt, wgk, wut, wuk, CT[ti + 1][0]) if ti + 1 < ntiles else None
                down(pend[0], pend[1], wdt, wdk, t0)
                pend = nxt

    def phase3(ti, t0):
        P.dma("sp", h1b[:, :, :n], ov[:, :, t0:t0 + n], r=[("h1d", ti)], w=["h1b"])
        for dm in range(8):
            P.add("dve", lambda e, dm=dm: e.scalar_tensor_tensor(out=yacc[:, dm, t0:t0 + n], in0=yacc[:, dm, t0:t0 + n], scalar=modt[:, 5, ti, dm:dm + 1], in1=h1b[:, dm, :n], op0=ALU.mult, op1=ALU.add),
                  r=["yacc", "h1b", "modt"], w=["yacc"])
        P.dma("sp", ov[:, :, t0:t0 + n], yacc[:, :, t0:t0 + n], r=["yacc", "h1b"], w=[("h1d", ti)])
        if final:
            P.add("act", lambda e: e.activation(out=h1b[:, :, :n], in_=yacc[:, :, t0:t0 + n], func=AF.Square), r=["yacc"], w=["h1b"])
            for k in range(8):
                P.add("pe", lambda e, k=k: e.matmul(ssps[:, :n], ones[:, :], h1b[:, k, :n], start=(k == 0), stop=(k == 7)), r=["h1b", "ones"], w=["ssps"])
            P.add("act", lambda e: e.activation(out=rsb[:, :n], in_=ssps[:, :n], func=AF.Sqrt, scale=1.0 / 1024, bias=epsb[:, 0:1]), r=["ssps", "epsb"], w=["rsb", "ssps"])
            P.add("dve", lambda e: e.reciprocal(out=rsb[:, :n], in_=rsb[:, :n]), r=["rsb"], w=["rsb"])
            for k in range(8):
                P.add("dve", lambda e, k=k: e.scalar_tensor_tensor(out=h1b[:, k, :n], in0=yacc[:, k, t0:t0 + n], scalar=fng[:, k:k + 1], in1=rsb[:, :n], op0=ALU.mult, op1=ALU.mult),
                      r=["yacc", "rsb", "fng", "ssps"], w=["h1b"])
            P.dma("sp", outT.rearrange("(k p) t -> p k t", p=128)[:, :, t0:t0 + n], h1b[:, :, :n], r=["h1b"])

    for ti, (t0, _) in enumerate(CT):
        phase3(ti, t0)
    print("C2 ops", P.n_ops())
    return P.finish()


NTOK = 4352
NCH = 34
QT = [(0, 256)] + [(256 + 512 * i, 512) for i in range(8)]


def bconsts(P):
    C = {}
    C["ones"] = P.sb([128, 128], F32, "ones")
    P.add("pool", lambda e: e.memset(C["ones"][:], 1.0), w=["ones"])
    C["epsb"] = P.sb([128, 1], F32, "epsb")
    P.add("pool", lambda e: e.memset(C["epsb"][:], EPS), w=["epsb"])
    return C


def emit_mla(P, C, D, mixT):
    scale = 96 ** -0.5
    qng = P.sb([128, 2], F32, "qng")
    P.dma("sp", qng[:], D["c_qng"][:, :], w=["qng"])
    kvg = P.sb([128, 1], F32, "kvg")
    P.dma("sp", kvg[:], D["c_kvg"][:, :], w=["kvg"])
    wq = P.sb([128, 2, 2, 96], BF16, "wq")
    wqP = P.sb([128, 2, 2, 96], BF16, "wqP")
    P.dma("pool", wq[:].rearrange("p k h c -> p k (h c)"), D["c_wq"].rearrange("(k p) c -> p k c", p=128), w=["wq"])
    P.dma("pool", wqP[:].rearrange("p k h c -> p k (h c)"), D["c_wqP"].rearrange("(k p) c -> p k c", p=128), w=["wqP"])
    wkn = P.sb([128, 2, 96], BF16, "wkn")
    P.dma("pool", wkn[:].rearrange("p h c -> p (h c)"), D["c_wkn"][:, :], w=["wkn"])
    wv = P.sb([128, 128], BF16, "wv")
    P.dma("pool", wv[:], D["c_wv"][:, :], w=["wv"])
    sel = P.sb([32, 96], BF16, "sel")
    P.dma("pool", sel[:], D["c_sel"][:, :], w=["sel"])
    qT = P.sb([96, 2, NTOK], BF16, "mqT")
    kT = P.sb([96, 2, NTOK], BF16, "mkT")
    vaug = P.sb([128, NCH, 2, 65], BF16, "mvaug")
    P.add("pool", lambda e: e.memset(vaug[:], 1.0), w=["mvaug"])
    ckvn = P.sb([128, NTOK], BF16, "ckvn")
    cq = P.sb([128, 2, 512], F32, "m_cq")
    ckv = P.sb([128, 512], F32, "m_ckv")
    sq = P.sb([128, 2, 512], F32, "m_sq")
    rs = P.sb([128, 512], F32, "m_rs")
    rs2 = P.sb([128, 512], F32, "m_rs2")
    cqn = P.sb([128, 2, 512], BF16, "m_cqn")
    ct = P.sb([96, 512], F32, "m_ct")
    st = P.sb([96, 512], F32, "m_st")
    kr = P.sb([32, 512], F32, "m_kr")
    krP = P.sb([32, 512], F32, "m_krP")
    krr = P.sb([32, 512], BF16, "m_krr")
    t1 = P.sb([96, 512], F32, "m_t1")
    t2 = P.sb([96, 512], F32, "m_t2")
    ssps = P.ps([128, 512], F32, "m_ssps")
    pA = P.ps([128, 512], F32, "m_pA")
    pB = P.ps([128, 512], F32, "m_pB")

    ct32 = P.sb([32, 512], F32, "m_ct32")
    st32 = P.sb([32, 512], F32, "m_st32")

    def tile_all(t0, n):
        P.dma("sp", cq[:, :, :n], D["c_cq"].rearrange("(k p) t -> p k t", p=128)[:, :, t0:t0 + n], w=["m_cq"])
        P.dma("sp", ckv[:, :n], D["c_ckv"][:, t0:t0 + n], w=["m_ckv"])
        P.dma("sp", ct[:, :n], D["c_ct96"][:, t0:t0 + n], w=["m_ct"])
        P.dma("sp", st[:, :n], D["c_st96"][:, t0:t0 + n], w=["m_st"])
        P.dma("sp", ct32[:, :n], D["c_ct96"][64:96, t0:t0 + n], w=["m_ct32"])
        P.dma("sp", st32[:, :n], D["c_st96"][64:96, t0:t0 + n], w=["m_st32"])
        P.dma("sp", kr[:, :n], D["c_kr"][:, t0:t0 + n], w=["m_kr"])
        P.dma("sp", krP[:, :n], D["c_krP"][:, t0:t0 + n], w=["m_krP"])
        P.add("act", lambda e: e.activation(out=sq[:, :, :n], in_=cq[:, :, :n], func=AF.Square), r=["m_cq"], w=["m_sq"])
        for k in range(2):
            P.add("pe", lambda e, k=k: e.matmul(ssps[:, :n], C["ones"][:, :], sq[:, k, :n], start=(k == 0), stop=(k == 1)), r=["m_sq", "ones"], w=["m_ssps"])
        P.add("act", lambda e: e.activation(out=rs[:, :n], in_=ssps[:, :n], func=AF.Sqrt, scale=1.0 / 256, bias=C["epsb"][:, 0:1]), r=["m_ssps", "epsb"], w=["m_rs"])
        P.add("dve", lambda e: e.reciprocal(out=rs[:, :n], in_=rs[:, :n]), r=["m_rs"], w=["m_rs"])
        for k in range(2):
            P.add("dve", lambda e, k=k: e.scalar_tensor_tensor(out=cqn[:, k, :n], in0=cq[:, k, :n], scalar=qng[:, k:k + 1], in1=rs[:, :n], op0=ALU.mult, op1=ALU.mult),
                  r=["m_cq", "qng", "m_rs"], w=["m_cqn"])
        P.add("act", lambda e: e.activation(out=sq[:, 0, :n], in_=ckv[:, :n], func=AF.Square), r=["m_ckv"], w=["m_sq"])
        P.add("pe", lambda e: e.matmul(ssps[:, :n], C["ones"][:, :], sq[:, 0, :n], start=True, stop=True), r=["m_sq", "ones"], w=["m_ssps"])
        P.add("act", lambda e: e.activation(out=rs2[:, :n], in_=ssps[:, :n], func=AF.Sqrt, scale=1.0 / 128, bias=C["epsb"][:, 0:1]), r=["m_ssps", "epsb"], w=["m_rs2"])
        P.add("dve", lambda e: e.reciprocal(out=rs2[:, :n], in_=rs2[:, :n]), r=["m_rs2"], w=["m_rs2"])
        P.add("dve", lambda e: e.scalar_tensor_tensor(out=ckvn[:, t0:t0 + n], in0=ckv[:, :n], scalar=kvg[:, 0:1], in1=rs2[:, :n], op0=ALU.mult, op1=ALU.mult),
              r=["m_ckv", "kvg", "m_rs2"], w=["ckvn"])
        P.add("pool", lambda e: e.tensor_tensor(out=kr[:, :n], in0=kr[:, :n], in1=ct32[:, :n], op=ALU.mult), r=["m_kr", "m_ct32"], w=["m_kr"])
        P.add("pool", lambda e: e.tensor_tensor(out=krP[:, :n], in0=krP[:, :n], in1=st32[:, :n], op=ALU.mult), r=["m_krP", "m_st32"], w=["m_krP"])
        P.add("pool", lambda e: e.tensor_tensor(out=krr[:, :n], in0=kr[:, :n], in1=krP[:, :n], op=ALU.add), r=["m_kr", "m_krP"], w=["m_krr"])
        for h in range(2):
            for k in range(2):
                P.add("pe", lambda e, k=k, h=h: e.matmul(pA[:96, :n], wq[:, k, h, :], cqn[:, k, :n], start=(k == 0), stop=(k == 1)), r=["wq", "m_cqn"], w=["m_pA"])
            for k in range(2):
                P.add("pe", lambda e, k=k, h=h: e.matmul(pB[:96, :n], wqP[:, k, h, :], cqn[:, k, :n], start=(k == 0), stop=(k == 1)), r=["wqP", "m_cqn"], w=["m_pB"])
            P.add("dve", lambda e: e.tensor_tensor(out=t1[:, :n], in0=pA[:96, :n], in1=ct[:, :n], op=ALU.mult), r=["m_pA", "m_ct"], w=["m_t1"])
            P.add("dve", lambda e: e.tensor_tensor(out=t2[:, :n], in0=pB[:96, :n], in1=st[:, :n], op=ALU.mult), r=["m_pB", "m_st"], w=["m_t2"])
            P.add("pool", lambda e, h=h: e.tensor_tensor(out=qT[:, h, t0:t0 + n], in0=t1[:, :n], in1=t2[:, :n], op=ALU.add), r=["m_t1", "m_t2"], w=["mqT"])
            P.add("pe", lambda e, h=h: e.matmul(pA[:96, :n], wkn[:, h, :], ckvn[:, t0:t0 + n], start=True, stop=False), r=["wkn", "ckvn"], w=["m_pA"])
            P.add("pe", lambda e, h=h: e.matmul(pA[:96, :n], sel[:, :], krr[:, :n], start=False, stop=True), r=["sel", "m_krr"], w=["m_pA"])
            P.add("act", lambda e, h=h: e.copy(out=kT[:, h, t0:t0 + n], in_=pA[:96, :n]), r=["m_pA"], w=["mkT"])
        for s in range(n // 128):
            c = (t0 + s * 128) // 128
            P.add("pe", lambda e, s=s: e.matmul(pB[:, 0:128], ckvn[:, t0 + s * 128:t0 + (s + 1) * 128], wv[:, :], start=True, stop=True), r=["ckvn", "wv"], w=["m_pB"])
            P.add("act", lambda e, c=c: e.copy(out=vaug[:, c, :, 0:64], in_=pB[:, 0:128].rearrange("p (h d) -> p h d", h=2)), r=["m_pB"], w=["mvaug"])

    for (t0, n) in QT:
        tile_all(t0, n)

    sps = Rot([P.ps([128, 512], F32, f"m_sps{i}") for i in range(3)], "m_sps")
    ops_ = Rot([P.ps([128, 512], F32, f"m_ops{i}") for i in range(2)], "m_ops")
    E = Rot([P.sb([128, 512], BF16, f"m_E{i}") for i in range(4)], "m_E")
    oa = Rot([P.sb([65, 512], F32, f"m_oa{i}") for i in range(2)], "m_oa")
    rc = Rot([P.sb([64, 512], F32, f"m_rc{i}") for i in range(2)], "m_rc")
    oo = Rot([P.sb([64, 512], F32, f"m_oo{i}") for i in range(2)], "m_oo")

    def attn_tile(h, t0, n, chunks):
        op_, ok = ops_.next()
        nchk = len(chunks)
        pend = []

        def issue_s(c):
            sp, sk = sps.next()
            P.add("pe", lambda e, sp=sp, c=c: e.matmul(sp[:, :n], kT[:, h, c * 128:(c + 1) * 128], qT[:, h, t0:t0 + n], start=True, stop=True), r=["mkT", "mqT"], w=[sk])
            pend.append((sp, sk))
        issue_s(chunks[0])
        if nchk > 1:
            issue_s(chunks[1])
        for ci, c in enumerate(chunks):
            if ci + 2 < nchk:
                issue_s(chunks[ci + 2])
            sp, sk = pend.pop(0)
            Et, ek = E.next()
            P.add("act", lambda e, sp=sp, Et=Et: e.activation(out=Et[:, :n], in_=sp[:, :n], func=AF.Exp, scale=scale), r=[sk], w=[ek])
            P.add("pe", lambda e, Et=Et, c=c, ci=ci: e.matmul(op_[:65, :n], vaug[:, c, h, :], Et[:, :n], start=(ci == 0), stop=(ci == nchk - 1)), r=[ek, "mvaug"], w=[ok])
        oat, oak = oa.next()
        P.add("act", lambda e: e.copy(out=oat[:, :n], in_=op_[:65, :n]), r=[ok], w=[oak])
        sp, sk = sps.next()
        P.add("pe", lambda e: e.matmul(sp[:64, :n], C["ones"][64:65, 0:64], oat[64:65, :n], start=True, stop=True), r=[oak, "ones"], w=[sk])
        rct, rck = rc.next()
        P.add("dve", lambda e: e.reciprocal(out=rct[:, :n], in_=sp[:64, :n]), r=[sk], w=[rck])
        oot, ook = oo.next()
        P.add("dve", lambda e: e.tensor_tensor(out=oot[:, :n], in0=oat[0:64, :n], in1=rct[:, :n], op=ALU.mult), r=[oak, rck], w=[ook])
        P.dma("sp", mixT[h * 64:(h + 1) * 64, t0:t0 + n], oot[:, :n], r=[ook])

    for h in range(2):
        attn_tile(h, 0, 256, [0, 1])
        for i in range(8):
            attn_tile(h, 256 + 512 * i, 512, list(range(NCH)))


MLA_IN = [("c_cq", [256, NTOK]), ("c_ckv", [128, NTOK]), ("c_kr", [32, NTOK]), ("c_krP", [32, NTOK]), ("c_ct96", [96, NTOK]), ("c_st96", [96, NTOK]),
          ("c_qng", [128, 2]), ("c_kvg", [128, 1]), ("c_wq", [256, 192]), ("c_wqP", [256, 192]), ("c_wkn", [128, 192]), ("c_wv", [128, 128]), ("c_sel", [32, 96])]


def build_mla():
    P = Prog()
    D = {nm: P.dram_in(nm, shp) for nm, shp in MLA_IN}
    out = P.dram_out("mix", [128, NTOK])
    C = bconsts(P)
    emit_mla(P, C, D, out)
    print("mla ops", P.n_ops())
    return P.finish()


SWA_IN = [("a_q", [128, NTOK]), ("a_qP", [128, NTOK]), ("a_k", [64, NTOK]), ("a_kP", [64, NTOK]), ("a_vtok", [NTOK, 64]),
          ("a_cos", [64, NTOK]), ("a_sin", [64, NTOK]), ("a_sink", [128, 2]), ("a_maskP", [128, 128]), ("a_maskN", [128, 128])]


def build_swa():
    P = Prog()
    D = {nm: P.dram_in(nm, shp) for nm, shp in SWA_IN}
    out = P.dram_out("mix", [128, NTOK])
    C = bconsts(P)
    scale = 64 ** -0.5
    aqT = P.sb([64, 2, NTOK], BF16, "aqT")
    akT = P.sb([64, NTOK], BF16, "akT")
    vaug = P.sb([128, NCH, 65], BF16, "avaug")
    P.add("pool", lambda e: e.memset(vaug[:], 1.0), w=["avaug"])
    P.dma("pool", vaug[:, :, 0:64], D["a_vtok"].rearrange("(c p) d -> p c d", p=128), w=["avaug"])
    es = P.sb([128, 2], F32, "a_es")
    P.dma("sp", es[:], D["a_sink"][:, :], w=["a_es"])
    P.add("act", lambda e: e.activation(out=es[:], in_=es[:], func=AF.Exp), r=["a_es"], w=["a_es"])
    mP = P.sb([128, 128], BF16, "a_mP")
    mN = P.sb([128, 128], BF16, "a_mN")
    P.dma("pool", mP[:], D["a_maskP"][:, :], w=["a_mP"])
    P.dma("pool", mN[:], D["a_maskN"][:, :], w=["a_mN"])
    q = P.sb([64, 2, 512], F32, "a_q")
    qP = P.sb([64, 2, 512], F32, "a_qP")
    k = P.sb([64, 512], F32, "a_k")
    kP = P.sb([64, 512], F32, "a_kP")
    cs = P.sb([64, 512], F32, "a_cs")
    sn = P.sb([64, 512], F32, "a_sn")
    t1 = P.sb([64, 512], F32, "a_t1")
    t2 = P.sb([64, 512], F32, "a_t2")

    def rope_tile(t0, n):
        P.dma("sp", q[:, :, :n], D["a_q"].rearrange("(h d) t -> d h t", d=64)[:, :, t0:t0 + n], w=["a_q"])
        P.dma("sp", qP[:, :, :n], D["a_qP"].rearrange("(h d) t -> d h t", d=64)[:, :, t0:t0 + n], w=["a_qP"])
        P.dma("sp", k[:, :n], D["a_k"][:, t0:t0 + n], w=["a_k"])
        P.dma("sp", kP[:, :n], D["a_kP"][:, t0:t0 + n], w=["a_kP"])
        P.dma("sp", cs[:, :n], D["a_cos"][:, t0:t0 + n], w=["a_cs"])
        P.dma("sp", sn[:, :n], D["a_sin"][:, t0:t0 + n], w=["a_sn"])
        for h in range(2):
            P.add("dve", lambda e, h=h: e.tensor_tensor(out=t1[:, :n], in0=q[:, h, :n], in1=cs[:, :n], op=ALU.mult), r=["a_q", "a_cs"], w=["a_t1"])
            P.add("pool", lambda e, h=h: e.tensor_tensor(out=t2[:, :n], in0=qP[:, h, :n], in1=sn[:, :n], op=ALU.mult), r=["a_qP", "a_sn"], w=["a_t2"])
            P.add("dve", lambda e, h=h: e.tensor_tensor(out=aqT[:, h, t0:t0 + n], in0=t1[:, :n], in1=t2[:, :n], op=ALU.add), r=["a_t1", "a_t2"], w=["aqT"])
        P.add("dve", lambda e: e.tensor_tensor(out=t1[:, :n], in0=k[:, :n], in1=cs[:, :n], op=ALU.mult), r=["a_k", "a_cs"], w=["a_t1"])
        P.add("pool", lambda e: e.tensor_tensor(out=t2[:, :n], in0=kP[:, :n], in1=sn[:, :n], op=ALU.mult), r=["a_kP", "a_sn"], w=["a_t2"])
        P.add("dve", lambda e: e.tensor_tensor(out=akT[:, t0:t0 + n], in0=t1[:, :n], in1=t2[:, :n], op=ALU.add), r=["a_t1", "a_t2"], w=["akT"])

    for (t0, n) in QT:
        rope_tile(t0, n)

    sp = [P.ps([128, 512], F32, f"a_sp{i}") for i in range(3)]
    ops_ = Rot([P.ps([128, 512], F32, f"a_op{i}") for i in range(2)], "a_op")
    bc = P.ps([128, 512], F32, "a_bc")
    E = Rot([P.sb([128, 5 * 256], BF16, f"a_E{i}") for i in range(2)], "a_E")
    oa = Rot([P.sb([65, 256], F32, f"a_oa{i}") for i in range(2)], "a_oa")
    rc = Rot([P.sb([64, 256], F32, f"a_rc{i}") for i in range(2)], "a_rc")
    oo = Rot([P.sb([64, 256], F32, f"a_oo{i}") for i in range(2)], "a_oo")
    outv = out.rearrange("(h d) t -> d h t", d=64)

    def block(q0, chunks):
        nck = len(chunks)
        for ci, (c, mk) in enumerate(chunks):
            b, off = ci // 2, (ci % 2) * 256
            P.add("pe", lambda e, b=b, off=off, c=c: e.matmul(sp[b][:, off:off + 256].rearrange("p (h q) -> p h q", h=2), akT[:, c * 128:(c + 1) * 128],
                                                             aqT[:, :, q0:q0 + 128], start=True, stop=True), r=["akT", "aqT"], w=[("a_sp", b)])
        Et, ek = E.next()
        for b in range((nck + 1) // 2):
            w_ = min(512, nck * 256 - b * 512)
            P.add("act", lambda e, b=b, w_=w_: e.activation(out=Et[:, b * 512:b * 512 + w_], in_=sp[b][:, :w_], func=AF.Exp, scale=scale), r=[("a_sp", b)], w=[ek])
        for ci, (c, mk) in enumerate(chunks):
            if mk is None:
                continue
            m = mP if mk == "P" else mN
            for h in range(2):
                o_ = ci * 256 + h * 128
                P.add("pool", lambda e, o_=o_, m=m: e.tensor_tensor(out=Et[:, o_:o_ + 128], in0=Et[:, o_:o_ + 128], in1=m[:, :], op=ALU.mult), r=[ek, "a_mP", "a_mN"], w=[ek])
        op_, ok = ops_.next()
        for ci, (c, mk) in enumerate(chunks):
            P.add("pe", lambda e, ci=ci, c=c: e.matmul(op_[:65, :256], vaug[:, c, :], Et[:, ci * 256:(ci + 1) * 256], start=(ci == 0), stop=(ci == nck - 1)), r=[ek, "avaug"], w=[ok])
        oat, oak = oa.next()
        P.add("act", lambda e: e.copy(out=oat[:, :], in_=op_[:65, :256]), r=[ok], w=[oak])
        for h in range(2):
            P.add("dve", lambda e, h=h: e.tensor_scalar(out=oat[64:65, h * 128:(h + 1) * 128], in0=oat[64:65, h * 128:(h + 1) * 128], scalar1=es[64:65, h:h + 1], scalar2=None, op0=ALU.add),
                  r=[oak, "a_es"], w=[oak])
        P.add("pe", lambda e: e.matmul(bc[:64, :256], C["ones"][64:65, 0:64], oat[64:65, :], start=True, stop=True), r=[oak, "ones"], w=["a_bc"])
        rct, rck = rc.next()
        P.add("dve", lambda e: e.reciprocal(out=rct[:, :], in_=bc[:64, :256]), r=["a_bc"], w=[rck])
        oot, ook = oo.next()
        P.add("dve", lambda e: e.tensor_tensor(out=oot[:, :], in0=oat[0:64, :], in1=rct[:, :], op=ALU.mult), r=[oak, rck], w=[ook])
        P.dma("sp", outv[:, :, q0:q0 + 128], oot[:, :].rearrange("d (h q) -> d h q", h=2), r=[ook])

    block(0, [(0, None), (1, None)])
    block(128, [(0, None), (1, None)])
    for nb in range(32):
        ch = [(0, None), (1, None)]
        if nb > 0:
            ch.append((nb + 1, "P"))
        ch.append((nb + 2, None))
        if nb < 31:
            ch.append((nb + 3, "N"))
        block(256 + 128 * nb, ch)
    print("swa ops", P.n_ops())
    return P.finish()


RET_IN = [("d_q", [128, NTOK]), ("d_qP", [128, NTOK]), ("d_k", [128, NTOK]), ("d_kP", [128, NTOK]), ("d_gate", [128, NTOK]),
          ("d_vtok", [NTOK, 128]), ("d_ktok", [NTOK, 128]), ("d_kPtok", [NTOK, 128]), ("d_cos", [64, NTOK]), ("d_sin", [64, NTOK]),
          ("d_costok", [NTOK, 64]), ("d_sintok", [NTOK, 64]), ("d_ldp", [128, 2]), ("d_ldr", [128, 4]), ("d_g", [128, 1]),
          ("d_relu", [128, 128]), ("d_rell", [128, 128]), ("d_um", [128, 128]), ("d_lm", [128, 128]), ("d_pos1", [128, 128]), ("d_posr", [128, 128]),
          ("d_pk", [128, 2]), ("d_bd", [128, 128]), ("d_bd64", [128, 128])]


def build_ret():
    P = Prog()
    D = {nm: P.dram_in(nm, shp) for nm, shp in RET_IN}
    out = P.dram_out("mix", [128, NTOK])
    C = bconsts(P)

    def ld(nm, shp, dt=F32, q="sp"):
        t = P.sb(shp, dt, "r_" + nm)
        P.dma(q, t[:], D[nm][:, :], w=["r_" + nm])
        return t
    ldp = ld("d_ldp", [128, 2]); ldr = ld("d_ldr", [128, 4]); g = ld("d_g", [128, 1])
    relu = ld("d_relu", [128, 128]); rell = ld("d_rell", [128, 128]); um = ld("d_um", [128, 128]); lm = ld("d_lm", [128, 128])
    pos1 = ld("d_pos1", [128, 128]); posr = ld("d_posr", [128, 128]); pk = ld("d_pk", [128, 2]); bd = ld("d_bd", [128, 128]); bd64 = ld("d_bd64", [128, 128])
    P.add("act", lambda e: e.activation(out=ldp[:], in_=ldp[:], func=AF.Exp), r=["r_d_ldp"], w=["r_d_ldp"])
    P.add("dve", lambda e: e.tensor_scalar(out=ldp[:], in0=ldp[:], scalar1=-1.0, scalar2=None, op0=ALU.mult), r=["r_d_ldp"], w=["r_d_ldp"])
    P.add("act", lambda e: e.activation(out=ldr[:], in_=ldr[:], func=AF.Exp), r=["r_d_ldr"], w=["r_d_ldr"])
    P.add("dve", lambda e: e.tensor_scalar(out=ldr[:], in0=ldr[:], scalar1=-1.0, scalar2=None, op0=ALU.mult), r=["r_d_ldr"], w=["r_d_ldr"])
    Qd = P.sb([128, 2, 128], F32, "r_Qd")
    for d, pt in ((0, pos1), (1, posr)):
        P.add("act", lambda e, d=d, pt=pt: e.activation(out=Qd[:, d, :], in_=pt[:, :], func=AF.Exp, scale=ldp[:, d:d + 1]), r=["r_d_ldp", "r_d_pos1", "r_d_posr"], w=["r_Qd"])
    P.add("dve", lambda e: e.tensor_scalar(out=Qd[:], in0=Qd[:], scalar1=0.125, scalar2=None, op0=ALU.mult), r=["r_Qd"], w=["r_Qd"])
    c128 = P.sb([128, 1], F32, "r_c128")
    P.add("pool", lambda e: e.memset(c128[:], 128.0), w=["r_c128"])
    cd = P.sb([128, 2], F32, "r_cd")
    for d in range(2):
        P.add("act", lambda e, d=d: e.activation(out=cd[:, d:d + 1], in_=c128[:, :], func=AF.Exp, scale=ldp[:, d:d + 1]), r=["r_d_ldp", "r_c128"], w=["r_cd"])
    Kd = P.sb([128, 2, 2], F32, "r_Kd")
    for d in range(2):
        P.add("act", lambda e, d=d: e.activation(out=Kd[:, d, :], in_=ldr[:, 2 * d:2 * d + 2], func=AF.Exp, scale=pk[:, d:d + 1]), r=["r_d_ldr", "r_d_pk"], w=["r_Kd"])
    DT = P.sb([128, 2, 128], F32, "r_DT")
    dt2 = P.sb([128, 128], F32, "r_dt2")
    for h in range(2):
        P.add("act", lambda e, h=h: e.activation(out=DT[:, h, :], in_=relu[:, :], func=AF.Exp, scale=ldr[:, h:h + 1]), r=["r_d_ldr", "r_d_relu"], w=["r_DT"])
        P.add("dve", lambda e, h=h: e.tensor_tensor(out=DT[:, h, :], in0=DT[:, h, :], in1=um[:, :], op=ALU.mult), r=["r_DT", "r_d_um"], w=["r_DT"])
        P.add("act", lambda e, h=h: e.activation(out=dt2[:, :], in_=rell[:, :], func=AF.Exp, scale=ldr[:, 2 + h:3 + h]), r=["r_d_ldr", "r_d_rell"], w=["r_dt2"])
        P.add("dve", lambda e, h=h: e.tensor_tensor(out=dt2[:, :], in0=dt2[:, :], in1=lm[:, :], op=ALU.mult), r=["r_dt2", "r_d_lm"], w=["r_dt2"])
        P.add("dve", lambda e, h=h: e.tensor_tensor(out=DT[:, h, :], in0=DT[:, h, :], in1=dt2[:, :], op=ALU.add), r=["r_DT", "r_dt2"], w=["r_DT"])
    P.add("dve", lambda e: e.tensor_scalar(out=DT[:], in0=DT[:], scalar1=0.125, scalar2=None, op0=ALU.mult), r=["r_DT"], w=["r_DT"])

    qdf = P.sb([128, NCH, 128], BF16, "r_qdf"); qdr = P.sb([128, NCH, 128], BF16, "r_qdr")
    qTb = P.sb([128, 2, NTOK], BF16, "r_qTb"); kTb = P.sb([128, NTOK], BF16, "r_kTb")
    P.add("pool", lambda e: e.memset(qTb[:], 0.0), w=["r_qTb"])
    kdf = P.sb([128, NCH, 128], BF16, "r_kdf"); kdr = P.sb([128, NCH, 128], BF16, "r_kdr")
    vt = P.sb([128, NCH, 128], BF16, "r_vt")
    vpad = P.sb([128, NCH, 2, 128], BF16, "r_vpad")
    P.add("pool", lambda e: e.memset(vpad[:], 0.0), w=["r_vpad"])
    vv = D["d_vtok"].rearrange("(c p) d -> p c d", p=128)
    P.dma("pool", vt[:], vv, w=["r_vt"])
    P.dma("pool", vpad[:, :, 0, 0:64], vv[:, :, 0:64], w=["r_vpad"])
    P.dma("pool", vpad[:, :, 1, 64:128], vv[:, :, 64:128], w=["r_vpad"])
    q = P.sb([128, 512], F32, "r_q"); qP = P.sb([128, 512], F32, "r_qP"); k = P.sb([128, 512], F32, "r_k"); kP = P.sb([128, 512], F32, "r_kP")
    cs = P.sb([128, 512], F32, "r_cs"); sn = P.sb([128, 512], F32, "r_sn")
    t1 = P.sb([128, 512], F32, "r_t1"); t2 = P.sb([128, 512], F32, "r_t2"); qr = P.sb([128, 512], F32, "r_qr")
    kt = P.sb([128, 4, 128], F32, "r_kt"); kPt = P.sb([128, 4, 128], F32, "r_kPt"); ct = P.sb([128, 4, 64], F32, "r_ct"); st = P.sb([128, 4, 64], F32, "r_st")
    krt = P.sb([128, 4, 128], F32, "r_krt"); kt2 = P.sb([128, 4, 128], F32, "r_kt2")

    def prep(t0, n):
        ns = n // 128
        c0 = t0 // 128
        for nm, t in (("d_q", q), ("d_qP", qP), ("d_k", k), ("d_kP", kP)):
            P.dma("sp", t[:, :n], D[nm][:, t0:t0 + n], w=[t.name])
        for hh in range(2):
            P.dma("sp", cs[hh * 64:(hh + 1) * 64, :n], D["d_cos"][:, t0:t0 + n], w=[cs.name])
            P.dma("sp", sn[hh * 64:(hh + 1) * 64, :n], D["d_sin"][:, t0:t0 + n], w=[sn.name])
        P.add("dve", lambda e: e.tensor_tensor(out=t1[:, :n], in0=q[:, :n], in1=cs[:, :n], op=ALU.mult), r=[q.name, cs.name], w=["r_t1"])
        P.add("pool", lambda e: e.tensor_tensor(out=t2[:, :n], in0=qP[:, :n], in1=sn[:, :n], op=ALU.mult), r=[qP.name, sn.name], w=["r_t2"])
        P.add("dve", lambda e: e.tensor_tensor(out=qr[:, :n], in0=t1[:, :n], in1=t2[:, :n], op=ALU.add), r=["r_t1", "r_t2"], w=["r_qr"])
        P.add("act", lambda e: e.copy(out=qTb[0:64, 0, t0:t0 + n], in_=qr[0:64, :n]), r=["r_qr"], w=["r_qTb"])
        P.add("act", lambda e: e.copy(out=qTb[64:128, 1, t0:t0 + n], in_=qr[64:128, :n]), r=["r_qr"], w=["r_qTb"])
        for s in range(ns):
            P.add("dve", lambda e, s=s: e.tensor_tensor(out=qdf[:, c0 + s, :], in0=qr[:, s * 128:(s + 1) * 128], in1=Qd[:, 0, :], op=ALU.mult), r=["r_qr", "r_Qd"], w=["r_qdf"])
            P.add("pool", lambda e, s=s: e.tensor_tensor(out=qdr[:, c0 + s, :], in0=qr[:, s * 128:(s + 1) * 128], in1=Qd[:, 1, :], op=ALU.mult), r=["r_qr", "r_Qd"], w=["r_qdr"])
        P.add("dve", lambda e: e.tensor_tensor(out=t1[:, :n], in0=k[:, :n], in1=cs[:, :n], op=ALU.mult), r=[k.name, cs.name], w=["r_t1"])
        P.add("pool", lambda e: e.tensor_tensor(out=t2[:, :n], in0=kP[:, :n], in1=sn[:, :n], op=ALU.mult), r=[kP.name, sn.name], w=["r_t2"])
        P.add("dve", lambda e: e.tensor_tensor(out=kTb[:, t0:t0 + n], in0=t1[:, :n], in1=t2[:, :n], op=ALU.add), r=["r_t1", "r_t2"], w=["r_kTb"])
        P.dma("sp", kt[:, :ns, :], D["d_ktok"].rearrange("(c p) d -> p c d", p=128)[:, c0:c0 + ns, :], w=["r_kt"])
        P.dma("sp", kPt[:, :ns, :], D["d_kPtok"].rearrange("(c p) d -> p c d", p=128)[:, c0:c0 + ns, :], w=["r_kPt"])
        P.dma("sp", ct[:, :ns, :], D["d_costok"].rearrange("(c p) d -> p c d", p=128)[:, c0:c0 + ns, :], w=["r_ct"])
        P.dma("sp", st[:, :ns, :], D["d_sintok"].rearrange("(c p) d -> p c d", p=128)[:, c0:c0 + ns, :], w=["r_st"])
        for h in range(2):
            hs = slice(h * 64, (h + 1) * 64)
            P.add("dve", lambda e, hs=hs: e.tensor_tensor(out=krt[:, :ns, hs], in0=kt[:, :ns, hs], in1=ct[:, :ns, :], op=ALU.mult), r=["r_kt", "r_ct"], w=["r_krt"])
            P.add("pool", lambda e, hs=hs: e.tensor_tensor(out=kt2[:, :ns, hs], in0=kPt[:, :ns, hs], in1=st[:, :ns, :], op=ALU.mult), r=["r_kPt", "r_st"], w=["r_kt2"])
        P.add("dve", lambda e: e.tensor_tensor(out=krt[:, :ns, :], in0=krt[:, :ns, :], in1=kt2[:, :ns, :], op=ALU.add), r=["r_krt", "r_kt2"], w=["r_krt"])
        for h in range(2):
            hs = slice(h * 64, (h + 1) * 64)
            P.add("dve", lambda e, hs=hs, h=h: e.tensor_scalar(out=kdf[:, c0:c0 + ns, hs], in0=krt[:, :ns, hs], scalar1=Kd[:, 0, h:h + 1], scalar2=None, op0=ALU.mult), r=["r_krt", "r_Kd"], w=["r_kdf"])
            P.add("pool", lambda e, hs=hs, h=h: e.tensor_scalar(out=kdr[:, c0:c0 + ns, hs], in0=krt[:, :ns, hs], scalar1=Kd[:, 1, h:h + 1], scalar2=None, op0=ALU.mult), r=["r_krt", "r_Kd"], w=["r_kdr"])

    RS = 3
    for (t0, n) in QT:
        prep(t0, n)
    if RS < 2:
        return P.finish()

    Sf = P.sb([128, NCH, 128], BF16, "r_Sf"); Sr = P.sb([128, NCH, 128], BF16, "r_Sr")
    S = P.sb([128, 128], F32, "r_S")
    gp = Rot([P.ps([128, 512], F32, f"r_gp{i}") for i in range(2)], "r_gp")
    tg = Rot([P.sb([128, 128], F32, f"r_tg{i}") for i in range(2)], "r_tg")

    def scan(order, kd, kdkey, Sall, skey, d):
        P.add("pool", lambda e: e.memset(S[:], 0.0), r=[], w=["r_S"])
        P.add("pool", lambda e: e.memset(Sall[:, order[0], :], 0.0), w=[skey])
        for idx in range(len(order) - 1):
            c = order[idx]
            g_, gk = gp.next()
            P.add("pe", lambda e, c=c, g_=g_: e.matmul(g_[:, 0:128], kd[:, c, :], vt[:, c, :], start=True, stop=True), r=[kdkey, "r_vt"], w=[gk])
            tg_, tk = tg.next()
            P.add("dve", lambda e, g_=g_, tg_=tg_: e.tensor_tensor(out=tg_[:, :], in0=g_[:, 0:128], in1=bd[:, :], op=ALU.mult), r=[gk, "r_d_bd"], w=[tk])
            P.add("dve", lambda e, tg_=tg_: e.scalar_tensor_tensor(out=S[:, :], in0=S[:, :], scalar=cd[:, d:d + 1], in1=tg_[:, :], op0=ALU.mult, op1=ALU.add), r=[tk, "r_S", "r_cd"], w=["r_S"])
            P.add("act", lambda e, nx=order[idx + 1]: e.copy(out=Sall[:, nx, :], in_=S[:, :]), r=["r_S"], w=[skey])

    scan(list(range(NCH)), kdf, "r_kdf", Sf, "r_Sf", 0)
    scan([1, 0] + list(range(NCH - 1, 1, -1)), kdr, "r_kdr", Sr, "r_Sr", 1)

    if RS < 3:
        return P.finish()
    bp = Rot([P.ps([128, 512], F32, f"r_bp{i}") for i in range(2)], "r_bp")
    op_ = Rot([P.ps([128, 512], F32, f"r_op{i}") for i in range(2)], "r_op")
    mv = P.ps([128, 512], F32, "r_mv")
    AT = Rot([P.sb([128, 2, 128], BF16, f"r_AT{i}") for i in range(2)], "r_AT")
    osb = P.sb([128, 512], F32, "r_osb"); dd = P.sb([128, 512], F32, "r_dd"); sq = P.sb([128, 512], F32, "r_sq"); rs = P.sb([128, 512], F32, "r_rs")
    gt = P.sb([128, 512], F32, "r_gt"); yo = P.sb([128, 512], F32, "r_yo")

    def out_tile(t0, n):
        ns = n // 128
        c0 = t0 // 128
        o_, ok = op_.next()
        for s in range(ns):
            c = c0 + s
            b_, bk = bp.next()
            for h in range(2):
                hs = slice(h * 64, (h + 1) * 64)
                P.add("pe", lambda e, h=h, hs=hs, c=c, b_=b_: e.matmul(b_[:, h * 128:(h + 1) * 128], kTb[:, c * 128:(c + 1) * 128], qTb[:, h, c * 128:(c + 1) * 128], start=True, stop=True),
                      r=["r_kTb", "r_qTb"], w=[bk])
            at, ak = AT.next()
            P.add("dve", lambda e, b_=b_, at=at: e.tensor_tensor(out=at[:, :, :], in0=b_[:, 0:256].rearrange("p (h q) -> p h q", h=2), in1=DT[:, :, :], op=ALU.mult), r=[bk, "r_DT"], w=[ak])
            reg = o_[:, s * 128:(s + 1) * 128]
            P.add("pe", lambda e, reg=reg, c=c: e.matmul(reg, Sf[:, c, :], qdf[:, c, :], start=True, stop=False), r=["r_Sf", "r_qdf"], w=[ok])
            P.add("pe", lambda e, reg=reg, c=c: e.matmul(reg, Sr[:, c, :], qdr[:, c, :], start=False, stop=False), r=["r_Sr", "r_qdr"], w=[ok])
            P.add("pe", lambda e, reg=reg, c=c, at=at: e.matmul(reg, vpad[:, c, 0, :], at[:, 0, :], start=False, stop=False), r=["r_vpad", ak], w=[ok])
            P.add("pe", lambda e, reg=reg, c=c, at=at: e.matmul(reg, vpad[:, c, 1, :], at[:, 1, :], start=False, stop=True), r=["r_vpad", ak], w=[ok])
        P.dma("sp", gt[:, :n], D["d_gate"][:, t0:t0 + n], w=["r_gt"])
        P.add("act", lambda e: e.copy(out=osb[:, :n], in_=o_[:, :n]), r=[ok], w=["r_osb"])
        P.add("pe", lambda e: e.matmul(mv[:, :n], bd64[:, :], osb[:, :n], start=True, stop=True), r=["r_d_bd64", "r_osb"], w=["r_mv"])
        P.add("dve", lambda e: e.tensor_tensor(out=dd[:, :n], in0=osb[:, :n], in1=mv[:, :n], op=ALU.subtract), r=["r_osb", "r_mv"], w=["r_dd"])
        P.add("act", lambda e: e.activation(out=sq[:, :n], in_=dd[:, :n], func=AF.Square), r=["r_dd"], w=["r_sq"])
        P.add("pe", lambda e: e.matmul(mv[:, :n], bd64[:, :], sq[:, :n], start=True, stop=True), r=["r_d_bd64", "r_sq"], w=["r_mv"])
        P.add("act", lambda e: e.activation(out=rs[:, :n], in_=mv[:, :n], func=AF.Sqrt, bias=C["epsb"][:, 0:1]), r=["r_mv", "epsb"], w=["r_rs"])
        P.add("dve", lambda e: e.reciprocal(out=rs[:, :n], in_=rs[:, :n]), r=["r_rs"], w=["r_rs"])
        P.add("dve", lambda e: e.tensor_tensor(out=dd[:, :n], in0=dd[:, :n], in1=rs[:, :n], op=ALU.mult), r=["r_dd", "r_rs"], w=["r_dd"])
        P.add("act", lambda e: e.activation(out=gt[:, :n], in_=gt[:, :n], func=AF.Silu), r=["r_gt"], w=["r_gt"])
        P.add("dve", lambda e: e.scalar_tensor_tensor(out=yo[:, :n], in0=dd[:, :n], scalar=g[:, 0:1], in1=gt[:, :n], op0=ALU.mult, op1=ALU.mult), r=["r_dd", "r_gt", "r_d_g"], w=["r_yo"])
        P.dma("sp", out[:, t0:t0 + n], yo[:, :n], r=["r_yo"])

    for (t0, n) in QT:
        out_tile(t0, n)
    print("ret ops", P.n_ops())
    return P.finish()


GDN_IN = [("b_q", [128, NTOK]), ("b_k", [128, NTOK]), ("b_v", [128, NTOK]), ("b_gate", [128, NTOK]), ("b_ab", [NTOK, 8]),
          ("b_cw", [64, 30]), ("b_dtb", [128, NCH * 8]), ("b_alog", [128, NCH * 8]), ("b_g", [64, 1]),
          ("b_triF", [128, 128]), ("b_triR", [128, 128]), ("b_ident", [128, 128]), ("b_um", [128, 128]), ("b_lm", [128, 128]),
          ("b_us", [128, 128]), ("b_ls", [128, 128])]


def build_gdn(nsteps=NCH, limit=10**9):
    P = Prog()
    _real_add = P.add
    _cnt = [0]
    _on = [False]

    def _ladd(eng, fn, r=(), w=()):
        if _on[0]:
            _cnt[0] += 1
            if _cnt[0] > limit:
                return None
        extra = [k for k in r if isinstance(k, tuple) and k[0] == 'bk' and k not in w]
        return _real_add(eng, fn, r, list(w) + extra)
    P.add = _ladd
    D = {nm: P.dram_in(nm, shp) for nm, shp in GDN_IN}
    out = P.dram_out("mix", [128, NTOK])
    C = bconsts(P)
    ones = C["ones"]

    def ld(nm, shp):
        t = P.sb(shp, F32, "g_" + nm)
        P.dma("sp", t[:], D[nm][:, :], w=["g_" + nm])
        return t
    cw = ld("b_cw", [64, 30])
    gg = ld("b_g", [64, 1])
    tri = [ld("b_triF", [128, 128]), ld("b_triR", [128, 128])]
    ident = ld("b_ident", [128, 128])
    msk = [ld("b_um", [128, 128]), ld("b_lm", [128, 128])]
    smsk = [ld("b_us", [128, 128]), ld("b_ls", [128, 128])]
    CK = ["g_b_triF", "g_b_triR", "g_b_ident", "g_b_um", "g_b_lm", "g_b_us", "g_b_ls", "ones"]
    banks = [P.ps([128, 512], F32, f"g_bk{j}") for j in range(8)]

    def X(j, r, rows=128, cols=128):
        return banks[j][0:rows, r * 128:r * 128 + cols]

    qn = P.sb([64, 2, NTOK], F32, "g_qn"); kn = P.sb([64, 2, NTOK], F32, "g_kn"); oacc = P.sb([64, 2, NTOK], F32, "g_oacc")
    ktok = P.sb([128, NCH, 128], F32, "g_ktok"); vtok = P.sb([128, NCH, 128], F32, "g_vtok")
    P.add("pool", lambda e: e.memset(oacc[:], 0.0), w=["g_oacc"])
    ab = P.sb([128, NCH * 8], F32, "g_ab"); dtb = ld("b_dtb", [128, NCH * 8]); alog = ld("b_alog", [128, NCH * 8])
    gtok = P.sb([128, NCH * 8], F32, "g_gtok"); btok = P.sb([128, NCH * 8], F32, "g_btok")
    P.dma("sp", ab[:].rearrange("p (c e) -> p c e", e=8), D["b_ab"].rearrange("(c p) e -> p c e", p=128), w=["g_ab"])
    P.add("act", lambda e: e.activation(out=btok[:], in_=ab[:], func=AF.Sigmoid), r=["g_ab"], w=["g_btok"])
    P.add("dve", lambda e: e.tensor_tensor(out=gtok[:], in0=ab[:], in1=dtb[:], op=ALU.add), r=["g_ab", "g_b_dtb"], w=["g_gtok"])
    P.add("act", lambda e: e.activation(out=gtok[:], in_=gtok[:], func=AF.Exp), r=["g_gtok"], w=["g_gtok"])
    P.add("act", lambda e: e.activation(out=gtok[:], in_=gtok[:], func=AF.Ln, bias=1.0), r=["g_gtok"], w=["g_gtok"])
    P.add("act", lambda e: e.activation(out=alog[:], in_=alog[:], func=AF.Exp), r=["g_b_alog"], w=["g_b_alog"])
    P.add("dve", lambda e: e.scalar_tensor_tensor(out=gtok[:], in0=gtok[:], scalar=-1.0, in1=alog[:], op0=ALU.mult, op1=ALU.mult), r=["g_gtok", "g_b_alog"], w=["g_gtok"])

    xr = P.sb([64, 2, 516], F32, "g_xr"); acc = P.sb([64, 2, 512], F32, "g_acc"); sq = P.sb([64, 2, 512], F32, "g_sq"); rs = P.sb([64, 2, 512], F32, "g_rs")

    def conv_tile(gi, src, t0, n):
        s0, s1 = (0, 256) if t0 < 256 else (256, NTOK)
        lo, hi = max(t0 - 2, s0), min(t0 + n + 2, s1)
        P.add("pool", lambda e: e.memset(xr[:], 0.0), w=["g_xr"])
        P.dma("sp", xr[:, :, lo - (t0 - 2):hi - (t0 - 2)], D[src].rearrange("(h d) t -> d h t", d=64)[:, :, lo:hi], w=["g_xr"])
        for h in range(2):
            eng = "dve"
            for tap in range(5):
                wcol = cw[:, gi * 10 + h * 5 + tap:gi * 10 + h * 5 + tap + 1]
                if tap == 0:
                    P.add(eng, lambda e, h=h, wcol=wcol: e.tensor_scalar(out=acc[:, h, :n], in0=xr[:, h, 0:n], scalar1=wcol, scalar2=None, op0=ALU.mult),
                          r=["g_xr", "g_b_cw"], w=[("g_acc", h)])
                else:
                    P.add(eng, lambda e, h=h, wcol=wcol, tap=tap: e.scalar_tensor_tensor(out=acc[:, h, :n], in0=xr[:, h, tap:tap + n], scalar=wcol, in1=acc[:, h, :n], op0=ALU.mult, op1=ALU.add),
                          r=["g_xr", "g_b_cw", ("g_acc", h)], w=[("g_acc", h)])
        P.add("act", lambda e: e.activation(out=acc[:, :, :n], in_=acc[:, :, :n], func=AF.Silu), r=[("g_acc", 0), ("g_acc", 1)], w=[("g_acc", 0), ("g_acc", 1)])

    def l2_tile(dst, dkey, t0, n, scl):
        P.add("act", lambda e: e.activation(out=sq[:, :, :n], in_=acc[:, :, :n], func=AF.Square), r=[("g_acc", 0), ("g_acc", 1)], w=["g_sq"])
        for h in range(2):
            P.add("pe", lambda e, h=h: e.matmul(banks[h][0:64, :n], ones[0:64, 0:64], sq[:, h, :n], start=True, stop=True), r=["g_sq", "ones"], w=[("bk", h)])
            P.add("act", lambda e, h=h: e.activation(out=rs[:, h, :n], in_=banks[h][0:64, :n], func=AF.Sqrt, bias=C["epsb"][0:64, 0:1]), r=[("bk", h), "epsb"], w=["g_rs"])
        P.add("dve", lambda e: e.reciprocal(out=rs[:, :, :n], in_=rs[:, :, :n]), r=["g_rs"], w=["g_rs"])
        P.add("dve", lambda e: e.scalar_tensor_tensor(out=dst[:, :, t0:t0 + n], in0=acc[:, :, :n], scalar=scl, in1=rs[:, :, :n], op0=ALU.mult, op1=ALU.mult),
              r=[("g_acc", 0), ("g_acc", 1), "g_rs"], w=[dkey])

    def tr_tile(srcfn, skeys, dst, dkey, t0, n):
        for s in range(n // 128):
            c = t0 // 128 + s
            j = 2 + (s % 2)
            for h in range(2):
                P.add("pe", lambda e, h=h, s=s, j=j: e.matmul(banks[j][:, h * 64:(h + 1) * 64], srcfn(h, s), ident[0:64, 0:64], start=True, stop=True),
                      r=skeys + ["g_b_ident"], w=[("bk", j)])
            P.add("act", lambda e, c=c, j=j: e.copy(out=dst[:, c, :], in_=banks[j][:, 0:128]), r=[("bk", j)], w=[dkey])

    for (t0, n) in QT:
        conv_tile(0, "b_q", t0, n)
        l2_tile(qn, "g_qn", t0, n, 0.125)
        conv_tile(1, "b_k", t0, n)
        l2_tile(kn, "g_kn", t0, n, 1.0)
        tr_tile(lambda h, s, t0=t0: kn[:, h, t0 + s * 128:t0 + (s + 1) * 128], ["g_kn"], ktok, "g_ktok", t0, n)
        conv_tile(2, "b_v", t0, n)
        tr_tile(lambda h, s: acc[:, h, s * 128:(s + 1) * 128], [("g_acc", 0), ("g_acc", 1)], vtok, "g_vtok", t0, n)

    NI = 4
    def tl(nm, shp):
        return [P.sb(shp, F32, f"g_{nm}{i}") for i in range(NI)]
    grep = tl("grep", [128, 128]); brep = tl("brep", [128, 128]); EB = tl("EB", [64, 128]); bBs = tl("bBs", [64, 128])
    DTm = tl("DTm", [128, 128]); DTs = tl("DTs", [128, 128]); MT = tl("MT", [128, 128]); Pa = tl("Pa", [128, 128]); PTa = tl("PTa", [128, 128])
    Pb = tl("Pb", [128, 128]); PTb = tl("PTb", [128, 128]); RT = tl("RT", [128, 128]); AT = tl("AT", [128, 128])
    wT = tl("wT", [64, 128]); u = tl("u", [128, 64]); vb = tl("vb", [128, 64]); kbe = tl("kbe", [128, 64])
    qd2 = [tl(f"qd{p}_", [64, 128]) for p in range(2)]; kd2 = [tl(f"kd{p}_", [128, 64]) for p in range(2)]; cols2 = [tl(f"cols{p}_", [128, 8]) for p in range(2)]
    kbT = tl("kbT", [64, 128]); vnew = tl("vnew", [128, 64]); Sst = tl("S", [64, 64])
    for i in range(NI):
        P.add("pool", lambda e, i=i: e.memset(Sst[i][:], 0.0), w=[("S", i)])
    orders = [list(range(NCH)), [1, 0] + list(range(NCH - 1, 1, -1))]

    def pre(i, c, d, h, par):
        K = lambda nm: (nm, i)
        bk = ("bk", i)
        gcol = gtok[:, c * 8 + d * 4 + h:c * 8 + d * 4 + h + 1]
        bcol = btok[:, c * 8 + d * 4 + 2 + h:c * 8 + d * 4 + 2 + h + 1]
        ch = slice(c * 128, (c + 1) * 128)
        hs = slice(h * 64, (h + 1) * 64)
        cl = cols2[par][i]
        qd_ = qd2[par][i]
        kd_ = kd2[par][i]
        P.add("dve", lambda e: e.tensor_scalar(out=grep[i][:, :], in0=ones[:, :], scalar1=gcol, scalar2=None, op0=ALU.mult), r=["g_gtok", "ones"], w=[K("grep")])
        P.add("pool", lambda e: e.tensor_scalar(out=brep[i][:, :], in0=ones[:, :], scalar1=bcol, scalar2=None, op0=ALU.mult), r=["g_btok", "ones"], w=[K("brep")])
        P.add("pe", lambda e: e.matmul(X(i, 0), grep[i][:, :], tri[d][:, :], start=True, stop=True), r=[K("grep")] + CK, w=[bk])
        yield
        P.add("pe", lambda e: e.matmul(X(i, 2), brep[i][:, :], ident[:, :], start=True, stop=True), r=[K("brep")] + CK, w=[bk])
        yield
        P.add("dve", lambda e: e.tensor_tensor(out=grep[i][:, :], in0=X(i, 0), in1=ident[:, :], op=ALU.mult), r=[bk] + CK, w=[K("grep")])
        P.add("dve", lambda e: e.reduce_sum(out=cl[:, 0:1], in_=grep[i][:, :], axis=AX.X), r=[K("grep")], w=[K(("cols", par))])
        lc = 127 if d == 0 else 0
        P.add("act", lambda e: e.copy(out=cl[:, 1:2], in_=banks[i][:, lc:lc + 1]), r=[bk], w=[K(("cols", par))])
        P.add("act", lambda e: e.activation(out=EB[i][:, :], in_=X(i, 0, 64), func=AF.Exp), r=[bk], w=[K("EB")])
        P.add("dve", lambda e: e.tensor_scalar(out=grep[i][:, :], in0=X(i, 0), scalar1=cl[:, 0:1], scalar2=0.0, op0=ALU.subtract, op1=ALU.min), r=[bk, K(("cols", par))], w=[K("grep")])
        P.add("dve", lambda e: e.tensor_copy(out=bBs[i][:, :], in_=X(i, 2, 64)), r=[bk], w=[K("bBs")])
        P.add("act", lambda e: e.activation(out=DTm[i][:, :], in_=grep[i][:, :], func=AF.Exp), r=[K("grep")], w=[K("DTm")])
        P.add("pool", lambda e: e.tensor_tensor(out=DTm[i][:, :], in0=DTm[i][:, :], in1=msk[d][:, :], op=ALU.mult), r=[K("DTm")] + CK, w=[K("DTm")])
        P.add("pool", lambda e: e.tensor_tensor(out=DTs[i][:, :], in0=DTm[i][:, :], in1=smsk[d][:, :], op=ALU.mult), r=[K("DTm")] + CK, w=[K("DTs")])
        P.add("act", lambda e: e.activation(out=cl[:, 2:3], in_=cl[:, 0:1], func=AF.Exp, scale=-1.0, bias=cl[:, 1:2]), r=[K(("cols", par))], w=[K(("cols", par))])
        P.add("act", lambda e: e.activation(out=cl[:, 3:4], in_=cl[:, 1:2], func=AF.Exp), r=[K(("cols", par))], w=[K(("cols", par))])
        P.add("act", lambda e: e.activation(out=cl[:, 4:5], in_=cl[:, 0:1], func=AF.Exp), r=[K(("cols", par))], w=[K(("cols", par))])
        P.add("dve", lambda e: e.tensor_tensor(out=cl[:, 5:6], in0=cl[:, 4:5], in1=bcol, op=ALU.mult), r=[K(("cols", par)), "g_btok"], w=[K(("cols", par))])
        P.add("dve", lambda e: e.tensor_tensor(out=kbT[i][:, :], in0=kn[:, h, ch], in1=bBs[i][:, :], op=ALU.mult), r=["g_kn", K("bBs")], w=[K("kbT")])
        P.add("dve", lambda e: e.tensor_tensor(out=qd_[:, :], in0=qn[:, h, ch], in1=EB[i][:, :], op=ALU.mult), r=["g_qn", K("EB")], w=[K(("qd", par))])
        P.add("pool", lambda e: e.tensor_scalar(out=vb[i][:, :], in0=vtok[:, c, hs], scalar1=bcol, scalar2=None, op0=ALU.mult), r=["g_vtok", "g_btok"], w=[K("vb")])
        P.add("pool", lambda e: e.tensor_scalar(out=kbe[i][:, :], in0=ktok[:, c, hs], scalar1=cl[:, 5:6], scalar2=None, op0=ALU.mult), r=["g_ktok", K(("cols", par))], w=[K("kbe")])
        P.add("pool", lambda e: e.tensor_scalar(out=kd_[:, :], in0=ktok[:, c, hs], scalar1=cl[:, 2:3], scalar2=None, op0=ALU.mult), r=["g_ktok", K(("cols", par))], w=[K(("kd", par))])
        P.add("pe", lambda e: e.matmul(X(i, 0), kn[:, h, ch], kbT[i][:, :], start=True, stop=True), r=["g_kn", K("kbT")], w=[bk])
        yield
        P.add("pe", lambda e: e.matmul(X(i, 1), kn[:, h, ch], qn[:, h, ch], start=True, stop=True), r=["g_kn", "g_qn"], w=[bk])
        yield
        P.add("dve", lambda e: e.scalar_tensor_tensor(out=MT[i][:, :], in0=X(i, 0), scalar=-1.0, in1=DTs[i][:, :], op0=ALU.mult, op1=ALU.mult), r=[bk, K("DTs")], w=[K("MT")])
        P.add("dve", lambda e: e.tensor_tensor(out=AT[i][:, :], in0=X(i, 1), in1=DTm[i][:, :], op=ALU.mult), r=[bk, K("DTm")], w=[K("AT")])
        P.add("pe", lambda e: e.matmul(X(i, 2), MT[i][:, :], ident[:, :], start=True, stop=True), r=[K("MT")] + CK, w=[bk])
        yield
        P.add("act", lambda e: e.copy(out=Pa[i][:, :], in_=X(i, 2)), r=[bk], w=[K("Pa")])
        P.add("pool", lambda e: e.tensor_tensor(out=RT[i][:, :], in0=MT[i][:, :], in1=ident[:, :], op=ALU.add), r=[K("MT")] + CK, w=[K("RT")])
        Pc, PTc, Pn, PTn = Pa[i], MT[i], Pb[i], PTb[i]
        kPc, kPTc, kPn, kPTn = K("Pa"), K("MT"), K("Pb"), K("PTb")
        for lvl in range(1, 7):
            P.add("pe", lambda e, Pc=Pc, PTc=PTc: e.matmul(X(i, 0), PTc[:, :], Pc[:, :], start=True, stop=True), r=[kPc, kPTc], w=[bk])
            yield
            if lvl < 6:
                P.add("pe", lambda e, Pc=Pc, PTc=PTc: e.matmul(X(i, 1), Pc[:, :], PTc[:, :], start=True, stop=True), r=[kPc, kPTc], w=[bk])
                yield
            P.add("act", lambda e, Pn=Pn: e.copy(out=Pn[:, :], in_=X(i, 0)), r=[bk], w=[kPn])
            if lvl < 6:
                P.add("dve", lambda e, PTn=PTn: e.tensor_copy(out=PTn[:, :], in_=X(i, 1)), r=[bk], w=[kPTn])
            P.add("pe", lambda e, Pn=Pn: e.matmul(X(i, 2), Pn[:, :], RT[i][:, :], start=True, stop=True), r=[kPn, K("RT")], w=[bk])
            yield
            P.add("dve", lambda e: e.tensor_tensor(out=RT[i][:, :], in0=RT[i][:, :], in1=X(i, 2), op=ALU.add), r=[bk, K("RT")], w=[K("RT")])
            if lvl == 1:
                Pc, PTc, Pn, PTn = Pb[i], PTb[i], Pa[i], PTa[i]
                kPc, kPTc, kPn, kPTn = K("Pb"), K("PTb"), K("Pa"), K("PTa")
            else:
                Pc, PTc, Pn, PTn = Pn, PTn, Pc, PTc
                kPc, kPTc, kPn, kPTn = kPn, kPTn, kPc, kPTc
        P.add("pe", lambda e: e.matmul(X(i, 0, 128, 64), RT[i][:, :], vb[i][:, :], start=True, stop=True), r=[K("RT"), K("vb")], w=[bk])
        yield
        P.add("pe", lambda e: e.matmul(X(i, 1, 64, 128), kbe[i][:, :], RT[i][:, :], start=True, stop=True), r=[K("RT"), K("kbe")], w=[bk])
        yield
        P.add("act", lambda e: e.copy(out=u[i][:, :], in_=X(i, 0, 128, 64)), r=[bk], w=[K("u")])
        P.add("dve", lambda e: e.tensor_copy(out=wT[i][:, :], in_=X(i, 1, 64, 128)), r=[bk], w=[K("wT")])

    def chain(i, c, d, h, par):
        K = lambda nm: (nm, i)
        bk = ("bk", 4 + i)
        j = 4 + i
        ch = slice(c * 128, (c + 1) * 128)
        cl = cols2[par][i]
        qd_ = qd2[par][i]
        kd_ = kd2[par][i]
        P.add("pe", lambda e: e.matmul(X(j, 0, 128, 64), wT[i][:, :], Sst[i][:, :], start=True, stop=True), r=[K("wT"), ("S", i)], w=[bk])
        yield
        P.add("dve", lambda e: e.tensor_tensor(out=vnew[i][:, :], in0=u[i][:, :], in1=X(j, 0, 128, 64), op=ALU.subtract), r=[bk, K("u")], w=[K("vnew")])
        P.add("pe", lambda e: e.matmul(X(j, 1, 64, 128), Sst[i][:, :], qd_[:, :], start=True, stop=False), r=[("S", i), K(("qd", par))], w=[bk])
        yield
        P.add("pe", lambda e: e.matmul(X(j, 1, 64, 128), vnew[i][:, :], AT[i][:, :], start=False, stop=True), r=[K("vnew"), K("AT")], w=[bk])
        yield
        P.add("pe", lambda e: e.matmul(X(j, 2, 64, 64), kd_[:, :], vnew[i][:, :], start=True, stop=True), r=[K(("kd", par)), K("vnew")], w=[bk])
        yield
        P.add("dve", lambda e: e.tensor_tensor(out=oacc[:, h, ch], in0=oacc[:, h, ch], in1=X(j, 1, 64, 128), op=ALU.add), r=[bk, "g_oacc"], w=["g_oacc"])
        P.add("dve", lambda e: e.scalar_tensor_tensor(out=Sst[i][:, :], in0=Sst[i][:, :], scalar=cl[0:64, 3:4], in1=X(j, 2, 64, 64), op0=ALU.mult, op1=ALU.add),
              r=[bk, ("S", i), K(("cols", par))], w=[("S", i)])

    _on[0] = True

    def rr(gens):
        live = list(gens)
        while live:
            nxt = []
            for g_ in live:
                try:
                    next(g_)
                    nxt.append(g_)
                except StopIteration:
                    pass
            live = nxt
    prev = []
    for s in range(nsteps + 1):
        pool = list(prev)
        insts = [(h * 2 + d, orders[d][s], d, h, s % 2) for h in range(2) for d in range(2)] if s < nsteps else []
        pool += [pre(*a_) for a_ in insts]
        rr(pool)
        prev = [chain(*a_) for a_ in insts]
    _on[0] = False
    print('gdn inst ops', _cnt[0])
    gt = xr; yo = acc

    def fin(t0, n):
        P.dma("sp", gt[:, :, :n], D["b_gate"].rearrange("(h d) t -> d h t", d=64)[:, :, t0:t0 + n], w=["g_xr"])
        P.add("act", lambda e: e.activation(out=sq[:, :, :n], in_=oacc[:, :, t0:t0 + n], func=AF.Square), r=["g_oacc"], w=["g_sq"])
        for h in range(2):
            P.add("pe", lambda e, h=h: e.matmul(banks[h][0:64, :n], ones[0:64, 0:64], sq[:, h, :n], start=True, stop=True), r=["g_sq", "ones"], w=[("bk", h)])
            P.add("act", lambda e, h=h: e.activation(out=rs[:, h, :n], in_=banks[h][0:64, :n], func=AF.Sqrt, scale=1.0 / 64, bias=C["epsb"][0:64, 0:1]), r=[("bk", h), "epsb"], w=["g_rs"])
        P.add("dve", lambda e: e.reciprocal(out=rs[:, :, :n], in_=rs[:, :, :n]), r=["g_rs"], w=["g_rs"])
        P.add("dve", lambda e: e.tensor_tensor(out=yo[:, :, :n], in0=oacc[:, :, t0:t0 + n], in1=rs[:, :, :n], op=ALU.mult), r=["g_oacc", "g_rs"], w=[("g_acc", 0), ("g_acc", 1)])
        P.add("act", lambda e: e.activation(out=gt[:, :, :n], in_=gt[:, :, :n], func=AF.Silu), r=["g_xr"], w=["g_xr"])
        P.add("dve", lambda e: e.scalar_tensor_tensor(out=yo[:, :, :n], in0=yo[:, :, :n], scalar=gg[:, 0:1], in1=gt[:, :, :n], op0=ALU.mult, op1=ALU.mult), r=[("g_acc", 0), ("g_acc", 1), "g_xr", "g_b_g"], w=[("g_acc", 0), ("g_acc", 1)])
        P.dma("sp", out.rearrange("(h d) t -> d h t", d=64)[:, :, t0:t0 + n], yo[:, :, :n], r=[("g_acc", 0), ("g_acc", 1)])

    for (t0, n) in QT:
        fin(t0, n)
    print("gdn ops", P.n_ops())
    return P.finish()


def build_M():
    P = Prog()
    scT = P.dram_in("scT", [1024, 5])
    wm = P.dram_in("wm", [1024, 3072])
    bm = P.dram_in("bm", [128, 24])
    modo = P.dram_out("modo", [128, 120])
    sc = P.sb([128, 8, 5], F32, "sc")
    P.dma("sp", sc[:], scT.rearrange("(k p) j -> p k j", p=128), w=["sc"])
    P.add("act", lambda e: e.activation(out=sc[:], in_=sc[:], func=AF.Silu), r=["sc"], w=["sc"])
    bms = P.sb([128, 24], F32, "bms")
    P.dma("sp", bms[:], bm[:, :], w=["bms"])
    w = P.sb([128, 8, 3072], F32, "wms")
    for k in range(8):
        P.dma("sp", w[:, k, :], wm[k * 128:(k + 1) * 128, :], w=[("wms", k)])
    ps = P.ps([128, 512], F32, "mps")
    ob = P.sb([128, 120], F32, "ob")
    for cc in range(24):
        for k in range(8):
            P.add("pe", lambda e, cc=cc, k=k: e.matmul(ps[:, cc * 5:(cc + 1) * 5], w[:, k, cc * 128:(cc + 1) * 128], sc[:, k, :], start=(k == 0), stop=(k == 7)),
                  r=["sc"] + [("wms", kk) for kk in range(8)], w=["mps"])
    for cc in range(24):
        P.add("dve", lambda e, cc=cc: e.tensor_scalar(out=ob[:, cc * 5:(cc + 1) * 5], in0=ps[:, cc * 5:(cc + 1) * 5], scalar1=bms[:, cc:cc + 1], scalar2=None, op0=ALU.add),
              r=["mps", "bms"], w=["ob"])
    P.dma("sp", modo[:, :], ob[:], r=["ob"])
    return P.finish()


OFF = {}
_o = 0
for nm, n in [("Aq",256),("Ak",128),("Av",128),("Bqkv",768),("Bgate",256),("Bab",16),("Ccq",256),("Cckv",128),("Ckr",32),("Dq",256),("Dk",256),("Dv",256),("Dgate",256)]:
    OFF[nm] = (_o, n); _o += n

def rope_perm(dim):
    q = dim // 4
    perm = np.concatenate([np.arange(q) + q, np.arange(q), np.arange(q) + 3 * q, np.arange(q) + 2 * q])
    sign = np.concatenate([-np.ones(q), np.ones(q), -np.ones(q), np.ones(q)]).astype(np.float32)
    return perm, sign

def rope_tables(rot_dim, rows=64, grid_w=64, theta=10000.0):
    n_freq = rot_dim // 4
    inv_freq = (theta ** (-np.arange(n_freq, dtype=np.float32) / n_freq)).astype(np.float32)
    row = np.repeat(np.arange(rows, dtype=np.float32), grid_w)
    col = np.tile(np.arange(grid_w, dtype=np.float32), rows)
    ang_r = row[:, None] * inv_freq
    ang_c = col[:, None] * inv_freq
    ang = np.concatenate([ang_r, ang_r, ang_c, ang_c], axis=-1).astype(np.float32)
    return np.cos(ang).astype(np.float32), np.sin(ang).astype(np.float32)

def rope_tabs_T(rot_dim):
    cos, sin = rope_tables(rot_dim)
    perm, sign = rope_perm(rot_dim)
    cT = np.ones((rot_dim, 4352), np.float32); sT = np.zeros((rot_dim, 4352), np.float32)
    cT[:, 256:] = cos.T; sT[:, 256:] = (sin * sign[None, :]).T
    return cT, sT

def perm_heads(w, dim):
    perm, _ = rope_perm(dim)
    nh = w.shape[1] // dim
    idx = np.concatenate([h * dim + perm for h in range(nh)])
    return w[:, idx]


def fm(v, nk):
    return np.ascontiguousarray(v.reshape(nk, 128).T)


def mla_inputs(P_, hf, W):
    hs = [2 * hf, 2 * hf + 1]
    perm32, _ = rope_perm(32)
    ct, st = rope_tabs_T(32)
    ct96 = np.ones((96, 4352), np.float32); st96 = np.zeros((96, 4352), np.float32)
    ct96[64:] = ct; st96[64:] = st
    wq = np.concatenate([W["mla_w_q_up"][:, h * 96:(h + 1) * 96] for h in hs], axis=1)
    wqP = np.zeros((256, 192), np.float32)
    for i, h in enumerate(hs):
        wqP[:, i * 96 + 64:i * 96 + 96] = W["mla_w_q_up"][:, h * 96 + 64 + perm32]
    wkn = np.zeros((128, 192), np.float32)
    for i, h in enumerate(hs):
        wkn[:, i * 96:i * 96 + 64] = W["mla_w_kv_up"][:, h * 128:h * 128 + 64]
    wv = np.concatenate([W["mla_w_kv_up"][:, h * 128 + 64:h * 128 + 128] for h in hs], axis=1)
    sel = np.zeros((32, 96), np.float32); sel[np.arange(32), 64 + np.arange(32)] = 1
    return {"c_cq": P_["Ccq"], "c_ckv": P_["Cckv"], "c_kr": P_["Ckr"], "c_krP": P_["CkrP"], "c_ct96": ct96, "c_st96": st96,
            "c_qng": fm(W["mla_q_norm"], 2), "c_kvg": fm(W["mla_kv_norm"], 1), "c_wq": np.ascontiguousarray(wq), "c_wqP": wqP,
            "c_wkn": wkn, "c_wv": np.ascontiguousarray(wv), "c_sel": sel}


def swa_inputs(PF, PT, hf, W):
    cT, sT = rope_tabs_T(64)
    j = np.arange(128)[:, None]; i = np.arange(128)[None, :]
    sink = W["swa_sink"][2 * hf:2 * hf + 2]
    return {"a_q": PF["Aq"][hf * 128:(hf + 1) * 128], "a_qP": PF["AqP"][hf * 128:(hf + 1) * 128],
            "a_k": PF["Ak"][hf * 64:(hf + 1) * 64], "a_kP": PF["AkP"][hf * 64:(hf + 1) * 64],
            "a_vtok": np.ascontiguousarray(PT["Av"][:, hf * 64:(hf + 1) * 64]), "a_cos": cT, "a_sin": sT,
            "a_sink": np.ascontiguousarray(np.broadcast_to(sink[None, :], (128, 2))).astype(np.float32),
            "a_maskP": (j >= i).astype(np.float32), "a_maskN": (j <= i).astype(np.float32)}


def ret_inputs(PF, PT, hf, W):
    cT, sT = rope_tabs_T(64)
    hs = [2 * hf, 2 * hf + 1]
    sl = slice(hf * 128, (hf + 1) * 128)
    ld = W["ret_log_decay"]
    ldp = np.zeros((128, 2), np.float32); ldr = np.zeros((128, 4), np.float32)
    for d in range(2):
        for i, h in enumerate(hs):
            ldp[i * 64:(i + 1) * 64, d] = ld[d, h]
            ldr[:, 2 * d + i] = ld[d, h]
    j = np.arange(128)[:, None].astype(np.float32); i = np.arange(128)[None, :].astype(np.float32)
    bd = np.zeros((128, 128), np.float32); bd[:64, :64] = 1; bd[64:, 64:] = 1
    pk = np.stack([127 - np.arange(128), np.arange(128)], 1).astype(np.float32)
    return {"d_q": PF["Dq"][sl], "d_qP": PF["DqP"][sl], "d_k": PF["Dk"][sl], "d_kP": PF["DkP"][sl], "d_gate": PF["Dgate"][sl],
            "d_vtok": np.ascontiguousarray(PT["Dv"][:, sl]), "d_ktok": np.ascontiguousarray(PT["Dk"][:, sl]), "d_kPtok": np.ascontiguousarray(PT["DkP"][:, sl]),
            "d_cos": cT, "d_sin": sT, "d_costok": np.ascontiguousarray(cT.T), "d_sintok": np.ascontiguousarray(sT.T),
            "d_ldp": ldp, "d_ldr": ldr, "d_g": np.ascontiguousarray(W["ret_norm"][sl].reshape(128, 1)),
            "d_relu": np.maximum(i - j, 0) + 0 * j, "d_rell": np.maximum(j - i, 0) + 0 * i, "d_um": (i >= j).astype(np.float32), "d_lm": (j >= i).astype(np.float32),
            "d_pos1": (i + 1) + 0 * j, "d_posr": (128 - i) + 0 * j, "d_pk": pk, "d_bd": bd, "d_bd64": bd / 64}


def gdn_inputs(PF, PT, hf, W):
    hs = [2 * hf, 2 * hf + 1]
    qkv = PF["Bqkv"]
    sel = lambda base: np.ascontiguousarray(np.concatenate([qkv[base + h * 64: base + (h + 1) * 64] for h in hs], 0))
    ab = PT["Bab"]
    abl = np.zeros((4352, 8), np.float32)
    dtb = np.zeros((8,), np.float32); alog = np.zeros((8,), np.float32)
    for d in range(2):
        for w in range(2):
            for hl, h in enumerate(hs):
                abl[:, d * 4 + w * 2 + hl] = ab[:, d * 8 + w * 4 + h]
        for hl, h in enumerate(hs):
            dtb[d * 4 + hl] = W["gdn_dt_bias"][d, h]; alog[d * 4 + hl] = W["gdn_a_log"][d, h]
    cwt = np.zeros((64, 3, 2, 5), np.float32)
    conv = W["gdn_conv"]
    for gi in range(3):
        for hl, h in enumerate(hs):
            cwt[:, gi, hl, :] = conv[:, gi * 256 + h * 64: gi * 256 + (h + 1) * 64].T
    k = np.arange(128)[:, None]; i = np.arange(128)[None, :]
    f = lambda m: m.astype(np.float32)
    return {"b_q": sel(0), "b_k": sel(256), "b_v": sel(512), "b_gate": PF["Bgate"][hf * 128:(hf + 1) * 128], "b_ab": abl,
            "b_cw": cwt.reshape(64, 30), "b_dtb": np.ascontiguousarray(np.broadcast_to(np.tile(dtb, 34)[None], (128, 272))),
            "b_alog": np.ascontiguousarray(np.broadcast_to(np.tile(alog, 34)[None], (128, 272))), "b_g": np.ascontiguousarray(W["gdn_norm"].reshape(64, 1)),
            "b_triF": f(k <= i), "b_triR": f(k >= i), "b_ident": np.eye(128, dtype=np.float32), "b_um": f(i >= k), "b_lm": f(k >= i),
            "b_us": f(i > k), "b_ls": f(k > i)}


_PROGS = {}
_TRACE = [False]


def _prog(name, fn):
    if name not in _PROGS:
        _PROGS[name] = fn()
    return _PROGS[name]


def _run(name, fn, in_maps):
    nc = fn()
    in_maps = [{k: np.ascontiguousarray(v, dtype=np.float32) for k, v in m.items()} for m in in_maps]
    res = run_bass_kernel_spmd(nc, in_maps, core_ids=list(range(8)), trace=_TRACE[0])
    if _TRACE[0]:
        print('STAGE', name, 'exec_ns', res.exec_time_ns, flush=True)
    return res.results


A_TILES = TILES
HALF = 2176


def _mod_table(mod_l, b, hf, tiles_ctx):
    nt = len(tiles_ctx)
    t = np.zeros((128, 6, nt, 8), np.float32)
    for ti, is_ctx in enumerate(tiles_ctx):
        v = mod_l[4 if is_ctx else b].reshape(6, 8, 128)
        t[:, :, ti, :] = v.transpose(2, 0, 1)
    return t


def kernel(x, c, ctx, c_ctx, w_mod, b_mod, norm1, norm2, w_in, w_out, swa_sink, gdn_conv, gdn_a_log,
           gdn_dt_bias, gdn_norm, mla_q_norm, mla_kv_norm, mla_w_q_up, mla_w_kv_up, ret_log_decay,
           ret_norm, ffn_w_gate, ffn_w_up, ffn_w_down, moe_router, moe_w_gate, moe_w_up, moe_w_down,
           final_norm, _nlayers=4):
    f32 = np.float32
    x = np.asarray(x, f32); ctx = np.asarray(ctx, f32)
    B = 4
    scT = np.ascontiguousarray(np.concatenate([np.asarray(c, f32), np.asarray(c_ctx, f32)[None]], 0).T)
    wm_all = np.asarray(w_mod, f32).transpose(1, 0, 2).reshape(1024, 4 * 6144)
    bm_all = np.asarray(b_mod, f32).reshape(4 * 6144)
    ims = []
    for core in range(8):
        sl = slice(core * 3072, (core + 1) * 3072)
        ims.append({"scT": scT, "wm": np.ascontiguousarray(wm_all[:, sl]), "bm": np.ascontiguousarray(bm_all[sl].reshape(24, 128).T)})
    res = _run("M", build_M, ims)
    mod = np.zeros((5, 4 * 6144), f32)
    for core in range(8):
        mo = res[core]["modo"].reshape(128, 24, 5)
        mod[:, core * 3072:(core + 1) * 3072] = mo.transpose(2, 1, 0).reshape(5, 3072)
    mod = mod.reshape(5, 4, 6144)

    hT = [np.ascontiguousarray(np.concatenate([ctx[b], x[b]], 0).T) for b in range(B)]
    p64, _ = rope_perm(64)
    p32, _ = rope_perm(32)
    ident = np.eye(128, dtype=f32)
    out_final = None
    for l in range(_nlayers):
        W = {"w_in": np.asarray(w_in[l], f32), "swa_sink": np.asarray(swa_sink[l], f32), "gdn_conv": np.asarray(gdn_conv[l], f32),
             "gdn_a_log": np.asarray(gdn_a_log[l], f32), "gdn_dt_bias": np.asarray(gdn_dt_bias[l], f32), "gdn_norm": np.asarray(gdn_norm[l], f32),
             "mla_q_norm": np.asarray(mla_q_norm[l], f32), "mla_kv_norm": np.asarray(mla_kv_norm[l], f32), "mla_w_q_up": np.asarray(mla_w_q_up[l], f32),
             "mla_w_kv_up": np.asarray(mla_w_kv_up[l], f32), "ret_log_decay": np.asarray(ret_log_decay[l], f32), "ret_norm": np.asarray(ret_norm[l], f32)}
        wi = W["w_in"]
        blk = lambda nm: wi[:, OFF[nm][0]:OFF[nm][0] + OFF[nm][1]]
        fcols = {"Aq": blk("Aq"), "AqP": perm_heads(blk("Aq"), 64), "Ak": blk("Ak"), "AkP": perm_heads(blk("Ak"), 64), "Bqkv": blk("Bqkv"),
                 "Bgate": blk("Bgate"), "Ccq": blk("Ccq"), "Cckv": blk("Cckv"), "Ckr": blk("Ckr"), "CkrP": perm_heads(blk("Ckr"), 32),
                 "Dq": blk("Dq"), "DqP": perm_heads(blk("Dq"), 64), "Dk": blk("Dk"), "DkP": perm_heads(blk("Dk"), 64), "Dgate": blk("Dgate")}
        tcols = {"Av": blk("Av"), "Bab": blk("Bab"), "Dv": blk("Dv"), "Dk": blk("Dk"), "DkP": perm_heads(blk("Dk"), 64)}
        win_ext = np.ascontiguousarray(np.concatenate([fcols[nm] for nm, _ in F_BLOCKS] + [tcols[nm] for nm, _ in T_BLOCKS], 1))
        gn1 = fm(np.asarray(norm1[l], f32), 8)
        gn2 = fm(np.asarray(norm2[l], f32), 8)
        ims = []
        for core in range(8):
            b, hf = core // 2, core % 2
            mt = _mod_table(mod[:, l], b, hf, [hf == 0 and ti == 0 for ti in range(5)])
            ims.append({"hT": np.ascontiguousarray(hT[b][:, hf * HALF:(hf + 1) * HALF]), "modt": mt.reshape(128, 240), "gn": gn1, "win": win_ext})
        res = _run("A", build_A, ims)
        PFs, PTs = [], []
        for b in range(B):
            pT = np.concatenate([res[2 * b]["projT"], res[2 * b + 1]["projT"]], 1)
            pK = np.concatenate([res[2 * b]["projTok"], res[2 * b + 1]["projTok"]], 0)
            PF, PT = {}, {}
            o = 0
            for nm, n in F_BLOCKS:
                PF[nm] = pT[o:o + n]; o += n
            o = 0
            for nm, n in T_BLOCKS:
                PT[nm] = pK[:, o:o + n]; o += n
            PFs.append(PF); PTs.append(PT)
        mixT = [np.zeros((1024, NTOK), f32) for _ in range(B)]
        for gi, (nm, bfn, ifn) in enumerate((("swa", build_swa, swa_inputs), ("gdn", build_gdn, gdn_inputs), ("mla", build_mla, None), ("ret", build_ret, ret_inputs))):
            ims = []
            for core in range(8):
                b, hf = core // 2, core % 2
                ims.append(mla_inputs(PFs[b], hf, W) if nm == "mla" else ifn(PFs[b], PTs[b], hf, W))
            res = _run(nm, bfn, ims)
            for core in range(8):
                b, hf = core // 2, core % 2
                mixT[b][gi * 256 + hf * 128: gi * 256 + (hf + 1) * 128] = res[core]["mix"]
        moe = (l % 2 == 1)
        final = (l == 3)
        i2 = l // 2
        nt = 9
        nl = 1
        span = nt * 256
        padw = nl * span
        if moe:
            wts = {"wg": np.asarray(moe_w_gate[i2], f32), "wu": np.asarray(moe_w_up[i2], f32), "wd": np.asarray(moe_w_down[i2], f32),
                   "wr": np.asarray(moe_router[i2], f32), "ident": ident}
        else:
            wts = {"wg": np.asarray(ffn_w_gate[i2], f32)[None], "wu": np.asarray(ffn_w_up[i2], f32)[None], "wd": np.asarray(ffn_w_down[i2], f32)[None]}
        wts["wout"] = np.asarray(w_out[l], f32)
        wts["gn"] = gn2
        if final:
            wts["fn"] = fm(np.asarray(final_norm, f32), 8)
        newh = [np.zeros((1024, NTOK), f32) for _ in range(B)]
        outs = [np.zeros((1024, NTOK), f32) for _ in range(B)]
        for r in range(nl):
            ims = []
            for core in range(8):
                b, hf = core // 2, core % 2
                hp = np.zeros((1024, padw), f32); mp = np.zeros((1024, padw), f32)
                hp[:, :HALF] = hT[b][:, hf * HALF:(hf + 1) * HALF]
                mp[:, :HALF] = mixT[b][:, hf * HALF:(hf + 1) * HALF]
                mt = _mod_table(mod[:, l], b, hf, [hf == 0 and (r * nt + ti) == 0 for ti in range(nt)])
                d = {"hT": np.ascontiguousarray(hp[:, r * span:(r + 1) * span]), "mixT": np.ascontiguousarray(mp[:, r * span:(r + 1) * span]),
                     "modt": mt.reshape(128, 6 * nt * 8)}
                d.update(wts)
                ims.append(d)
            res = _run(("C", moe, final, nt), lambda: build_C2(moe, final, nt), ims)
            for core in range(8):
                b, hf = core // 2, core % 2
                lo = r * span
                hi = min((r + 1) * span, HALF)
                if hi > lo:
                    newh[b][:, hf * HALF + lo: hf * HALF + hi] = res[core]["h2T"][:, :hi - lo]
                    if final:
                        outs[b][:, hf * HALF + lo: hf * HALF + hi] = res[core]["outT"][:, :hi - lo]
        hT = newh
        if final:
            out_final = np.stack([np.ascontiguousarray(outs[b][:, 256:].T) for b in range(B)], 0)
    if _nlayers < 4:
        return hT
    return out_final.astype(np.float32)
```
